# Optimizing a Trainium2 kernel written in Bass

```python
import jax, jax.numpy as jnp
from jax import lax
import numpy as np

D_MODEL = 1024
BATCH = 8
SEQ = 4096
DEPTH = 1

CHUNK = 64
Q_BLOCK = 128
EPS = 1e-6
NEG_INF = -1e30

MLA_HEADS = 8
MLA_NOPE_DIM = 64
MLA_ROPE_DIM = 32
MLA_V_DIM = 64
MLA_Q_RANK = 256
MLA_KV_RANK = 128
ROPE_THETA = 10000.0
MLA_WIDTH = MLA_HEADS * MLA_V_DIM

FOX_HEADS = 8
FOX_HEAD_DIM = 64
FOX_WIDTH = FOX_HEADS * FOX_HEAD_DIM
FORGET_BIAS_MEAN = 3.0

N_GROUPS = 4
EXPERTS_PER_GROUP = 8
N_EXPERTS = N_GROUPS * EXPERTS_PER_GROUP
TOP_K = 2
EXPERT_FF = 256

IN_SPLITS = [MLA_Q_RANK, MLA_KV_RANK, MLA_ROPE_DIM,
             FOX_WIDTH, FOX_WIDTH, FOX_WIDTH, FOX_HEADS,
             D_MODEL, D_MODEL]
D_IN = int(sum(IN_SPLITS))
SPLIT_IDX = [int(v) for v in np.cumsum(IN_SPLITS)[:-1]]

kernel_name = "hybrid_mla_fox_hmoe_block"


def rms_norm(x, g):
    xf = x.astype(jnp.float32)
    y = xf * lax.rsqrt(jnp.mean(xf * xf, axis=-1, keepdims=True) + EPS)
    return (y * g.astype(jnp.float32)).astype(x.dtype)


def rope(x, positions):
    d = x.shape[-1]
    half = d // 2
    inv_freq = ROPE_THETA ** (-jnp.arange(half, dtype=jnp.float32) / half)
    ang = positions.astype(jnp.float32)[:, :, None, None] * inv_freq
    cos, sin = jnp.cos(ang), jnp.sin(ang)
    xf = x.astype(jnp.float32)
    x1, x2 = xf[..., :half], xf[..., half:]
    out = jnp.concatenate([x1 * cos - x2 * sin, x2 * cos + x1 * sin], axis=-1)
    return out.astype(x.dtype)


def blocked_attention(q, k, v, scale, decay=None):
    S = q.shape[1]
    outs = []
    for i in range(S // Q_BLOCK):
        q0, q1 = i * Q_BLOCK, (i + 1) * Q_BLOCK
        logits = jnp.einsum('bqhd,bkhd->bhqk', q[:, q0:q1], k[:, :q1]).astype(jnp.float32) * scale
        q_pos = jnp.arange(q0, q1)[:, None]
        k_pos = jnp.arange(q1)[None, :]
        if decay is None:
            allowed = (k_pos // CHUNK) <= (q_pos // CHUNK)
        else:
            logits = logits + decay[:, :, q0:q1, None] - decay[:, :, None, :q1]
            allowed = k_pos <= q_pos
        logits = jnp.where(allowed, logits, NEG_INF)
        probs = jax.nn.softmax(logits, axis=-1).astype(v.dtype)
        outs.append(jnp.einsum('bhqk,bkhd->bqhd', probs, v[:, :q1]))
    return jnp.concatenate(outs, axis=1)


def hybrid_mixer(xn, positions, w_in, b_forget, g_cq, g_ckv, w_uq, w_uk, w_uv,
                 w_o_mla, w_o_fox, w_out):
    B, S, _ = xn.shape
    z = xn @ w_in
    c_q, c_kv, k_r, fq, fk, fv, f_logit, gate_a, gate_b = jnp.split(z, SPLIT_IDX, axis=-1)

    q = (rms_norm(c_q, g_cq) @ w_uq).reshape(B, S, MLA_HEADS, MLA_NOPE_DIM + MLA_ROPE_DIM)
    q = jnp.concatenate([q[..., :MLA_NOPE_DIM], rope(q[..., MLA_NOPE_DIM:], positions)], axis=-1)
    ckv = rms_norm(c_kv, g_ckv)
    k_nope = (ckv @ w_uk).reshape(B, S, MLA_HEADS, MLA_NOPE_DIM)
    v_a = (ckv @ w_uv).reshape(B, S, MLA_HEADS, MLA_V_DIM)
    k_rope = rope(k_r[:, :, None, :], positions)
    k_a = jnp.concatenate(
        [k_nope, jnp.broadcast_to(k_rope, (B, S, MLA_HEADS, MLA_ROPE_DIM))], axis=-1)
    o_a = blocked_attention(q, k_a, v_a, (MLA_NOPE_DIM + MLA_ROPE_DIM) ** -0.5)
    y_a = o_a.reshape(B, S, MLA_WIDTH) @ w_o_mla

    q_b = fq.reshape(B, S, FOX_HEADS, FOX_HEAD_DIM)
    k_b = fk.reshape(B, S, FOX_HEADS, FOX_HEAD_DIM)
    v_b = fv.reshape(B, S, FOX_HEADS, FOX_HEAD_DIM)
    log_f = jax.nn.log_sigmoid(f_logit.astype(jnp.float32) + b_forget.astype(jnp.float32))
    decay = jnp.transpose(jnp.cumsum(log_f, axis=1), (0, 2, 1))
    o_b = blocked_attention(q_b, k_b, v_b, FOX_HEAD_DIM ** -0.5, decay)
    y_b = o_b.reshape(B, S, FOX_WIDTH) @ w_o_fox

    mixed = jax.nn.sigmoid(gate_a) * y_a + jax.nn.sigmoid(gate_b) * y_b
    return mixed @ w_out


def hierarchical_moe(hn, w_group, b_group, w_router, b_router, w_e_gate, w_e_up, w_e_down):
    B, S, D = hn.shape
    t = hn.reshape(B * S, D)
    p_group = jax.nn.softmax((t @ w_group).astype(jnp.float32) + b_group.astype(jnp.float32), axis=-1)
    g_p, g_idx = lax.top_k(p_group, 1)
    exp_logits = ((t @ w_router).astype(jnp.float32) + b_router.astype(jnp.float32)
                  ).reshape(-1, N_GROUPS, EXPERTS_PER_GROUP)
    sel = jnp.take_along_axis(exp_logits, g_idx[:, :, None], axis=1)[:, 0]
    e_p, e_idx = lax.top_k(jax.nn.softmax(sel, axis=-1), TOP_K)
    e_p = e_p / jnp.sum(e_p, axis=-1, keepdims=True)
    weights = g_p * e_p
    expert_id = g_idx * EXPERTS_PER_GROUP + e_idx
    combine = jnp.sum(jax.nn.one_hot(expert_id, N_EXPERTS, dtype=jnp.float32)
                      * weights[..., None], axis=1)
    y = jnp.zeros((B * S, D), jnp.float32)
    for e in range(N_EXPERTS):
        a = jax.nn.silu(t @ w_e_gate[e]) * (t @ w_e_up[e])
        y = y + combine[:, e:e + 1] * (a @ w_e_down[e]).astype(jnp.float32)
    return y.reshape(B, S, D).astype(hn.dtype)


def setup_inputs(seed: int = 0) -> dict:
    key = jax.random.key(seed)
    ks = jax.random.split(key, 24)
    f32 = jnp.float32

    def w(k, shape, fan_in):
        return jax.random.normal(k, shape, f32) * (fan_in ** -0.5)

    def gain(k, n):
        return 1.0 + 0.02 * jax.random.normal(k, (n,), f32)

    x = jax.random.normal(ks[0], (BATCH, SEQ, D_MODEL), f32)
    offset = jax.random.randint(ks[1], (BATCH, 1), 0, 4096, dtype=jnp.int32)
    positions = offset + jnp.arange(SEQ, dtype=jnp.int32)[None, :]
    return {
        "x": x,
        "positions": positions,
        "g_mix": gain(ks[2], D_MODEL),
        "w_in": w(ks[3], (D_MODEL, D_IN), D_MODEL),
        "b_forget": FORGET_BIAS_MEAN + 0.5 * jax.random.normal(ks[4], (FOX_HEADS,), f32),
        "g_cq": gain(ks[5], MLA_Q_RANK),
        "g_ckv": gain(ks[6], MLA_KV_RANK),
        "w_uq": w(ks[7], (MLA_Q_RANK, MLA_HEADS * (MLA_NOPE_DIM + MLA_ROPE_DIM)), MLA_Q_RANK),
        "w_uk": w(ks[8], (MLA_KV_RANK, MLA_HEADS * MLA_NOPE_DIM), MLA_KV_RANK),
        "w_uv": w(ks[9], (MLA_KV_RANK, MLA_HEADS * MLA_V_DIM), MLA_KV_RANK),
        "w_o_mla": w(ks[10], (MLA_WIDTH, D_MODEL), MLA_WIDTH),
        "w_o_fox": w(ks[11], (FOX_WIDTH, D_MODEL), FOX_WIDTH),
        "w_out": w(ks[12], (D_MODEL, D_MODEL), D_MODEL),
        "g_ffn": gain(ks[13], D_MODEL),
        "w_group": w(ks[14], (D_MODEL, N_GROUPS), D_MODEL),
        "b_group": 0.01 * jax.random.normal(ks[15], (N_GROUPS,), f32),
        "w_router": w(ks[16], (D_MODEL, N_EXPERTS), D_MODEL),
        "b_router": 0.01 * jax.random.normal(ks[17], (N_EXPERTS,), f32),
        "w_e_gate": w(ks[18], (N_EXPERTS, D_MODEL, EXPERT_FF), D_MODEL),
        "w_e_up": w(ks[19], (N_EXPERTS, D_MODEL, EXPERT_FF), D_MODEL),
        "w_e_down": w(ks[20], (N_EXPERTS, EXPERT_FF, D_MODEL), EXPERT_FF),
        "g_final": gain(ks[21], D_MODEL),
    }


def reference(x, positions, g_mix, w_in, b_forget, g_cq, g_ckv, w_uq, w_uk, w_uv,
              w_o_mla, w_o_fox, w_out, g_ffn, w_group, b_group, w_router, b_router,
              w_e_gate, w_e_up, w_e_down, g_final):
    h = x
    for _ in range(DEPTH):
        h = h + hybrid_mixer(rms_norm(h, g_mix), positions, w_in, b_forget, g_cq, g_ckv,
                             w_uq, w_uk, w_uv, w_o_mla, w_o_fox, w_out)
        h = h + hierarchical_moe(rms_norm(h, g_ffn), w_group, b_group, w_router, b_router,
                                 w_e_gate, w_e_up, w_e_down)
    return rms_norm(h, g_final)
```

```python
import contextlib
import numpy as np
import ml_dtypes
import concourse.bass as bass
import concourse.mybir as mybir
from concourse.bass_utils import run_bass_kernel_spmd

F32 = mybir.dt.float32
BF16 = mybir.dt.bfloat16
I32 = mybir.dt.int32
ALU = mybir.AluOpType
AF = mybir.ActivationFunctionType
AX = mybir.AxisListType

PE, ACT, DVE, POOL, SP = "tensor", "scalar", "vector", "gpsimd", "sync"
ENGINES = [PE, ACT, DVE, POOL, SP]

S = 4096
D = 1024
NT = 32
NQ = 8
NH = 8
NE = 32
EFF = 256
EPS = 1e-6
MLA_SCALE = 96.0 ** -0.5
FOX_SCALE = 0.125
NEG = -30000.0
TWO_PI_HI = 6.28125
TWO_PI_LO = 6.283185307179586 - 6.28125


class Op:
    __slots__ = ("eng", "fn", "deps", "is_dma", "marked", "count", "sem", "semval", "prewait", "nosem")

    def __init__(self, eng, fn, is_dma):
        self.eng = eng
        self.fn = fn
        self.deps = []
        self.is_dma = is_dma
        self.marked = False
        self.count = 0
        self.sem = None
        self.semval = 0
        self.prewait = None
        self.nosem = False


class Prog:
    def __init__(self, nc, dma_pool=8):
        self.nc = nc
        self.ops = {e: [] for e in ENGINES}
        self.last_write = {}
        self.readers = {}
        self.dma_pool = dma_pool
        self.dma_count = {e: 0 for e in ENGINES}

    def op(self, eng, fn, reads=(), writes=(), dma=False, nosem=False):
        o = Op(eng, fn, dma)
        o.nosem = nosem
        deps = {}
        for r in reads:
            w = self.last_write.get(r)
            if w is not None:
                deps[id(w)] = (w, "raw")
        for w_ in writes:
            for rd in self.readers.get(w_, ()):
                if id(rd) not in deps:
                    deps[id(rd)] = (rd, "war")
            w = self.last_write.get(w_)
            if w is not None:
                deps[id(w)] = (w, "waw")
        for d, kind in deps.values():
            if d is o:
                continue
            if (not d.is_dma) and (not dma) and d.eng == eng:
                if eng == PE or kind == "war":
                    continue
            o.deps.append(d)
            d.marked = True
        for r in reads:
            self.readers.setdefault(r, []).append(o)
        for w_ in writes:
            self.last_write[w_] = o
            self.readers[w_] = []
        if dma and not nosem:
            k = self.dma_count[eng]
            self.dma_count[eng] = k + 1
            o.sem = (eng, k % self.dma_pool)
            o.semval = 16 * (k // self.dma_pool + 1)
            if k >= self.dma_pool:
                o.prewait = (o.sem, o.semval - 16)
        self.ops[eng].append(o)
        return o

    def emit(self, name):
        nc = self.nc
        for e in ENGINES:
            c = 0
            for o in self.ops[e]:
                if not o.is_dma and o.marked:
                    c += 1
                    o.count = c
        with contextlib.ExitStack() as st:
            esem = {e: st.enter_context(nc.semaphore("s_%s_%s" % (name, e))) for e in ENGINES}
            dsem = {}
            for e in ENGINES:
                for i in range(min(self.dma_pool, self.dma_count[e])):
                    dsem[(e, i)] = st.enter_context(nc.semaphore("d_%s_%s_%d" % (name, e, i)))
            allsems = list(esem.values()) + list(dsem.values())
            with nc.Block() as cblk:
                def _clr(engobj):
                    for s_ in allsems:
                        engobj.sem_clear(s_)
                cblk.sync(_clr)
            block = st.enter_context(nc.Block())

            def run_engine(e, engobj):
                seen = {}
                for o in self.ops[e]:
                    need = {}
                    for d in o.deps:
                        if d.is_dma:
                            key = ("d", d.sem)
                            val = d.semval
                        else:
                            key = ("e", d.eng)
                            val = d.count
                        if need.get(key, 0) < val:
                            need[key] = val
                    if o.prewait is not None:
                        key = ("d", o.prewait[0])
                        if need.get(key, 0) < o.prewait[1]:
                            need[key] = o.prewait[1]
                    for key, val in need.items():
                        if seen.get(key, 0) >= val:
                            continue
                        seen[key] = val
                        s = dsem[key[1]] if key[0] == "d" else esem[key[1]]
                        engobj.wait_ge(s, val)
                    ins = o.fn(engobj)
                    if o.nosem:
                        continue
                    if o.is_dma:
                        ins.then_inc(dsem[o.sem], 16)
                    elif o.marked:
                        ins.then_inc(esem[e], 1)
                k = self.dma_count[e]
                for i in range(min(self.dma_pool, k)):
                    uses = (k - 1 - i) // self.dma_pool + 1
                    if uses > 0:
                        engobj.wait_ge(dsem[(e, i)], 16 * uses)

            for e in ENGINES:
                if not self.ops[e]:
                    continue
                getattr(block, e)(lambda engobj, e=e: run_engine(e, engobj))


def build_program(stop_after=99, debug=None):
    nc = bass.Bass("TRN2", target_bir_lowering=False)
    debug = debug or []

    def din(name, shape, dt=F32):
        return nc.dram_tensor(name, list(shape), dt, kind="ExternalInput").ap()

    x_d = din("x", [S, D])
    pos_d = din("pos", [1, S], I32)
    g_mix_d = din("g_mix", [1, D])
    g_ffn_d = din("g_ffn", [1, D])
    g_fin_d = din("g_final", [1, D])
    g_cq_d = din("g_cq", [1, 256])
    g_ckv_d = din("g_ckv", [1, 128])
    bf_d = din("b_forget", [1, 8])
    br_d = din("b_r36", [1, 36])
    w_lat_d = din("w_lat", [D, 384])
    w_kr_d = din("w_kr2", [D, 192])
    w_fq_d = din("w_fq", [D, 512])
    w_fk_d = din("w_fk", [D, 512])
    w_fv_d = din("w_fv", [D, 512])
    w_f_d = din("w_f", [D, 8])
    w_ga_d = din("w_ga", [D, D])
    w_gb_d = din("w_gb", [D, D])
    w_uq_d = din("w_uq", [256, 768])
    w_uqs_d = din("w_uq_sw", [256, 768])
    w_uk_d = din("w_uk", [128, 512])
    w_uv_d = din("w_uv", [128, 512])
    w_oa_d = din("w_o_mla", [512, D])
    w_ob_d = din("w_o_fox", [512, D])
    w_out_d = din("w_out", [D, D])
    w_r_d = din("w_r36", [D, 36])
    w_eg_d = din("w_e_gate", [NE * D * EFF // 2048, 2048])
    w_eu_d = din("w_e_up", [NE * D * EFF // 2048, 2048])
    w_ed_d = din("w_e_down", [NE * EFF * D // 2048, 2048])
    cf_d = din("consts_f", [128, 512])
    cb_d = din("consts_b", [128, 1024], BF16)
    out_d = nc.dram_tensor("out", [S, D], F32, kind="ExternalOutput").ap()

    ga_s = nc.dram_tensor("ga_s", [D, S], BF16).ap()
    gb_s = nc.dram_tensor("gb_s", [D, S], BF16).ap()
    oa_s = nc.dram_tensor("oa_s", [512, S], BF16).ap()
    ob_s = nc.dram_tensor("ob_s", [512, S], BF16).ap()
    h_s = nc.dram_tensor("h_s", [S, D], F32).ap()

    dbg_outs = {}

    cf = nc.alloc_sbuf_tensor("cf", [128, 512], F32)
    cb = nc.alloc_sbuf_tensor("cb", [128, 1024], BF16)
    actT = nc.alloc_sbuf_tensor("actT", [128, 8, S], BF16)
    comb = nc.alloc_sbuf_tensor("comb", [128, NT, NE], F32)
    ps = nc.alloc_psum_tensor("ps", [128, 8, 512], F32)

    identf = cf[:, 0:128]
    utri = cf[:, 128:256]
    onesf = cf[:, 256:384]
    freq_col = cf[:, 384:385]
    freq_abs = cf[:, 385:386]
    identb = cb[:, 0:128]
    mask_fox = cb[:, 128:256]
    mask_mla = cb[:, 256:384]
    esel = cb[0:8, 384:384 + 8 * 65]


    def psb(b):
        return ps[:, b, :]

    def psb16(b):
        return ps[:, b, :].bitcast(BF16)

    def add_dbg(P, name, ap, shape, dt, reads):
        t = nc.dram_tensor("dbg_" + name, list(shape), dt, kind="ExternalOutput").ap()
        dbg_outs[name] = t
        P.op(SP, lambda e: e.dma_start(out=t, in_=ap), reads=reads, dma=True)

    def wload(P, dst, src_ap, res, eng=POOL):
        P.op(eng, lambda e: e.dma_start(out=dst, in_=src_ap), writes=[res], dma=True)

    def kc_view(w_ap):
        return w_ap.rearrange("(kc p) n -> p kc n", p=128)

    P = Prog(nc)
    wload(P, cf[:], cf_d, "cf", eng=SP)
    wload(P, cb[:], cb_d, "cb", eng=SP)
    with contextlib.ExitStack() as st:
        gmix = st.enter_context(nc.sbuf_tensor("gmix", [128, D], F32))
        xt = [st.enter_context(nc.sbuf_tensor("xt%d" % i, [128, D], F32)) for i in range(3)]
        xs = [st.enter_context(nc.sbuf_tensor("xs%d" % i, [128, D], BF16)) for i in range(2)]
        sq = st.enter_context(nc.sbuf_tensor("sq", [128, D], BF16))
        stat = st.enter_context(nc.sbuf_tensor("stat", [128, NT, 2], F32))
        P.op(SP, lambda e: e.dma_start(out=gmix[:], in_=g_mix_d[0].partition_broadcast(128)), writes=["gmix"], dma=True)
        P.op(DVE, lambda e: e.memset(stat[:], 0.0), writes=["stat"])

        def p1_copy(t):
            bk = t % 2
            P.op(ACT, lambda e: e.copy(
                actT[:, :, t * 128:(t + 1) * 128], psb16(bk).rearrange("p (a b) -> p a b", a=8)),
                reads=[("ps", bk)], writes=[("actT", t)])

        for t in range(NT):
            xb = xt[t % 3]
            xsb = xs[t % 2]
            bk = t % 2
            P.op(SP, lambda e, xb=xb, t=t: e.dma_start(out=xb[:], in_=x_d[t * 128:(t + 1) * 128, :]),
                 writes=[("xt", t % 3)], dma=True)
            P.op(ACT, lambda e, xb=xb, t=t: e.activation(sq[:], xb[:], AF.Square, accum_out=stat[:, t, 0:1]),
                 reads=[("xt", t % 3), "stat"], writes=["sq", ("stat", t)])
            P.op(ACT, lambda e, t=t: e.activation(stat[:, t, 1:2], stat[:, t, 0:1], AF.Sqrt, bias=EPS, scale=1.0 / D),
                 reads=[("stat", t)], writes=[("stat1", t)])
            if t > 0:
                p1_copy(t - 1)
            P.op(DVE, lambda e, t=t: e.reciprocal(stat[:, t, 1:2], stat[:, t, 1:2]),
                 reads=[("stat1", t)], writes=[("stat1", t)])
            P.op(DVE, lambda e, xb=xb, xsb=xsb, t=t: e.scalar_tensor_tensor(
                xsb[:], xb[:], stat[:, t, 1:2], gmix[:], ALU.mult, ALU.mult),
                reads=[("xt", t % 3), ("stat1", t), "gmix"], writes=[("xs", t % 2)])
            for kc in range(8):
                P.op(PE, lambda e, xsb=xsb, kc=kc, bk=bk: e.transpose(
                    psb16(bk)[:, kc * 128:(kc + 1) * 128], xsb[:, kc * 128:(kc + 1) * 128], identb),
                    reads=[("xs", t % 2), "cb"], writes=[("ps", bk)])
        p1_copy(NT - 1)
        if "xnT" in debug:
            add_dbg(P, "xnT", actT[:, :, 0:512], [128, 8, 512], BF16, [("actT", t) for t in range(4)])
    P.emit("p1")
    if stop_after <= 1:
        return nc, dbg_outs

    st_f = contextlib.ExitStack()
    Gc = st_f.enter_context(nc.sbuf_tensor("Gc", [128, NT, 8], F32))
    FsT = st_f.enter_context(nc.sbuf_tensor("FsT", [8, S], BF16))
    P = Prog(nc)
    with contextlib.ExitStack() as st:
        wf = st.enter_context(nc.sbuf_tensor("wf", [128, 8, 8], BF16))
        wga = st.enter_context(nc.sbuf_tensor("wga", [128, 8, D], BF16))
        wgb = st.enter_context(nc.sbuf_tensor("wgb", [128, 8, D], BF16))
        bfb = st.enter_context(nc.sbuf_tensor("bfb", [128, 8], F32))
        lf = st.enter_context(nc.sbuf_tensor("lf", [128, NT, 8], F32))
        tot = st.enter_context(nc.sbuf_tensor("tot", [128, NT, 8], F32))
        off = st.enter_context(nc.sbuf_tensor("off", [128, NT, 8], F32))
        gst = [st.enter_context(nc.sbuf_tensor("gst%d" % i, [128, 512], BF16)) for i in range(4)]
        wload(P, wf[:], kc_view(w_f_d), "wf")
        wload(P, wga[:], kc_view(w_ga_d), "wga")
        wload(P, wgb[:], kc_view(w_gb_d), "wgb")
        P.op(SP, lambda e: e.dma_start(out=bfb[:], in_=bf_d[0].partition_broadcast(128)), writes=["bfb"], dma=True)
        for t in range(NT):
            for kc in range(8):
                P.op(PE, lambda e, t=t, kc=kc: e.matmul(
                    psb(0)[:, t * 8:(t + 1) * 8], actT[:, kc, t * 128:(t + 1) * 128], wf[:, kc, :],
                    start=(kc == 0), stop=(kc == 7)),
                    reads=[("actT", t), "wf"], writes=[("ps", 0)])
        P.op(DVE, lambda e: e.tensor_tensor(
            lf[:], psb(0)[:, 0:256].rearrange("p (t h) -> p t h", h=8),
            bfb[:].unsqueeze(1).broadcast_to([128, NT, 8]), ALU.add),
            reads=[("ps", 0), "bfb"], writes=["lf"])
        P.op(ACT, lambda e: e.activation(lf[:], lf[:], AF.Exp, scale=-1.0), reads=["lf"], writes=["lf"])
        P.op(ACT, lambda e: e.activation(lf[:], lf[:], AF.Ln, bias=1.0), reads=["lf"], writes=["lf"])
        lf2 = lf[:].rearrange("p t h -> p (t h)")
        P.op(PE, lambda e: e.matmul(psb(1)[:, 0:256], utri, lf2, start=True, stop=True),
             reads=["lf", "cf"], writes=[("ps", 1)])
        P.op(PE, lambda e: e.matmul(psb(2)[:, 0:256], onesf, lf2, start=True, stop=True),
             reads=["lf", "cf"], writes=[("ps", 2)])
        P.op(DVE, lambda e: e.tensor_copy(tot[:], psb(2)[:, 0:256].rearrange("p (t h) -> p t h", h=8)),
             reads=[("ps", 2)], writes=["tot"])
        P.op(DVE, lambda e: e.memset(off[:, 0, :], 0.0), writes=["off"])
        for t in range(1, NT):
            P.op(DVE, lambda e, t=t: e.tensor_tensor(off[:, t, :], off[:, t - 1, :], tot[:, t - 1, :], ALU.add),
                 reads=["off", "tot"], writes=["off"])
        P.op(DVE, lambda e: e.tensor_tensor(
            Gc[:], psb(1)[:, 0:256].rearrange("p (t h) -> p t h", h=8), off[:], ALU.add),
            reads=[("ps", 1), "off"], writes=["Gc"])
        for g4 in range(8):
            bk = 3 + (g4 % 2)
            for i in range(4):
                t = g4 * 4 + i
                P.op(PE, lambda e, t=t, i=i, bk=bk: e.transpose(
                    psb(bk)[0:8, i * 128:(i + 1) * 128], Gc[:, t, :], identf),
                    reads=["Gc", "cf"], writes=[("ps", bk)])
            P.op(ACT, lambda e, g4=g4, bk=bk: e.mul(FsT[:, g4 * 512:(g4 + 1) * 512], psb(bk)[0:8, :], -1.0 / FOX_SCALE),
                 reads=[("ps", bk)], writes=["FsT"])
        n = 0
        for (wg_, dst) in ((wga, ga_s), (wgb, gb_s)):
            wname = "wga" if wg_ is wga else "wgb"
            for dc in range(8):
                for q in range(NQ):
                    bk = 5 + (n % 3)
                    sb = gst[n % 4]
                    for kc in range(8):
                        P.op(PE, lambda e, wg_=wg_, dc=dc, q=q, kc=kc, bk=bk: e.matmul(
                            psb(bk), wg_[:, kc, dc * 128:(dc + 1) * 128], actT[:, kc, q * 512:(q + 1) * 512],
                            start=(kc == 0), stop=(kc == 7)),
                            reads=[wname] + [("actT", q * 4 + i) for i in range(4)], writes=[("ps", bk)])
                    P.op(ACT, lambda e, sb=sb, bk=bk: e.activation(sb[:], psb(bk), AF.Sigmoid),
                         reads=[("ps", bk)], writes=[("gst", n % 4)])
                    P.op(SP, lambda e, sb=sb, dst=dst, dc=dc, q=q: e.dma_start(
                        out=dst[dc * 128:(dc + 1) * 128, q * 512:(q + 1) * 512], in_=sb[:]),
                        reads=[("gst", n % 4)], dma=True)
                    n += 1
        if "Gc" in debug:
            add_dbg(P, "Gc", Gc[:], [128, NT, 8], F32, ["Gc"])
            add_dbg(P, "FsT", FsT[:], [8, S], BF16, ["FsT"])
    P.emit("p2a")
    if stop_after <= 2:
        return nc, dbg_outs

    def attention_head(P, h, kdim, qT, kT, vx, scale, bias_fn, mask, o_dst, bufs, hname):
        pt, rrow, rbc, ost = bufs
        pairs = [(I, J) for I in range(NQ) for J in range(4 * I + 4)]
        npairs = len(pairs)

        def emit_qk(n):
            I, J = pairs[n]
            j = J - 4 * I
            c0 = max(0, j) * 128
            sbk = n % 3
            P.op(PE, lambda e: e.matmul(
                psb(sbk)[:, c0:512], kT[0:kdim, J * 128:(J + 1) * 128], qT[0:kdim, I * 512 + c0:(I + 1) * 512],
                start=True, stop=(j < 0)),
                reads=[hname + "q", hname + "k"], writes=[("ps", sbk)])
            if j >= 0:
                P.op(PE, lambda e: e.matmul(
                    psb(sbk)[:, c0:c0 + 128], identb, mask, start=False, stop=True),
                    reads=["cb"], writes=[("ps", sbk)])

        def emit_exp(n):
            I, J = pairs[n]
            j = J - 4 * I
            c0 = max(0, j) * 128
            sbk = n % 3
            ptb = pt[n % 3]
            bias = bias_fn(J)
            P.op(ACT, lambda e: e.activation(
                ptb[:, c0:512], psb(sbk)[:, c0:512], AF.Exp, bias=bias, scale=scale),
                reads=[("ps", sbk), "Gc"], writes=[("pt", n % 3)])

        def emit_pv(n):
            I, J = pairs[n]
            j = J - 4 * I
            c0 = max(0, j) * 128
            ob = 3 + (I % 2)
            nJ = 4 * I + 4
            ptb = pt[n % 3]
            P.op(PE, lambda e: e.matmul(
                psb(ob)[0:65, c0:512], vx[:, J, :], ptb[:, c0:512], start=(J == 0), stop=(J == nJ - 1)),
                reads=[("pt", n % 3), hname + "v"], writes=[("ps", ob)])

        def finalize(I):
            ob = 3 + (I % 2)
            P.op(DVE, lambda e: e.reciprocal(rrow[64:65, :], psb(ob)[64:65, :]),
                 reads=[("ps", ob)], writes=["rrow"])
            P.op(PE, lambda e: e.matmul(psb(5)[0:64, :], onesf[64:65, 0:64], rrow[64:65, :], start=True, stop=True),
                 reads=["rrow", "cf"], writes=[("ps", 5)])
            P.op(ACT, lambda e: e.copy(rbc[0:64, :], psb(5)[0:64, :]), reads=[("ps", 5)], writes=["rbc"])
            osb = ost[I % 2]
            P.op(DVE, lambda e: e.tensor_tensor(osb[0:64, :], psb(ob)[0:64, :], rbc[0:64, :], ALU.mult),
                 reads=[("ps", ob), "rbc"], writes=[("ost", I % 2)])
            P.op(SP, lambda e: e.dma_start(
                out=o_dst[h * 64:(h + 1) * 64, I * 512:(I + 1) * 512], in_=osb[0:64, :]),
                reads=[("ost", I % 2)], writes=["odram"], dma=True)

        pending = []
        emit_qk(0)
        emit_exp(0)
        emit_qk(1)
        emit_exp(1)
        for n in range(npairs):
            if n + 2 < npairs:
                emit_qk(n + 2)
                emit_exp(n + 2)
            emit_pv(n)
            I, J = pairs[n]
            if J == 4 * I + 3:
                pending.append((n + 3, I))
            while pending and pending[0][0] <= n:
                finalize(pending.pop(0)[1])
        for _, I in pending:
            finalize(I)

    P = Prog(nc)
    with contextlib.ExitStack() as st:
        wfq = st.enter_context(nc.sbuf_tensor("wfq", [128, 8, 512], BF16))
        wfk = st.enter_context(nc.sbuf_tensor("wfk", [128, 8, 512], BF16))
        wfv = st.enter_context(nc.sbuf_tensor("wfv", [128, 8, 512], BF16))
        qTs = [st.enter_context(nc.sbuf_tensor("fqT%d" % i, [65, S], BF16)) for i in range(2)]
        kTs = [st.enter_context(nc.sbuf_tensor("fkT%d" % i, [65, S], BF16)) for i in range(2)]
        vxs = [st.enter_context(nc.sbuf_tensor("fvx%d" % i, [128, NT, 65], BF16)) for i in range(2)]
        pt = [st.enter_context(nc.sbuf_tensor("pt%d" % i, [128, 512], BF16)) for i in range(3)]
        rrow = st.enter_context(nc.sbuf_tensor("rrow", [65, 512], F32))
        rbc = st.enter_context(nc.sbuf_tensor("rbc", [64, 512], F32))
        ost = [st.enter_context(nc.sbuf_tensor("ost%d" % i, [64, 512], BF16)) for i in range(2)]
        wload(P, wfq[:], kc_view(w_fq_d), "wfq")
        wload(P, wfk[:], kc_view(w_fk_d), "wfk")
        wload(P, wfv[:], kc_view(w_fv_d), "wfv")
        for i in range(2):
            P.op(POOL, lambda e, i=i: e.memset(kTs[i][64:65, :], 1.0), writes=[("fk", i)])
            P.op(POOL, lambda e, i=i: e.memset(vxs[i][:, :, 64:65], 1.0), writes=[("fv", i)])
        allact = [("actT", t) for t in range(NT)]
        for h in range(NH):
            b = h % 2
            qT, kT, vx = qTs[b], kTs[b], vxs[b]
            hn = "f%d" % b
            for q in range(NQ):
                bk = 6 + (q % 2)
                P.op(PE, lambda e, h=h, q=q, bk=bk: e.matmul(
                    psb(bk)[0:65, :], esel[:, h * 65:(h + 1) * 65], FsT[:, q * 512:(q + 1) * 512], start=True, stop=False),
                    reads=["FsT", "cb"], writes=[("ps", bk)])
                for kc in range(8):
                    P.op(PE, lambda e, h=h, q=q, kc=kc, bk=bk: e.matmul(
                        psb(bk)[0:64, :], wfq[:, kc, h * 64:(h + 1) * 64], actT[:, kc, q * 512:(q + 1) * 512],
                        start=False, stop=(kc == 7)),
                        reads=["wfq"] + allact[q * 4:q * 4 + 4], writes=[("ps", bk)])
                P.op(ACT, lambda e, qT=qT, q=q, bk=bk: e.copy(qT[0:65, q * 512:(q + 1) * 512], psb(bk)[0:65, :]),
                     reads=[("ps", bk)], writes=[hn + "q"])
            for q in range(NQ):
                bk = 6 + (q % 2)
                for kc in range(8):
                    P.op(PE, lambda e, h=h, q=q, kc=kc, bk=bk: e.matmul(
                        psb(bk)[0:64, :], wfk[:, kc, h * 64:(h + 1) * 64], actT[:, kc, q * 512:(q + 1) * 512],
                        start=(kc == 0), stop=(kc == 7)),
                        reads=["wfk"] + allact[q * 4:q * 4 + 4], writes=[("ps", bk)])
                P.op(DVE, lambda e, kT=kT, q=q, bk=bk: e.tensor_copy(kT[0:64, q * 512:(q + 1) * 512], psb(bk)[0:64, :]),
                     reads=[("ps", bk), ("fk", b)], writes=[hn + "k"])
            for g8 in range(4):
                bk = 6 + (g8 % 2)
                for i in range(8):
                    t = g8 * 8 + i
                    for kc in range(8):
                        P.op(PE, lambda e, h=h, t=t, i=i, kc=kc, bk=bk: e.matmul(
                            psb(bk)[:, i * 64:(i + 1) * 64], actT[:, kc, t * 128:(t + 1) * 128], wfv[:, kc, h * 64:(h + 1) * 64],
                            start=(kc == 0), stop=(kc == 7)),
                            reads=["wfv", ("actT", t)], writes=[("ps", bk)])
                P.op(DVE, lambda e, vx=vx, g8=g8, bk=bk: e.tensor_copy(
                    vx[:, g8 * 8:(g8 + 1) * 8, 0:64], psb(bk).rearrange("p (t d) -> p t d", d=64)),
                    reads=[("ps", bk), ("fv", b)], writes=[hn + "v"])
            attention_head(P, h, 65, qT, kT, vx, FOX_SCALE, lambda J, h=h: Gc[:, J, h:h + 1], mask_fox,
                           ob_s, (pt, rrow, rbc, ost), hn)
        if "ob" in debug:
            add_dbg(P, "ob", ob_s, [512, S], BF16, ["odram"])
    P.emit("p3")
    st_f.close()
    if stop_after <= 3:
        return nc, dbg_outs

    st_a = contextlib.ExitStack()
    cqnT = st_a.enter_context(nc.sbuf_tensor("cqnT", [128, 3, S], BF16))
    krT = st_a.enter_context(nc.sbuf_tensor("krT", [96, S], BF16))
    cosT = st_a.enter_context(nc.sbuf_tensor("cosT", [96, S], BF16))
    sinT = st_a.enter_context(nc.sbuf_tensor("sinT", [96, S], BF16))
    R = slice(64, 96)
    allc = [("cqnT", t) for t in range(NT)]
    P = Prog(nc)
    with contextlib.ExitStack() as st:
        wlat = st.enter_context(nc.sbuf_tensor("wlat", [128, 8, 384], BF16))
        wkr = st.enter_context(nc.sbuf_tensor("wkr", [128, 8, 192], BF16))
        gcq = st.enter_context(nc.sbuf_tensor("gcq", [128, 384], F32))
        lat = [st.enter_context(nc.sbuf_tensor("lat%d" % i, [128, 384], BF16)) for i in range(2)]
        lsq = st.enter_context(nc.sbuf_tensor("lsq", [128, 384], BF16))
        lst = st.enter_context(nc.sbuf_tensor("lst", [128, NT, 4], F32))
        posi = st.enter_context(nc.sbuf_tensor("posi", [96, 512], I32))
        ang = st.enter_context(nc.sbuf_tensor("ang", [96, 512], F32))
        uu = st.enter_context(nc.sbuf_tensor("uu", [96, 512], F32))
        ki = st.enter_context(nc.sbuf_tensor("ki", [96, 512], I32))
        kf = st.enter_context(nc.sbuf_tensor("kf", [96, 512], F32))
        rr = st.enter_context(nc.sbuf_tensor("rr", [96, 512], F32))
        hs_ = st.enter_context(nc.sbuf_tensor("hs_", [96, 512], F32))
        t1 = st.enter_context(nc.sbuf_tensor("t1", [96, 512], F32))
        t2 = st.enter_context(nc.sbuf_tensor("t2", [96, 512], F32))
        wload(P, wlat[:], kc_view(w_lat_d), "wlat")
        wload(P, wkr[:], kc_view(w_kr_d), "wkr")
        P.op(SP, lambda e: e.dma_start(out=gcq[:, 0:256], in_=g_cq_d[0].partition_broadcast(128)), writes=["gcq"], dma=True)
        P.op(SP, lambda e: e.dma_start(out=gcq[:, 256:384], in_=g_ckv_d[0].partition_broadcast(128)), writes=["gcq"], dma=True)
        for q in range(NQ):
            tk = slice(q * 512, (q + 1) * 512)
            P.op(SP, lambda e, tk=tk: e.dma_start(out=posi[R, :], in_=pos_d[0, tk].partition_broadcast(32)),
                 writes=["posi"], dma=True)
            P.op(DVE, lambda e: e.tensor_copy(ang[R, :], posi[R, :]), reads=["posi"], writes=["ang"])
            P.op(DVE, lambda e: e.tensor_scalar(ang[R, :], ang[R, :], freq_abs[R, :], None, ALU.mult),
                 reads=["ang", "cf"], writes=["ang"])
            P.op(DVE, lambda e: e.tensor_scalar(uu[R, :], ang[R, :], 1.0 / (2 * np.pi), None, ALU.mult),
                 reads=["ang"], writes=["uu"])
            P.op(DVE, lambda e: e.tensor_copy(ki[R, :], uu[R, :]), reads=["uu"], writes=["ki"])
            P.op(DVE, lambda e: e.tensor_copy(kf[R, :], ki[R, :]), reads=["ki"], writes=["kf"])
            P.op(DVE, lambda e: e.scalar_tensor_tensor(rr[R, :], kf[R, :], -TWO_PI_HI, ang[R, :], ALU.mult, ALU.add),
                 reads=["kf", "ang"], writes=["rr"])
            P.op(DVE, lambda e: e.scalar_tensor_tensor(rr[R, :], kf[R, :], -TWO_PI_LO, rr[R, :], ALU.mult, ALU.add),
                 reads=["kf", "rr"], writes=["rr"])
            P.op(DVE, lambda e: e.tensor_scalar(rr[R, :], rr[R, :], 3.1415925, -3.1415925, ALU.min, ALU.max),
                 reads=["rr"], writes=["rr"])
            P.op(ACT, lambda e, tk=tk: e.activation(sinT[R, tk], rr[R, :], AF.Sin, scale=cf[R, 386:387]),
                 reads=["rr", "cf"], writes=["sinT"])
            P.op(ACT, lambda e: e.activation(hs_[R, :], rr[R, :], AF.Sin, scale=0.5), reads=["rr"], writes=["hs_"])
            P.op(DVE, lambda e: e.tensor_tensor(hs_[R, :], hs_[R, :], hs_[R, :], ALU.mult), reads=["hs_"], writes=["hs_"])
            P.op(DVE, lambda e, tk=tk: e.tensor_scalar(cosT[R, tk], hs_[R, :], -2.0, 1.0, ALU.mult, ALU.add),
                 reads=["hs_"], writes=["cosT"])
        P.op(DVE, lambda e: e.memset(lst[:], 0.0), writes=["lst"])
        for t in range(NT):
            bk = 6 + (t % 2)
            lb = lat[t % 2]
            for kc in range(8):
                P.op(PE, lambda e, t=t, kc=kc, bk=bk: e.matmul(
                    psb(bk)[:, 0:384], actT[:, kc, t * 128:(t + 1) * 128], wlat[:, kc, :], start=(kc == 0), stop=(kc == 7)),
                    reads=["wlat", ("actT", t)], writes=[("ps", bk)])
            P.op(ACT, lambda e, t=t, bk=bk: e.activation(lsq[:, 0:256], psb(bk)[:, 0:256], AF.Square, accum_out=lst[:, t, 0:1]),
                 reads=[("ps", bk), "lst"], writes=["lsq", ("lst", t)])
            P.op(ACT, lambda e, t=t, bk=bk: e.activation(lsq[:, 256:384], psb(bk)[:, 256:384], AF.Square, accum_out=lst[:, t, 1:2]),
                 reads=[("ps", bk), ("lst", t)], writes=["lsq", ("lst", t)])
            P.op(ACT, lambda e, t=t: e.activation(lst[:, t, 2:3], lst[:, t, 0:1], AF.Sqrt, bias=EPS, scale=1.0 / 256),
                 reads=[("lst", t)], writes=[("lst2", t)])
            P.op(ACT, lambda e, t=t: e.activation(lst[:, t, 3:4], lst[:, t, 1:2], AF.Sqrt, bias=EPS, scale=1.0 / 128),
                 reads=[("lst", t)], writes=[("lst3", t)])
            P.op(DVE, lambda e, t=t: e.reciprocal(lst[:, t, 2:4], lst[:, t, 2:4]),
                 reads=[("lst2", t), ("lst3", t)], writes=[("lst2", t), ("lst3", t)])
            P.op(DVE, lambda e, t=t, bk=bk, lb=lb: e.scalar_tensor_tensor(
                lb[:, 0:256], psb(bk)[:, 0:256], lst[:, t, 2:3], gcq[:, 0:256], ALU.mult, ALU.mult),
                reads=[("ps", bk), ("lst2", t), "gcq"], writes=[("lat", t % 2)])
            P.op(DVE, lambda e, t=t, bk=bk, lb=lb: e.scalar_tensor_tensor(
                lb[:, 256:384], psb(bk)[:, 256:384], lst[:, t, 3:4], gcq[:, 256:384], ALU.mult, ALU.mult),
                reads=[("ps", bk), ("lst3", t), "gcq"], writes=[("lat", t % 2)])
            tb = 4 + (t % 2)
            for c in range(3):
                P.op(PE, lambda e, lb=lb, c=c, tb=tb: e.transpose(
                    psb16(tb)[:, c * 128:(c + 1) * 128], lb[:, c * 128:(c + 1) * 128], identb),
                    reads=[("lat", t % 2), "cb"], writes=[("ps", tb)])
            P.op(ACT, lambda e, t=t, tb=tb: e.copy(
                cqnT[:, :, t * 128:(t + 1) * 128], psb16(tb)[:, 0:384].rearrange("p (a b) -> p a b", a=3)),
                reads=[("ps", tb)], writes=[("cqnT", t)])
        for q in range(NQ):
            tk = slice(q * 512, (q + 1) * 512)
            for v in range(2):
                for kc in range(8):
                    P.op(PE, lambda e, q=q, v=v, kc=kc: e.matmul(
                        psb(6 + v)[0:96, :], wkr[:, kc, v * 96:(v + 1) * 96], actT[:, kc, q * 512:(q + 1) * 512],
                        start=(kc == 0), stop=(kc == 7)),
                        reads=["wkr"] + [("actT", q * 4 + i) for i in range(4)], writes=[("ps", 6 + v)])
            P.op(DVE, lambda e, tk=tk: e.tensor_tensor(t1[R, :], psb(6)[R, :], cosT[R, tk], ALU.mult),
                 reads=[("ps", 6), "cosT"], writes=["t1"])
            P.op(DVE, lambda e, tk=tk: e.tensor_tensor(t2[R, :], psb(7)[R, :], sinT[R, tk], ALU.mult),
                 reads=[("ps", 7), "sinT"], writes=["t2"])
            P.op(POOL, lambda e, tk=tk: e.tensor_tensor(krT[R, tk], t1[R, :], t2[R, :], ALU.add),
                 reads=["t1", "t2"], writes=["krT"])
        if "lat" in debug:
            add_dbg(P, "cqnT", cqnT[:, :, 0:512], [128, 3, 512], BF16, allc)
            add_dbg(P, "krT", krT[R, 0:512], [32, 512], BF16, ["krT"])
    P.emit("p4a")

    P = Prog(nc)
    with contextlib.ExitStack() as st:
        wuq = st.enter_context(nc.sbuf_tensor("wuq", [128, 2, 768], BF16))
        wuqs = st.enter_context(nc.sbuf_tensor("wuqs", [128, 2, 768], BF16))
        wuk = st.enter_context(nc.sbuf_tensor("wuk", [128, 512], BF16))
        wuv = st.enter_context(nc.sbuf_tensor("wuv", [128, 512], BF16))
        qTs = [st.enter_context(nc.sbuf_tensor("aqT%d" % i, [96, S], BF16)) for i in range(2)]
        kTs = [st.enter_context(nc.sbuf_tensor("akT%d" % i, [96, S], BF16)) for i in range(2)]
        vxs = [st.enter_context(nc.sbuf_tensor("avx%d" % i, [128, NT, 65], BF16)) for i in range(2)]
        pt = [st.enter_context(nc.sbuf_tensor("apt%d" % i, [128, 512], BF16)) for i in range(3)]
        rrow = st.enter_context(nc.sbuf_tensor("arrow", [65, 512], F32))
        rbc = st.enter_context(nc.sbuf_tensor("arbc", [64, 512], F32))
        ost = [st.enter_context(nc.sbuf_tensor("aost%d" % i, [64, 512], BF16)) for i in range(2)]
        t1 = st.enter_context(nc.sbuf_tensor("t1b", [96, 512], F32))
        t2 = st.enter_context(nc.sbuf_tensor("t2b", [96, 512], F32))
        wload(P, wuq[:], kc_view(w_uq_d), "wuq")
        wload(P, wuqs[:], kc_view(w_uqs_d), "wuqs")
        wload(P, wuk[:], w_uk_d, "wuk")
        wload(P, wuv[:], w_uv_d, "wuv")
        for i in range(2):
            P.op(POOL, lambda e, i=i: e.memset(vxs[i][:, :, 64:65], 1.0), writes=[("av", i)])
        for h in range(NH):
            b = h % 2
            qT, kT, vx = qTs[b], kTs[b], vxs[b]
            hn = "a%d" % b
            for q in range(NQ):
                tk = slice(q * 512, (q + 1) * 512)
                for v, w_ in enumerate((wuq, wuqs)):
                    for kc in range(2):
                        P.op(PE, lambda e, h=h, q=q, kc=kc, v=v, w_=w_: e.matmul(
                            psb(6 + v)[0:96, :], w_[:, kc, h * 96:(h + 1) * 96], cqnT[:, kc, q * 512:(q + 1) * 512],
                            start=(kc == 0), stop=(kc == 1)),
                            reads=["wuq", "wuqs"] + allc[q * 4:q * 4 + 4], writes=[("ps", 6 + v)])
                P.op(ACT, lambda e, qT=qT, tk=tk: e.copy(qT[0:64, tk], psb(6)[0:64, :]),
                     reads=[("ps", 6)], writes=[hn + "q"])
                P.op(DVE, lambda e, tk=tk: e.tensor_tensor(t1[R, :], psb(6)[R, :], cosT[R, tk], ALU.mult),
                     reads=[("ps", 6), "cosT"], writes=["t1"])
                P.op(DVE, lambda e, tk=tk: e.tensor_tensor(t2[R, :], psb(7)[R, :], sinT[R, tk], ALU.mult),
                     reads=[("ps", 7), "sinT"], writes=["t2"])
                P.op(POOL, lambda e, qT=qT, tk=tk: e.tensor_tensor(qT[R, tk], t1[R, :], t2[R, :], ALU.add),
                     reads=["t1", "t2"], writes=[hn + "q"])
            for q in range(NQ):
                tk = slice(q * 512, (q + 1) * 512)
                bk = 6 + (q % 2)
                P.op(PE, lambda e, h=h, q=q, bk=bk: e.matmul(
                    psb(bk)[0:64, :], wuk[:, h * 64:(h + 1) * 64], cqnT[:, 2, q * 512:(q + 1) * 512], start=True, stop=True),
                    reads=["wuk"] + allc[q * 4:q * 4 + 4], writes=[("ps", bk)])
                P.op(ACT, lambda e, kT=kT, tk=tk, bk=bk: e.copy(kT[0:64, tk], psb(bk)[0:64, :]),
                     reads=[("ps", bk)], writes=[hn + "k"])
            P.op(POOL, lambda e, kT=kT: e.tensor_copy(kT[R, :], krT[R, :]), reads=["krT"], writes=[hn + "k"])
            for g8 in range(4):
                bk = 6 + (g8 % 2)
                for i in range(8):
                    t = g8 * 8 + i
                    P.op(PE, lambda e, h=h, t=t, i=i, bk=bk: e.matmul(
                        psb(bk)[:, i * 64:(i + 1) * 64], cqnT[:, 2, t * 128:(t + 1) * 128], wuv[:, h * 64:(h + 1) * 64],
                        start=True, stop=True),
                        reads=["wuv", ("cqnT", t)], writes=[("ps", bk)])
                P.op(DVE, lambda e, vx=vx, g8=g8, bk=bk: e.tensor_copy(
                    vx[:, g8 * 8:(g8 + 1) * 8, 0:64], psb(bk).rearrange("p (t d) -> p t d", d=64)),
                    reads=[("ps", bk), ("av", b)], writes=[hn + "v"])
            attention_head(P, h, 96, qT, kT, vx, MLA_SCALE, lambda J: 0.0, mask_mla,
                           oa_s, (pt, rrow, rbc, ost), hn)
        if "oa" in debug:
            add_dbg(P, "oa", oa_s, [512, S], BF16, ["odram"])
    P.emit("p4")
    st_a.close()
    if stop_after <= 4:
        return nc, dbg_outs

    P = Prog(nc)
    with contextlib.ExitStack() as st:
        woa = st.enter_context(nc.sbuf_tensor("woa", [128, 4, D], BF16))
        wob = st.enter_context(nc.sbuf_tensor("wob", [128, 4, D], BF16))
        wout = st.enter_context(nc.sbuf_tensor("wout", [128, 8, D], BF16))
        wr = st.enter_context(nc.sbuf_tensor("wr", [128, 8, 36], BF16))
        gffn = st.enter_context(nc.sbuf_tensor("gffn", [128, D], F32))
        brb = st.enter_context(nc.sbuf_tensor("brb", [128, 36], F32))
        gat = [[st.enter_context(nc.sbuf_tensor("gat%d_%d" % (b_, i), [128, 8, 512], BF16)) for i in range(2)] for b_ in range(2)]
        oin = [[st.enter_context(nc.sbuf_tensor("oin%d_%d" % (b_, i), [128, 4, 512], BF16)) for i in range(2)] for b_ in range(2)]
        mixT = [st.enter_context(nc.sbuf_tensor("mixT%d" % i, [128, 8, 512], BF16)) for i in range(2)]
        m1 = [st.enter_context(nc.sbuf_tensor("m1_%d" % i, [128, 512], F32)) for i in range(2)]
        m2 = [st.enter_context(nc.sbuf_tensor("m2_%d" % i, [128, 512], F32)) for i in range(2)]
        xh = [st.enter_context(nc.sbuf_tensor("xh%d" % i, [128, D], F32)) for i in range(3)]
        hsb = [st.enter_context(nc.sbuf_tensor("hsb%d" % i, [128, D], BF16)) for i in range(2)]
        sq5 = st.enter_context(nc.sbuf_tensor("sq5", [128, D], BF16))
        st5 = st.enter_context(nc.sbuf_tensor("st5", [128, NT, 2], F32))
        rt = st.enter_context(nc.sbuf_tensor("rt", [128, 128], F32))
        wload(P, woa[:], w_oa_d.rearrange("(c p) n -> p c n", p=128), "woa")
        wload(P, wob[:], w_ob_d.rearrange("(c p) n -> p c n", p=128), "wob")
        wload(P, wout[:], kc_view(w_out_d), "wout")
        wload(P, wr[:], kc_view(w_r_d), "wr")
        P.op(SP, lambda e: e.dma_start(out=gffn[:], in_=g_ffn_d[0].partition_broadcast(128)), writes=["gffn"], dma=True)
        P.op(SP, lambda e: e.dma_start(out=brb[:], in_=br_d[0].partition_broadcast(128)), writes=["brb"], dma=True)
        P.op(DVE, lambda e: e.memset(st5[:], 0.0), writes=["st5"])
        xi = [0]

        def merge_loads(q):
            tk = slice(q * 512, (q + 1) * 512)
            b_ = q % 2
            P.op(SP, lambda e: e.dma_start(out=gat[b_][0][:], in_=ga_s[:, tk].rearrange("(c p) t -> p c t", p=128)),
                 writes=[("gat", b_, 0)], dma=True)
            P.op(SP, lambda e: e.dma_start(out=oin[b_][0][:], in_=oa_s[:, tk].rearrange("(c p) t -> p c t", p=128)),
                 writes=[("oin", b_, 0)], dma=True)
            P.op(SP, lambda e: e.dma_start(out=gat[b_][1][:], in_=gb_s[:, tk].rearrange("(c p) t -> p c t", p=128)),
                 writes=[("gat", b_, 1)], dma=True)
            P.op(SP, lambda e: e.dma_start(out=oin[b_][1][:], in_=ob_s[:, tk].rearrange("(c p) t -> p c t", p=128)),
                 writes=[("oin", b_, 1)], dma=True)

        def merge_dc(q, dc):
            b_ = q % 2
            mb = dc % 2
            for v, w_ in enumerate((woa, wob)):
                for pc in range(4):
                    P.op(PE, lambda e, v=v, w_=w_, pc=pc: e.matmul(
                        psb(v), w_[:, pc, dc * 128:(dc + 1) * 128], oin[b_][v][:, pc, :], start=(pc == 0), stop=(pc == 3)),
                        reads=["woa", "wob", ("oin", b_, v)], writes=[("ps", v)])
            P.op(DVE, lambda e: e.tensor_tensor(m1[mb][:], psb(0), gat[b_][0][:, dc, :], ALU.mult),
                 reads=[("ps", 0), ("gat", b_, 0)], writes=[("m1", mb)])
            P.op(DVE, lambda e: e.tensor_tensor(m2[mb][:], psb(1), gat[b_][1][:, dc, :], ALU.mult),
                 reads=[("ps", 1), ("gat", b_, 1)], writes=[("m2", mb)])
            P.op(POOL, lambda e: e.tensor_tensor(mixT[b_][:, dc, :], m1[mb][:], m2[mb][:], ALU.add),
                 reads=[("m1", mb), ("m2", mb)], writes=[("mixT", b_, dc)])

        def xload(t):
            P.op(SP, lambda e: e.dma_start(out=xh[t % 3][:], in_=x_d[t * 128:(t + 1) * 128, :]),
                 writes=[("xh", t % 3)], dma=True)

        merge_loads(0)
        xload(0)
        xload(1)
        for dc in range(8):
            merge_dc(0, dc)
        for q in range(NQ):
            if q + 1 < NQ:
                merge_loads(q + 1)
            for sub in range(4):
                t = q * 4 + sub
                xb = xh[t % 3]
                xr = ("xh", t % 3)
                if t + 2 < NT:
                    xload(t + 2)
                for ch in range(2):
                    bk = 2 + ch
                    for kc in range(8):
                        P.op(PE, lambda e, sub=sub, ch=ch, kc=kc, bk=bk, q=q: e.matmul(
                            psb(bk), mixT[q % 2][:, kc, sub * 128:(sub + 1) * 128], wout[:, kc, ch * 512:(ch + 1) * 512],
                            start=(kc == 0), stop=(kc == 7)),
                            reads=["wout"] + [("mixT", q % 2, d_) for d_ in range(8)], writes=[("ps", bk)])
                    P.op(DVE, lambda e, xb=xb, ch=ch, bk=bk: e.tensor_tensor(
                        xb[:, ch * 512:(ch + 1) * 512], psb(bk), xb[:, ch * 512:(ch + 1) * 512], ALU.add),
                        reads=[("ps", bk), xr], writes=[xr])
                P.op(SP, lambda e, xb=xb, t=t: e.dma_start(out=h_s[t * 128:(t + 1) * 128, :], in_=xb[:]),
                     reads=[xr], dma=True)
                if q + 1 < NQ:
                    merge_dc(q + 1, 2 * sub)
                P.op(ACT, lambda e, xb=xb, t=t: e.activation(sq5[:], xb[:], AF.Square, accum_out=st5[:, t, 0:1]),
                     reads=[xr, "st5"], writes=["sq5", ("st5", t)])
                P.op(ACT, lambda e, t=t: e.activation(st5[:, t, 1:2], st5[:, t, 0:1], AF.Sqrt, bias=EPS, scale=1.0 / D),
                     reads=[("st5", t)], writes=[("st51", t)])
                P.op(DVE, lambda e, t=t: e.reciprocal(st5[:, t, 1:2], st5[:, t, 1:2]),
                     reads=[("st51", t)], writes=[("st51", t)])
                hb = hsb[t % 2]
                P.op(DVE, lambda e, xb=xb, hb=hb, t=t: e.scalar_tensor_tensor(
                    hb[:], xb[:], st5[:, t, 1:2], gffn[:], ALU.mult, ALU.mult),
                    reads=[xr, ("st51", t), "gffn"], writes=[("hsb", t % 2)])
                tb = 4 + (t % 2)
                for kc in range(8):
                    P.op(PE, lambda e, hb=hb, kc=kc, tb=tb: e.transpose(
                        psb16(tb)[:, kc * 128:(kc + 1) * 128], hb[:, kc * 128:(kc + 1) * 128], identb),
                        reads=[("hsb", t % 2), "cb"], writes=[("ps", tb)])
                P.op(ACT, lambda e, t=t, tb=tb: e.copy(
                    actT[:, :, t * 128:(t + 1) * 128], psb16(tb).rearrange("p (a b) -> p a b", a=8)),
                    reads=[("ps", tb)], writes=[("actT", t)])
                if q + 1 < NQ:
                    merge_dc(q + 1, 2 * sub + 1)
                for kc in range(8):
                    P.op(PE, lambda e, t=t, kc=kc: e.matmul(
                        psb(6)[:, 0:36], actT[:, kc, t * 128:(t + 1) * 128], wr[:, kc, :], start=(kc == 0), stop=(kc == 7)),
                        reads=[("actT", t), "wr"], writes=[("ps", 6)])
                L = rt[:, 0:36]
                P.op(DVE, lambda e: e.tensor_tensor(rt[:, 0:36], psb(6)[:, 0:36], brb[:], ALU.add),
                     reads=[("ps", 6), "brb"], writes=["rt"])
                P.op(DVE, lambda e: e.reduce_max(rt[:, 36:37], rt[:, 0:4], axis=AX.X), reads=["rt"], writes=["rt"])
                P.op(DVE, lambda e: e.tensor_scalar(rt[:, 96:100], rt[:, 0:4], rt[:, 36:37], None, ALU.subtract),
                     reads=["rt"], writes=["rt"])
                P.op(DVE, lambda e: e.memset(rt[:, 37:38], 0.0), reads=["rt"], writes=["rt"])
                P.op(ACT, lambda e: e.activation(rt[:, 100:104], rt[:, 96:100], AF.Exp, accum_out=rt[:, 37:38]),
                     reads=["rt"], writes=["rt"])
                P.op(DVE, lambda e: e.reciprocal(rt[:, 37:38], rt[:, 37:38]), reads=["rt"], writes=["rt"])
                P.op(DVE, lambda e: e.tensor_scalar(rt[:, 38:42], rt[:, 0:4], rt[:, 36:37], None, ALU.is_equal),
                     reads=["rt"], writes=["rt"])
                P.op(DVE, lambda e: e.tensor_scalar(rt[:, 42:50], rt[:, 4:12], rt[:, 38:39], None, ALU.mult),
                     reads=["rt"], writes=["rt"])
                for g in range(1, 4):
                    P.op(DVE, lambda e, g=g: e.scalar_tensor_tensor(
                        rt[:, 42:50], rt[:, 4 + 8 * g:12 + 8 * g], rt[:, 38 + g:39 + g], rt[:, 42:50], ALU.mult, ALU.add),
                        reads=["rt"], writes=["rt"])
                P.op(DVE, lambda e: e.reduce_max(rt[:, 50:51], rt[:, 42:50], axis=AX.X), reads=["rt"], writes=["rt"])
                P.op(DVE, lambda e: e.tensor_scalar(rt[:, 51:59], rt[:, 42:50], rt[:, 50:51], None, ALU.is_equal),
                     reads=["rt"], writes=["rt"])
                P.op(DVE, lambda e: e.scalar_tensor_tensor(rt[:, 59:67], rt[:, 51:59], -1e30, rt[:, 42:50], ALU.mult, ALU.add),
                     reads=["rt"], writes=["rt"])
                P.op(DVE, lambda e: e.reduce_max(rt[:, 67:68], rt[:, 59:67], axis=AX.X), reads=["rt"], writes=["rt"])
                P.op(DVE, lambda e: e.tensor_scalar(rt[:, 68:76], rt[:, 59:67], rt[:, 67:68], None, ALU.is_equal),
                     reads=["rt"], writes=["rt"])
                P.op(DVE, lambda e: e.tensor_tensor(rt[:, 76:77], rt[:, 67:68], rt[:, 50:51], ALU.subtract),
                     reads=["rt"], writes=["rt"])
                P.op(ACT, lambda e: e.activation(rt[:, 76:77], rt[:, 76:77], AF.Exp), reads=["rt"], writes=["rt"])
                P.op(DVE, lambda e: e.tensor_scalar(rt[:, 77:78], rt[:, 76:77], 1.0, None, ALU.add), reads=["rt"], writes=["rt"])
                P.op(DVE, lambda e: e.reciprocal(rt[:, 77:78], rt[:, 77:78]), reads=["rt"], writes=["rt"])
                P.op(DVE, lambda e: e.tensor_tensor(rt[:, 78:79], rt[:, 77:78], rt[:, 37:38], ALU.mult),
                     reads=["rt"], writes=["rt"])
                P.op(DVE, lambda e: e.tensor_tensor(rt[:, 79:80], rt[:, 78:79], rt[:, 76:77], ALU.mult),
                     reads=["rt"], writes=["rt"])
                P.op(DVE, lambda e: e.tensor_scalar(rt[:, 80:88], rt[:, 51:59], rt[:, 78:79], None, ALU.mult),
                     reads=["rt"], writes=["rt"])
                P.op(DVE, lambda e: e.scalar_tensor_tensor(rt[:, 80:88], rt[:, 68:76], rt[:, 79:80], rt[:, 80:88], ALU.mult, ALU.add),
                     reads=["rt"], writes=["rt"])
                for g in range(4):
                    P.op(DVE, lambda e, g=g, t=t: e.tensor_scalar(
                        comb[:, t, 8 * g:8 * g + 8], rt[:, 80:88], rt[:, 38 + g:39 + g], None, ALU.mult),
                        reads=["rt"], writes=[("comb", t)])
        if "p5" in debug:
            add_dbg(P, "comb", comb[:], [128, NT, NE], F32, [("comb", t) for t in range(NT)])
            add_dbg(P, "hnT", actT[:, :, 2048:2560], [128, 8, 512], BF16, [("actT", t) for t in range(16, 20)])
    P.emit("p5")
    if stop_after <= 5:
        return nc, dbg_outs

    P = Prog(nc)
    TT = 2048
    NS8 = TT // 128
    with contextlib.ExitStack() as st:
        gfin = st.enter_context(nc.sbuf_tensor("gfin", [128, D], F32))
        yacc = st.enter_context(nc.sbuf_tensor("yacc", [128, TT // 128, D], F32))
        wgu = [st.enter_context(nc.sbuf_tensor("wgu%d" % i, [128, 2, 8, EFF], BF16)) for i in range(2)]
        wdn = [st.enter_context(nc.sbuf_tensor("wdn%d" % i, [128, 2, D], BF16)) for i in range(2)]
        sg = [st.enter_context(nc.sbuf_tensor("sg%d" % i, [128, 512], F32)) for i in range(2)]
        aT = [st.enter_context(nc.sbuf_tensor("aT%d" % i, [128, 2, 512], BF16)) for i in range(2)]
        sq6 = st.enter_context(nc.sbuf_tensor("sq6", [128, D], BF16))
        st6 = st.enter_context(nc.sbuf_tensor("st6", [128, NT, 2], F32))
        P.op(SP, lambda e: e.dma_start(out=gfin[:], in_=g_fin_d[0].partition_broadcast(128)), writes=["gfin"], dma=True)
        P.op(DVE, lambda e: e.memset(st6[:], 0.0), writes=["st6"])
        wg_v = w_eg_d.rearrange("(e r) c -> e (r c)", e=NE).rearrange("e (kc p f) -> e p kc f", p=128, f=EFF)
        wu_v = w_eu_d.rearrange("(e r) c -> e (r c)", e=NE).rearrange("e (kc p f) -> e p kc f", p=128, f=EFF)
        wd_v = w_ed_d.rearrange("(e r) c -> e (r c)", e=NE).rearrange("e (c p n) -> e p c n", p=128, n=D)
        it = 0
        dn = 0
        for tt in range(S // TT):
            for s8 in range(NS8):
                t = tt * NS8 + s8
                P.op(SP, lambda e, s8=s8, t=t: e.dma_start(out=yacc[:, s8, :], in_=h_s[t * 128:(t + 1) * 128, :]),
                     writes=[("yacc", s8)], dma=True)
            if "y0" in debug and tt == 1:
                add_dbg(P, "y0", yacc[:, 0, :], [128, D], F32, [("yacc", 0)])
            for ex in range(NE):
                wb = it % 2
                P.op(POOL, lambda e, ex=ex, wb=wb: e.dma_start(out=wgu[wb][:, 0, :, :], in_=wg_v[ex]), writes=[("wgu", wb)], dma=True)
                P.op(POOL, lambda e, ex=ex, wb=wb: e.dma_start(out=wgu[wb][:, 1, :, :], in_=wu_v[ex]), writes=[("wgu", wb)], dma=True)
                P.op(POOL, lambda e, ex=ex, wb=wb: e.dma_start(out=wdn[wb][:], in_=wd_v[ex]), writes=[("wdn", wb)], dma=True)
                if "p6" in debug and it == 0:
                    add_dbg(P, "wgu0", wgu[0][:], [128, 2, 8, EFF], BF16, [("wgu", 0)])
                    add_dbg(P, "wdn0", wdn[0][:], [128, 2, D], BF16, [("wdn", 0)])
                for half in range(TT // 512):
                    tok0 = tt * TT + half * 512
                    ab = aT[half % 2]
                    for c in range(2):
                        gb_, ub_ = 2 * c, 2 * c + 1
                        for v, bk in ((0, gb_), (1, ub_)):
                            for kc in range(8):
                                P.op(PE, lambda e, wb=wb, v=v, kc=kc, c=c, bk=bk, tok0=tok0: e.matmul(
                                    psb(bk), wgu[wb][:, v, kc, c * 128:(c + 1) * 128], actT[:, kc, tok0:tok0 + 512],
                                    start=(kc == 0), stop=(kc == 7)),
                                    reads=[("wgu", wb)], writes=[("ps", bk)])
                        sgb = sg[c]
                        P.op(ACT, lambda e, sgb=sgb, gb_=gb_: e.activation(sgb[:], psb(gb_), AF.Silu),
                             reads=[("ps", gb_)], writes=[("sg", c)])
                        P.op(DVE, lambda e, ab=ab, c=c, sgb=sgb, ub_=ub_: e.tensor_tensor(ab[:, c, :], psb(ub_), sgb[:], ALU.mult),
                             reads=[("ps", ub_), ("sg", c)], writes=[("aT", half % 2, c)])
                    for sub in range(4):
                        s8 = half * 4 + sub
                        t = tt * NS8 + s8
                        for ch in range(2):
                            bk = 4 + (dn % 4)
                            dn += 1
                            for c in range(2):
                                P.op(PE, lambda e, ab=ab, c=c, sub=sub, wb=wb, ch=ch, bk=bk: e.matmul(
                                    psb(bk), ab[:, c, sub * 128:(sub + 1) * 128], wdn[wb][:, c, ch * 512:(ch + 1) * 512],
                                    start=(c == 0), stop=(c == 1)),
                                    reads=[("aT", half % 2, 0), ("aT", half % 2, 1), ("wdn", wb)], writes=[("ps", bk)])
                            P.op(DVE, lambda e, s8=s8, ch=ch, bk=bk, t=t, ex=ex: e.scalar_tensor_tensor(
                                yacc[:, s8, ch * 512:(ch + 1) * 512], psb(bk), comb[:, t, ex:ex + 1],
                                yacc[:, s8, ch * 512:(ch + 1) * 512], ALU.mult, ALU.add),
                                reads=[("ps", bk), ("yacc", s8)], writes=[("yacc", s8)])
                it += 1
            for s8 in range(NS8):
                t = tt * NS8 + s8
                P.op(ACT, lambda e, s8=s8, t=t: e.activation(sq6[:], yacc[:, s8, :], AF.Square, accum_out=st6[:, t, 0:1]),
                     reads=[("yacc", s8), "st6"], writes=["sq6", ("st6", t)])
                P.op(ACT, lambda e, t=t: e.activation(st6[:, t, 1:2], st6[:, t, 0:1], AF.Sqrt, bias=EPS, scale=1.0 / D),
                     reads=[("st6", t)], writes=[("st61", t)])
                P.op(DVE, lambda e, t=t: e.reciprocal(st6[:, t, 1:2], st6[:, t, 1:2]), reads=[("st61", t)], writes=[("st61", t)])
                P.op(DVE, lambda e, s8=s8, t=t: e.scalar_tensor_tensor(
                    yacc[:, s8, :], yacc[:, s8, :], st6[:, t, 1:2], gfin[:], ALU.mult, ALU.mult),
                    reads=[("yacc", s8), ("st61", t), "gfin"], writes=[("yacc", s8)])
                P.op(SP, lambda e, s8=s8, t=t: e.dma_start(out=out_d[t * 128:(t + 1) * 128, :], in_=yacc[:, s8, :]),
                     reads=[("yacc", s8)], dma=True)
    P.emit("p6")
    return nc, dbg_outs


def _consts():
    cfm = np.zeros((128, 512), np.float32)
    cfm[:, 0:128] = np.eye(128, dtype=np.float32)
    cfm[:, 128:256] = np.triu(np.ones((128, 128), np.float32))
    cfm[:, 256:384] = 1.0
    p = np.arange(128)
    j = p % 16
    freq = (10000.0 ** (-(j.astype(np.float32)) / np.float32(16.0))).astype(np.float32)
    sgn = np.where((p % 32) < 16, -1.0, 1.0).astype(np.float32)
    cfm[:, 384] = sgn * freq
    cfm[:, 385] = freq
    cfm[:, 386] = sgn
    cbm = np.zeros((128, 1024), np.float32)
    cbm[:, 0:128] = np.eye(128)
    k = np.arange(128)[:, None]
    q = np.arange(128)[None, :]
    cbm[:, 128:256] = np.where(k > q, NEG, 0.0)
    cbm[:, 256:384] = np.where((k // 64) > (q // 64), NEG, 0.0)
    for h in range(8):
        cbm[h, 384 + h * 65 + 64] = 1.0
    return cfm, cbm.astype(ml_dtypes.bfloat16)


def _prep_inputs(inp):
    f = lambda a: np.ascontiguousarray(np.asarray(a, dtype=np.float32))
    w_in = f(inp["w_in"])
    kr = w_in[:, 384:416]
    swap = np.concatenate([np.arange(16, 32), np.arange(0, 16)])
    w_kr2 = np.concatenate([kr, kr, kr, kr, kr, kr[:, swap]], axis=1)
    w_uq = f(inp["w_uq"])
    idx = np.arange(768).reshape(8, 96).copy()
    idx[:, 64:96] = idx[:, 64:96][:, swap]
    w_uq_sw = w_uq[:, idx.reshape(-1)]
    cfm, cbm = _consts()
    shared = {
        "g_mix": f(inp["g_mix"]).reshape(1, -1),
        "g_ffn": f(inp["g_ffn"]).reshape(1, -1),
        "g_final": f(inp["g_final"]).reshape(1, -1),
        "g_cq": f(inp["g_cq"]).reshape(1, -1),
        "g_ckv": f(inp["g_ckv"]).reshape(1, -1),
        "b_forget": f(inp["b_forget"]).reshape(1, -1),
        "b_r36": np.concatenate([f(inp["b_group"]), f(inp["b_router"])]).reshape(1, -1),
        "w_lat": np.ascontiguousarray(w_in[:, 0:384]),
        "w_kr2": np.ascontiguousarray(w_kr2),
        "w_fq": np.ascontiguousarray(w_in[:, 416:928]),
        "w_fk": np.ascontiguousarray(w_in[:, 928:1440]),
        "w_fv": np.ascontiguousarray(w_in[:, 1440:1952]),
        "w_f": np.ascontiguousarray(w_in[:, 1952:1960]),
        "w_ga": np.ascontiguousarray(w_in[:, 1960:2984]),
        "w_gb": np.ascontiguousarray(w_in[:, 2984:4008]),
        "w_uq": w_uq,
        "w_uq_sw": np.ascontiguousarray(w_uq_sw),
        "w_uk": f(inp["w_uk"]),
        "w_uv": f(inp["w_uv"]),
        "w_o_mla": f(inp["w_o_mla"]),
        "w_o_fox": f(inp["w_o_fox"]),
        "w_out": f(inp["w_out"]),
        "w_r36": np.ascontiguousarray(np.concatenate([f(inp["w_group"]), f(inp["w_router"])], axis=1)),
        "w_e_gate": f(inp["w_e_gate"]).reshape(-1, 2048),
        "w_e_up": f(inp["w_e_up"]).reshape(-1, 2048),
        "w_e_down": f(inp["w_e_down"]).reshape(-1, 2048),
        "consts_f": cfm,
        "consts_b": cbm,
    }
    x = f(inp["x"])
    pos = np.ascontiguousarray(np.asarray(inp["positions"], dtype=np.int32))
    in_maps = []
    for b in range(8):
        m = dict(shared)
        m["x"] = x[b]
        m["pos"] = pos[b].reshape(1, -1)
        in_maps.append(m)
    return in_maps


def kernel(**inputs):
    in_maps = _prep_inputs(inputs)
    nc, _ = build_program()
    res = run_bass_kernel_spmd(nc, in_maps, core_ids=list(range(8)))
    out = np.stack([np.asarray(r["out"], dtype=np.float32) for r in res.results], axis=0)
    return out
```

```python
import contextlib
import numpy as np
import ml_dtypes
import concourse.bass as bass
import concourse.mybir as mybir
from concourse.bass_utils import run_bass_kernel_spmd

F32 = mybir.dt.float32
BF16 = mybir.dt.bfloat16
I32 = mybir.dt.int32
ALU = mybir.AluOpType
AF = mybir.ActivationFunctionType
AX = mybir.AxisListType

PE, ACT, DVE, POOL, SP = "tensor", "scalar", "vector", "gpsimd", "sync"
ENGINES = [PE, ACT, DVE, POOL, SP]

S = 4096
D = 1024
NT = 32
NQ = 8
NH = 8
NE = 32
EFF = 256
EPS = 1e-6
MLA_SCALE = 96.0 ** -0.5
FOX_SCALE = 0.125
NEG = -30000.0
TWO_PI_HI = 6.28125
TWO_PI_LO = 6.283185307179586 - 6.28125


class Op:
    __slots__ = ("eng", "fn", "deps", "is_dma", "marked", "count", "sem", "semval", "prewait", "nosem")

    def __init__(self, eng, fn, is_dma):
        self.eng = eng
        self.fn = fn
        self.deps = []
        self.is_dma = is_dma
        self.marked = False
        self.count = 0
        self.sem = None
        self.semval = 0
        self.prewait = None
        self.nosem = False


class Prog:
    def __init__(self, nc, dma_pool=8):
        self.nc = nc
        self.ops = {e: [] for e in ENGINES}
        self.last_write = {}
        self.readers = {}
        self.dma_pool = dma_pool
        self.dma_count = {e: 0 for e in ENGINES}

    def op(self, eng, fn, reads=(), writes=(), dma=False, nosem=False):
        o = Op(eng, fn, dma)
        o.nosem = nosem
        deps = {}
        for r in reads:
            w = self.last_write.get(r)
            if w is not None:
                deps[id(w)] = (w, "raw")
        for w_ in writes:
            for rd in self.readers.get(w_, ()):
                if id(rd) not in deps:
                    deps[id(rd)] = (rd, "war")
            w = self.last_write.get(w_)
            if w is not None:
                deps[id(w)] = (w, "waw")
        for d, kind in deps.values():
            if d is o:
                continue
            if (not d.is_dma) and (not dma) and d.eng == eng:
                if eng == PE or kind == "war":
                    continue
            o.deps.append(d)
            d.marked = True
        for r in reads:
            self.readers.setdefault(r, []).append(o)
        for w_ in writes:
            self.last_write[w_] = o
            self.readers[w_] = []
        if dma and not nosem:
            k = self.dma_count[eng]
            self.dma_count[eng] = k + 1
            o.sem = (eng, k % self.dma_pool)
            o.semval = 16 * (k // self.dma_pool + 1)
            if k >= self.dma_pool:
                o.prewait = (o.sem, o.semval - 16)
        self.ops[eng].append(o)
        return o

    def emit(self, name):
        nc = self.nc
        for e in ENGINES:
            c = 0
            for o in self.ops[e]:
                if not o.is_dma and o.marked:
                    c += 1
                    o.count = c
        with contextlib.ExitStack() as st:
            esem = {e: st.enter_context(nc.semaphore("s_%s_%s" % (name, e))) for e in ENGINES}
            dsem = {}
            for e in ENGINES:
                for i in range(min(self.dma_pool, self.dma_count[e])):
                    dsem[(e, i)] = st.enter_context(nc.semaphore("d_%s_%s_%d" % (name, e, i)))
            allsems = list(esem.values()) + list(dsem.values())
            with nc.Block() as cblk:
                def _clr(engobj):
                    for s_ in allsems:
                        engobj.sem_clear(s_)
                cblk.sync(_clr)
            block = st.enter_context(nc.Block())

            def run_engine(e, engobj):
                seen = {}
                for o in self.ops[e]:
                    need = {}
                    for d in o.deps:
                        if d.is_dma:
                            key = ("d", d.sem)
                            val = d.semval
                        else:
                            key = ("e", d.eng)
                            val = d.count
                        if need.get(key, 0) < val:
                            need[key] = val
                    if o.prewait is not None:
                        key = ("d", o.prewait[0])
                        if need.get(key, 0) < o.prewait[1]:
                            need[key] = o.prewait[1]
                    for key, val in need.items():
                        if seen.get(key, 0) >= val:
                            continue
                        seen[key] = val
                        s = dsem[key[1]] if key[0] == "d" else esem[key[1]]
                        engobj.wait_ge(s, val)
                    ins = o.fn(engobj)
                    if o.nosem:
                        continue
                    if o.is_dma:
                        ins.then_inc(dsem[o.sem], 16)
                    elif o.marked:
                        ins.then_inc(esem[e], 1)
                k = self.dma_count[e]
                for i in range(min(self.dma_pool, k)):
                    uses = (k - 1 - i) // self.dma_pool + 1
                    if uses > 0:
                        engobj.wait_ge(dsem[(e, i)], 16 * uses)

            for e in ENGINES:
                if not self.ops[e]:
                    continue
                getattr(block, e)(lambda engobj, e=e: run_engine(e, engobj))


def build_program(stop_after=99, debug=None):
    nc = bass.Bass("TRN2", target_bir_lowering=False)
    debug = debug or []

    def din(name, shape, dt=F32):
        return nc.dram_tensor(name, list(shape), dt, kind="ExternalInput").ap()

    x_d = din("x", [S, D])
    pos_d = din("pos", [1, S], I32)
    g_mix_d = din("g_mix", [1, D])
    g_ffn_d = din("g_ffn", [1, D])
    g_fin_d = din("g_final", [1, D])
    g_cq_d = din("g_cq", [1, 256])
    g_ckv_d = din("g_ckv", [1, 128])
    bf_d = din("b_forget", [1, 8])
    br_d = din("b_r36", [1, 36])
    w_lat_d = din("w_lat", [D, 384])
    w_kr_d = din("w_kr2", [D, 192])
    w_fq_d = din("w_fq", [D, 512])
    w_fk_d = din("w_fk", [D, 512])
    w_fv_d = din("w_fv", [D, 512])
    w_f_d = din("w_f", [D, 8])
    w_ga_d = din("w_ga", [D, D])
    w_gb_d = din("w_gb", [D, D])
    w_uq_d = din("w_uq", [256, 768])
    w_uqs_d = din("w_uq_sw", [256, 768])
    w_uk_d = din("w_uk", [128, 512])
    w_uv_d = din("w_uv", [128, 512])
    w_oa_d = din("w_o_mla", [512, D])
    w_ob_d = din("w_o_fox", [512, D])
    w_out_d = din("w_out", [D, D])
    w_r_d = din("w_r36", [D, 36])
    w_eg_d = din("w_e_gate", [NE * D * EFF // 2048, 2048])
    w_eu_d = din("w_e_up", [NE * D * EFF // 2048, 2048])
    w_ed_d = din("w_e_down", [NE * EFF * D // 2048, 2048])
    cf_d = din("consts_f", [128, 512])
    cb_d = din("consts_b", [128, 1024], BF16)
    out_d = nc.dram_tensor("out", [S, D], F32, kind="ExternalOutput").ap()

    ga_s = nc.dram_tensor("ga_s", [D, S], BF16).ap()
    gb_s = nc.dram_tensor("gb_s", [D, S], BF16).ap()
    oa_s = nc.dram_tensor("oa_s", [512, S], BF16).ap()
    ob_s = nc.dram_tensor("ob_s", [512, S], BF16).ap()
    h_s = nc.dram_tensor("h_s", [S, D], F32).ap()

    dbg_outs = {}

    cf = nc.alloc_sbuf_tensor("cf", [128, 512], F32)
    cb = nc.alloc_sbuf_tensor("cb", [128, 1024], BF16)
    actT = nc.alloc_sbuf_tensor("actT", [128, 8, S], BF16)
    comb = nc.alloc_sbuf_tensor("comb", [128, NT, NE], F32)
    ps = nc.alloc_psum_tensor("ps", [128, 8, 512], F32)

    identf = cf[:, 0:128]
    utri = cf[:, 128:256]
    onesf = cf[:, 256:384]
    freq_col = cf[:, 384:385]
    freq_abs = cf[:, 385:386]
    identb = cb[:, 0:128]
    mask_fox = cb[:, 128:256]
    mask_mla = cb[:, 256:384]
    esel = cb[0:8, 384:384 + 8 * 65]


    def psb(b):
        return ps[:, b, :]

    def psb16(b):
        return ps[:, b, :].bitcast(BF16)

    def add_dbg(P, name, ap, shape, dt, reads):
        t = nc.dram_tensor("dbg_" + name, list(shape), dt, kind="ExternalOutput").ap()
        dbg_outs[name] = t
        P.op(SP, lambda e: e.dma_start(out=t, in_=ap), reads=reads, dma=True)

    def wload(P, dst, src_ap, res, eng=POOL):
        P.op(eng, lambda e: e.dma_start(out=dst, in_=src_ap), writes=[res], dma=True)

    def kc_view(w_ap):
        return w_ap.rearrange("(kc p) n -> p kc n", p=128)

    P = Prog(nc)
    wload(P, cf[:], cf_d, "cf", eng=SP)
    wload(P, cb[:], cb_d, "cb", eng=SP)
    with contextlib.ExitStack() as st:
        gmix = st.enter_context(nc.sbuf_tensor("gmix", [128, D], F32))
        xt = [st.enter_context(nc.sbuf_tensor("xt%d" % i, [128, D], F32)) for i in range(3)]
        xs = [st.enter_context(nc.sbuf_tensor("xs%d" % i, [128, D], BF16)) for i in range(2)]
        sq = st.enter_context(nc.sbuf_tensor("sq", [128, D], BF16))
        stat = st.enter_context(nc.sbuf_tensor("stat", [128, NT, 2], F32))
        P.op(SP, lambda e: e.dma_start(out=gmix[:], in_=g_mix_d[0].partition_broadcast(128)), writes=["gmix"], dma=True)
        P.op(DVE, lambda e: e.memset(stat[:], 0.0), writes=["stat"])

        def p1_copy(t):
            bk = t % 2
            P.op(ACT, lambda e: e.copy(
                actT[:, :, t * 128:(t + 1) * 128], psb16(bk).rearrange("p (a b) -> p a b", a=8)),
                reads=[("ps", bk)], writes=[("actT", t)])

        for t in range(NT):
            xb = xt[t % 3]
            xsb = xs[t % 2]
            bk = t % 2
            P.op(SP, lambda e, xb=xb, t=t: e.dma_start(out=xb[:], in_=x_d[t * 128:(t + 1) * 128, :]),
                 writes=[("xt", t % 3)], dma=True)
            P.op(ACT, lambda e, xb=xb, t=t: e.activation(sq[:], xb[:], AF.Square, accum_out=stat[:, t, 0:1]),
                 reads=[("xt", t % 3), "stat"], writes=["sq", ("stat", t)])
            P.op(ACT, lambda e, t=t: e.activation(stat[:, t, 1:2], stat[:, t, 0:1], AF.Sqrt, bias=EPS, scale=1.0 / D),
                 reads=[("stat", t)], writes=[("stat1", t)])
            if t > 0:
                p1_copy(t - 1)
            P.op(DVE, lambda e, t=t: e.reciprocal(stat[:, t, 1:2], stat[:, t, 1:2]),
                 reads=[("stat1", t)], writes=[("stat1", t)])
            P.op(DVE, lambda e, xb=xb, xsb=xsb, t=t: e.scalar_tensor_tensor(
                xsb[:], xb[:], stat[:, t, 1:2], gmix[:], ALU.mult, ALU.mult),
                reads=[("xt", t % 3), ("stat1", t), "gmix"], writes=[("xs", t % 2)])
            for kc in range(8):
                P.op(PE, lambda e, xsb=xsb, kc=kc, bk=bk: e.transpose(
                    psb16(bk)[:, kc * 128:(kc + 1) * 128], xsb[:, kc * 128:(kc + 1) * 128], identb),
                    reads=[("xs", t % 2), "cb"], writes=[("ps", bk)])
        p1_copy(NT - 1)
        if "xnT" in debug:
            add_dbg(P, "xnT", actT[:, :, 0:512], [128, 8, 512], BF16, [("actT", t) for t in range(4)])
    P.emit("p1")
    if stop_after <= 1:
        return nc, dbg_outs

    st_f = contextlib.ExitStack()
    Gc = st_f.enter_context(nc.sbuf_tensor("Gc", [128, NT, 8], F32))
    FsT = st_f.enter_context(nc.sbuf_tensor("FsT", [8, S], BF16))
    P = Prog(nc)
    with contextlib.ExitStack() as st:
        wf = st.enter_context(nc.sbuf_tensor("wf", [128, 8, 8], BF16))
        wga = st.enter_context(nc.sbuf_tensor("wga", [128, 8, D], BF16))
        wgb = st.enter_context(nc.sbuf_tensor("wgb", [128, 8, D], BF16))
        bfb = st.enter_context(nc.sbuf_tensor("bfb", [128, 8], F32))
        lf = st.enter_context(nc.sbuf_tensor("lf", [128, NT, 8], F32))
        tot = st.enter_context(nc.sbuf_tensor("tot", [128, NT, 8], F32))
        off = st.enter_context(nc.sbuf_tensor("off", [128, NT, 8], F32))
        gst = [st.enter_context(nc.sbuf_tensor("gst%d" % i, [128, 512], BF16)) for i in range(4)]
        wload(P, wf[:], kc_view(w_f_d), "wf")
        wload(P, wga[:], kc_view(w_ga_d), "wga")
        wload(P, wgb[:], kc_view(w_gb_d), "wgb")
        P.op(SP, lambda e: e.dma_start(out=bfb[:], in_=bf_d[0].partition_broadcast(128)), writes=["bfb"], dma=True)
        for t in range(NT):
            for kc in range(8):
                P.op(PE, lambda e, t=t, kc=kc: e.matmul(
                    psb(0)[:, t * 8:(t + 1) * 8], actT[:, kc, t * 128:(t + 1) * 128], wf[:, kc, :],
                    start=(kc == 0), stop=(kc == 7)),
                    reads=[("actT", t), "wf"], writes=[("ps", 0)])
        P.op(DVE, lambda e: e.tensor_tensor(
            lf[:], psb(0)[:, 0:256].rearrange("p (t h) -> p t h", h=8),
            bfb[:].unsqueeze(1).broadcast_to([128, NT, 8]), ALU.add),
            reads=[("ps", 0), "bfb"], writes=["lf"])
        P.op(ACT, lambda e: e.activation(lf[:], lf[:], AF.Exp, scale=-1.0), reads=["lf"], writes=["lf"])
        P.op(ACT, lambda e: e.activation(lf[:], lf[:], AF.Ln, bias=1.0), reads=["lf"], writes=["lf"])
        lf2 = lf[:].rearrange("p t h -> p (t h)")
        P.op(PE, lambda e: e.matmul(psb(1)[:, 0:256], utri, lf2, start=True, stop=True),
             reads=["lf", "cf"], writes=[("ps", 1)])
        P.op(PE, lambda e: e.matmul(psb(2)[:, 0:256], onesf, lf2, start=True, stop=True),
             reads=["lf", "cf"], writes=[("ps", 2)])
        P.op(DVE, lambda e: e.tensor_copy(tot[:], psb(2)[:, 0:256].rearrange("p (t h) -> p t h", h=8)),
             reads=[("ps", 2)], writes=["tot"])
        P.op(DVE, lambda e: e.memset(off[:, 0, :], 0.0), writes=["off"])
        for t in range(1, NT):
            P.op(DVE, lambda e, t=t: e.tensor_tensor(off[:, t, :], off[:, t - 1, :], tot[:, t - 1, :], ALU.add),
                 reads=["off", "tot"], writes=["off"])
        P.op(DVE, lambda e: e.tensor_tensor(
            Gc[:], psb(1)[:, 0:256].rearrange("p (t h) -> p t h", h=8), off[:], ALU.add),
            reads=[("ps", 1), "off"], writes=["Gc"])
        for g4 in range(8):
            bk = 3 + (g4 % 2)
            for i in range(4):
                t = g4 * 4 + i
                P.op(PE, lambda e, t=t, i=i, bk=bk: e.transpose(
                    psb(bk)[0:8, i * 128:(i + 1) * 128], Gc[:, t, :], identf),
                    reads=["Gc", "cf"], writes=[("ps", bk)])
            P.op(ACT, lambda e, g4=g4, bk=bk: e.mul(FsT[:, g4 * 512:(g4 + 1) * 512], psb(bk)[0:8, :], -1.0 / FOX_SCALE),
                 reads=[("ps", bk)], writes=["FsT"])
        n = 0
        for (wg_, dst) in ((wga, ga_s), (wgb, gb_s)):
            wname = "wga" if wg_ is wga else "wgb"
            for dc in range(8):
                for q in range(NQ):
                    bk = 5 + (n % 3)
                    sb = gst[n % 4]
                    for kc in range(8):
                        P.op(PE, lambda e, wg_=wg_, dc=dc, q=q, kc=kc, bk=bk: e.matmul(
                            psb(bk), wg_[:, kc, dc * 128:(dc + 1) * 128], actT[:, kc, q * 512:(q + 1) * 512],
                            start=(kc == 0), stop=(kc == 7)),
                            reads=[wname] + [("actT", q * 4 + i) for i in range(4)], writes=[("ps", bk)])
                    P.op(ACT, lambda e, sb=sb, bk=bk: e.activation(sb[:], psb(bk), AF.Sigmoid),
                         reads=[("ps", bk)], writes=[("gst", n % 4)])
                    P.op(SP, lambda e, sb=sb, dst=dst, dc=dc, q=q: e.dma_start(
                        out=dst[dc * 128:(dc + 1) * 128, q * 512:(q + 1) * 512], in_=sb[:]),
                        reads=[("gst", n % 4)], dma=True)
                    n += 1
        if "Gc" in debug:
            add_dbg(P, "Gc", Gc[:], [128, NT, 8], F32, ["Gc"])
            add_dbg(P, "FsT", FsT[:], [8, S], BF16, ["FsT"])
    P.emit("p2a")
    if stop_after <= 2:
        return nc, dbg_outs

    def attention_head(P, h, kdim, qT, kT, vx, scale, bias_fn, mask, o_dst, bufs, hname):
        pt, rrow, rbc, ost = bufs
        pairs = [(I, J) for I in range(NQ) for J in range(4 * I + 4)]
        npairs = len(pairs)

        def emit_qk(n):
            I, J = pairs[n]
            j = J - 4 * I
            c0 = max(0, j) * 128
            sbk = n % 3
            P.op(PE, lambda e: e.matmul(
                psb(sbk)[:, c0:512], kT[0:kdim, J * 128:(J + 1) * 128], qT[0:kdim, I * 512 + c0:(I + 1) * 512],
                start=True, stop=(j < 0)),
                reads=[hname + "q", hname + "k"], writes=[("ps", sbk)])
            if j >= 0:
                P.op(PE, lambda e: e.matmul(
                    psb(sbk)[:, c0:c0 + 128], identb, mask, start=False, stop=True),
                    reads=["cb"], writes=[("ps", sbk)])

        def emit_exp(n):
            I, J = pairs[n]
            j = J - 4 * I
            c0 = max(0, j) * 128
            sbk = n % 3
            ptb = pt[n % 3]
            bias = bias_fn(J)
            P.op(ACT, lambda e: e.activation(
                ptb[:, c0:512], psb(sbk)[:, c0:512], AF.Exp, bias=bias, scale=scale),
                reads=[("ps", sbk), "Gc"], writes=[("pt", n % 3)])

        def emit_pv(n):
            I, J = pairs[n]
            j = J - 4 * I
            c0 = max(0, j) * 128
            ob = 3 + (I % 2)
            nJ = 4 * I + 4
            ptb = pt[n % 3]
            P.op(PE, lambda e: e.matmul(
                psb(ob)[0:65, c0:512], vx[:, J, :], ptb[:, c0:512], start=(J == 0), stop=(J == nJ - 1)),
                reads=[("pt", n % 3), hname + "v"], writes=[("ps", ob)])

        def finalize(I):
            ob = 3 + (I % 2)
            P.op(DVE, lambda e: e.reciprocal(rrow[64:65, :], psb(ob)[64:65, :]),
                 reads=[("ps", ob)], writes=["rrow"])
            P.op(PE, lambda e: e.matmul(psb(5)[0:64, :], onesf[64:65, 0:64], rrow[64:65, :], start=True, stop=True),
                 reads=["rrow", "cf"], writes=[("ps", 5)])
            P.op(ACT, lambda e: e.copy(rbc[0:64, :], psb(5)[0:64, :]), reads=[("ps", 5)], writes=["rbc"])
            osb = ost[I % 2]
            P.op(DVE, lambda e: e.tensor_tensor(osb[0:64, :], psb(ob)[0:64, :], rbc[0:64, :], ALU.mult),
                 reads=[("ps", ob), "rbc"], writes=[("ost", I % 2)])
            P.op(SP, lambda e: e.dma_start(
                out=o_dst[h * 64:(h + 1) * 64, I * 512:(I + 1) * 512], in_=osb[0:64, :]),
                reads=[("ost", I % 2)], writes=["odram"], dma=True)

        pending = []
        emit_qk(0)
        emit_exp(0)
        emit_qk(1)
        emit_exp(1)
        for n in range(npairs):
            if n + 2 < npairs:
                emit_qk(n + 2)
                emit_exp(n + 2)
            emit_pv(n)
            I, J = pairs[n]
            if J == 4 * I + 3:
                pending.append((n + 3, I))
            while pending and pending[0][0] <= n:
                finalize(pending.pop(0)[1])
        for _, I in pending:
            finalize(I)

    P = Prog(nc)
    with contextlib.ExitStack() as st:
        wfq = st.enter_context(nc.sbuf_tensor("wfq", [128, 8, 512], BF16))
        wfk = st.enter_context(nc.sbuf_tensor("wfk", [128, 8, 512], BF16))
        wfv = st.enter_context(nc.sbuf_tensor("wfv", [128, 8, 512], BF16))
        qTs = [st.enter_context(nc.sbuf_tensor("fqT%d" % i, [65, S], BF16)) for i in range(2)]
        kTs = [st.enter_context(nc.sbuf_tensor("fkT%d" % i, [65, S], BF16)) for i in range(2)]
        vxs = [st.enter_context(nc.sbuf_tensor("fvx%d" % i, [128, NT, 65], BF16)) for i in range(2)]
        pt = [st.enter_context(nc.sbuf_tensor("pt%d" % i, [128, 512], BF16)) for i in range(3)]
        rrow = st.enter_context(nc.sbuf_tensor("rrow", [65, 512], F32))
        rbc = st.enter_context(nc.sbuf_tensor("rbc", [64, 512], F32))
        ost = [st.enter_context(nc.sbuf_tensor("ost%d" % i, [64, 512], BF16)) for i in range(2)]
        wload(P, wfq[:], kc_view(w_fq_d), "wfq")
        wload(P, wfk[:], kc_view(w_fk_d), "wfk")
        wload(P, wfv[:], kc_view(w_fv_d), "wfv")
        for i in range(2):
            P.op(POOL, lambda e, i=i: e.memset(kTs[i][64:65, :], 1.0), writes=[("fk", i)])
            P.op(POOL, lambda e, i=i: e.memset(vxs[i][:, :, 64:65], 1.0), writes=[("fv", i)])
        allact = [("actT", t) for t in range(NT)]
        for h in range(NH):
            b = h % 2
            qT, kT, vx = qTs[b], kTs[b], vxs[b]
            hn = "f%d" % b
            for q in range(NQ):
                bk = 6 + (q % 2)
                P.op(PE, lambda e, h=h, q=q, bk=bk: e.matmul(
                    psb(bk)[0:65, :], esel[:, h * 65:(h + 1) * 65], FsT[:, q * 512:(q + 1) * 512], start=True, stop=False),
                    reads=["FsT", "cb"], writes=[("ps", bk)])
                for kc in range(8):
                    P.op(PE, lambda e, h=h, q=q, kc=kc, bk=bk: e.matmul(
                        psb(bk)[0:64, :], wfq[:, kc, h * 64:(h + 1) * 64], actT[:, kc, q * 512:(q + 1) * 512],
                        start=False, stop=(kc == 7)),
                        reads=["wfq"] + allact[q * 4:q * 4 + 4], writes=[("ps", bk)])
                P.op(ACT, lambda e, qT=qT, q=q, bk=bk: e.copy(qT[0:65, q * 512:(q + 1) * 512], psb(bk)[0:65, :]),
                     reads=[("ps", bk)], writes=[hn + "q"])
            for q in range(NQ):
                bk = 6 + (q % 2)
                for kc in range(8):
                    P.op(PE, lambda e, h=h, q=q, kc=kc, bk=bk: e.matmul(
                        psb(bk)[0:64, :], wfk[:, kc, h * 64:(h + 1) * 64], actT[:, kc, q * 512:(q + 1) * 512],
                        start=(kc == 0), stop=(kc == 7)),
                        reads=["wfk"] + allact[q * 4:q * 4 + 4], writes=[("ps", bk)])
                P.op(DVE, lambda e, kT=kT, q=q, bk=bk: e.tensor_copy(kT[0:64, q * 512:(q + 1) * 512], psb(bk)[0:64, :]),
                     reads=[("ps", bk), ("fk", b)], writes=[hn + "k"])
            for g8 in range(4):
                bk = 6 + (g8 % 2)
                for i in range(8):
                    t = g8 * 8 + i
                    for kc in range(8):
                        P.op(PE, lambda e, h=h, t=t, i=i, kc=kc, bk=bk: e.matmul(
                            psb(bk)[:, i * 64:(i + 1) * 64], actT[:, kc, t * 128:(t + 1) * 128], wfv[:, kc, h * 64:(h + 1) * 64],
                            start=(kc == 0), stop=(kc == 7)),
                            reads=["wfv", ("actT", t)], writes=[("ps", bk)])
                P.op(DVE, lambda e, vx=vx, g8=g8, bk=bk: e.tensor_copy(
                    vx[:, g8 * 8:(g8 + 1) * 8, 0:64], psb(bk).rearrange("p (t d) -> p t d", d=64)),
                    reads=[("ps", bk), ("fv", b)], writes=[hn + "v"])
            attention_head(P, h, 65, qT, kT, vx, FOX_SCALE, lambda J, h=h: Gc[:, J, h:h + 1], mask_fox,
                           ob_s, (pt, rrow, rbc, ost), hn)
        if "ob" in debug:
            add_dbg(P, "ob", ob_s, [512, S], BF16, ["odram"])
    P.emit("p3")
    st_f.close()
    if stop_after <= 3:
        return nc, dbg_outs

    st_a = contextlib.ExitStack()
    cqnT = st_a.enter_context(nc.sbuf_tensor("cqnT", [128, 3, S], BF16))
    krT = st_a.enter_context(nc.sbuf_tensor("krT", [96, S], BF16))
    cosT = st_a.enter_context(nc.sbuf_tensor("cosT", [96, S], BF16))
    sinT = st_a.enter_context(nc.sbuf_tensor("sinT", [96, S], BF16))
    R = slice(64, 96)
    allc = [("cqnT", t) for t in range(NT)]
    P = Prog(nc)
    with contextlib.ExitStack() as st:
        wlat = st.enter_context(nc.sbuf_tensor("wlat", [128, 8, 384], BF16))
        wkr = st.enter_context(nc.sbuf_tensor("wkr", [128, 8, 192], BF16))
        gcq = st.enter_context(nc.sbuf_tensor("gcq", [128, 384], F32))
        lat = [st.enter_context(nc.sbuf_tensor("lat%d" % i, [128, 384], BF16)) for i in range(2)]
        lsq = st.enter_context(nc.sbuf_tensor("lsq", [128, 384], BF16))
        lst = st.enter_context(nc.sbuf_tensor("lst", [128, NT, 4], F32))
        posi = st.enter_context(nc.sbuf_tensor("posi", [96, 512], I32))
        ang = st.enter_context(nc.sbuf_tensor("ang", [96, 512], F32))
        uu = st.enter_context(nc.sbuf_tensor("uu", [96, 512], F32))
        ki = st.enter_context(nc.sbuf_tensor("ki", [96, 512], I32))
        kf = st.enter_context(nc.sbuf_tensor("kf", [96, 512], F32))
        rr = st.enter_context(nc.sbuf_tensor("rr", [96, 512], F32))
        hs_ = st.enter_context(nc.sbuf_tensor("hs_", [96, 512], F32))
        t1 = st.enter_context(nc.sbuf_tensor("t1", [96, 512], F32))
        t2 = st.enter_context(nc.sbuf_tensor("t2", [96, 512], F32))
        wload(P, wlat[:], kc_view(w_lat_d), "wlat")
        wload(P, wkr[:], kc_view(w_kr_d), "wkr")
        P.op(SP, lambda e: e.dma_start(out=gcq[:, 0:256], in_=g_cq_d[0].partition_broadcast(128)), writes=["gcq"], dma=True)
        P.op(SP, lambda e: e.dma_start(out=gcq[:, 256:384], in_=g_ckv_d[0].partition_broadcast(128)), writes=["gcq"], dma=True)
        for q in range(NQ):
            tk = slice(q * 512, (q + 1) * 512)
            P.op(SP, lambda e, tk=tk: e.dma_start(out=posi[R, :], in_=pos_d[0, tk].partition_broadcast(32)),
                 writes=["posi"], dma=True)
            P.op(DVE, lambda e: e.tensor_copy(ang[R, :], posi[R, :]), reads=["posi"], writes=["ang"])
            P.op(DVE, lambda e: e.tensor_scalar(ang[R, :], ang[R, :], freq_abs[R, :], None, ALU.mult),
                 reads=["ang", "cf"], writes=["ang"])
            P.op(DVE, lambda e: e.tensor_scalar(uu[R, :], ang[R, :], 1.0 / (2 * np.pi), None, ALU.mult),
                 reads=["ang"], writes=["uu"])
            P.op(DVE, lambda e: e.tensor_copy(ki[R, :], uu[R, :]), reads=["uu"], writes=["ki"])
            P.op(DVE, lambda e: e.tensor_copy(kf[R, :], ki[R, :]), reads=["ki"], writes=["kf"])
            P.op(DVE, lambda e: e.scalar_tensor_tensor(rr[R, :], kf[R, :], -TWO_PI_HI, ang[R, :], ALU.mult, ALU.add),
                 reads=["kf", "ang"], writes=["rr"])
            P.op(DVE, lambda e: e.scalar_tensor_tensor(rr[R, :], kf[R, :], -TWO_PI_LO, rr[R, :], ALU.mult, ALU.add),
                 reads=["kf", "rr"], writes=["rr"])
            P.op(DVE, lambda e: e.tensor_scalar(rr[R, :], rr[R, :], 3.1415925, -3.1415925, ALU.min, ALU.max),
                 reads=["rr"], writes=["rr"])
            P.op(ACT, lambda e, tk=tk: e.activation(sinT[R, tk], rr[R, :], AF.Sin, scale=cf[R, 386:387]),
                 reads=["rr", "cf"], writes=["sinT"])
            P.op(ACT, lambda e: e.activation(hs_[R, :], rr[R, :], AF.Sin, scale=0.5), reads=["rr"], writes=["hs_"])
            P.op(DVE, lambda e: e.tensor_tensor(hs_[R, :], hs_[R, :], hs_[R, :], ALU.mult), reads=["hs_"], writes=["hs_"])
            P.op(DVE, lambda e, tk=tk: e.tensor_scalar(cosT[R, tk], hs_[R, :], -2.0, 1.0, ALU.mult, ALU.add),
                 reads=["hs_"], writes=["cosT"])
        P.op(DVE, lambda e: e.memset(lst[:], 0.0), writes=["lst"])
        for t in range(NT):
            bk = 6 + (t % 2)
            lb = lat[t % 2]
            for kc in range(8):
                P.op(PE, lambda e, t=t, kc=kc, bk=bk: e.matmul(
                    psb(bk)[:, 0:384], actT[:, kc, t * 128:(t + 1) * 128], wlat[:, kc, :], start=(kc == 0), stop=(kc == 7)),
                    reads=["wlat", ("actT", t)], writes=[("ps", bk)])
            P.op(ACT, lambda e, t=t, bk=bk: e.activation(lsq[:, 0:256], psb(bk)[:, 0:256], AF.Square, accum_out=lst[:, t, 0:1]),
                 reads=[("ps", bk), "lst"], writes=["lsq", ("lst", t)])
            P.op(ACT, lambda e, t=t, bk=bk: e.activation(lsq[:, 256:384], psb(bk)[:, 256:384], AF.Square, accum_out=lst[:, t, 1:2]),
                 reads=[("ps", bk), ("lst", t)], writes=["lsq", ("lst", t)])
            P.op(ACT, lambda e, t=t: e.activation(lst[:, t, 2:3], lst[:, t, 0:1], AF.Sqrt, bias=EPS, scale=1.0 / 256),
                 reads=[("lst", t)], writes=[("lst2", t)])
            P.op(ACT, lambda e, t=t: e.activation(lst[:, t, 3:4], lst[:, t, 1:2], AF.Sqrt, bias=EPS, scale=1.0 / 128),
                 reads=[("lst", t)], writes=[("lst3", t)])
            P.op(DVE, lambda e, t=t: e.reciprocal(lst[:, t, 2:4], lst[:, t, 2:4]),
                 reads=[("lst2", t), ("lst3", t)], writes=[("lst2", t), ("lst3", t)])
            P.op(DVE, lambda e, t=t, bk=bk, lb=lb: e.scalar_tensor_tensor(
                lb[:, 0:256], psb(bk)[:, 0:256], lst[:, t, 2:3], gcq[:, 0:256], ALU.mult, ALU.mult),
                reads=[("ps", bk), ("lst2", t), "gcq"], writes=[("lat", t % 2)])
            P.op(DVE, lambda e, t=t, bk=bk, lb=lb: e.scalar_tensor_tensor(
                lb[:, 256:384], psb(bk)[:, 256:384], lst[:, t, 3:4], gcq[:, 256:384], ALU.mult, ALU.mult),
                reads=[("ps", bk), ("lst3", t), "gcq"], writes=[("lat", t % 2)])
            tb = 4 + (t % 2)
            for c in range(3):
                P.op(PE, lambda e, lb=lb, c=c, tb=tb: e.transpose(
                    psb16(tb)[:, c * 128:(c + 1) * 128], lb[:, c * 128:(c + 1) * 128], identb),
                    reads=[("lat", t % 2), "cb"], writes=[("ps", tb)])
            P.op(ACT, lambda e, t=t, tb=tb: e.copy(
                cqnT[:, :, t * 128:(t + 1) * 128], psb16(tb)[:, 0:384].rearrange("p (a b) -> p a b", a=3)),
                reads=[("ps", tb)], writes=[("cqnT", t)])
        for q in range(NQ):
            tk = slice(q * 512, (q + 1) * 512)
            for v in range(2):
                for kc in range(8):
                    P.op(PE, lambda e, q=q, v=v, kc=kc: e.matmul(
                        psb(6 + v)[0:96, :], wkr[:, kc, v * 96:(v + 1) * 96], actT[:, kc, q * 512:(q + 1) * 512],
                        start=(kc == 0), stop=(kc == 7)),
                        reads=["wkr"] + [("actT", q * 4 + i) for i in range(4)], writes=[("ps", 6 + v)])
            P.op(DVE, lambda e, tk=tk: e.tensor_tensor(t1[R, :], psb(6)[R, :], cosT[R, tk], ALU.mult),
                 reads=[("ps", 6), "cosT"], writes=["t1"])
            P.op(DVE, lambda e, tk=tk: e.tensor_tensor(t2[R, :], psb(7)[R, :], sinT[R, tk], ALU.mult),
                 reads=[("ps", 7), "sinT"], writes=["t2"])
            P.op(POOL, lambda e, tk=tk: e.tensor_tensor(krT[R, tk], t1[R, :], t2[R, :], ALU.add),
                 reads=["t1", "t2"], writes=["krT"])
        if "lat" in debug:
            add_dbg(P, "cqnT", cqnT[:, :, 0:512], [128, 3, 512], BF16, allc)
            add_dbg(P, "krT", krT[R, 0:512], [32, 512], BF16, ["krT"])
    P.emit("p4a")

    P = Prog(nc)
    with contextlib.ExitStack() as st:
        wuq = st.enter_context(nc.sbuf_tensor("wuq", [128, 2, 768], BF16))
        wuqs = st.enter_context(nc.sbuf_tensor("wuqs", [128, 2, 768], BF16))
        wuk = st.enter_context(nc.sbuf_tensor("wuk", [128, 512], BF16))
        wuv = st.enter_context(nc.sbuf_tensor("wuv", [128, 512], BF16))
        qTs = [st.enter_context(nc.sbuf_tensor("aqT%d" % i, [96, S], BF16)) for i in range(2)]
        kTs = [st.enter_context(nc.sbuf_tensor("akT%d" % i, [96, S], BF16)) for i in range(2)]
        vxs = [st.enter_context(nc.sbuf_tensor("avx%d" % i, [128, NT, 65], BF16)) for i in range(2)]
        pt = [st.enter_context(nc.sbuf_tensor("apt%d" % i, [128, 512], BF16)) for i in range(3)]
        rrow = st.enter_context(nc.sbuf_tensor("arrow", [65, 512], F32))
        rbc = st.enter_context(nc.sbuf_tensor("arbc", [64, 512], F32))
        ost = [st.enter_context(nc.sbuf_tensor("aost%d" % i, [64, 512], BF16)) for i in range(2)]
        t1 = st.enter_context(nc.sbuf_tensor("t1b", [96, 512], F32))
        t2 = st.enter_context(nc.sbuf_tensor("t2b", [96, 512], F32))
        wload(P, wuq[:], kc_view(w_uq_d), "wuq")
        wload(P, wuqs[:], kc_view(w_uqs_d), "wuqs")
        wload(P, wuk[:], w_uk_d, "wuk")
        wload(P, wuv[:], w_uv_d, "wuv")
        for i in range(2):
            P.op(POOL, lambda e, i=i: e.memset(vxs[i][:, :, 64:65], 1.0), writes=[("av", i)])
        for h in range(NH):
            b = h % 2
            qT, kT, vx = qTs[b], kTs[b], vxs[b]
            hn = "a%d" % b
            for q in range(NQ):
                tk = slice(q * 512, (q + 1) * 512)
                for v, w_ in enumerate((wuq, wuqs)):
                    for kc in range(2):
                        P.op(PE, lambda e, h=h, q=q, kc=kc, v=v, w_=w_: e.matmul(
                            psb(6 + v)[0:96, :], w_[:, kc, h * 96:(h + 1) * 96], cqnT[:, kc, q * 512:(q + 1) * 512],
                            start=(kc == 0), stop=(kc == 1)),
                            reads=["wuq", "wuqs"] + allc[q * 4:q * 4 + 4], writes=[("ps", 6 + v)])
                P.op(ACT, lambda e, qT=qT, tk=tk: e.copy(qT[0:64, tk], psb(6)[0:64, :]),
                     reads=[("ps", 6)], writes=[hn + "q"])
                P.op(DVE, lambda e, tk=tk: e.tensor_tensor(t1[R, :], psb(6)[R, :], cosT[R, tk], ALU.mult),
                     reads=[("ps", 6), "cosT"], writes=["t1"])
                P.op(DVE, lambda e, tk=tk: e.tensor_tensor(t2[R, :], psb(7)[R, :], sinT[R, tk], ALU.mult),
                     reads=[("ps", 7), "sinT"], writes=["t2"])
                P.op(POOL, lambda e, qT=qT, tk=tk: e.tensor_tensor(qT[R, tk], t1[R, :], t2[R, :], ALU.add),
                     reads=["t1", "t2"], writes=[hn + "q"])
            for q in range(NQ):
                tk = slice(q * 512, (q + 1) * 512)
                bk = 6 + (q % 2)
                P.op(PE, lambda e, h=h, q=q, bk=bk: e.matmul(
                    psb(bk)[0:64, :], wuk[:, h * 64:(h + 1) * 64], cqnT[:, 2, q * 512:(q + 1) * 512], start=True, stop=True),
                    reads=["wuk"] + allc[q * 4:q * 4 + 4], writes=[("ps", bk)])
                P.op(ACT, lambda e, kT=kT, tk=tk, bk=bk: e.copy(kT[0:64, tk], psb(bk)[0:64, :]),
                     reads=[("ps", bk)], writes=[hn + "k"])
            P.op(POOL, lambda e, kT=kT: e.tensor_copy(kT[R, :], krT[R, :]), reads=["krT"], writes=[hn + "k"])
            for g8 in range(4):
                bk = 6 + (g8 % 2)
                for i in range(8):
                    t = g8 * 8 + i
                    P.op(PE, lambda e, h=h, t=t, i=i, bk=bk: e.matmul(
                        psb(bk)[:, i * 64:(i + 1) * 64], cqnT[:, 2, t * 128:(t + 1) * 128], wuv[:, h * 64:(h + 1) * 64],
                        start=True, stop=True),
                        reads=["wuv", ("cqnT", t)], writes=[("ps", bk)])
                P.op(DVE, lambda e, vx=vx, g8=g8, bk=bk: e.tensor_copy(
                    vx[:, g8 * 8:(g8 + 1) * 8, 0:64], psb(bk).rearrange("p (t d) -> p t d", d=64)),
                    reads=[("ps", bk), ("av", b)], writes=[hn + "v"])
            attention_head(P, h, 96, qT, kT, vx, MLA_SCALE, lambda J: 0.0, mask_mla,
                           oa_s, (pt, rrow, rbc, ost), hn)
        if "oa" in debug:
            add_dbg(P, "oa", oa_s, [512, S], BF16, ["odram"])
    P.emit("p4")
    st_a.close()
    if stop_after <= 4:
        return nc, dbg_outs

    P = Prog(nc)
    with contextlib.ExitStack() as st:
        woa = st.enter_context(nc.sbuf_tensor("woa", [128, 4, D], BF16))
        wob = st.enter_context(nc.sbuf_tensor("wob", [128, 4, D], BF16))
        wout = st.enter_context(nc.sbuf_tensor("wout", [128, 8, D], BF16))
        wr = st.enter_context(nc.sbuf_tensor("wr", [128, 8, 36], BF16))
        gffn = st.enter_context(nc.sbuf_tensor("gffn", [128, D], F32))
        brb = st.enter_context(nc.sbuf_tensor("brb", [128, 36], F32))
        gat = [[st.enter_context(nc.sbuf_tensor("gat%d_%d" % (b_, i), [128, 8, 512], BF16)) for i in range(2)] for b_ in range(2)]
        oin = [[st.enter_context(nc.sbuf_tensor("oin%d_%d" % (b_, i), [128, 4, 512], BF16)) for i in range(2)] for b_ in range(2)]
        mixT = [st.enter_context(nc.sbuf_tensor("mixT%d" % i, [128, 8, 512], BF16)) for i in range(2)]
        m1 = [st.enter_context(nc.sbuf_tensor("m1_0", [128, 512], F32))] * 2
        m2 = [st.enter_context(nc.sbuf_tensor("m2_0", [128, 512], F32))] * 2
        xh = [st.enter_context(nc.sbuf_tensor("xh%d" % i, [128, D], F32)) for i in range(3)]
        hsb = [st.enter_context(nc.sbuf_tensor("hsb%d" % i, [128, D], BF16)) for i in range(2)]
        sq5 = st.enter_context(nc.sbuf_tensor("sq5", [128, D], BF16))
        st5 = st.enter_context(nc.sbuf_tensor("st5", [128, NT, 2], F32))
        Lr = st.enter_context(nc.sbuf_tensor("Lr", [128, NT, 36], F32))
        wload(P, woa[:], w_oa_d.rearrange("(c p) n -> p c n", p=128), "woa")
        wload(P, wob[:], w_ob_d.rearrange("(c p) n -> p c n", p=128), "wob")
        wload(P, wout[:], kc_view(w_out_d), "wout")
        wload(P, wr[:], kc_view(w_r_d), "wr")
        P.op(SP, lambda e: e.dma_start(out=gffn[:], in_=g_ffn_d[0].partition_broadcast(128)), writes=["gffn"], dma=True)
        P.op(SP, lambda e: e.dma_start(out=brb[:], in_=br_d[0].partition_broadcast(128)), writes=["brb"], dma=True)
        P.op(DVE, lambda e: e.memset(st5[:], 0.0), writes=["st5"])
        xi = [0]

        def merge_loads(q):
            tk = slice(q * 512, (q + 1) * 512)
            b_ = q % 2
            P.op(SP, lambda e: e.dma_start(out=gat[b_][0][:], in_=ga_s[:, tk].rearrange("(c p) t -> p c t", p=128)),
                 writes=[("gat", b_, 0)], dma=True)
            P.op(SP, lambda e: e.dma_start(out=oin[b_][0][:], in_=oa_s[:, tk].rearrange("(c p) t -> p c t", p=128)),
                 writes=[("oin", b_, 0)], dma=True)
            P.op(SP, lambda e: e.dma_start(out=gat[b_][1][:], in_=gb_s[:, tk].rearrange("(c p) t -> p c t", p=128)),
                 writes=[("gat", b_, 1)], dma=True)
            P.op(SP, lambda e: e.dma_start(out=oin[b_][1][:], in_=ob_s[:, tk].rearrange("(c p) t -> p c t", p=128)),
                 writes=[("oin", b_, 1)], dma=True)

        def merge_dc(q, dc):
            b_ = q % 2
            mb = 0
            for v, w_ in enumerate((woa, wob)):
                for pc in range(4):
                    P.op(PE, lambda e, v=v, w_=w_, pc=pc: e.matmul(
                        psb(v), w_[:, pc, dc * 128:(dc + 1) * 128], oin[b_][v][:, pc, :], start=(pc == 0), stop=(pc == 3)),
                        reads=["woa", "wob", ("oin", b_, v)], writes=[("ps", v)])
            P.op(DVE, lambda e: e.tensor_tensor(m1[mb][:], psb(0), gat[b_][0][:, dc, :], ALU.mult),
                 reads=[("ps", 0), ("gat", b_, 0)], writes=[("m1", mb)])
            P.op(DVE, lambda e: e.tensor_tensor(m2[mb][:], psb(1), gat[b_][1][:, dc, :], ALU.mult),
                 reads=[("ps", 1), ("gat", b_, 1)], writes=[("m2", mb)])
            P.op(POOL, lambda e: e.tensor_tensor(mixT[b_][:, dc, :], m1[mb][:], m2[mb][:], ALU.add),
                 reads=[("m1", mb), ("m2", mb)], writes=[("mixT", b_, dc)])

        merge_loads(0)
        for dc in range(8):
            merge_dc(0, dc)
        for q in range(NQ):
            if q + 1 < NQ:
                merge_loads(q + 1)
            for sub in range(4):
                t = q * 4 + sub
                xb = xh[xi[0] % 3]
                xr = ("xh", xi[0] % 3)
                xi[0] += 1
                P.op(SP, lambda e, xb=xb, t=t: e.dma_start(out=xb[:], in_=x_d[t * 128:(t + 1) * 128, :]),
                     writes=[xr], dma=True)
                for ch in range(2):
                    bk = 2 + ch
                    for kc in range(8):
                        P.op(PE, lambda e, sub=sub, ch=ch, kc=kc, bk=bk, q=q: e.matmul(
                            psb(bk), mixT[q % 2][:, kc, sub * 128:(sub + 1) * 128], wout[:, kc, ch * 512:(ch + 1) * 512],
                            start=(kc == 0), stop=(kc == 7)),
                            reads=["wout"] + [("mixT", q % 2, d_) for d_ in range(8)], writes=[("ps", bk)])
                    P.op(DVE, lambda e, xb=xb, ch=ch, bk=bk: e.tensor_tensor(
                        xb[:, ch * 512:(ch + 1) * 512], psb(bk), xb[:, ch * 512:(ch + 1) * 512], ALU.add),
                        reads=[("ps", bk), xr], writes=[xr])
                P.op(SP, lambda e, xb=xb, t=t: e.dma_start(out=h_s[t * 128:(t + 1) * 128, :], in_=xb[:]),
                     reads=[xr], dma=True)
                if q + 1 < NQ:
                    merge_dc(q + 1, 2 * sub)
                P.op(ACT, lambda e, xb=xb, t=t: e.activation(sq5[:], xb[:], AF.Square, accum_out=st5[:, t, 0:1]),
                     reads=[xr, "st5"], writes=["sq5", ("st5", t)])
                P.op(ACT, lambda e, t=t: e.activation(st5[:, t, 1:2], st5[:, t, 0:1], AF.Sqrt, bias=EPS, scale=1.0 / D),
                     reads=[("st5", t)], writes=[("st51", t)])
                P.op(DVE, lambda e, t=t: e.reciprocal(st5[:, t, 1:2], st5[:, t, 1:2]),
                     reads=[("st51", t)], writes=[("st51", t)])
                hb = hsb[t % 2]
                P.op(DVE, lambda e, xb=xb, hb=hb, t=t: e.scalar_tensor_tensor(
                    hb[:], xb[:], st5[:, t, 1:2], gffn[:], ALU.mult, ALU.mult),
                    reads=[xr, ("st51", t), "gffn"], writes=[("hsb", t % 2)])
                tb = 4 + (t % 2)
                for kc in range(8):
                    P.op(PE, lambda e, hb=hb, kc=kc, tb=tb: e.transpose(
                        psb16(tb)[:, kc * 128:(kc + 1) * 128], hb[:, kc * 128:(kc + 1) * 128], identb),
                        reads=[("hsb", t % 2), "cb"], writes=[("ps", tb)])
                P.op(ACT, lambda e, t=t, tb=tb: e.copy(
                    actT[:, :, t * 128:(t + 1) * 128], psb16(tb).rearrange("p (a b) -> p a b", a=8)),
                    reads=[("ps", tb)], writes=[("actT", t)])
                if q + 1 < NQ:
                    merge_dc(q + 1, 2 * sub + 1)
                for kc in range(8):
                    P.op(PE, lambda e, t=t, kc=kc: e.matmul(
                        psb(6)[:, 0:36], actT[:, kc, t * 128:(t + 1) * 128], wr[:, kc, :], start=(kc == 0), stop=(kc == 7)),
                        reads=[("actT", t), "wr"], writes=[("ps", 6)])
                P.op(DVE, lambda e, t=t: e.tensor_tensor(Lr[:, t, :], psb(6)[:, 0:36], brb[:], ALU.add),
                     reads=[("ps", 6), "brb"], writes=[("Lr", t)])

        allL = [("Lr", t) for t in range(NT)]
        scr = mixT[0][:].rearrange("p a b -> p (a b)").bitcast(F32)

        class V:
            def __init__(self, ap):
                self.ap = ap

            def __getitem__(self, idx):
                return self.ap if (isinstance(idx, slice) and idx == slice(None)) else self.ap[idx]

        def v3(o, k):
            return V(scr[:, o:o + NT * k].rearrange("p (t k) -> p t k", k=k))

        def v2(o):
            return V(scr[:, o:o + NT])

        sel, sel2, tmp8, is1, is2 = v3(0, 8), v3(256, 8), v3(512, 8), v3(768, 8), v3(1024, 8)
        gd, grp = v3(1280, 4), v3(1408, 4)
        gmax, gp, mx1, mx2, e2, w1, w2 = (v2(1536 + 32 * i) for i in range(7))
        mixall = [("mixT", 0, d_) for d_ in range(8)]

        def b3(ap2, k):
            return ap2.unsqueeze(2).broadcast_to([128, NT, k])

        def dv(fn, reads, writes):
            P.op(DVE, fn, reads=reads, writes=writes)

        Lg = Lr[:, :, 0:4]
        dv(lambda e: e.reduce_max(gmax[:], Lg, axis=AX.X), allL, ["gmax"] + mixall)
        dv(lambda e: e.tensor_tensor(gd[:], Lg, b3(gmax[:], 4), ALU.subtract), allL + ["gmax"], ["gd"])
        P.op(ACT, lambda e: e.activation(gd[:], gd[:], AF.Exp), reads=["gd"], writes=["gd"])
        dv(lambda e: e.reduce_sum(gp[:], gd[:], axis=AX.X), ["gd"], ["gp"])
        dv(lambda e: e.reciprocal(gp[:], gp[:]), ["gp"], ["gp"])
        dv(lambda e: e.tensor_tensor(grp[:], Lg, b3(gmax[:], 4), ALU.is_equal), allL + ["gmax"], ["grp"])
        dv(lambda e: e.tensor_tensor(sel[:], Lr[:, :, 4:12], grp[:, :, 0:1].broadcast_to([128, NT, 8]), ALU.mult),
           allL + ["grp"], ["sel"])
        for g in range(1, 4):
            dv(lambda e, g=g: e.tensor_tensor(tmp8[:], Lr[:, :, 4 + 8 * g:12 + 8 * g],
                                              grp[:, :, g:g + 1].broadcast_to([128, NT, 8]), ALU.mult),
               allL + ["grp", "sel"], ["tmp8"])
            dv(lambda e: e.tensor_tensor(sel[:], sel[:], tmp8[:], ALU.add), ["sel", "tmp8"], ["sel"])
        dv(lambda e: e.reduce_max(mx1[:], sel[:], axis=AX.X), ["sel"], ["mx1"])
        dv(lambda e: e.tensor_tensor(is1[:], sel[:], b3(mx1[:], 8), ALU.is_equal), ["sel", "mx1"], ["is1"])
        dv(lambda e: e.scalar_tensor_tensor(sel2[:], is1[:], -1e30, sel[:], ALU.mult, ALU.add), ["is1", "sel"], ["sel2"])
        dv(lambda e: e.reduce_max(mx2[:], sel2[:], axis=AX.X), ["sel2"], ["mx2"])
        dv(lambda e: e.tensor_tensor(is2[:], sel2[:], b3(mx2[:], 8), ALU.is_equal), ["sel2", "mx2"], ["is2"])
        dv(lambda e: e.tensor_tensor(e2[:], mx2[:], mx1[:], ALU.subtract), ["mx1", "mx2"], ["e2"])
        P.op(ACT, lambda e: e.activation(e2[:], e2[:], AF.Exp), reads=["e2"], writes=["e2"])
        dv(lambda e: e.tensor_scalar(w1[:], e2[:], 1.0, None, ALU.add), ["e2"], ["w1"])
        dv(lambda e: e.reciprocal(w1[:], w1[:]), ["w1"], ["w1"])
        dv(lambda e: e.tensor_tensor(w1[:], w1[:], gp[:], ALU.mult), ["w1", "gp"], ["w1"])
        dv(lambda e: e.tensor_tensor(w2[:], w1[:], e2[:], ALU.mult), ["w1", "e2"], ["w2"])
        dv(lambda e: e.tensor_tensor(is1[:], is1[:], b3(w1[:], 8), ALU.mult), ["is1", "w1", "sel2"], ["is1"])
        dv(lambda e: e.tensor_tensor(is2[:], is2[:], b3(w2[:], 8), ALU.mult), ["is2", "w2"], ["is2"])
        dv(lambda e: e.tensor_tensor(is1[:], is1[:], is2[:], ALU.add), ["is1", "is2"], ["is1"])
        for g in range(4):
            dv(lambda e, g=g: e.tensor_tensor(comb[:, :, 8 * g:8 * g + 8], is1[:],
                                              grp[:, :, g:g + 1].broadcast_to([128, NT, 8]), ALU.mult),
               ["is1", "grp"], [("comb", g)])
        if "p5" in debug:
            add_dbg(P, "comb", comb[:], [128, NT, NE], F32, [("comb", g) for g in range(4)])
            add_dbg(P, "hnT", actT[:, :, 2048:2560], [128, 8, 512], BF16, [("actT", t) for t in range(16, 20)])
    P.emit("p5")
    if stop_after <= 5:
        return nc, dbg_outs

    P = Prog(nc)
    TT = 2048
    NS8 = TT // 128
    with contextlib.ExitStack() as st:
        gfin = st.enter_context(nc.sbuf_tensor("gfin", [128, D], F32))
        yacc = st.enter_context(nc.sbuf_tensor("yacc", [128, TT // 128, D], F32))
        wgu = [st.enter_context(nc.sbuf_tensor("wgu%d" % i, [128, 2, 8, EFF], BF16)) for i in range(2)]
        wdn = [st.enter_context(nc.sbuf_tensor("wdn%d" % i, [128, 2, D], BF16)) for i in range(2)]
        sg = [st.enter_context(nc.sbuf_tensor("sg%d" % i, [128, 512], F32)) for i in range(2)]
        aT = [st.enter_context(nc.sbuf_tensor("aT%d" % i, [128, 2, 512], BF16)) for i in range(2)]
        sq6 = st.enter_context(nc.sbuf_tensor("sq6", [128, D], BF16))
        st6 = st.enter_context(nc.sbuf_tensor("st6", [128, NT, 2], F32))
        P.op(SP, lambda e: e.dma_start(out=gfin[:], in_=g_fin_d[0].partition_broadcast(128)), writes=["gfin"], dma=True)
        P.op(DVE, lambda e: e.memset(st6[:], 0.0), writes=["st6"])
        wg_v = w_eg_d.rearrange("(e r) c -> e (r c)", e=NE).rearrange("e (kc p f) -> e p kc f", p=128, f=EFF)
        wu_v = w_eu_d.rearrange("(e r) c -> e (r c)", e=NE).rearrange("e (kc p f) -> e p kc f", p=128, f=EFF)
        wd_v = w_ed_d.rearrange("(e r) c -> e (r c)", e=NE).rearrange("e (c p n) -> e p c n", p=128, n=D)
        it = 0
        dn = 0
        for tt in range(S // TT):
            for s8 in range(NS8):
                t = tt * NS8 + s8
                P.op(SP, lambda e, s8=s8, t=t: e.dma_start(out=yacc[:, s8, :], in_=h_s[t * 128:(t + 1) * 128, :]),
                     writes=[("yacc", s8)], dma=True)
            if "y0" in debug and tt == 1:
                add_dbg(P, "y0", yacc[:, 0, :], [128, D], F32, [("yacc", 0)])
            for ex in range(NE):
                wb = it % 2
                P.op(POOL, lambda e, ex=ex, wb=wb: e.dma_start(out=wgu[wb][:, 0, :, :], in_=wg_v[ex]), writes=[("wgu", wb)], dma=True)
                P.op(POOL, lambda e, ex=ex, wb=wb: e.dma_start(out=wgu[wb][:, 1, :, :], in_=wu_v[ex]), writes=[("wgu", wb)], dma=True)
                P.op(POOL, lambda e, ex=ex, wb=wb: e.dma_start(out=wdn[wb][:], in_=wd_v[ex]), writes=[("wdn", wb)], dma=True)
                if "p6" in debug and it == 0:
                    add_dbg(P, "wgu0", wgu[0][:], [128, 2, 8, EFF], BF16, [("wgu", 0)])
                    add_dbg(P, "wdn0", wdn[0][:], [128, 2, D], BF16, [("wdn", 0)])
                for half in range(TT // 512):
                    tok0 = tt * TT + half * 512
                    ab = aT[half % 2]
                    for c in range(2):
                        gb_, ub_ = 2 * c, 2 * c + 1
                        for v, bk in ((0, gb_), (1, ub_)):
                            for kc in range(8):
                                P.op(PE, lambda e, wb=wb, v=v, kc=kc, c=c, bk=bk, tok0=tok0: e.matmul(
                                    psb(bk), wgu[wb][:, v, kc, c * 128:(c + 1) * 128], actT[:, kc, tok0:tok0 + 512],
                                    start=(kc == 0), stop=(kc == 7)),
                                    reads=[("wgu", wb)], writes=[("ps", bk)])
                        sgb = sg[c]
                        P.op(ACT, lambda e, sgb=sgb, gb_=gb_: e.activation(sgb[:], psb(gb_), AF.Silu),
                             reads=[("ps", gb_)], writes=[("sg", c)])
                        P.op(DVE, lambda e, ab=ab, c=c, sgb=sgb, ub_=ub_: e.tensor_tensor(ab[:, c, :], psb(ub_), sgb[:], ALU.mult),
                             reads=[("ps", ub_), ("sg", c)], writes=[("aT", half % 2, c)])
                    for sub in range(4):
                        s8 = half * 4 + sub
                        t = tt * NS8 + s8
                        for ch in range(2):
                            bk = 4 + (dn % 4)
                            dn += 1
                            for c in range(2):
                                P.op(PE, lambda e, ab=ab, c=c, sub=sub, wb=wb, ch=ch, bk=bk: e.matmul(
                                    psb(bk), ab[:, c, sub * 128:(sub + 1) * 128], wdn[wb][:, c, ch * 512:(ch + 1) * 512],
                                    start=(c == 0), stop=(c == 1)),
                                    reads=[("aT", half % 2, 0), ("aT", half % 2, 1), ("wdn", wb)], writes=[("ps", bk)])
                            P.op(DVE, lambda e, s8=s8, ch=ch, bk=bk, t=t, ex=ex: e.scalar_tensor_tensor(
                                yacc[:, s8, ch * 512:(ch + 1) * 512], psb(bk), comb[:, t, ex:ex + 1],
                                yacc[:, s8, ch * 512:(ch + 1) * 512], ALU.mult, ALU.add),
                                reads=[("ps", bk), ("yacc", s8)], writes=[("yacc", s8)])
                it += 1
            for s8 in range(NS8):
                t = tt * NS8 + s8
                P.op(ACT, lambda e, s8=s8, t=t: e.activation(sq6[:], yacc[:, s8, :], AF.Square, accum_out=st6[:, t, 0:1]),
                     reads=[("yacc", s8), "st6"], writes=["sq6", ("st6", t)])
                P.op(ACT, lambda e, t=t: e.activation(st6[:, t, 1:2], st6[:, t, 0:1], AF.Sqrt, bias=EPS, scale=1.0 / D),
                     reads=[("st6", t)], writes=[("st61", t)])
                P.op(DVE, lambda e, t=t: e.reciprocal(st6[:, t, 1:2], st6[:, t, 1:2]), reads=[("st61", t)], writes=[("st61", t)])
                P.op(DVE, lambda e, s8=s8, t=t: e.scalar_tensor_tensor(
                    yacc[:, s8, :], yacc[:, s8, :], st6[:, t, 1:2], gfin[:], ALU.mult, ALU.mult),
                    reads=[("yacc", s8), ("st61", t), "gfin"], writes=[("yacc", s8)])
                P.op(SP, lambda e, s8=s8, t=t: e.dma_start(out=out_d[t * 128:(t + 1) * 128, :], in_=yacc[:, s8, :]),
                     reads=[("yacc", s8)], dma=True)
    P.emit("p6")
    return nc, dbg_outs


def _consts():
    cfm = np.zeros((128, 512), np.float32)
    cfm[:, 0:128] = np.eye(128, dtype=np.float32)
    cfm[:, 128:256] = np.triu(np.ones((128, 128), np.float32))
    cfm[:, 256:384] = 1.0
    p = np.arange(128)
    j = p % 16
    freq = (10000.0 ** (-(j.astype(np.float32)) / np.float32(16.0))).astype(np.float32)
    sgn = np.where((p % 32) < 16, -1.0, 1.0).astype(np.float32)
    cfm[:, 384] = sgn * freq
    cfm[:, 385] = freq
    cfm[:, 386] = sgn
    cbm = np.zeros((128, 1024), np.float32)
    cbm[:, 0:128] = np.eye(128)
    k = np.arange(128)[:, None]
    q = np.arange(128)[None, :]
    cbm[:, 128:256] = np.where(k > q, NEG, 0.0)
    cbm[:, 256:384] = np.where((k // 64) > (q // 64), NEG, 0.0)
    for h in range(8):
        cbm[h, 384 + h * 65 + 64] = 1.0
    return cfm, cbm.astype(ml_dtypes.bfloat16)


def _prep_inputs(inp):
    f = lambda a: np.ascontiguousarray(np.asarray(a, dtype=np.float32))
    w_in = f(inp["w_in"])
    kr = w_in[:, 384:416]
    swap = np.concatenate([np.arange(16, 32), np.arange(0, 16)])
    w_kr2 = np.concatenate([kr, kr, kr, kr, kr, kr[:, swap]], axis=1)
    w_uq = f(inp["w_uq"])
    idx = np.arange(768).reshape(8, 96).copy()
    idx[:, 64:96] = idx[:, 64:96][:, swap]
    w_uq_sw = w_uq[:, idx.reshape(-1)]
    cfm, cbm = _consts()
    shared = {
        "g_mix": f(inp["g_mix"]).reshape(1, -1),
        "g_ffn": f(inp["g_ffn"]).reshape(1, -1),
        "g_final": f(inp["g_final"]).reshape(1, -1),
        "g_cq": f(inp["g_cq"]).reshape(1, -1),
        "g_ckv": f(inp["g_ckv"]).reshape(1, -1),
        "b_forget": f(inp["b_forget"]).reshape(1, -1),
        "b_r36": np.concatenate([f(inp["b_group"]), f(inp["b_router"])]).reshape(1, -1),
        "w_lat": np.ascontiguousarray(w_in[:, 0:384]),
        "w_kr2": np.ascontiguousarray(w_kr2),
        "w_fq": np.ascontiguousarray(w_in[:, 416:928]),
        "w_fk": np.ascontiguousarray(w_in[:, 928:1440]),
        "w_fv": np.ascontiguousarray(w_in[:, 1440:1952]),
        "w_f": np.ascontiguousarray(w_in[:, 1952:1960]),
        "w_ga": np.ascontiguousarray(w_in[:, 1960:2984]),
        "w_gb": np.ascontiguousarray(w_in[:, 2984:4008]),
        "w_uq": w_uq,
        "w_uq_sw": np.ascontiguousarray(w_uq_sw),
        "w_uk": f(inp["w_uk"]),
        "w_uv": f(inp["w_uv"]),
        "w_o_mla": f(inp["w_o_mla"]),
        "w_o_fox": f(inp["w_o_fox"]),
        "w_out": f(inp["w_out"]),
        "w_r36": np.ascontiguousarray(np.concatenate([f(inp["w_group"]), f(inp["w_router"])], axis=1)),
        "w_e_gate": f(inp["w_e_gate"]).reshape(-1, 2048),
        "w_e_up": f(inp["w_e_up"]).reshape(-1, 2048),
        "w_e_down": f(inp["w_e_down"]).reshape(-1, 2048),
        "consts_f": cfm,
        "consts_b": cbm,
    }
    x = f(inp["x"])
    pos = np.ascontiguousarray(np.asarray(inp["positions"], dtype=np.int32))
    in_maps = []
    for b in range(8):
        m = dict(shared)
        m["x"] = x[b]
        m["pos"] = pos[b].reshape(1, -1)
        in_maps.append(m)
    return in_maps


def kernel(**inputs):
    in_maps = _prep_inputs(inputs)
    nc, _ = build_program()
    res = run_bass_kernel_spmd(nc, in_maps, core_ids=list(range(8)))
    out = np.stack([np.asarray(r["out"], dtype=np.float32) for r in res.results], axis=0)
    return out
```

```python
import contextlib
import numpy as np
import ml_dtypes
import concourse.bass as bass
import concourse.mybir as mybir
from concourse.bass_utils import run_bass_kernel_spmd

F32 = mybir.dt.float32
BF16 = mybir.dt.bfloat16
I32 = mybir.dt.int32
ALU = mybir.AluOpType
AF = mybir.ActivationFunctionType
AX = mybir.AxisListType

PE, ACT, DVE, POOL, SP = "tensor", "scalar", "vector", "gpsimd", "sync"
ENGINES = [PE, ACT, DVE, POOL, SP]

S = 4096
D = 1024
NT = 32
NQ = 8
NH = 8
NE = 32
EFF = 256
EPS = 1e-6
MLA_SCALE = 96.0 ** -0.5
FOX_SCALE = 0.125
NEG = -30000.0
TWO_PI_HI = 6.28125
TWO_PI_LO = 6.283185307179586 - 6.28125


class Op:
    __slots__ = ("eng", "fn", "deps", "is_dma", "marked", "count", "sem", "semval", "prewait", "nosem")

    def __init__(self, eng, fn, is_dma):
        self.eng = eng
        self.fn = fn
        self.deps = []
        self.is_dma = is_dma
        self.marked = False
        self.count = 0
        self.sem = None
        self.semval = 0
        self.prewait = None
        self.nosem = False


class Prog:
    def __init__(self, nc, dma_pool=8):
        self.nc = nc
        self.ops = {e: [] for e in ENGINES}
        self.last_write = {}
        self.readers = {}
        self.dma_pool = dma_pool
        self.dma_count = {e: 0 for e in ENGINES}

    def op(self, eng, fn, reads=(), writes=(), dma=False, nosem=False):
        o = Op(eng, fn, dma)
        o.nosem = nosem
        deps = {}
        for r in reads:
            w = self.last_write.get(r)
            if w is not None:
                deps[id(w)] = (w, "raw")
        for w_ in writes:
            for rd in self.readers.get(w_, ()):
                if id(rd) not in deps:
                    deps[id(rd)] = (rd, "war")
            w = self.last_write.get(w_)
            if w is not None:
                deps[id(w)] = (w, "waw")
        for d, kind in deps.values():
            if d is o:
                continue
            if (not d.is_dma) and (not dma) and d.eng == eng:
                if eng == PE or kind == "war":
                    continue
            o.deps.append(d)
            d.marked = True
        for r in reads:
            self.readers.setdefault(r, []).append(o)
        for w_ in writes:
            self.last_write[w_] = o
            self.readers[w_] = []
        if dma and not nosem:
            k = self.dma_count[eng]
            self.dma_count[eng] = k + 1
            o.sem = (eng, k % self.dma_pool)
            o.semval = 16 * (k // self.dma_pool + 1)
            if k >= self.dma_pool:
                o.prewait = (o.sem, o.semval - 16)
        self.ops[eng].append(o)
        return o

    def emit(self, name):
        nc = self.nc
        for e in ENGINES:
            c = 0
            for o in self.ops[e]:
                if not o.is_dma and o.marked:
                    c += 1
                    o.count = c
        with contextlib.ExitStack() as st:
            esem = {e: st.enter_context(nc.semaphore("s_%s_%s" % (name, e))) for e in ENGINES}
            dsem = {}
            for e in ENGINES:
                for i in range(min(self.dma_pool, self.dma_count[e])):
                    dsem[(e, i)] = st.enter_context(nc.semaphore("d_%s_%s_%d" % (name, e, i)))
            allsems = list(esem.values()) + list(dsem.values())
            with nc.Block() as cblk:
                def _clr(engobj):
                    for s_ in allsems:
                        engobj.sem_clear(s_)
                cblk.sync(_clr)
            block = st.enter_context(nc.Block())

            def run_engine(e, engobj):
                seen = {}
                for o in self.ops[e]:
                    need = {}
                    for d in o.deps:
                        if d.is_dma:
                            key = ("d", d.sem)
                            val = d.semval
                        else:
                            key = ("e", d.eng)
                            val = d.count
                        if need.get(key, 0) < val:
                            need[key] = val
                    if o.prewait is not None:
                        key = ("d", o.prewait[0])
                        if need.get(key, 0) < o.prewait[1]:
                            need[key] = o.prewait[1]
                    for key, val in need.items():
                        if seen.get(key, 0) >= val:
                            continue
                        seen[key] = val
                        s = dsem[key[1]] if key[0] == "d" else esem[key[1]]
                        engobj.wait_ge(s, val)
                    ins = o.fn(engobj)
                    if o.nosem:
                        continue
                    if o.is_dma:
                        ins.then_inc(dsem[o.sem], 16)
                    elif o.marked:
                        ins.then_inc(esem[e], 1)
                k = self.dma_count[e]
                for i in range(min(self.dma_pool, k)):
                    uses = (k - 1 - i) // self.dma_pool + 1
                    if uses > 0:
                        engobj.wait_ge(dsem[(e, i)], 16 * uses)

            for e in ENGINES:
                if not self.ops[e]:
                    continue
                getattr(block, e)(lambda engobj, e=e: run_engine(e, engobj))


def build_program(stop_after=99, debug=None):
    nc = bass.Bass("TRN2", target_bir_lowering=False)
    debug = debug or []

    def din(name, shape, dt=F32):
        return nc.dram_tensor(name, list(shape), dt, kind="ExternalInput").ap()

    x_d = din("x", [S, D])
    pos_d = din("pos", [1, S], I32)
    g_mix_d = din("g_mix", [1, D])
    g_ffn_d = din("g_ffn", [1, D])
    g_fin_d = din("g_final", [1, D])
    g_cq_d = din("g_cq", [1, 256])
    g_ckv_d = din("g_ckv", [1, 128])
    bf_d = din("b_forget", [1, 8])
    br_d = din("b_r36", [1, 36])
    w_lat_d = din("w_lat", [D, 384])
    w_kr_d = din("w_kr2", [D, 192])
    w_fq_d = din("w_fq", [D, 512])
    w_fk_d = din("w_fk", [D, 512])
    w_fv_d = din("w_fv", [D, 512])
    w_f_d = din("w_f", [D, 8])
    w_ga_d = din("w_ga", [D, D])
    w_gb_d = din("w_gb", [D, D])
    w_uq_d = din("w_uq", [256, 768])
    w_uqs_d = din("w_uq_sw", [256, 768])
    w_uk_d = din("w_uk", [128, 512])
    w_uv_d = din("w_uv", [128, 512])
    w_oa_d = din("w_o_mla", [512, D])
    w_ob_d = din("w_o_fox", [512, D])
    w_out_d = din("w_out", [D, D])
    w_r_d = din("w_r36", [D, 36])
    w_eg_d = din("w_e_gate", [NE * D * EFF // 2048, 2048])
    w_eu_d = din("w_e_up", [NE * D * EFF // 2048, 2048])
    w_ed_d = din("w_e_down", [NE * EFF * D // 2048, 2048])
    cf_d = din("consts_f", [128, 512])
    cb_d = din("consts_b", [128, 1024], BF16)
    out_d = nc.dram_tensor("out", [S, D], F32, kind="ExternalOutput").ap()

    ga_s = nc.dram_tensor("ga_s", [D, S], BF16).ap()
    gb_s = nc.dram_tensor("gb_s", [D, S], BF16).ap()
    oa_s = nc.dram_tensor("oa_s", [512, S], BF16).ap()
    ob_s = nc.dram_tensor("ob_s", [512, S], BF16).ap()
    h_s = nc.dram_tensor("h_s", [S, D], F32).ap()

    dbg_outs = {}

    cf = nc.alloc_sbuf_tensor("cf", [128, 512], F32)
    cb = nc.alloc_sbuf_tensor("cb", [128, 1024], BF16)
    actT = nc.alloc_sbuf_tensor("actT", [128, 8, S], BF16)
    comb = nc.alloc_sbuf_tensor("comb", [128, NT, NE], F32)
    ps = nc.alloc_psum_tensor("ps", [128, 8, 512], F32)

    identf = cf[:, 0:128]
    utri = cf[:, 128:256]
    onesf = cf[:, 256:384]
    freq_col = cf[:, 384:385]
    freq_abs = cf[:, 385:386]
    identb = cb[:, 0:128]
    mask_fox = cb[:, 128:256]
    mask_mla = cb[:, 256:384]
    esel = cb[0:8, 384:384 + 8 * 65]


    def psb(b):
        return ps[:, b, :]

    def psb16(b):
        return ps[:, b, :].bitcast(BF16)

    def add_dbg(P, name, ap, shape, dt, reads):
        t = nc.dram_tensor("dbg_" + name, list(shape), dt, kind="ExternalOutput").ap()
        dbg_outs[name] = t
        P.op(SP, lambda e: e.dma_start(out=t, in_=ap), reads=reads, dma=True)

    def wload(P, dst, src_ap, res, eng=POOL):
        P.op(eng, lambda e: e.dma_start(out=dst, in_=src_ap), writes=[res], dma=True)

    def kc_view(w_ap):
        return w_ap.rearrange("(kc p) n -> p kc n", p=128)

    P = Prog(nc)
    wload(P, cf[:], cf_d, "cf", eng=SP)
    wload(P, cb[:], cb_d, "cb", eng=SP)
    with contextlib.ExitStack() as st:
        gmix = st.enter_context(nc.sbuf_tensor("gmix", [128, D], F32))
        xt = [st.enter_context(nc.sbuf_tensor("xt%d" % i, [128, D], F32)) for i in range(3)]
        xs = [st.enter_context(nc.sbuf_tensor("xs%d" % i, [128, D], BF16)) for i in range(2)]
        sq = st.enter_context(nc.sbuf_tensor("sq", [128, D], BF16))
        stat = st.enter_context(nc.sbuf_tensor("stat", [128, NT, 2], F32))
        P.op(SP, lambda e: e.dma_start(out=gmix[:], in_=g_mix_d[0].partition_broadcast(128)), writes=["gmix"], dma=True)
        P.op(DVE, lambda e: e.memset(stat[:], 0.0), writes=["stat"])

        def p1_copy(t):
            bk = t % 2
            P.op(ACT, lambda e: e.copy(
                actT[:, :, t * 128:(t + 1) * 128], psb16(bk).rearrange("p (a b) -> p a b", a=8)),
                reads=[("ps", bk)], writes=[("actT", t)])

        for t in range(NT):
            xb = xt[t % 3]
            xsb = xs[t % 2]
            bk = t % 2
            P.op(SP, lambda e, xb=xb, t=t: e.dma_start(out=xb[:], in_=x_d[t * 128:(t + 1) * 128, :]),
                 writes=[("xt", t % 3)], dma=True)
            P.op(ACT, lambda e, xb=xb, t=t: e.activation(sq[:], xb[:], AF.Square, accum_out=stat[:, t, 0:1]),
                 reads=[("xt", t % 3), "stat"], writes=["sq", ("stat", t)])
            P.op(ACT, lambda e, t=t: e.activation(stat[:, t, 1:2], stat[:, t, 0:1], AF.Sqrt, bias=EPS, scale=1.0 / D),
                 reads=[("stat", t)], writes=[("stat1", t)])
            if t > 0:
                p1_copy(t - 1)
            P.op(DVE, lambda e, t=t: e.reciprocal(stat[:, t, 1:2], stat[:, t, 1:2]),
                 reads=[("stat1", t)], writes=[("stat1", t)])
            P.op(DVE, lambda e, xb=xb, xsb=xsb, t=t: e.scalar_tensor_tensor(
                xsb[:], xb[:], stat[:, t, 1:2], gmix[:], ALU.mult, ALU.mult),
                reads=[("xt", t % 3), ("stat1", t), "gmix"], writes=[("xs", t % 2)])
            for kc in range(8):
                P.op(PE, lambda e, xsb=xsb, kc=kc, bk=bk: e.transpose(
                    psb16(bk)[:, kc * 128:(kc + 1) * 128], xsb[:, kc * 128:(kc + 1) * 128], identb),
                    reads=[("xs", t % 2), "cb"], writes=[("ps", bk)])
        p1_copy(NT - 1)
        if "xnT" in debug:
            add_dbg(P, "xnT", actT[:, :, 0:512], [128, 8, 512], BF16, [("actT", t) for t in range(4)])
    P.emit("p1")
    if stop_after <= 1:
        return nc, dbg_outs

    st_f = contextlib.ExitStack()
    Gc = st_f.enter_context(nc.sbuf_tensor("Gc", [128, NT, 8], F32))
    FsT = st_f.enter_context(nc.sbuf_tensor("FsT", [8, S], BF16))
    P = Prog(nc)
    with contextlib.ExitStack() as st:
        wf = st.enter_context(nc.sbuf_tensor("wf", [128, 8, 8], BF16))
        wga = st.enter_context(nc.sbuf_tensor("wga", [128, 8, D], BF16))
        wgb = st.enter_context(nc.sbuf_tensor("wgb", [128, 8, D], BF16))
        bfb = st.enter_context(nc.sbuf_tensor("bfb", [128, 8], F32))
        lf = st.enter_context(nc.sbuf_tensor("lf", [128, NT, 8], F32))
        tot = st.enter_context(nc.sbuf_tensor("tot", [128, NT, 8], F32))
        off = st.enter_context(nc.sbuf_tensor("off", [128, NT, 8], F32))
        gst = [st.enter_context(nc.sbuf_tensor("gst%d" % i, [128, 512], BF16)) for i in range(4)]
        wload(P, wf[:], kc_view(w_f_d), "wf")
        wload(P, wga[:], kc_view(w_ga_d), "wga")
        wload(P, wgb[:], kc_view(w_gb_d), "wgb")
        P.op(SP, lambda e: e.dma_start(out=bfb[:], in_=bf_d[0].partition_broadcast(128)), writes=["bfb"], dma=True)
        for t in range(NT):
            for kc in range(8):
                P.op(PE, lambda e, t=t, kc=kc: e.matmul(
                    psb(0)[:, t * 8:(t + 1) * 8], actT[:, kc, t * 128:(t + 1) * 128], wf[:, kc, :],
                    start=(kc == 0), stop=(kc == 7)),
                    reads=[("actT", t), "wf"], writes=[("ps", 0)])
        P.op(DVE, lambda e: e.tensor_tensor(
            lf[:], psb(0)[:, 0:256].rearrange("p (t h) -> p t h", h=8),
            bfb[:].unsqueeze(1).broadcast_to([128, NT, 8]), ALU.add),
            reads=[("ps", 0), "bfb"], writes=["lf"])
        P.op(ACT, lambda e: e.activation(lf[:], lf[:], AF.Exp, scale=-1.0), reads=["lf"], writes=["lf"])
        P.op(ACT, lambda e: e.activation(lf[:], lf[:], AF.Ln, bias=1.0), reads=["lf"], writes=["lf"])
        lf2 = lf[:].rearrange("p t h -> p (t h)")
        P.op(PE, lambda e: e.matmul(psb(1)[:, 0:256], utri, lf2, start=True, stop=True),
             reads=["lf", "cf"], writes=[("ps", 1)])
        P.op(PE, lambda e: e.matmul(psb(2)[:, 0:256], onesf, lf2, start=True, stop=True),
             reads=["lf", "cf"], writes=[("ps", 2)])
        P.op(DVE, lambda e: e.tensor_copy(tot[:], psb(2)[:, 0:256].rearrange("p (t h) -> p t h", h=8)),
             reads=[("ps", 2)], writes=["tot"])
        P.op(DVE, lambda e: e.memset(off[:, 0, :], 0.0), writes=["off"])
        for t in range(1, NT):
            P.op(DVE, lambda e, t=t: e.tensor_tensor(off[:, t, :], off[:, t - 1, :], tot[:, t - 1, :], ALU.add),
                 reads=["off", "tot"], writes=["off"])
        P.op(DVE, lambda e: e.tensor_tensor(
            Gc[:], psb(1)[:, 0:256].rearrange("p (t h) -> p t h", h=8), off[:], ALU.add),
            reads=[("ps", 1), "off"], writes=["Gc"])
        for g4 in range(8):
            bk = 3 + (g4 % 2)
            for i in range(4):
                t = g4 * 4 + i
                P.op(PE, lambda e, t=t, i=i, bk=bk: e.transpose(
                    psb(bk)[0:8, i * 128:(i + 1) * 128], Gc[:, t, :], identf),
                    reads=["Gc", "cf"], writes=[("ps", bk)])
            P.op(ACT, lambda e, g4=g4, bk=bk: e.mul(FsT[:, g4 * 512:(g4 + 1) * 512], psb(bk)[0:8, :], -1.0 / FOX_SCALE),
                 reads=[("ps", bk)], writes=["FsT"])
        n = 0
        for (wg_, dst) in ((wga, ga_s), (wgb, gb_s)):
            wname = "wga" if wg_ is wga else "wgb"
            for dc in range(8):
                for q in range(NQ):
                    bk = 5 + (n % 3)
                    sb = gst[n % 4]
                    for kc in range(8):
                        P.op(PE, lambda e, wg_=wg_, dc=dc, q=q, kc=kc, bk=bk: e.matmul(
                            psb(bk), wg_[:, kc, dc * 128:(dc + 1) * 128], actT[:, kc, q * 512:(q + 1) * 512],
                            start=(kc == 0), stop=(kc == 7)),
                            reads=[wname] + [("actT", q * 4 + i) for i in range(4)], writes=[("ps", bk)])
                    P.op(ACT, lambda e, sb=sb, bk=bk: e.activation(sb[:], psb(bk), AF.Sigmoid),
                         reads=[("ps", bk)], writes=[("gst", n % 4)])
                    P.op(SP, lambda e, sb=sb, dst=dst, dc=dc, q=q: e.dma_start(
                        out=dst[dc * 128:(dc + 1) * 128, q * 512:(q + 1) * 512], in_=sb[:]),
                        reads=[("gst", n % 4)], dma=True)
                    n += 1
        if "Gc" in debug:
            add_dbg(P, "Gc", Gc[:], [128, NT, 8], F32, ["Gc"])
            add_dbg(P, "FsT", FsT[:], [8, S], BF16, ["FsT"])
    P.emit("p2a")
    if stop_after <= 2:
        return nc, dbg_outs

    def attention_head(P, h, kdim, qT, kT, vx, scale, bias_fn, mask, o_dst, bufs, hname, interleave=()):
        pt, rrow, rbc, ost = bufs
        pairs = [(I, J) for I in range(NQ) for J in range(4 * I + 4)]
        npairs = len(pairs)

        def emit_qk(n):
            I, J = pairs[n]
            j = J - 4 * I
            c0 = max(0, j) * 128
            sbk = n % 3
            P.op(PE, lambda e: e.matmul(
                psb(sbk)[:, c0:512], kT[0:kdim, J * 128:(J + 1) * 128], qT[0:kdim, I * 512 + c0:(I + 1) * 512],
                start=True, stop=(j < 0)),
                reads=[hname + "q", hname + "k"], writes=[("ps", sbk)])
            if j >= 0:
                P.op(PE, lambda e: e.matmul(
                    psb(sbk)[:, c0:c0 + 128], identb, mask, start=False, stop=True),
                    reads=["cb"], writes=[("ps", sbk)])

        def emit_exp(n):
            I, J = pairs[n]
            j = J - 4 * I
            c0 = max(0, j) * 128
            sbk = n % 3
            ptb = pt[n % 3]
            bias = bias_fn(J)
            P.op(ACT, lambda e: e.activation(
                ptb[:, c0:512], psb(sbk)[:, c0:512], AF.Exp, bias=bias, scale=scale),
                reads=[("ps", sbk), "Gc"], writes=[("pt", n % 3)])

        def emit_pv(n):
            I, J = pairs[n]
            j = J - 4 * I
            c0 = max(0, j) * 128
            ob = 3 + (I % 2)
            nJ = 4 * I + 4
            ptb = pt[n % 3]
            P.op(PE, lambda e: e.matmul(
                psb(ob)[0:65, c0:512], vx[:, J, :], ptb[:, c0:512], start=(J == 0), stop=(J == nJ - 1)),
                reads=[("pt", n % 3), hname + "v"], writes=[("ps", ob)])

        def finalize(I):
            ob = 3 + (I % 2)
            P.op(DVE, lambda e: e.reciprocal(rrow[64:65, :], psb(ob)[64:65, :]),
                 reads=[("ps", ob)], writes=["rrow"])
            P.op(PE, lambda e: e.matmul(psb(5)[0:64, :], onesf[64:65, 0:64], rrow[64:65, :], start=True, stop=True),
                 reads=["rrow", "cf"], writes=[("ps", 5)])
            P.op(ACT, lambda e: e.copy(rbc[0:64, :], psb(5)[0:64, :]), reads=[("ps", 5)], writes=["rbc"])
            osb = ost[I % 2]
            P.op(DVE, lambda e: e.tensor_tensor(osb[0:64, :], psb(ob)[0:64, :], rbc[0:64, :], ALU.mult),
                 reads=[("ps", ob), "rbc"], writes=[("ost", I % 2)])
            P.op(SP, lambda e: e.dma_start(
                out=o_dst[h * 64:(h + 1) * 64, I * 512:(I + 1) * 512], in_=osb[0:64, :]),
                reads=[("ost", I % 2)], writes=["odram"], dma=True)

        pending = []
        chunks = list(interleave)
        step = max(1, (npairs - 8) // (len(chunks) + 1)) if chunks else 0
        emit_qk(0)
        emit_exp(0)
        emit_qk(1)
        emit_exp(1)
        for n in range(npairs):
            if n + 2 < npairs:
                emit_qk(n + 2)
                emit_exp(n + 2)
            emit_pv(n)
            I, J = pairs[n]
            if J == 4 * I + 3:
                pending.append((n + 3, I))
            while pending and pending[0][0] <= n:
                finalize(pending.pop(0)[1])
            if chunks and n % step == step - 1:
                chunks.pop(0)()
        for _, I in pending:
            finalize(I)
        for c in chunks:
            c()

    P = Prog(nc)
    with contextlib.ExitStack() as st:
        wfq = st.enter_context(nc.sbuf_tensor("wfq", [128, 8, 512], BF16))
        wfk = st.enter_context(nc.sbuf_tensor("wfk", [128, 8, 512], BF16))
        wfv = st.enter_context(nc.sbuf_tensor("wfv", [128, 8, 512], BF16))
        qTs = [st.enter_context(nc.sbuf_tensor("fqT%d" % i, [65, S], BF16)) for i in range(2)]
        kTs = [st.enter_context(nc.sbuf_tensor("fkT%d" % i, [65, S], BF16)) for i in range(2)]
        vxs = [st.enter_context(nc.sbuf_tensor("fvx%d" % i, [128, NT, 65], BF16)) for i in range(2)]
        pt = [st.enter_context(nc.sbuf_tensor("pt%d" % i, [128, 512], BF16)) for i in range(3)]
        rrow = st.enter_context(nc.sbuf_tensor("rrow", [65, 512], F32))
        rbc = st.enter_context(nc.sbuf_tensor("rbc", [64, 512], F32))
        ost = [st.enter_context(nc.sbuf_tensor("ost%d" % i, [64, 512], BF16)) for i in range(2)]
        wload(P, wfq[:], kc_view(w_fq_d), "wfq")
        wload(P, wfk[:], kc_view(w_fk_d), "wfk")
        wload(P, wfv[:], kc_view(w_fv_d), "wfv")
        for i in range(2):
            P.op(POOL, lambda e, i=i: e.memset(kTs[i][64:65, :], 1.0), writes=[("fk", i)])
            P.op(POOL, lambda e, i=i: e.memset(vxs[i][:, :, 64:65], 1.0), writes=[("fv", i)])
        allact = [("actT", t) for t in range(NT)]
        def fox_chunks(h):
            b = h % 2
            qT, kT, vx = qTs[b], kTs[b], vxs[b]
            hn = "f%d" % b
            out = []

            def cq(q):
                bk = 6 + (q % 2)
                P.op(PE, lambda e: e.matmul(
                    psb(bk)[0:65, :], esel[:, h * 65:(h + 1) * 65], FsT[:, q * 512:(q + 1) * 512], start=True, stop=False),
                    reads=["FsT", "cb"], writes=[("ps", bk)])
                for kc in range(8):
                    P.op(PE, lambda e, kc=kc: e.matmul(
                        psb(bk)[0:64, :], wfq[:, kc, h * 64:(h + 1) * 64], actT[:, kc, q * 512:(q + 1) * 512],
                        start=False, stop=(kc == 7)),
                        reads=["wfq"] + allact[q * 4:q * 4 + 4], writes=[("ps", bk)])
                P.op(ACT, lambda e: e.copy(qT[0:65, q * 512:(q + 1) * 512], psb(bk)[0:65, :]),
                     reads=[("ps", bk)], writes=[hn + "q"])

            def ck(q):
                bk = 6 + (q % 2)
                for kc in range(8):
                    P.op(PE, lambda e, kc=kc: e.matmul(
                        psb(bk)[0:64, :], wfk[:, kc, h * 64:(h + 1) * 64], actT[:, kc, q * 512:(q + 1) * 512],
                        start=(kc == 0), stop=(kc == 7)),
                        reads=["wfk"] + allact[q * 4:q * 4 + 4], writes=[("ps", bk)])
                P.op(DVE, lambda e: e.tensor_copy(kT[0:64, q * 512:(q + 1) * 512], psb(bk)[0:64, :]),
                     reads=[("ps", bk), ("fk", b)], writes=[hn + "k"])

            def cv(g8):
                bk = 6 + (g8 % 2)
                for i in range(8):
                    t = g8 * 8 + i
                    for kc in range(8):
                        P.op(PE, lambda e, t=t, i=i, kc=kc: e.matmul(
                            psb(bk)[:, i * 64:(i + 1) * 64], actT[:, kc, t * 128:(t + 1) * 128], wfv[:, kc, h * 64:(h + 1) * 64],
                            start=(kc == 0), stop=(kc == 7)),
                            reads=["wfv", ("actT", t)], writes=[("ps", bk)])
                P.op(DVE, lambda e: e.tensor_copy(
                    vx[:, g8 * 8:(g8 + 1) * 8, 0:64], psb(bk).rearrange("p (t d) -> p t d", d=64)),
                    reads=[("ps", bk), ("fv", b)], writes=[hn + "v"])

            for q in range(NQ):
                out.append(lambda q=q: cq(q))
            for q in range(NQ):
                out.append(lambda q=q: ck(q))
            for g8 in range(4):
                out.append(lambda g8=g8: cv(g8))
            return out

        for c in fox_chunks(0):
            c()
        for h in range(NH):
            b = h % 2
            nxt = fox_chunks(h + 1) if h + 1 < NH else []
            attention_head(P, h, 65, qTs[b], kTs[b], vxs[b], FOX_SCALE, lambda J, h=h: Gc[:, J, h:h + 1], mask_fox,
                           ob_s, (pt, rrow, rbc, ost), "f%d" % b, interleave=nxt)
        if "ob" in debug:
            add_dbg(P, "ob", ob_s, [512, S], BF16, ["odram"])
    P.emit("p3")
    st_f.close()
    if stop_after <= 3:
        return nc, dbg_outs

    st_a = contextlib.ExitStack()
    cqnT = st_a.enter_context(nc.sbuf_tensor("cqnT", [128, 3, S], BF16))
    krT = st_a.enter_context(nc.sbuf_tensor("krT", [96, S], BF16))
    cosT = st_a.enter_context(nc.sbuf_tensor("cosT", [96, S], BF16))
    sinT = st_a.enter_context(nc.sbuf_tensor("sinT", [96, S], BF16))
    R = slice(64, 96)
    allc = [("cqnT", t) for t in range(NT)]
    P = Prog(nc)
    with contextlib.ExitStack() as st:
        wlat = st.enter_context(nc.sbuf_tensor("wlat", [128, 8, 384], BF16))
        wkr = st.enter_context(nc.sbuf_tensor("wkr", [128, 8, 192], BF16))
        gcq = st.enter_context(nc.sbuf_tensor("gcq", [128, 384], F32))
        lat = [st.enter_context(nc.sbuf_tensor("lat%d" % i, [128, 384], BF16)) for i in range(2)]
        lsq = st.enter_context(nc.sbuf_tensor("lsq", [128, 384], BF16))
        lst = st.enter_context(nc.sbuf_tensor("lst", [128, NT, 4], F32))
        posi = st.enter_context(nc.sbuf_tensor("posi", [96, 512], I32))
        ang = st.enter_context(nc.sbuf_tensor("ang", [96, 512], F32))
        uu = st.enter_context(nc.sbuf_tensor("uu", [96, 512], F32))
        ki = st.enter_context(nc.sbuf_tensor("ki", [96, 512], I32))
        kf = st.enter_context(nc.sbuf_tensor("kf", [96, 512], F32))
        rr = st.enter_context(nc.sbuf_tensor("rr", [96, 512], F32))
        hs_ = st.enter_context(nc.sbuf_tensor("hs_", [96, 512], F32))
        t1 = st.enter_context(nc.sbuf_tensor("t1", [96, 512], F32))
        t2 = st.enter_context(nc.sbuf_tensor("t2", [96, 512], F32))
        wload(P, wlat[:], kc_view(w_lat_d), "wlat")
        wload(P, wkr[:], kc_view(w_kr_d), "wkr")
        P.op(SP, lambda e: e.dma_start(out=gcq[:, 0:256], in_=g_cq_d[0].partition_broadcast(128)), writes=["gcq"], dma=True)
        P.op(SP, lambda e: e.dma_start(out=gcq[:, 256:384], in_=g_ckv_d[0].partition_broadcast(128)), writes=["gcq"], dma=True)
        for q in range(NQ):
            tk = slice(q * 512, (q + 1) * 512)
            P.op(SP, lambda e, tk=tk: e.dma_start(out=posi[R, :], in_=pos_d[0, tk].partition_broadcast(32)),
                 writes=["posi"], dma=True)
            P.op(DVE, lambda e: e.tensor_copy(ang[R, :], posi[R, :]), reads=["posi"], writes=["ang"])
            P.op(DVE, lambda e: e.tensor_scalar(ang[R, :], ang[R, :], freq_abs[R, :], None, ALU.mult),
                 reads=["ang", "cf"], writes=["ang"])
            P.op(DVE, lambda e: e.tensor_scalar(uu[R, :], ang[R, :], 1.0 / (2 * np.pi), None, ALU.mult),
                 reads=["ang"], writes=["uu"])
            P.op(DVE, lambda e: e.tensor_copy(ki[R, :], uu[R, :]), reads=["uu"], writes=["ki"])
            P.op(DVE, lambda e: e.tensor_copy(kf[R, :], ki[R, :]), reads=["ki"], writes=["kf"])
            P.op(DVE, lambda e: e.scalar_tensor_tensor(rr[R, :], kf[R, :], -TWO_PI_HI, ang[R, :], ALU.mult, ALU.add),
                 reads=["kf", "ang"], writes=["rr"])
            P.op(DVE, lambda e: e.scalar_tensor_tensor(rr[R, :], kf[R, :], -TWO_PI_LO, rr[R, :], ALU.mult, ALU.add),
                 reads=["kf", "rr"], writes=["rr"])
            P.op(DVE, lambda e: e.tensor_scalar(rr[R, :], rr[R, :], 3.1415925, -3.1415925, ALU.min, ALU.max),
                 reads=["rr"], writes=["rr"])
            P.op(ACT, lambda e, tk=tk: e.activation(sinT[R, tk], rr[R, :], AF.Sin, scale=cf[R, 386:387]),
                 reads=["rr", "cf"], writes=["sinT"])
            P.op(ACT, lambda e: e.activation(hs_[R, :], rr[R, :], AF.Sin, scale=0.5), reads=["rr"], writes=["hs_"])
            P.op(DVE, lambda e: e.tensor_tensor(hs_[R, :], hs_[R, :], hs_[R, :], ALU.mult), reads=["hs_"], writes=["hs_"])
            P.op(DVE, lambda e, tk=tk: e.tensor_scalar(cosT[R, tk], hs_[R, :], -2.0, 1.0, ALU.mult, ALU.add),
                 reads=["hs_"], writes=["cosT"])
        P.op(DVE, lambda e: e.memset(lst[:], 0.0), writes=["lst"])
        for t in range(NT):
            bk = 6 + (t % 2)
            lb = lat[t % 2]
            for kc in range(8):
                P.op(PE, lambda e, t=t, kc=kc, bk=bk: e.matmul(
                    psb(bk)[:, 0:384], actT[:, kc, t * 128:(t + 1) * 128], wlat[:, kc, :], start=(kc == 0), stop=(kc == 7)),
                    reads=["wlat", ("actT", t)], writes=[("ps", bk)])
            P.op(ACT, lambda e, t=t, bk=bk: e.activation(lsq[:, 0:256], psb(bk)[:, 0:256], AF.Square, accum_out=lst[:, t, 0:1]),
                 reads=[("ps", bk), "lst"], writes=["lsq", ("lst", t)])
            P.op(ACT, lambda e, t=t, bk=bk: e.activation(lsq[:, 256:384], psb(bk)[:, 256:384], AF.Square, accum_out=lst[:, t, 1:2]),
                 reads=[("ps", bk), ("lst", t)], writes=["lsq", ("lst", t)])
            P.op(ACT, lambda e, t=t: e.activation(lst[:, t, 2:3], lst[:, t, 0:1], AF.Sqrt, bias=EPS, scale=1.0 / 256),
                 reads=[("lst", t)], writes=[("lst2", t)])
            P.op(ACT, lambda e, t=t: e.activation(lst[:, t, 3:4], lst[:, t, 1:2], AF.Sqrt, bias=EPS, scale=1.0 / 128),
                 reads=[("lst", t)], writes=[("lst3", t)])
            P.op(DVE, lambda e, t=t: e.reciprocal(lst[:, t, 2:4], lst[:, t, 2:4]),
                 reads=[("lst2", t), ("lst3", t)], writes=[("lst2", t), ("lst3", t)])
            P.op(DVE, lambda e, t=t, bk=bk, lb=lb: e.scalar_tensor_tensor(
                lb[:, 0:256], psb(bk)[:, 0:256], lst[:, t, 2:3], gcq[:, 0:256], ALU.mult, ALU.mult),
                reads=[("ps", bk), ("lst2", t), "gcq"], writes=[("lat", t % 2)])
            P.op(DVE, lambda e, t=t, bk=bk, lb=lb: e.scalar_tensor_tensor(
                lb[:, 256:384], psb(bk)[:, 256:384], lst[:, t, 3:4], gcq[:, 256:384], ALU.mult, ALU.mult),
                reads=[("ps", bk), ("lst3", t), "gcq"], writes=[("lat", t % 2)])
            tb = 4 + (t % 2)
            for c in range(3):
                P.op(PE, lambda e, lb=lb, c=c, tb=tb: e.transpose(
                    psb16(tb)[:, c * 128:(c + 1) * 128], lb[:, c * 128:(c + 1) * 128], identb),
                    reads=[("lat", t % 2), "cb"], writes=[("ps", tb)])
            P.op(ACT, lambda e, t=t, tb=tb: e.copy(
                cqnT[:, :, t * 128:(t + 1) * 128], psb16(tb)[:, 0:384].rearrange("p (a b) -> p a b", a=3)),
                reads=[("ps", tb)], writes=[("cqnT", t)])
        for q in range(NQ):
            tk = slice(q * 512, (q + 1) * 512)
            for v in range(2):
                for kc in range(8):
                    P.op(PE, lambda e, q=q, v=v, kc=kc: e.matmul(
                        psb(6 + v)[0:96, :], wkr[:, kc, v * 96:(v + 1) * 96], actT[:, kc, q * 512:(q + 1) * 512],
                        start=(kc == 0), stop=(kc == 7)),
                        reads=["wkr"] + [("actT", q * 4 + i) for i in range(4)], writes=[("ps", 6 + v)])
            P.op(DVE, lambda e, tk=tk: e.tensor_tensor(t1[R, :], psb(6)[R, :], cosT[R, tk], ALU.mult),
                 reads=[("ps", 6), "cosT"], writes=["t1"])
            P.op(DVE, lambda e, tk=tk: e.tensor_tensor(t2[R, :], psb(7)[R, :], sinT[R, tk], ALU.mult),
                 reads=[("ps", 7), "sinT"], writes=["t2"])
            P.op(POOL, lambda e, tk=tk: e.tensor_tensor(krT[R, tk], t1[R, :], t2[R, :], ALU.add),
                 reads=["t1", "t2"], writes=["krT"])
        if "lat" in debug:
            add_dbg(P, "cqnT", cqnT[:, :, 0:512], [128, 3, 512], BF16, allc)
            add_dbg(P, "krT", krT[R, 0:512], [32, 512], BF16, ["krT"])
    P.emit("p4a")

    P = Prog(nc)
    with contextlib.ExitStack() as st:
        wuq = st.enter_context(nc.sbuf_tensor("wuq", [128, 2, 768], BF16))
        wuqs = st.enter_context(nc.sbuf_tensor("wuqs", [128, 2, 768], BF16))
        wuk = st.enter_context(nc.sbuf_tensor("wuk", [128, 512], BF16))
        wuv = st.enter_context(nc.sbuf_tensor("wuv", [128, 512], BF16))
        qTs = [st.enter_context(nc.sbuf_tensor("aqT%d" % i, [96, S], BF16)) for i in range(2)]
        kTs = [st.enter_context(nc.sbuf_tensor("akT%d" % i, [96, S], BF16)) for i in range(2)]
        vxs = [st.enter_context(nc.sbuf_tensor("avx%d" % i, [128, NT, 65], BF16)) for i in range(2)]
        pt = [st.enter_context(nc.sbuf_tensor("apt%d" % i, [128, 512], BF16)) for i in range(3)]
        rrow = st.enter_context(nc.sbuf_tensor("arrow", [65, 512], F32))
        rbc = st.enter_context(nc.sbuf_tensor("arbc", [64, 512], F32))
        ost = [st.enter_context(nc.sbuf_tensor("aost%d" % i, [64, 512], BF16)) for i in range(2)]
        t1 = st.enter_context(nc.sbuf_tensor("t1b", [96, 512], F32))
        t2 = st.enter_context(nc.sbuf_tensor("t2b", [96, 512], F32))
        wload(P, wuq[:], kc_view(w_uq_d), "wuq")
        wload(P, wuqs[:], kc_view(w_uqs_d), "wuqs")
        wload(P, wuk[:], w_uk_d, "wuk")
        wload(P, wuv[:], w_uv_d, "wuv")
        for i in range(2):
            P.op(POOL, lambda e, i=i: e.memset(vxs[i][:, :, 64:65], 1.0), writes=[("av", i)])
        def mla_chunks(h):
            b = h % 2
            qT, kT, vx = qTs[b], kTs[b], vxs[b]
            hn = "a%d" % b
            out = []

            def cq(q):
                tk = slice(q * 512, (q + 1) * 512)
                for v, w_ in enumerate((wuq, wuqs)):
                    for kc in range(2):
                        P.op(PE, lambda e, kc=kc, v=v, w_=w_: e.matmul(
                            psb(6 + v)[0:96, :], w_[:, kc, h * 96:(h + 1) * 96], cqnT[:, kc, q * 512:(q + 1) * 512],
                            start=(kc == 0), stop=(kc == 1)),
                            reads=["wuq", "wuqs"] + allc[q * 4:q * 4 + 4], writes=[("ps", 6 + v)])
                P.op(ACT, lambda e: e.copy(qT[0:64, tk], psb(6)[0:64, :]),
                     reads=[("ps", 6)], writes=[hn + "q"])
                P.op(DVE, lambda e: e.tensor_tensor(t1[R, :], psb(6)[R, :], cosT[R, tk], ALU.mult),
                     reads=[("ps", 6), "cosT"], writes=["t1"])
                P.op(DVE, lambda e: e.tensor_tensor(t2[R, :], psb(7)[R, :], sinT[R, tk], ALU.mult),
                     reads=[("ps", 7), "sinT"], writes=["t2"])
                P.op(POOL, lambda e: e.tensor_tensor(qT[R, tk], t1[R, :], t2[R, :], ALU.add),
                     reads=["t1", "t2"], writes=[hn + "q"])

            def ck(q):
                tk = slice(q * 512, (q + 1) * 512)
                bk = 6 + (q % 2)
                P.op(PE, lambda e: e.matmul(
                    psb(bk)[0:64, :], wuk[:, h * 64:(h + 1) * 64], cqnT[:, 2, q * 512:(q + 1) * 512], start=True, stop=True),
                    reads=["wuk"] + allc[q * 4:q * 4 + 4], writes=[("ps", bk)])
                P.op(ACT, lambda e: e.copy(kT[0:64, tk], psb(bk)[0:64, :]),
                     reads=[("ps", bk)], writes=[hn + "k"])

            def ckr():
                P.op(POOL, lambda e: e.tensor_copy(kT[R, :], krT[R, :]), reads=["krT"], writes=[hn + "k"])

            def cv(g8):
                bk = 6 + (g8 % 2)
                for i in range(8):
                    t = g8 * 8 + i
                    P.op(PE, lambda e, t=t, i=i: e.matmul(
                        psb(bk)[:, i * 64:(i + 1) * 64], cqnT[:, 2, t * 128:(t + 1) * 128], wuv[:, h * 64:(h + 1) * 64],
                        start=True, stop=True),
                        reads=["wuv", ("cqnT", t)], writes=[("ps", bk)])
                P.op(DVE, lambda e: e.tensor_copy(
                    vx[:, g8 * 8:(g8 + 1) * 8, 0:64], psb(bk).rearrange("p (t d) -> p t d", d=64)),
                    reads=[("ps", bk), ("av", b)], writes=[hn + "v"])

            for q in range(NQ):
                out.append(lambda q=q: cq(q))
            for q in range(0, NQ, 2):
                out.append(lambda q=q: (ck(q), ck(q + 1)))
            out.append(ckr)
            for g8 in range(4):
                out.append(lambda g8=g8: cv(g8))
            return out

        for c in mla_chunks(0):
            c()
        for h in range(NH):
            b = h % 2
            nxt = mla_chunks(h + 1) if h + 1 < NH else []
            attention_head(P, h, 96, qTs[b], kTs[b], vxs[b], MLA_SCALE, lambda J: 0.0, mask_mla,
                           oa_s, (pt, rrow, rbc, ost), "a%d" % b, interleave=nxt)
        if "oa" in debug:
            add_dbg(P, "oa", oa_s, [512, S], BF16, ["odram"])
    P.emit("p4")
    st_a.close()
    if stop_after <= 4:
        return nc, dbg_outs

    P = Prog(nc)
    with contextlib.ExitStack() as st:
        woa = st.enter_context(nc.sbuf_tensor("woa", [128, 4, D], BF16))
        wob = st.enter_context(nc.sbuf_tensor("wob", [128, 4, D], BF16))
        wout = st.enter_context(nc.sbuf_tensor("wout", [128, 8, D], BF16))
        wr = st.enter_context(nc.sbuf_tensor("wr", [128, 8, 36], BF16))
        gffn = st.enter_context(nc.sbuf_tensor("gffn", [128, D], F32))
        brb = st.enter_context(nc.sbuf_tensor("brb", [128, 36], F32))
        gat = [[st.enter_context(nc.sbuf_tensor("gat%d_%d" % (b_, i), [128, 8, 512], BF16)) for i in range(2)] for b_ in range(2)]
        oin = [[st.enter_context(nc.sbuf_tensor("oin%d_%d" % (b_, i), [128, 4, 512], BF16)) for i in range(2)] for b_ in range(2)]
        mixT = [st.enter_context(nc.sbuf_tensor("mixT%d" % i, [128, 8, 512], BF16)) for i in range(2)]
        m1 = [st.enter_context(nc.sbuf_tensor("m1_0", [128, 512], F32))] * 2
        m2 = [st.enter_context(nc.sbuf_tensor("m2_0", [128, 512], F32))] * 2
        xh = [st.enter_context(nc.sbuf_tensor("xh%d" % i, [128, D], F32)) for i in range(3)]
        hsb = [st.enter_context(nc.sbuf_tensor("hsb%d" % i, [128, D], BF16)) for i in range(2)]
        sq5 = st.enter_context(nc.sbuf_tensor("sq5", [128, D], BF16))
        st5 = st.enter_context(nc.sbuf_tensor("st5", [128, NT, 2], F32))
        Lr = st.enter_context(nc.sbuf_tensor("Lr", [128, NT, 36], F32))
        wload(P, woa[:], w_oa_d.rearrange("(c p) n -> p c n", p=128), "woa")
        wload(P, wob[:], w_ob_d.rearrange("(c p) n -> p c n", p=128), "wob")
        wload(P, wout[:], kc_view(w_out_d), "wout")
        wload(P, wr[:], kc_view(w_r_d), "wr")
        P.op(SP, lambda e: e.dma_start(out=gffn[:], in_=g_ffn_d[0].partition_broadcast(128)), writes=["gffn"], dma=True)
        P.op(SP, lambda e: e.dma_start(out=brb[:], in_=br_d[0].partition_broadcast(128)), writes=["brb"], dma=True)
        P.op(DVE, lambda e: e.memset(st5[:], 0.0), writes=["st5"])
        xi = [0]

        def merge_loads(q):
            tk = slice(q * 512, (q + 1) * 512)
            b_ = q % 2
            P.op(SP, lambda e: e.dma_start(out=gat[b_][0][:], in_=ga_s[:, tk].rearrange("(c p) t -> p c t", p=128)),
                 writes=[("gat", b_, 0)], dma=True)
            P.op(SP, lambda e: e.dma_start(out=oin[b_][0][:], in_=oa_s[:, tk].rearrange("(c p) t -> p c t", p=128)),
                 writes=[("oin", b_, 0)], dma=True)
            P.op(SP, lambda e: e.dma_start(out=gat[b_][1][:], in_=gb_s[:, tk].rearrange("(c p) t -> p c t", p=128)),
                 writes=[("gat", b_, 1)], dma=True)
            P.op(SP, lambda e: e.dma_start(out=oin[b_][1][:], in_=ob_s[:, tk].rearrange("(c p) t -> p c t", p=128)),
                 writes=[("oin", b_, 1)], dma=True)

        def merge_dc(q, dc):
            b_ = q % 2
            mb = 0
            for v, w_ in enumerate((woa, wob)):
                for pc in range(4):
                    P.op(PE, lambda e, v=v, w_=w_, pc=pc: e.matmul(
                        psb(v), w_[:, pc, dc * 128:(dc + 1) * 128], oin[b_][v][:, pc, :], start=(pc == 0), stop=(pc == 3)),
                        reads=["woa", "wob", ("oin", b_, v)], writes=[("ps", v)])
            P.op(DVE, lambda e: e.tensor_tensor(m1[mb][:], psb(0), gat[b_][0][:, dc, :], ALU.mult),
                 reads=[("ps", 0), ("gat", b_, 0)], writes=[("m1", mb)])
            P.op(DVE, lambda e: e.tensor_tensor(m2[mb][:], psb(1), gat[b_][1][:, dc, :], ALU.mult),
                 reads=[("ps", 1), ("gat", b_, 1)], writes=[("m2", mb)])
            P.op(POOL, lambda e: e.tensor_tensor(mixT[b_][:, dc, :], m1[mb][:], m2[mb][:], ALU.add),
                 reads=[("m1", mb), ("m2", mb)], writes=[("mixT", b_, dc)])

        merge_loads(0)
        for dc in range(8):
            merge_dc(0, dc)
        for q in range(NQ):
            if q + 1 < NQ:
                merge_loads(q + 1)
            for sub in range(4):
                t = q * 4 + sub
                xb = xh[xi[0] % 3]
                xr = ("xh", xi[0] % 3)
                xi[0] += 1
                P.op(SP, lambda e, xb=xb, t=t: e.dma_start(out=xb[:], in_=x_d[t * 128:(t + 1) * 128, :]),
                     writes=[xr], dma=True)
                for ch in range(2):
                    bk = 2 + ch
                    for kc in range(8):
                        P.op(PE, lambda e, sub=sub, ch=ch, kc=kc, bk=bk, q=q: e.matmul(
                            psb(bk), mixT[q % 2][:, kc, sub * 128:(sub + 1) * 128], wout[:, kc, ch * 512:(ch + 1) * 512],
                            start=(kc == 0), stop=(kc == 7)),
                            reads=["wout"] + [("mixT", q % 2, d_) for d_ in range(8)], writes=[("ps", bk)])
                    P.op(DVE, lambda e, xb=xb, ch=ch, bk=bk: e.tensor_tensor(
                        xb[:, ch * 512:(ch + 1) * 512], psb(bk), xb[:, ch * 512:(ch + 1) * 512], ALU.add),
                        reads=[("ps", bk), xr], writes=[xr])
                P.op(SP, lambda e, xb=xb, t=t: e.dma_start(out=h_s[t * 128:(t + 1) * 128, :], in_=xb[:]),
                     reads=[xr], dma=True)
                if q + 1 < NQ:
                    merge_dc(q + 1, 2 * sub)
                P.op(ACT, lambda e, xb=xb, t=t: e.activation(sq5[:], xb[:], AF.Square, accum_out=st5[:, t, 0:1]),
                     reads=[xr, "st5"], writes=["sq5", ("st5", t)])
                P.op(ACT, lambda e, t=t: e.activation(st5[:, t, 1:2], st5[:, t, 0:1], AF.Sqrt, bias=EPS, scale=1.0 / D),
                     reads=[("st5", t)], writes=[("st51", t)])
                P.op(DVE, lambda e, t=t: e.reciprocal(st5[:, t, 1:2], st5[:, t, 1:2]),
                     reads=[("st51", t)], writes=[("st51", t)])
                hb = hsb[t % 2]
                P.op(DVE, lambda e, xb=xb, hb=hb, t=t: e.scalar_tensor_tensor(
                    hb[:], xb[:], st5[:, t, 1:2], gffn[:], ALU.mult, ALU.mult),
                    reads=[xr, ("st51", t), "gffn"], writes=[("hsb", t % 2)])
                tb = 4 + (t % 2)
                for kc in range(8):
                    P.op(PE, lambda e, hb=hb, kc=kc, tb=tb: e.transpose(
                        psb16(tb)[:, kc * 128:(kc + 1) * 128], hb[:, kc * 128:(kc + 1) * 128], identb),
                        reads=[("hsb", t % 2), "cb"], writes=[("ps", tb)])
                P.op(ACT, lambda e, t=t, tb=tb: e.copy(
                    actT[:, :, t * 128:(t + 1) * 128], psb16(tb).rearrange("p (a b) -> p a b", a=8)),
                    reads=[("ps", tb)], writes=[("actT", t)])
                if q + 1 < NQ:
                    merge_dc(q + 1, 2 * sub + 1)
                for kc in range(8):
                    P.op(PE, lambda e, t=t, kc=kc: e.matmul(
                        psb(6)[:, 0:36], actT[:, kc, t * 128:(t + 1) * 128], wr[:, kc, :], start=(kc == 0), stop=(kc == 7)),
                        reads=[("actT", t), "wr"], writes=[("ps", 6)])
                P.op(DVE, lambda e, t=t: e.tensor_tensor(Lr[:, t, :], psb(6)[:, 0:36], brb[:], ALU.add),
                     reads=[("ps", 6), "brb"], writes=[("Lr", t)])

        allL = [("Lr", t) for t in range(NT)]
        scr = mixT[0][:].rearrange("p a b -> p (a b)").bitcast(F32)

        class V:
            def __init__(self, ap):
                self.ap = ap

            def __getitem__(self, idx):
                return self.ap if (isinstance(idx, slice) and idx == slice(None)) else self.ap[idx]

        def v3(o, k):
            return V(scr[:, o:o + NT * k].rearrange("p (t k) -> p t k", k=k))

        def v2(o):
            return V(scr[:, o:o + NT])

        sel, sel2, tmp8, is1, is2 = v3(0, 8), v3(256, 8), v3(512, 8), v3(768, 8), v3(1024, 8)
        gd, grp = v3(1280, 4), v3(1408, 4)
        gmax, gp, mx1, mx2, e2, w1, w2 = (v2(1536 + 32 * i) for i in range(7))
        mixall = [("mixT", 0, d_) for d_ in range(8)]

        def b3(ap2, k):
            return ap2.unsqueeze(2).broadcast_to([128, NT, k])

        def dv(fn, reads, writes):
            P.op(DVE, fn, reads=reads, writes=writes)

        Lg = Lr[:, :, 0:4]
        dv(lambda e: e.reduce_max(gmax[:], Lg, axis=AX.X), allL, ["gmax"] + mixall)
        dv(lambda e: e.tensor_tensor(gd[:], Lg, b3(gmax[:], 4), ALU.subtract), allL + ["gmax"], ["gd"])
        P.op(ACT, lambda e: e.activation(gd[:], gd[:], AF.Exp), reads=["gd"], writes=["gd"])
        dv(lambda e: e.reduce_sum(gp[:], gd[:], axis=AX.X), ["gd"], ["gp"])
        dv(lambda e: e.reciprocal(gp[:], gp[:]), ["gp"], ["gp"])
        dv(lambda e: e.tensor_tensor(grp[:], Lg, b3(gmax[:], 4), ALU.is_equal), allL + ["gmax"], ["grp"])
        dv(lambda e: e.tensor_tensor(sel[:], Lr[:, :, 4:12], grp[:, :, 0:1].broadcast_to([128, NT, 8]), ALU.mult),
           allL + ["grp"], ["sel"])
        for g in range(1, 4):
            dv(lambda e, g=g: e.tensor_tensor(tmp8[:], Lr[:, :, 4 + 8 * g:12 + 8 * g],
                                              grp[:, :, g:g + 1].broadcast_to([128, NT, 8]), ALU.mult),
               allL + ["grp", "sel"], ["tmp8"])
            dv(lambda e: e.tensor_tensor(sel[:], sel[:], tmp8[:], ALU.add), ["sel", "tmp8"], ["sel"])
        dv(lambda e: e.reduce_max(mx1[:], sel[:], axis=AX.X), ["sel"], ["mx1"])
        dv(lambda e: e.tensor_tensor(is1[:], sel[:], b3(mx1[:], 8), ALU.is_equal), ["sel", "mx1"], ["is1"])
        dv(lambda e: e.scalar_tensor_tensor(sel2[:], is1[:], -1e30, sel[:], ALU.mult, ALU.add), ["is1", "sel"], ["sel2"])
        dv(lambda e: e.reduce_max(mx2[:], sel2[:], axis=AX.X), ["sel2"], ["mx2"])
        dv(lambda e: e.tensor_tensor(is2[:], sel2[:], b3(mx2[:], 8), ALU.is_equal), ["sel2", "mx2"], ["is2"])
        dv(lambda e: e.tensor_tensor(e2[:], mx2[:], mx1[:], ALU.subtract), ["mx1", "mx2"], ["e2"])
        P.op(ACT, lambda e: e.activation(e2[:], e2[:], AF.Exp), reads=["e2"], writes=["e2"])
        dv(lambda e: e.tensor_scalar(w1[:], e2[:], 1.0, None, ALU.add), ["e2"], ["w1"])
        dv(lambda e: e.reciprocal(w1[:], w1[:]), ["w1"], ["w1"])
        dv(lambda e: e.tensor_tensor(w1[:], w1[:], gp[:], ALU.mult), ["w1", "gp"], ["w1"])
        dv(lambda e: e.tensor_tensor(w2[:], w1[:], e2[:], ALU.mult), ["w1", "e2"], ["w2"])
        dv(lambda e: e.tensor_tensor(is1[:], is1[:], b3(w1[:], 8), ALU.mult), ["is1", "w1", "sel2"], ["is1"])
        dv(lambda e: e.tensor_tensor(is2[:], is2[:], b3(w2[:], 8), ALU.mult), ["is2", "w2"], ["is2"])
        dv(lambda e: e.tensor_tensor(is1[:], is1[:], is2[:], ALU.add), ["is1", "is2"], ["is1"])
        for g in range(4):
            dv(lambda e, g=g: e.tensor_tensor(comb[:, :, 8 * g:8 * g + 8], is1[:],
                                              grp[:, :, g:g + 1].broadcast_to([128, NT, 8]), ALU.mult),
               ["is1", "grp"], [("comb", g)])
        if "p5" in debug:
            add_dbg(P, "comb", comb[:], [128, NT, NE], F32, [("comb", g) for g in range(4)])
            add_dbg(P, "hnT", actT[:, :, 2048:2560], [128, 8, 512], BF16, [("actT", t) for t in range(16, 20)])
    P.emit("p5")
    if stop_after <= 5:
        return nc, dbg_outs

    P = Prog(nc)
    TT = 2048
    NS8 = TT // 128
    with contextlib.ExitStack() as st:
        gfin = st.enter_context(nc.sbuf_tensor("gfin", [128, D], F32))
        yacc = st.enter_context(nc.sbuf_tensor("yacc", [128, TT // 128, D], F32))
        wgu = [st.enter_context(nc.sbuf_tensor("wgu%d" % i, [128, 2, 8, EFF], BF16)) for i in range(2)]
        wdn = [st.enter_context(nc.sbuf_tensor("wdn%d" % i, [128, 2, D], BF16)) for i in range(2)]
        sg = [st.enter_context(nc.sbuf_tensor("sg%d" % i, [128, 512], F32)) for i in range(2)]
        aT = [st.enter_context(nc.sbuf_tensor("aT%d" % i, [128, 2, 512], BF16)) for i in range(2)]
        sq6 = st.enter_context(nc.sbuf_tensor("sq6", [128, D], BF16))
        st6 = st.enter_context(nc.sbuf_tensor("st6", [128, NT, 2], F32))
        P.op(SP, lambda e: e.dma_start(out=gfin[:], in_=g_fin_d[0].partition_broadcast(128)), writes=["gfin"], dma=True)
        P.op(DVE, lambda e: e.memset(st6[:], 0.0), writes=["st6"])
        wg_v = w_eg_d.rearrange("(e r) c -> e (r c)", e=NE).rearrange("e (kc p f) -> e p kc f", p=128, f=EFF)
        wu_v = w_eu_d.rearrange("(e r) c -> e (r c)", e=NE).rearrange("e (kc p f) -> e p kc f", p=128, f=EFF)
        wd_v = w_ed_d.rearrange("(e r) c -> e (r c)", e=NE).rearrange("e (c p n) -> e p c n", p=128, n=D)
        it = 0
        dn = 0
        for tt in range(S // TT):
            for s8 in range(NS8):
                t = tt * NS8 + s8
                P.op(SP, lambda e, s8=s8, t=t: e.dma_start(out=yacc[:, s8, :], in_=h_s[t * 128:(t + 1) * 128, :]),
                     writes=[("yacc", s8)], dma=True)
            if "y0" in debug and tt == 1:
                add_dbg(P, "y0", yacc[:, 0, :], [128, D], F32, [("yacc", 0)])
            for ex in range(NE):
                wb = it % 2
                P.op(POOL, lambda e, ex=ex, wb=wb: e.dma_start(out=wgu[wb][:, 0, :, :], in_=wg_v[ex]), writes=[("wgu", wb)], dma=True)
                P.op(POOL, lambda e, ex=ex, wb=wb: e.dma_start(out=wgu[wb][:, 1, :, :], in_=wu_v[ex]), writes=[("wgu", wb)], dma=True)
                P.op(POOL, lambda e, ex=ex, wb=wb: e.dma_start(out=wdn[wb][:], in_=wd_v[ex]), writes=[("wdn", wb)], dma=True)
                if "p6" in debug and it == 0:
                    add_dbg(P, "wgu0", wgu[0][:], [128, 2, 8, EFF], BF16, [("wgu", 0)])
                    add_dbg(P, "wdn0", wdn[0][:], [128, 2, D], BF16, [("wdn", 0)])
                for half in range(TT // 512):
                    tok0 = tt * TT + half * 512
                    ab = aT[half % 2]
                    for c in range(2):
                        gb_, ub_ = 2 * c, 2 * c + 1
                        for v, bk in ((0, gb_), (1, ub_)):
                            for kc in range(8):
                                P.op(PE, lambda e, wb=wb, v=v, kc=kc, c=c, bk=bk, tok0=tok0: e.matmul(
                                    psb(bk), wgu[wb][:, v, kc, c * 128:(c + 1) * 128], actT[:, kc, tok0:tok0 + 512],
                                    start=(kc == 0), stop=(kc == 7)),
                                    reads=[("wgu", wb)], writes=[("ps", bk)])
                        sgb = sg[c]
                        P.op(ACT, lambda e, sgb=sgb, gb_=gb_: e.activation(sgb[:], psb(gb_), AF.Silu),
                             reads=[("ps", gb_)], writes=[("sg", c)])
                        P.op(DVE, lambda e, ab=ab, c=c, sgb=sgb, ub_=ub_: e.tensor_tensor(ab[:, c, :], psb(ub_), sgb[:], ALU.mult),
                             reads=[("ps", ub_), ("sg", c)], writes=[("aT", half % 2, c)])
                    for sub in range(4):
                        s8 = half * 4 + sub
                        t = tt * NS8 + s8
                        for ch in range(2):
                            bk = 4 + (dn % 4)
                            dn += 1
                            for c in range(2):
                                P.op(PE, lambda e, ab=ab, c=c, sub=sub, wb=wb, ch=ch, bk=bk: e.matmul(
                                    psb(bk), ab[:, c, sub * 128:(sub + 1) * 128], wdn[wb][:, c, ch * 512:(ch + 1) * 512],
                                    start=(c == 0), stop=(c == 1)),
                                    reads=[("aT", half % 2, 0), ("aT", half % 2, 1), ("wdn", wb)], writes=[("ps", bk)])
                            P.op(DVE, lambda e, s8=s8, ch=ch, bk=bk, t=t, ex=ex: e.scalar_tensor_tensor(
                                yacc[:, s8, ch * 512:(ch + 1) * 512], psb(bk), comb[:, t, ex:ex + 1],
                                yacc[:, s8, ch * 512:(ch + 1) * 512], ALU.mult, ALU.add),
                                reads=[("ps", bk), ("yacc", s8)], writes=[("yacc", s8)])
                it += 1
            for s8 in range(NS8):
                t = tt * NS8 + s8
                P.op(ACT, lambda e, s8=s8, t=t: e.activation(sq6[:], yacc[:, s8, :], AF.Square, accum_out=st6[:, t, 0:1]),
                     reads=[("yacc", s8), "st6"], writes=["sq6", ("st6", t)])
                P.op(ACT, lambda e, t=t: e.activation(st6[:, t, 1:2], st6[:, t, 0:1], AF.Sqrt, bias=EPS, scale=1.0 / D),
                     reads=[("st6", t)], writes=[("st61", t)])
                P.op(DVE, lambda e, t=t: e.reciprocal(st6[:, t, 1:2], st6[:, t, 1:2]), reads=[("st61", t)], writes=[("st61", t)])
                P.op(DVE, lambda e, s8=s8, t=t: e.scalar_tensor_tensor(
                    yacc[:, s8, :], yacc[:, s8, :], st6[:, t, 1:2], gfin[:], ALU.mult, ALU.mult),
                    reads=[("yacc", s8), ("st61", t), "gfin"], writes=[("yacc", s8)])
                P.op(SP, lambda e, s8=s8, t=t: e.dma_start(out=out_d[t * 128:(t + 1) * 128, :], in_=yacc[:, s8, :]),
                     reads=[("yacc", s8)], dma=True)
    P.emit("p6")
    return nc, dbg_outs


def _consts():
    cfm = np.zeros((128, 512), np.float32)
    cfm[:, 0:128] = np.eye(128, dtype=np.float32)
    cfm[:, 128:256] = np.triu(np.ones((128, 128), np.float32))
    cfm[:, 256:384] = 1.0
    p = np.arange(128)
    j = p % 16
    freq = (10000.0 ** (-(j.astype(np.float32)) / np.float32(16.0))).astype(np.float32)
    sgn = np.where((p % 32) < 16, -1.0, 1.0).astype(np.float32)
    cfm[:, 384] = sgn * freq
    cfm[:, 385] = freq
    cfm[:, 386] = sgn
    cbm = np.zeros((128, 1024), np.float32)
    cbm[:, 0:128] = np.eye(128)
    k = np.arange(128)[:, None]
    q = np.arange(128)[None, :]
    cbm[:, 128:256] = np.where(k > q, NEG, 0.0)
    cbm[:, 256:384] = np.where((k // 64) > (q // 64), NEG, 0.0)
    for h in range(8):
        cbm[h, 384 + h * 65 + 64] = 1.0
    return cfm, cbm.astype(ml_dtypes.bfloat16)


def _prep_inputs(inp):
    f = lambda a: np.ascontiguousarray(np.asarray(a, dtype=np.float32))
    w_in = f(inp["w_in"])
    kr = w_in[:, 384:416]
    swap = np.concatenate([np.arange(16, 32), np.arange(0, 16)])
    w_kr2 = np.concatenate([kr, kr, kr, kr, kr, kr[:, swap]], axis=1)
    w_uq = f(inp["w_uq"])
    idx = np.arange(768).reshape(8, 96).copy()
    idx[:, 64:96] = idx[:, 64:96][:, swap]
    w_uq_sw = w_uq[:, idx.reshape(-1)]
    cfm, cbm = _consts()
    shared = {
        "g_mix": f(inp["g_mix"]).reshape(1, -1),
        "g_ffn": f(inp["g_ffn"]).reshape(1, -1),
        "g_final": f(inp["g_final"]).reshape(1, -1),
        "g_cq": f(inp["g_cq"]).reshape(1, -1),
        "g_ckv": f(inp["g_ckv"]).reshape(1, -1),
        "b_forget": f(inp["b_forget"]).reshape(1, -1),
        "b_r36": np.concatenate([f(inp["b_group"]), f(inp["b_router"])]).reshape(1, -1),
        "w_lat": np.ascontiguousarray(w_in[:, 0:384]),
        "w_kr2": np.ascontiguousarray(w_kr2),
        "w_fq": np.ascontiguousarray(w_in[:, 416:928]),
        "w_fk": np.ascontiguousarray(w_in[:, 928:1440]),
        "w_fv": np.ascontiguousarray(w_in[:, 1440:1952]),
        "w_f": np.ascontiguousarray(w_in[:, 1952:1960]),
        "w_ga": np.ascontiguousarray(w_in[:, 1960:2984]),
        "w_gb": np.ascontiguousarray(w_in[:, 2984:4008]),
        "w_uq": w_uq,
        "w_uq_sw": np.ascontiguousarray(w_uq_sw),
        "w_uk": f(inp["w_uk"]),
        "w_uv": f(inp["w_uv"]),
        "w_o_mla": f(inp["w_o_mla"]),
        "w_o_fox": f(inp["w_o_fox"]),
        "w_out": f(inp["w_out"]),
        "w_r36": np.ascontiguousarray(np.concatenate([f(inp["w_group"]), f(inp["w_router"])], axis=1)),
        "w_e_gate": f(inp["w_e_gate"]).reshape(-1, 2048),
        "w_e_up": f(inp["w_e_up"]).reshape(-1, 2048),
        "w_e_down": f(inp["w_e_down"]).reshape(-1, 2048),
        "consts_f": cfm,
        "consts_b": cbm,
    }
    x = f(inp["x"])
    pos = np.ascontiguousarray(np.asarray(inp["positions"], dtype=np.int32))
    in_maps = []
    for b in range(8):
        m = dict(shared)
        m["x"] = x[b]
        m["pos"] = pos[b].reshape(1, -1)
        in_maps.append(m)
    return in_maps


def kernel(**inputs):
    in_maps = _prep_inputs(inputs)
    nc, _ = build_program()
    res = run_bass_kernel_spmd(nc, in_maps, core_ids=list(range(8)))
    out = np.stack([np.asarray(r["out"], dtype=np.float32) for r in res.results], axis=0)
    return out
```

```python
import contextlib
import numpy as np
import ml_dtypes
import concourse.bass as bass
import concourse.mybir as mybir
from concourse.bass_utils import run_bass_kernel_spmd

F32 = mybir.dt.float32
BF16 = mybir.dt.bfloat16
I32 = mybir.dt.int32
ALU = mybir.AluOpType
AF = mybir.ActivationFunctionType
AX = mybir.AxisListType

PE, ACT, DVE, POOL, SP = "tensor", "scalar", "vector", "gpsimd", "sync"
ENGINES = [PE, ACT, DVE, POOL, SP]

S = 4096
D = 1024
NT = 32
NQ = 8
NH = 8
NE = 32
EFF = 256
EPS = 1e-6
MLA_SCALE = 96.0 ** -0.5
FOX_SCALE = 0.125
NEG = -30000.0
TWO_PI_HI = 6.28125
TWO_PI_LO = 6.283185307179586 - 6.28125


class Op:
    __slots__ = ("eng", "fn", "deps", "is_dma", "marked", "count", "sem", "semval", "prewait", "nosem")

    def __init__(self, eng, fn, is_dma):
        self.eng = eng
        self.fn = fn
        self.deps = []
        self.is_dma = is_dma
        self.marked = False
        self.count = 0
        self.sem = None
        self.semval = 0
        self.prewait = None
        self.nosem = False


class Prog:
    def __init__(self, nc, dma_pool=8):
        self.nc = nc
        self.ops = {e: [] for e in ENGINES}
        self.last_write = {}
        self.readers = {}
        self.dma_pool = dma_pool
        self.dma_count = {e: 0 for e in ENGINES}

    def op(self, eng, fn, reads=(), writes=(), dma=False, nosem=False):
        o = Op(eng, fn, dma)
        o.nosem = nosem
        deps = {}
        for r in reads:
            w = self.last_write.get(r)
            if w is not None:
                deps[id(w)] = (w, "raw")
        for w_ in writes:
            for rd in self.readers.get(w_, ()):
                if id(rd) not in deps:
                    deps[id(rd)] = (rd, "war")
            w = self.last_write.get(w_)
            if w is not None:
                deps[id(w)] = (w, "waw")
        for d, kind in deps.values():
            if d is o:
                continue
            if (not d.is_dma) and (not dma) and d.eng == eng:
                if eng == PE or kind == "war":
                    continue
            o.deps.append(d)
            d.marked = True
        for r in reads:
            self.readers.setdefault(r, []).append(o)
        for w_ in writes:
            self.last_write[w_] = o
            self.readers[w_] = []
        if dma and not nosem:
            k = self.dma_count[eng]
            self.dma_count[eng] = k + 1
            o.sem = (eng, k % self.dma_pool)
            o.semval = 16 * (k // self.dma_pool + 1)
            if k >= self.dma_pool:
                o.prewait = (o.sem, o.semval - 16)
        self.ops[eng].append(o)
        return o

    def emit(self, name):
        nc = self.nc
        for e in ENGINES:
            c = 0
            for o in self.ops[e]:
                if not o.is_dma and o.marked:
                    c += 1
                    o.count = c
        with contextlib.ExitStack() as st:
            esem = {e: st.enter_context(nc.semaphore("s_%s_%s" % (name, e))) for e in ENGINES}
            dsem = {}
            for e in ENGINES:
                for i in range(min(self.dma_pool, self.dma_count[e])):
                    dsem[(e, i)] = st.enter_context(nc.semaphore("d_%s_%s_%d" % (name, e, i)))
            allsems = list(esem.values()) + list(dsem.values())
            with nc.Block() as cblk:
                def _clr(engobj):
                    for s_ in allsems:
                        engobj.sem_clear(s_)
                cblk.sync(_clr)
            block = st.enter_context(nc.Block())

            def run_engine(e, engobj):
                seen = {}
                for o in self.ops[e]:
                    need = {}
                    for d in o.deps:
                        if d.is_dma:
                            key = ("d", d.sem)
                            val = d.semval
                        else:
                            key = ("e", d.eng)
                            val = d.count
                        if need.get(key, 0) < val:
                            need[key] = val
                    if o.prewait is not None:
                        key = ("d", o.prewait[0])
                        if need.get(key, 0) < o.prewait[1]:
                            need[key] = o.prewait[1]
                    for key, val in need.items():
                        if seen.get(key, 0) >= val:
                            continue
                        seen[key] = val
                        s = dsem[key[1]] if key[0] == "d" else esem[key[1]]
                        engobj.wait_ge(s, val)
                    ins = o.fn(engobj)
                    if o.nosem:
                        continue
                    if o.is_dma:
                        ins.then_inc(dsem[o.sem], 16)
                    elif o.marked:
                        ins.then_inc(esem[e], 1)
                k = self.dma_count[e]
                for i in range(min(self.dma_pool, k)):
                    uses = (k - 1 - i) // self.dma_pool + 1
                    if uses > 0:
                        engobj.wait_ge(dsem[(e, i)], 16 * uses)

            for e in ENGINES:
                if not self.ops[e]:
                    continue
                getattr(block, e)(lambda engobj, e=e: run_engine(e, engobj))


def build_program(stop_after=99, debug=None):
    nc = bass.Bass("TRN2", target_bir_lowering=False)
    debug = debug or []

    def din(name, shape, dt=F32):
        return nc.dram_tensor(name, list(shape), dt, kind="ExternalInput").ap()

    x_d = din("x", [S, D])
    pos_d = din("pos", [1, S], I32)
    g_mix_d = din("g_mix", [1, D])
    g_ffn_d = din("g_ffn", [1, D])
    g_fin_d = din("g_final", [1, D])
    g_cq_d = din("g_cq", [1, 256])
    g_ckv_d = din("g_ckv", [1, 128])
    bf_d = din("b_forget", [1, 8])
    br_d = din("b_r36", [1, 36])
    w_lat_d = din("w_lat", [D, 384])
    w_kr_d = din("w_kr2", [D, 192])
    w_fq_d = din("w_fq", [D, 512])
    w_fk_d = din("w_fk", [D, 512])
    w_fv_d = din("w_fv", [D, 512])
    w_f_d = din("w_f", [D, 8])
    w_ga_d = din("w_ga", [D, D])
    w_gb_d = din("w_gb", [D, D])
    w_uq_d = din("w_uq", [256, 768])
    w_uqs_d = din("w_uq_sw", [256, 768])
    w_uk_d = din("w_uk", [128, 512])
    w_uv_d = din("w_uv", [128, 512])
    w_oa_d = din("w_o_mla", [512, D])
    w_ob_d = din("w_o_fox", [512, D])
    w_out_d = din("w_out", [D, D])
    w_r_d = din("w_r36", [D, 36])
    w_eg_d = din("w_e_gate", [NE * D * EFF // 2048, 2048])
    w_eu_d = din("w_e_up", [NE * D * EFF // 2048, 2048])
    w_ed_d = din("w_e_down", [NE * EFF * D // 2048, 2048])
    cf_d = din("consts_f", [128, 512])
    cb_d = din("consts_b", [128, 1024], BF16)
    out_d = nc.dram_tensor("out", [S, D], F32, kind="ExternalOutput").ap()

    ga_s = nc.dram_tensor("ga_s", [D, S], BF16).ap()
    gb_s = nc.dram_tensor("gb_s", [D, S], BF16).ap()
    oa_s = nc.dram_tensor("oa_s", [512, S], BF16).ap()
    ob_s = nc.dram_tensor("ob_s", [512, S], BF16).ap()
    h_s = nc.dram_tensor("h_s", [S, D], F32).ap()

    dbg_outs = {}

    cf = nc.alloc_sbuf_tensor("cf", [128, 512], F32)
    cb = nc.alloc_sbuf_tensor("cb", [128, 1024], BF16)
    actT = nc.alloc_sbuf_tensor("actT", [128, 8, S], BF16)
    comb = nc.alloc_sbuf_tensor("comb", [128, NT, NE], F32)
    ps = nc.alloc_psum_tensor("ps", [128, 8, 512], F32)

    identf = cf[:, 0:128]
    utri = cf[:, 128:256]
    onesf = cf[:, 256:384]
    freq_col = cf[:, 384:385]
    freq_abs = cf[:, 385:386]
    identb = cb[:, 0:128]
    mask_fox = cb[:, 128:256]
    mask_mla = cb[:, 256:384]
    esel = cb[0:8, 384:384 + 8 * 65]


    def psb(b):
        return ps[:, b, :]

    def psb16(b):
        return ps[:, b, :].bitcast(BF16)

    def add_dbg(P, name, ap, shape, dt, reads):
        t = nc.dram_tensor("dbg_" + name, list(shape), dt, kind="ExternalOutput").ap()
        dbg_outs[name] = t
        P.op(SP, lambda e: e.dma_start(out=t, in_=ap), reads=reads, dma=True)

    def wload(P, dst, src_ap, res, eng=POOL):
        P.op(eng, lambda e: e.dma_start(out=dst, in_=src_ap), writes=[res], dma=True)

    def kc_view(w_ap):
        return w_ap.rearrange("(kc p) n -> p kc n", p=128)

    P = Prog(nc)
    wload(P, cf[:], cf_d, "cf", eng=SP)
    wload(P, cb[:], cb_d, "cb", eng=SP)
    with contextlib.ExitStack() as st:
        gmix = st.enter_context(nc.sbuf_tensor("gmix", [128, D], F32))
        xt = [st.enter_context(nc.sbuf_tensor("xt%d" % i, [128, D], F32)) for i in range(3)]
        xs = [st.enter_context(nc.sbuf_tensor("xs%d" % i, [128, D], BF16)) for i in range(2)]
        sq = st.enter_context(nc.sbuf_tensor("sq", [128, D], BF16))
        stat = st.enter_context(nc.sbuf_tensor("stat", [128, NT, 2], F32))
        P.op(SP, lambda e: e.dma_start(out=gmix[:], in_=g_mix_d[0].partition_broadcast(128)), writes=["gmix"], dma=True)
        P.op(DVE, lambda e: e.memset(stat[:], 0.0), writes=["stat"])

        def p1_copy(t):
            bk = t % 2
            P.op(ACT, lambda e: e.copy(
                actT[:, :, t * 128:(t + 1) * 128], psb16(bk).rearrange("p (a b) -> p a b", a=8)),
                reads=[("ps", bk)], writes=[("actT", t)])

        for t in range(NT):
            xb = xt[t % 3]
            xsb = xs[t % 2]
            bk = t % 2
            P.op(SP, lambda e, xb=xb, t=t: e.dma_start(out=xb[:], in_=x_d[t * 128:(t + 1) * 128, :]),
                 writes=[("xt", t % 3)], dma=True)
            P.op(ACT, lambda e, xb=xb, t=t: e.activation(sq[:], xb[:], AF.Square, accum_out=stat[:, t, 0:1]),
                 reads=[("xt", t % 3), "stat"], writes=["sq", ("stat", t)])
            P.op(ACT, lambda e, t=t: e.activation(stat[:, t, 1:2], stat[:, t, 0:1], AF.Sqrt, bias=EPS, scale=1.0 / D),
                 reads=[("stat", t)], writes=[("stat1", t)])
            if t > 0:
                p1_copy(t - 1)
            P.op(DVE, lambda e, t=t: e.reciprocal(stat[:, t, 1:2], stat[:, t, 1:2]),
                 reads=[("stat1", t)], writes=[("stat1", t)])
            P.op(DVE, lambda e, xb=xb, xsb=xsb, t=t: e.scalar_tensor_tensor(
                xsb[:], xb[:], stat[:, t, 1:2], gmix[:], ALU.mult, ALU.mult),
                reads=[("xt", t % 3), ("stat1", t), "gmix"], writes=[("xs", t % 2)])
            for kc in range(8):
                P.op(PE, lambda e, xsb=xsb, kc=kc, bk=bk: e.transpose(
                    psb16(bk)[:, kc * 128:(kc + 1) * 128], xsb[:, kc * 128:(kc + 1) * 128], identb),
                    reads=[("xs", t % 2), "cb"], writes=[("ps", bk)])
        p1_copy(NT - 1)
        if "xnT" in debug:
            add_dbg(P, "xnT", actT[:, :, 0:512], [128, 8, 512], BF16, [("actT", t) for t in range(4)])
    P.emit("p1")
    if stop_after <= 1:
        return nc, dbg_outs

    st_f = contextlib.ExitStack()
    Gc = st_f.enter_context(nc.sbuf_tensor("Gc", [128, NT, 8], F32))
    FsT = st_f.enter_context(nc.sbuf_tensor("FsT", [8, S], BF16))
    P = Prog(nc)
    with contextlib.ExitStack() as st:
        wf = st.enter_context(nc.sbuf_tensor("wf", [128, 8, 8], BF16))
        wga = st.enter_context(nc.sbuf_tensor("wga", [128, 8, D], BF16))
        wgb = st.enter_context(nc.sbuf_tensor("wgb", [128, 8, D], BF16))
        bfb = st.enter_context(nc.sbuf_tensor("bfb", [128, 8], F32))
        lf = st.enter_context(nc.sbuf_tensor("lf", [128, NT, 8], F32))
        tot = st.enter_context(nc.sbuf_tensor("tot", [128, NT, 8], F32))
        off = st.enter_context(nc.sbuf_tensor("off", [128, NT, 8], F32))
        gst = [st.enter_context(nc.sbuf_tensor("gst%d" % i, [128, 512], BF16)) for i in range(4)]
        wload(P, wf[:], kc_view(w_f_d), "wf")
        wload(P, wga[:], kc_view(w_ga_d), "wga")
        wload(P, wgb[:], kc_view(w_gb_d), "wgb")
        P.op(SP, lambda e: e.dma_start(out=bfb[:], in_=bf_d[0].partition_broadcast(128)), writes=["bfb"], dma=True)
        for t in range(NT):
            for kc in range(8):
                P.op(PE, lambda e, t=t, kc=kc: e.matmul(
                    psb(0)[:, t * 8:(t + 1) * 8], actT[:, kc, t * 128:(t + 1) * 128], wf[:, kc, :],
                    start=(kc == 0), stop=(kc == 7)),
                    reads=[("actT", t), "wf"], writes=[("ps", 0)])
        P.op(DVE, lambda e: e.tensor_tensor(
            lf[:], psb(0)[:, 0:256].rearrange("p (t h) -> p t h", h=8),
            bfb[:].unsqueeze(1).broadcast_to([128, NT, 8]), ALU.add),
            reads=[("ps", 0), "bfb"], writes=["lf"])
        P.op(ACT, lambda e: e.activation(lf[:], lf[:], AF.Exp, scale=-1.0), reads=["lf"], writes=["lf"])
        P.op(ACT, lambda e: e.activation(lf[:], lf[:], AF.Ln, bias=1.0), reads=["lf"], writes=["lf"])
        lf2 = lf[:].rearrange("p t h -> p (t h)")
        P.op(PE, lambda e: e.matmul(psb(1)[:, 0:256], utri, lf2, start=True, stop=True),
             reads=["lf", "cf"], writes=[("ps", 1)])
        P.op(PE, lambda e: e.matmul(psb(2)[:, 0:256], onesf, lf2, start=True, stop=True),
             reads=["lf", "cf"], writes=[("ps", 2)])
        P.op(DVE, lambda e: e.tensor_copy(tot[:], psb(2)[:, 0:256].rearrange("p (t h) -> p t h", h=8)),
             reads=[("ps", 2)], writes=["tot"])
        P.op(DVE, lambda e: e.memset(off[:, 0, :], 0.0), writes=["off"])
        for t in range(1, NT):
            P.op(DVE, lambda e, t=t: e.tensor_tensor(off[:, t, :], off[:, t - 1, :], tot[:, t - 1, :], ALU.add),
                 reads=["off", "tot"], writes=["off"])
        P.op(DVE, lambda e: e.tensor_tensor(
            Gc[:], psb(1)[:, 0:256].rearrange("p (t h) -> p t h", h=8), off[:], ALU.add),
            reads=[("ps", 1), "off"], writes=["Gc"])
        for g4 in range(8):
            bk = 3 + (g4 % 2)
            for i in range(4):
                t = g4 * 4 + i
                P.op(PE, lambda e, t=t, i=i, bk=bk: e.transpose(
                    psb(bk)[0:8, i * 128:(i + 1) * 128], Gc[:, t, :], identf),
                    reads=["Gc", "cf"], writes=[("ps", bk)])
            P.op(ACT, lambda e, g4=g4, bk=bk: e.mul(FsT[:, g4 * 512:(g4 + 1) * 512], psb(bk)[0:8, :], -1.0 / FOX_SCALE),
                 reads=[("ps", bk)], writes=["FsT"])
        n = 0
        for (wg_, dst) in ((wga, ga_s), (wgb, gb_s)):
            wname = "wga" if wg_ is wga else "wgb"
            for dc in range(8):
                for q in range(NQ):
                    bk = 5 + (n % 3)
                    sb = gst[n % 4]
                    for kc in range(8):
                        P.op(PE, lambda e, wg_=wg_, dc=dc, q=q, kc=kc, bk=bk: e.matmul(
                            psb(bk), wg_[:, kc, dc * 128:(dc + 1) * 128], actT[:, kc, q * 512:(q + 1) * 512],
                            start=(kc == 0), stop=(kc == 7)),
                            reads=[wname] + [("actT", q * 4 + i) for i in range(4)], writes=[("ps", bk)])
                    P.op(ACT, lambda e, sb=sb, bk=bk: e.activation(sb[:], psb(bk), AF.Sigmoid),
                         reads=[("ps", bk)], writes=[("gst", n % 4)])
                    P.op(SP, lambda e, sb=sb, dst=dst, dc=dc, q=q: e.dma_start(
                        out=dst[dc * 128:(dc + 1) * 128, q * 512:(q + 1) * 512], in_=sb[:]),
                        reads=[("gst", n % 4)], dma=True)
                    n += 1
        if "Gc" in debug:
            add_dbg(P, "Gc", Gc[:], [128, NT, 8], F32, ["Gc"])
            add_dbg(P, "FsT", FsT[:], [8, S], BF16, ["FsT"])
    P.emit("p2a")
    if stop_after <= 2:
        return nc, dbg_outs

    def attention_head(P, h, kdim, qT, kT, vx, scale, bias_fn, mask, o_dst, bufs, hname, interleave=()):
        pt, rrow, rbc, ost = bufs
        pairs = [(I, J) for I in range(NQ) for J in range(4 * I + 4)]
        npairs = len(pairs)

        def emit_qk(n):
            I, J = pairs[n]
            j = J - 4 * I
            c0 = max(0, j) * 128
            sbk = n % 3
            P.op(PE, lambda e: e.matmul(
                psb(sbk)[:, c0:512], kT[0:kdim, J * 128:(J + 1) * 128], qT[0:kdim, I * 512 + c0:(I + 1) * 512],
                start=True, stop=(j < 0)),
                reads=[hname + "q", hname + "k"], writes=[("ps", sbk)])
            if j >= 0:
                P.op(PE, lambda e: e.matmul(
                    psb(sbk)[:, c0:c0 + 128], identb, mask, start=False, stop=True),
                    reads=["cb"], writes=[("ps", sbk)])

        def emit_exp(n):
            I, J = pairs[n]
            j = J - 4 * I
            c0 = max(0, j) * 128
            sbk = n % 3
            ptb = pt[n % 3]
            bias = bias_fn(J)
            P.op(ACT, lambda e: e.activation(
                ptb[:, c0:512], psb(sbk)[:, c0:512], AF.Exp, bias=bias, scale=scale),
                reads=[("ps", sbk), "Gc"], writes=[("pt", n % 3)])

        def emit_pv(n):
            I, J = pairs[n]
            j = J - 4 * I
            c0 = max(0, j) * 128
            ob = 3 + (I % 2)
            nJ = 4 * I + 4
            ptb = pt[n % 3]
            P.op(PE, lambda e: e.matmul(
                psb(ob)[0:65, c0:512], vx[:, J, :], ptb[:, c0:512], start=(J == 0), stop=(J == nJ - 1)),
                reads=[("pt", n % 3), hname + "v"], writes=[("ps", ob)])

        def finalize(I):
            ob = 3 + (I % 2)
            P.op(DVE, lambda e: e.reciprocal(rrow[64:65, :], psb(ob)[64:65, :]),
                 reads=[("ps", ob)], writes=["rrow"])
            P.op(PE, lambda e: e.matmul(psb(5)[0:64, :], onesf[64:65, 0:64], rrow[64:65, :], start=True, stop=True),
                 reads=["rrow", "cf"], writes=[("ps", 5)])
            P.op(ACT, lambda e: e.copy(rbc[0:64, :], psb(5)[0:64, :]), reads=[("ps", 5)], writes=["rbc"])
            osb = ost[I % 2]
            P.op(DVE, lambda e: e.tensor_tensor(osb[0:64, :], psb(ob)[0:64, :], rbc[0:64, :], ALU.mult),
                 reads=[("ps", ob), "rbc"], writes=[("ost", I % 2)])
            P.op(SP, lambda e: e.dma_start(
                out=o_dst[h * 64:(h + 1) * 64, I * 512:(I + 1) * 512], in_=osb[0:64, :]),
                reads=[("ost", I % 2)], writes=["odram"], dma=True)

        pending = []
        chunks = list(interleave)
        evq = []
        step = max(1, (npairs - 8) // (len(chunks) + 1)) if chunks else 0
        emit_qk(0)
        emit_exp(0)
        emit_qk(1)
        emit_exp(1)
        for n in range(npairs):
            if n + 2 < npairs:
                emit_qk(n + 2)
                emit_exp(n + 2)
            emit_pv(n)
            I, J = pairs[n]
            if J == 4 * I + 3:
                pending.append((n + 3, I))
            while pending and pending[0][0] <= n:
                finalize(pending.pop(0)[1])
            while evq and evq[0][0] <= n:
                evq.pop(0)[1]()
            if chunks and n % step == step - 1:
                ev = chunks.pop(0)()
                if ev:
                    evq.append((n + 2, ev))
        for _, I in pending:
            finalize(I)
        for _, ev in evq:
            ev()
        for c in chunks:
            ev = c()
            if ev:
                ev()

    P = Prog(nc)
    with contextlib.ExitStack() as st:
        wfq = st.enter_context(nc.sbuf_tensor("wfq", [128, 8, 512], BF16))
        wfk = st.enter_context(nc.sbuf_tensor("wfk", [128, 8, 512], BF16))
        wfv = st.enter_context(nc.sbuf_tensor("wfv", [128, 8, 512], BF16))
        qTs = [st.enter_context(nc.sbuf_tensor("fqT%d" % i, [65, S], BF16)) for i in range(2)]
        kTs = [st.enter_context(nc.sbuf_tensor("fkT%d" % i, [65, S], BF16)) for i in range(2)]
        vxs = [st.enter_context(nc.sbuf_tensor("fvx%d" % i, [128, NT, 65], BF16)) for i in range(2)]
        pt = [st.enter_context(nc.sbuf_tensor("pt%d" % i, [128, 512], BF16)) for i in range(3)]
        rrow = st.enter_context(nc.sbuf_tensor("rrow", [65, 512], F32))
        rbc = st.enter_context(nc.sbuf_tensor("rbc", [64, 512], F32))
        ost = [st.enter_context(nc.sbuf_tensor("ost%d" % i, [64, 512], BF16)) for i in range(2)]
        wload(P, wfq[:], kc_view(w_fq_d), "wfq")
        wload(P, wfk[:], kc_view(w_fk_d), "wfk")
        wload(P, wfv[:], kc_view(w_fv_d), "wfv")
        for i in range(2):
            P.op(POOL, lambda e, i=i: e.memset(kTs[i][64:65, :], 1.0), writes=[("fk", i)])
            P.op(POOL, lambda e, i=i: e.memset(vxs[i][:, :, 64:65], 1.0), writes=[("fv", i)])
        allact = [("actT", t) for t in range(NT)]
        def fox_chunks(h):
            b = h % 2
            qT, kT, vx = qTs[b], kTs[b], vxs[b]
            hn = "f%d" % b
            out = []

            def cq(q):
                bk = 6 + (q % 2)
                P.op(PE, lambda e: e.matmul(
                    psb(bk)[0:65, :], esel[:, h * 65:(h + 1) * 65], FsT[:, q * 512:(q + 1) * 512], start=True, stop=False),
                    reads=["FsT", "cb"], writes=[("ps", bk)])
                for kc in range(8):
                    P.op(PE, lambda e, kc=kc: e.matmul(
                        psb(bk)[0:64, :], wfq[:, kc, h * 64:(h + 1) * 64], actT[:, kc, q * 512:(q + 1) * 512],
                        start=False, stop=(kc == 7)),
                        reads=["wfq"] + allact[q * 4:q * 4 + 4], writes=[("ps", bk)])
                return lambda: P.op(ACT, lambda e: e.copy(qT[0:65, q * 512:(q + 1) * 512], psb(bk)[0:65, :]),
                                    reads=[("ps", bk)], writes=[hn + "q"])

            def ck(q):
                bk = 6 + (q % 2)
                for kc in range(8):
                    P.op(PE, lambda e, kc=kc: e.matmul(
                        psb(bk)[0:64, :], wfk[:, kc, h * 64:(h + 1) * 64], actT[:, kc, q * 512:(q + 1) * 512],
                        start=(kc == 0), stop=(kc == 7)),
                        reads=["wfk"] + allact[q * 4:q * 4 + 4], writes=[("ps", bk)])
                return lambda: P.op(DVE, lambda e: e.tensor_copy(kT[0:64, q * 512:(q + 1) * 512], psb(bk)[0:64, :]),
                                    reads=[("ps", bk), ("fk", b)], writes=[hn + "k"])

            def cv(g8):
                bk = 6 + (g8 % 2)
                for i in range(8):
                    t = g8 * 8 + i
                    for kc in range(8):
                        P.op(PE, lambda e, t=t, i=i, kc=kc: e.matmul(
                            psb(bk)[:, i * 64:(i + 1) * 64], actT[:, kc, t * 128:(t + 1) * 128], wfv[:, kc, h * 64:(h + 1) * 64],
                            start=(kc == 0), stop=(kc == 7)),
                            reads=["wfv", ("actT", t)], writes=[("ps", bk)])
                return lambda: P.op(DVE, lambda e: e.tensor_copy(
                    vx[:, g8 * 8:(g8 + 1) * 8, 0:64], psb(bk).rearrange("p (t d) -> p t d", d=64)),
                    reads=[("ps", bk), ("fv", b)], writes=[hn + "v"])

            for q in range(NQ):
                out.append(lambda q=q: cq(q))
            for q in range(NQ):
                out.append(lambda q=q: ck(q))
            for g8 in range(4):
                out.append(lambda g8=g8: cv(g8))
            return out

        for c in fox_chunks(0):
            c()()
        for h in range(NH):
            b = h % 2
            nxt = fox_chunks(h + 1) if h + 1 < NH else []
            attention_head(P, h, 65, qTs[b], kTs[b], vxs[b], FOX_SCALE, lambda J, h=h: Gc[:, J, h:h + 1], mask_fox,
                           ob_s, (pt, rrow, rbc, ost), "f%d" % b, interleave=nxt)
        if "ob" in debug:
            add_dbg(P, "ob", ob_s, [512, S], BF16, ["odram"])
    P.emit("p3")
    st_f.close()
    if stop_after <= 3:
        return nc, dbg_outs

    st_a = contextlib.ExitStack()
    cqnT = st_a.enter_context(nc.sbuf_tensor("cqnT", [128, 3, S], BF16))
    krT = st_a.enter_context(nc.sbuf_tensor("krT", [96, S], BF16))
    cosT = st_a.enter_context(nc.sbuf_tensor("cosT", [96, S], BF16))
    sinT = st_a.enter_context(nc.sbuf_tensor("sinT", [96, S], BF16))
    R = slice(64, 96)
    allc = [("cqnT", t) for t in range(NT)]
    P = Prog(nc)
    with contextlib.ExitStack() as st:
        wlat = st.enter_context(nc.sbuf_tensor("wlat", [128, 8, 384], BF16))
        wkr = st.enter_context(nc.sbuf_tensor("wkr", [128, 8, 192], BF16))
        gcq = st.enter_context(nc.sbuf_tensor("gcq", [128, 384], F32))
        lat = [st.enter_context(nc.sbuf_tensor("lat%d" % i, [128, 384], BF16)) for i in range(2)]
        lsq = st.enter_context(nc.sbuf_tensor("lsq", [128, 384], BF16))
        lst = st.enter_context(nc.sbuf_tensor("lst", [128, NT, 4], F32))
        posi = st.enter_context(nc.sbuf_tensor("posi", [96, 512], I32))
        ang = st.enter_context(nc.sbuf_tensor("ang", [96, 512], F32))
        uu = st.enter_context(nc.sbuf_tensor("uu", [96, 512], F32))
        ki = st.enter_context(nc.sbuf_tensor("ki", [96, 512], I32))
        kf = st.enter_context(nc.sbuf_tensor("kf", [96, 512], F32))
        rr = st.enter_context(nc.sbuf_tensor("rr", [96, 512], F32))
        hs_ = st.enter_context(nc.sbuf_tensor("hs_", [96, 512], F32))
        t1 = st.enter_context(nc.sbuf_tensor("t1", [96, 512], F32))
        t2 = st.enter_context(nc.sbuf_tensor("t2", [96, 512], F32))
        wload(P, wlat[:], kc_view(w_lat_d), "wlat")
        wload(P, wkr[:], kc_view(w_kr_d), "wkr")
        P.op(SP, lambda e: e.dma_start(out=gcq[:, 0:256], in_=g_cq_d[0].partition_broadcast(128)), writes=["gcq"], dma=True)
        P.op(SP, lambda e: e.dma_start(out=gcq[:, 256:384], in_=g_ckv_d[0].partition_broadcast(128)), writes=["gcq"], dma=True)
        for q in range(NQ):
            tk = slice(q * 512, (q + 1) * 512)
            P.op(SP, lambda e, tk=tk: e.dma_start(out=posi[R, :], in_=pos_d[0, tk].partition_broadcast(32)),
                 writes=["posi"], dma=True)
            P.op(DVE, lambda e: e.tensor_copy(ang[R, :], posi[R, :]), reads=["posi"], writes=["ang"])
            P.op(DVE, lambda e: e.tensor_scalar(ang[R, :], ang[R, :], freq_abs[R, :], None, ALU.mult),
                 reads=["ang", "cf"], writes=["ang"])
            P.op(DVE, lambda e: e.tensor_scalar(uu[R, :], ang[R, :], 1.0 / (2 * np.pi), None, ALU.mult),
                 reads=["ang"], writes=["uu"])
            P.op(DVE, lambda e: e.tensor_copy(ki[R, :], uu[R, :]), reads=["uu"], writes=["ki"])
            P.op(DVE, lambda e: e.tensor_copy(kf[R, :], ki[R, :]), reads=["ki"], writes=["kf"])
            P.op(DVE, lambda e: e.scalar_tensor_tensor(rr[R, :], kf[R, :], -TWO_PI_HI, ang[R, :], ALU.mult, ALU.add),
                 reads=["kf", "ang"], writes=["rr"])
            P.op(DVE, lambda e: e.scalar_tensor_tensor(rr[R, :], kf[R, :], -TWO_PI_LO, rr[R, :], ALU.mult, ALU.add),
                 reads=["kf", "rr"], writes=["rr"])
            P.op(DVE, lambda e: e.tensor_scalar(rr[R, :], rr[R, :], 3.1415925, -3.1415925, ALU.min, ALU.max),
                 reads=["rr"], writes=["rr"])
            P.op(ACT, lambda e, tk=tk: e.activation(sinT[R, tk], rr[R, :], AF.Sin, scale=cf[R, 386:387]),
                 reads=["rr", "cf"], writes=["sinT"])
            P.op(ACT, lambda e: e.activation(hs_[R, :], rr[R, :], AF.Sin, scale=0.5), reads=["rr"], writes=["hs_"])
            P.op(DVE, lambda e: e.tensor_tensor(hs_[R, :], hs_[R, :], hs_[R, :], ALU.mult), reads=["hs_"], writes=["hs_"])
            P.op(DVE, lambda e, tk=tk: e.tensor_scalar(cosT[R, tk], hs_[R, :], -2.0, 1.0, ALU.mult, ALU.add),
                 reads=["hs_"], writes=["cosT"])
        P.op(DVE, lambda e: e.memset(lst[:], 0.0), writes=["lst"])

        def lat_copy(t):
            tb = 4 + (t % 2)
            P.op(ACT, lambda e: e.copy(
                cqnT[:, :, t * 128:(t + 1) * 128], psb16(tb)[:, 0:384].rearrange("p (a b) -> p a b", a=3)),
                reads=[("ps", tb)], writes=[("cqnT", t)])

        for t in range(NT):
            bk = 6 + (t % 2)
            lb = lat[t % 2]
            for kc in range(8):
                P.op(PE, lambda e, t=t, kc=kc, bk=bk: e.matmul(
                    psb(bk)[:, 0:384], actT[:, kc, t * 128:(t + 1) * 128], wlat[:, kc, :], start=(kc == 0), stop=(kc == 7)),
                    reads=["wlat", ("actT", t)], writes=[("ps", bk)])
            P.op(ACT, lambda e, t=t, bk=bk: e.activation(lsq[:, 0:256], psb(bk)[:, 0:256], AF.Square, accum_out=lst[:, t, 0:1]),
                 reads=[("ps", bk), "lst"], writes=["lsq", ("lst", t)])
            P.op(ACT, lambda e, t=t, bk=bk: e.activation(lsq[:, 256:384], psb(bk)[:, 256:384], AF.Square, accum_out=lst[:, t, 1:2]),
                 reads=[("ps", bk), ("lst", t)], writes=["lsq", ("lst", t)])
            P.op(ACT, lambda e, t=t: e.activation(lst[:, t, 2:3], lst[:, t, 0:1], AF.Sqrt, bias=EPS, scale=1.0 / 256),
                 reads=[("lst", t)], writes=[("lst2", t)])
            P.op(ACT, lambda e, t=t: e.activation(lst[:, t, 3:4], lst[:, t, 1:2], AF.Sqrt, bias=EPS, scale=1.0 / 128),
                 reads=[("lst", t)], writes=[("lst3", t)])
            if t > 0:
                lat_copy(t - 1)
            P.op(DVE, lambda e, t=t: e.reciprocal(lst[:, t, 2:4], lst[:, t, 2:4]),
                 reads=[("lst2", t), ("lst3", t)], writes=[("lst2", t), ("lst3", t)])
            P.op(DVE, lambda e, t=t, bk=bk, lb=lb: e.scalar_tensor_tensor(
                lb[:, 0:256], psb(bk)[:, 0:256], lst[:, t, 2:3], gcq[:, 0:256], ALU.mult, ALU.mult),
                reads=[("ps", bk), ("lst2", t), "gcq"], writes=[("lat", t % 2)])
            P.op(DVE, lambda e, t=t, bk=bk, lb=lb: e.scalar_tensor_tensor(
                lb[:, 256:384], psb(bk)[:, 256:384], lst[:, t, 3:4], gcq[:, 256:384], ALU.mult, ALU.mult),
                reads=[("ps", bk), ("lst3", t), "gcq"], writes=[("lat", t % 2)])
            tb = 4 + (t % 2)
            for c in range(3):
                P.op(PE, lambda e, lb=lb, c=c, tb=tb: e.transpose(
                    psb16(tb)[:, c * 128:(c + 1) * 128], lb[:, c * 128:(c + 1) * 128], identb),
                    reads=[("lat", t % 2), "cb"], writes=[("ps", tb)])
        lat_copy(NT - 1)
        for q in range(NQ):
            tk = slice(q * 512, (q + 1) * 512)
            b0 = 2 * (q % 2)
            for v in range(2):
                for kc in range(8):
                    P.op(PE, lambda e, q=q, v=v, kc=kc, b0=b0: e.matmul(
                        psb(b0 + v)[0:96, :], wkr[:, kc, v * 96:(v + 1) * 96], actT[:, kc, q * 512:(q + 1) * 512],
                        start=(kc == 0), stop=(kc == 7)),
                        reads=["wkr"] + [("actT", q * 4 + i) for i in range(4)], writes=[("ps", b0 + v)])
            P.op(DVE, lambda e, tk=tk, b0=b0: e.tensor_tensor(t1[R, :], psb(b0)[R, :], cosT[R, tk], ALU.mult),
                 reads=[("ps", b0), "cosT"], writes=["t1"])
            P.op(DVE, lambda e, tk=tk, b0=b0: e.tensor_tensor(t2[R, :], psb(b0 + 1)[R, :], sinT[R, tk], ALU.mult),
                 reads=[("ps", b0 + 1), "sinT"], writes=["t2"])
            P.op(POOL, lambda e, tk=tk: e.tensor_tensor(krT[R, tk], t1[R, :], t2[R, :], ALU.add),
                 reads=["t1", "t2"], writes=["krT"])
        if "lat" in debug:
            add_dbg(P, "cqnT", cqnT[:, :, 0:512], [128, 3, 512], BF16, allc)
            add_dbg(P, "krT", krT[R, 0:512], [32, 512], BF16, ["krT"])
    P.emit("p4a")

    P = Prog(nc)
    with contextlib.ExitStack() as st:
        wuq = st.enter_context(nc.sbuf_tensor("wuq", [128, 2, 768], BF16))
        wuqs = st.enter_context(nc.sbuf_tensor("wuqs", [128, 2, 768], BF16))
        wuk = st.enter_context(nc.sbuf_tensor("wuk", [128, 512], BF16))
        wuv = st.enter_context(nc.sbuf_tensor("wuv", [128, 512], BF16))
        qTs = [st.enter_context(nc.sbuf_tensor("aqT%d" % i, [96, S], BF16)) for i in range(2)]
        kTs = [st.enter_context(nc.sbuf_tensor("akT%d" % i, [96, S], BF16)) for i in range(2)]
        vxs = [st.enter_context(nc.sbuf_tensor("avx%d" % i, [128, NT, 65], BF16)) for i in range(2)]
        pt = [st.enter_context(nc.sbuf_tensor("apt%d" % i, [128, 512], BF16)) for i in range(3)]
        rrow = st.enter_context(nc.sbuf_tensor("arrow", [65, 512], F32))
        rbc = st.enter_context(nc.sbuf_tensor("arbc", [64, 512], F32))
        ost = [st.enter_context(nc.sbuf_tensor("aost%d" % i, [64, 512], BF16)) for i in range(2)]
        t1 = st.enter_context(nc.sbuf_tensor("t1b", [96, 512], F32))
        t2 = st.enter_context(nc.sbuf_tensor("t2b", [96, 512], F32))
        wload(P, wuq[:], kc_view(w_uq_d), "wuq")
        wload(P, wuqs[:], kc_view(w_uqs_d), "wuqs")
        wload(P, wuk[:], w_uk_d, "wuk")
        wload(P, wuv[:], w_uv_d, "wuv")
        for i in range(2):
            P.op(POOL, lambda e, i=i: e.memset(vxs[i][:, :, 64:65], 1.0), writes=[("av", i)])
        def mla_chunks(h):
            b = h % 2
            qT, kT, vx = qTs[b], kTs[b], vxs[b]
            hn = "a%d" % b
            out = []

            def cq(q):
                tk = slice(q * 512, (q + 1) * 512)
                for v, w_ in enumerate((wuq, wuqs)):
                    for kc in range(2):
                        P.op(PE, lambda e, kc=kc, v=v, w_=w_: e.matmul(
                            psb(6 + v)[0:96, :], w_[:, kc, h * 96:(h + 1) * 96], cqnT[:, kc, q * 512:(q + 1) * 512],
                            start=(kc == 0), stop=(kc == 1)),
                            reads=["wuq", "wuqs"] + allc[q * 4:q * 4 + 4], writes=[("ps", 6 + v)])
                def ev():
                    P.op(ACT, lambda e: e.copy(qT[0:64, tk], psb(6)[0:64, :]),
                         reads=[("ps", 6)], writes=[hn + "q"])
                    P.op(DVE, lambda e: e.tensor_tensor(t1[R, :], psb(6)[R, :], cosT[R, tk], ALU.mult),
                         reads=[("ps", 6), "cosT"], writes=["t1"])
                    P.op(DVE, lambda e: e.tensor_tensor(t2[R, :], psb(7)[R, :], sinT[R, tk], ALU.mult),
                         reads=[("ps", 7), "sinT"], writes=["t2"])
                    P.op(POOL, lambda e: e.tensor_tensor(qT[R, tk], t1[R, :], t2[R, :], ALU.add),
                         reads=["t1", "t2"], writes=[hn + "q"])
                return ev

            def ck(q):
                tk = slice(q * 512, (q + 1) * 512)
                bk = 6 + (q % 2)
                P.op(PE, lambda e: e.matmul(
                    psb(bk)[0:64, :], wuk[:, h * 64:(h + 1) * 64], cqnT[:, 2, q * 512:(q + 1) * 512], start=True, stop=True),
                    reads=["wuk"] + allc[q * 4:q * 4 + 4], writes=[("ps", bk)])
                return lambda: P.op(ACT, lambda e: e.copy(kT[0:64, tk], psb(bk)[0:64, :]),
                                    reads=[("ps", bk)], writes=[hn + "k"])

            def ck2(q):
                e0 = ck(q)
                e1 = ck(q + 1)
                return lambda: (e0(), e1())

            def ckr():
                P.op(POOL, lambda e: e.tensor_copy(kT[R, :], krT[R, :]), reads=["krT"], writes=[hn + "k"])
                return None

            def cv(g8):
                bk = 6 + (g8 % 2)
                for i in range(8):
                    t = g8 * 8 + i
                    P.op(PE, lambda e, t=t, i=i: e.matmul(
                        psb(bk)[:, i * 64:(i + 1) * 64], cqnT[:, 2, t * 128:(t + 1) * 128], wuv[:, h * 64:(h + 1) * 64],
                        start=True, stop=True),
                        reads=["wuv", ("cqnT", t)], writes=[("ps", bk)])
                return lambda: P.op(DVE, lambda e: e.tensor_copy(
                    vx[:, g8 * 8:(g8 + 1) * 8, 0:64], psb(bk).rearrange("p (t d) -> p t d", d=64)),
                    reads=[("ps", bk), ("av", b)], writes=[hn + "v"])

            for q in range(NQ):
                out.append(lambda q=q: cq(q))
            for q in range(0, NQ, 2):
                out.append(lambda q=q: ck2(q))
            out.append(ckr)
            for g8 in range(4):
                out.append(lambda g8=g8: cv(g8))
            return out

        for c in mla_chunks(0):
            ev = c()
            if ev:
                ev()
        for h in range(NH):
            b = h % 2
            nxt = mla_chunks(h + 1) if h + 1 < NH else []
            attention_head(P, h, 96, qTs[b], kTs[b], vxs[b], MLA_SCALE, lambda J: 0.0, mask_mla,
                           oa_s, (pt, rrow, rbc, ost), "a%d" % b, interleave=nxt)
        if "oa" in debug:
            add_dbg(P, "oa", oa_s, [512, S], BF16, ["odram"])
    P.emit("p4")
    st_a.close()
    if stop_after <= 4:
        return nc, dbg_outs

    P = Prog(nc)
    with contextlib.ExitStack() as st:
        woa = st.enter_context(nc.sbuf_tensor("woa", [128, 4, D], BF16))
        wob = st.enter_context(nc.sbuf_tensor("wob", [128, 4, D], BF16))
        wout = st.enter_context(nc.sbuf_tensor("wout", [128, 8, D], BF16))
        wr = st.enter_context(nc.sbuf_tensor("wr", [128, 8, 36], BF16))
        gffn = st.enter_context(nc.sbuf_tensor("gffn", [128, D], F32))
        brb = st.enter_context(nc.sbuf_tensor("brb", [128, 36], F32))
        gat = [[st.enter_context(nc.sbuf_tensor("gat%d_%d" % (b_, i), [128, 8, 512], BF16)) for i in range(2)] for b_ in range(2)]
        oin = [[st.enter_context(nc.sbuf_tensor("oin%d_%d" % (b_, i), [128, 4, 512], BF16)) for i in range(2)] for b_ in range(2)]
        mixT = [st.enter_context(nc.sbuf_tensor("mixT%d" % i, [128, 8, 512], BF16)) for i in range(2)]
        m1 = [st.enter_context(nc.sbuf_tensor("m1_0", [128, 512], F32))] * 2
        m2 = [st.enter_context(nc.sbuf_tensor("m2_0", [128, 512], F32))] * 2
        xh = [st.enter_context(nc.sbuf_tensor("xh%d" % i, [128, D], F32)) for i in range(3)]
        hsb = [st.enter_context(nc.sbuf_tensor("hsb%d" % i, [128, D], BF16)) for i in range(2)]
        sq5 = st.enter_context(nc.sbuf_tensor("sq5", [128, D], BF16))
        st5 = st.enter_context(nc.sbuf_tensor("st5", [128, NT, 2], F32))
        Lr = st.enter_context(nc.sbuf_tensor("Lr", [128, NT, 36], F32))
        wload(P, woa[:], w_oa_d.rearrange("(c p) n -> p c n", p=128), "woa")
        wload(P, wob[:], w_ob_d.rearrange("(c p) n -> p c n", p=128), "wob")
        wload(P, wout[:], kc_view(w_out_d), "wout")
        wload(P, wr[:], kc_view(w_r_d), "wr")
        P.op(SP, lambda e: e.dma_start(out=gffn[:], in_=g_ffn_d[0].partition_broadcast(128)), writes=["gffn"], dma=True)
        P.op(SP, lambda e: e.dma_start(out=brb[:], in_=br_d[0].partition_broadcast(128)), writes=["brb"], dma=True)
        P.op(DVE, lambda e: e.memset(st5[:], 0.0), writes=["st5"])
        xi = [0]

        def merge_loads(q):
            tk = slice(q * 512, (q + 1) * 512)
            b_ = q % 2
            P.op(SP, lambda e: e.dma_start(out=gat[b_][0][:], in_=ga_s[:, tk].rearrange("(c p) t -> p c t", p=128)),
                 writes=[("gat", b_, 0)], dma=True)
            P.op(SP, lambda e: e.dma_start(out=oin[b_][0][:], in_=oa_s[:, tk].rearrange("(c p) t -> p c t", p=128)),
                 writes=[("oin", b_, 0)], dma=True)
            P.op(SP, lambda e: e.dma_start(out=gat[b_][1][:], in_=gb_s[:, tk].rearrange("(c p) t -> p c t", p=128)),
                 writes=[("gat", b_, 1)], dma=True)
            P.op(SP, lambda e: e.dma_start(out=oin[b_][1][:], in_=ob_s[:, tk].rearrange("(c p) t -> p c t", p=128)),
                 writes=[("oin", b_, 1)], dma=True)

        def merge_dc(q, dc):
            b_ = q % 2
            mb = 0
            for v, w_ in enumerate((woa, wob)):
                for pc in range(4):
                    P.op(PE, lambda e, v=v, w_=w_, pc=pc: e.matmul(
                        psb(v), w_[:, pc, dc * 128:(dc + 1) * 128], oin[b_][v][:, pc, :], start=(pc == 0), stop=(pc == 3)),
                        reads=["woa", "wob", ("oin", b_, v)], writes=[("ps", v)])
            P.op(DVE, lambda e: e.tensor_tensor(m1[mb][:], psb(0), gat[b_][0][:, dc, :], ALU.mult),
                 reads=[("ps", 0), ("gat", b_, 0)], writes=[("m1", mb)])
            P.op(DVE, lambda e: e.tensor_tensor(m2[mb][:], psb(1), gat[b_][1][:, dc, :], ALU.mult),
                 reads=[("ps", 1), ("gat", b_, 1)], writes=[("m2", mb)])
            P.op(POOL, lambda e: e.tensor_tensor(mixT[b_][:, dc, :], m1[mb][:], m2[mb][:], ALU.add),
                 reads=[("m1", mb), ("m2", mb)], writes=[("mixT", b_, dc)])

        merge_loads(0)
        for dc in range(8):
            merge_dc(0, dc)
        for q in range(NQ):
            if q + 1 < NQ:
                merge_loads(q + 1)
            for sub in range(4):
                t = q * 4 + sub
                xb = xh[xi[0] % 3]
                xr = ("xh", xi[0] % 3)
                xi[0] += 1
                P.op(SP, lambda e, xb=xb, t=t: e.dma_start(out=xb[:], in_=x_d[t * 128:(t + 1) * 128, :]),
                     writes=[xr], dma=True)
                for ch in range(2):
                    bk = 2 + ch
                    for kc in range(8):
                        P.op(PE, lambda e, sub=sub, ch=ch, kc=kc, bk=bk, q=q: e.matmul(
                            psb(bk), mixT[q % 2][:, kc, sub * 128:(sub + 1) * 128], wout[:, kc, ch * 512:(ch + 1) * 512],
                            start=(kc == 0), stop=(kc == 7)),
                            reads=["wout"] + [("mixT", q % 2, d_) for d_ in range(8)], writes=[("ps", bk)])
                    P.op(DVE, lambda e, xb=xb, ch=ch, bk=bk: e.tensor_tensor(
                        xb[:, ch * 512:(ch + 1) * 512], psb(bk), xb[:, ch * 512:(ch + 1) * 512], ALU.add),
                        reads=[("ps", bk), xr], writes=[xr])
                P.op(SP, lambda e, xb=xb, t=t: e.dma_start(out=h_s[t * 128:(t + 1) * 128, :], in_=xb[:]),
                     reads=[xr], dma=True)
                if q + 1 < NQ:
                    merge_dc(q + 1, 2 * sub)
                P.op(ACT, lambda e, xb=xb, t=t: e.activation(sq5[:], xb[:], AF.Square, accum_out=st5[:, t, 0:1]),
                     reads=[xr, "st5"], writes=["sq5", ("st5", t)])
                P.op(ACT, lambda e, t=t: e.activation(st5[:, t, 1:2], st5[:, t, 0:1], AF.Sqrt, bias=EPS, scale=1.0 / D),
                     reads=[("st5", t)], writes=[("st51", t)])
                P.op(DVE, lambda e, t=t: e.reciprocal(st5[:, t, 1:2], st5[:, t, 1:2]),
                     reads=[("st51", t)], writes=[("st51", t)])
                hb = hsb[t % 2]
                P.op(DVE, lambda e, xb=xb, hb=hb, t=t: e.scalar_tensor_tensor(
                    hb[:], xb[:], st5[:, t, 1:2], gffn[:], ALU.mult, ALU.mult),
                    reads=[xr, ("st51", t), "gffn"], writes=[("hsb", t % 2)])
                tb = 4 + (t % 2)
                for kc in range(8):
                    P.op(PE, lambda e, hb=hb, kc=kc, tb=tb: e.transpose(
                        psb16(tb)[:, kc * 128:(kc + 1) * 128], hb[:, kc * 128:(kc + 1) * 128], identb),
                        reads=[("hsb", t % 2), "cb"], writes=[("ps", tb)])
                P.op(ACT, lambda e, t=t, tb=tb: e.copy(
                    actT[:, :, t * 128:(t + 1) * 128], psb16(tb).rearrange("p (a b) -> p a b", a=8)),
                    reads=[("ps", tb)], writes=[("actT", t)])
                if q + 1 < NQ:
                    merge_dc(q + 1, 2 * sub + 1)
                for kc in range(8):
                    P.op(PE, lambda e, t=t, kc=kc: e.matmul(
                        psb(6)[:, 0:36], actT[:, kc, t * 128:(t + 1) * 128], wr[:, kc, :], start=(kc == 0), stop=(kc == 7)),
                        reads=[("actT", t), "wr"], writes=[("ps", 6)])
                P.op(DVE, lambda e, t=t: e.tensor_tensor(Lr[:, t, :], psb(6)[:, 0:36], brb[:], ALU.add),
                     reads=[("ps", 6), "brb"], writes=[("Lr", t)])

        allL = [("Lr", t) for t in range(NT)]
        scr = mixT[0][:].rearrange("p a b -> p (a b)").bitcast(F32)

        class V:
            def __init__(self, ap):
                self.ap = ap

            def __getitem__(self, idx):
                return self.ap if (isinstance(idx, slice) and idx == slice(None)) else self.ap[idx]

        def v3(o, k):
            return V(scr[:, o:o + NT * k].rearrange("p (t k) -> p t k", k=k))

        def v2(o):
            return V(scr[:, o:o + NT])

        sel, sel2, tmp8, is1, is2 = v3(0, 8), v3(256, 8), v3(512, 8), v3(768, 8), v3(1024, 8)
        gd, grp = v3(1280, 4), v3(1408, 4)
        gmax, gp, mx1, mx2, e2, w1, w2 = (v2(1536 + 32 * i) for i in range(7))
        mixall = [("mixT", 0, d_) for d_ in range(8)]

        def b3(ap2, k):
            return ap2.unsqueeze(2).broadcast_to([128, NT, k])

        def dv(fn, reads, writes):
            P.op(DVE, fn, reads=reads, writes=writes)

        Lg = Lr[:, :, 0:4]
        dv(lambda e: e.reduce_max(gmax[:], Lg, axis=AX.X), allL, ["gmax"] + mixall)
        dv(lambda e: e.tensor_tensor(gd[:], Lg, b3(gmax[:], 4), ALU.subtract), allL + ["gmax"], ["gd"])
        P.op(ACT, lambda e: e.activation(gd[:], gd[:], AF.Exp), reads=["gd"], writes=["gd"])
        dv(lambda e: e.reduce_sum(gp[:], gd[:], axis=AX.X), ["gd"], ["gp"])
        dv(lambda e: e.reciprocal(gp[:], gp[:]), ["gp"], ["gp"])
        dv(lambda e: e.tensor_tensor(grp[:], Lg, b3(gmax[:], 4), ALU.is_equal), allL + ["gmax"], ["grp"])
        dv(lambda e: e.tensor_tensor(sel[:], Lr[:, :, 4:12], grp[:, :, 0:1].broadcast_to([128, NT, 8]), ALU.mult),
           allL + ["grp"], ["sel"])
        for g in range(1, 4):
            dv(lambda e, g=g: e.tensor_tensor(tmp8[:], Lr[:, :, 4 + 8 * g:12 + 8 * g],
                                              grp[:, :, g:g + 1].broadcast_to([128, NT, 8]), ALU.mult),
               allL + ["grp", "sel"], ["tmp8"])
            dv(lambda e: e.tensor_tensor(sel[:], sel[:], tmp8[:], ALU.add), ["sel", "tmp8"], ["sel"])
        dv(lambda e: e.reduce_max(mx1[:], sel[:], axis=AX.X), ["sel"], ["mx1"])
        dv(lambda e: e.tensor_tensor(is1[:], sel[:], b3(mx1[:], 8), ALU.is_equal), ["sel", "mx1"], ["is1"])
        dv(lambda e: e.scalar_tensor_tensor(sel2[:], is1[:], -1e30, sel[:], ALU.mult, ALU.add), ["is1", "sel"], ["sel2"])
        dv(lambda e: e.reduce_max(mx2[:], sel2[:], axis=AX.X), ["sel2"], ["mx2"])
        dv(lambda e: e.tensor_tensor(is2[:], sel2[:], b3(mx2[:], 8), ALU.is_equal), ["sel2", "mx2"], ["is2"])
        dv(lambda e: e.tensor_tensor(e2[:], mx2[:], mx1[:], ALU.subtract), ["mx1", "mx2"], ["e2"])
        P.op(ACT, lambda e: e.activation(e2[:], e2[:], AF.Exp), reads=["e2"], writes=["e2"])
        dv(lambda e: e.tensor_scalar(w1[:], e2[:], 1.0, None, ALU.add), ["e2"], ["w1"])
        dv(lambda e: e.reciprocal(w1[:], w1[:]), ["w1"], ["w1"])
        dv(lambda e: e.tensor_tensor(w1[:], w1[:], gp[:], ALU.mult), ["w1", "gp"], ["w1"])
        dv(lambda e: e.tensor_tensor(w2[:], w1[:], e2[:], ALU.mult), ["w1", "e2"], ["w2"])
        dv(lambda e: e.tensor_tensor(is1[:], is1[:], b3(w1[:], 8), ALU.mult), ["is1", "w1", "sel2"], ["is1"])
        dv(lambda e: e.tensor_tensor(is2[:], is2[:], b3(w2[:], 8), ALU.mult), ["is2", "w2"], ["is2"])
        dv(lambda e: e.tensor_tensor(is1[:], is1[:], is2[:], ALU.add), ["is1", "is2"], ["is1"])
        for g in range(4):
            dv(lambda e, g=g: e.tensor_tensor(comb[:, :, 8 * g:8 * g + 8], is1[:],
                                              grp[:, :, g:g + 1].broadcast_to([128, NT, 8]), ALU.mult),
               ["is1", "grp"], [("comb", g)])
        if "p5" in debug:
            add_dbg(P, "comb", comb[:], [128, NT, NE], F32, [("comb", g) for g in range(4)])
            add_dbg(P, "hnT", actT[:, :, 2048:2560], [128, 8, 512], BF16, [("actT", t) for t in range(16, 20)])
    P.emit("p5")
    if stop_after <= 5:
        return nc, dbg_outs

    P = Prog(nc)
    TT = 2048
    NS8 = TT // 128
    with contextlib.ExitStack() as st:
        gfin = st.enter_context(nc.sbuf_tensor("gfin", [128, D], F32))
        yacc = st.enter_context(nc.sbuf_tensor("yacc", [128, TT // 128, D], F32))
        wgu = [st.enter_context(nc.sbuf_tensor("wgu%d" % i, [128, 2, 8, EFF], BF16)) for i in range(2)]
        wdn = [st.enter_context(nc.sbuf_tensor("wdn%d" % i, [128, 2, D], BF16)) for i in range(2)]
        sg = [st.enter_context(nc.sbuf_tensor("sg%d" % i, [128, 512], F32)) for i in range(2)]
        aT = [st.enter_context(nc.sbuf_tensor("aT%d" % i, [128, 2, 512], BF16)) for i in range(2)]
        sq6 = st.enter_context(nc.sbuf_tensor("sq6", [128, D], BF16))
        st6 = st.enter_context(nc.sbuf_tensor("st6", [128, NT, 2], F32))
        P.op(SP, lambda e: e.dma_start(out=gfin[:], in_=g_fin_d[0].partition_broadcast(128)), writes=["gfin"], dma=True)
        P.op(DVE, lambda e: e.memset(st6[:], 0.0), writes=["st6"])
        wg_v = w_eg_d.rearrange("(e r) c -> e (r c)", e=NE).rearrange("e (kc p f) -> e p kc f", p=128, f=EFF)
        wu_v = w_eu_d.rearrange("(e r) c -> e (r c)", e=NE).rearrange("e (kc p f) -> e p kc f", p=128, f=EFF)
        wd_v = w_ed_d.rearrange("(e r) c -> e (r c)", e=NE).rearrange("e (c p n) -> e p c n", p=128, n=D)
        it = 0
        dn = 0
        for tt in range(S // TT):
            for s8 in range(NS8):
                t = tt * NS8 + s8
                P.op(SP, lambda e, s8=s8, t=t: e.dma_start(out=yacc[:, s8, :], in_=h_s[t * 128:(t + 1) * 128, :]),
                     writes=[("yacc", s8)], dma=True)
            if "y0" in debug and tt == 1:
                add_dbg(P, "y0", yacc[:, 0, :], [128, D], F32, [("yacc", 0)])
            def wloads(ex, wb):
                P.op(POOL, lambda e: e.dma_start(out=wgu[wb][:, 0, :, :], in_=wg_v[ex]), writes=[("wgu", wb)], dma=True)
                P.op(POOL, lambda e: e.dma_start(out=wgu[wb][:, 1, :, :], in_=wu_v[ex]), writes=[("wgu", wb)], dma=True)
                P.op(POOL, lambda e: e.dma_start(out=wdn[wb][:], in_=wd_v[ex]), writes=[("wdn", wb)], dma=True)

            def gu(ex, half, wb):
                tok0 = tt * TT + half * 512
                ab = aT[half % 2]
                for c in range(2):
                    gb_, ub_ = 2 * c, 2 * c + 1
                    for v, bk in ((0, gb_), (1, ub_)):
                        for kc in range(8):
                            P.op(PE, lambda e, v=v, kc=kc, c=c, bk=bk: e.matmul(
                                psb(bk), wgu[wb][:, v, kc, c * 128:(c + 1) * 128], actT[:, kc, tok0:tok0 + 512],
                                start=(kc == 0), stop=(kc == 7)),
                                reads=[("wgu", wb)], writes=[("ps", bk)])
                    sgb = sg[c]
                    P.op(ACT, lambda e, sgb=sgb, gb_=gb_: e.activation(sgb[:], psb(gb_), AF.Silu),
                         reads=[("ps", gb_)], writes=[("sg", c)])
                    P.op(DVE, lambda e, c=c, sgb=sgb, ub_=ub_: e.tensor_tensor(ab[:, c, :], psb(ub_), sgb[:], ALU.mult),
                         reads=[("ps", ub_), ("sg", c)], writes=[("aT", half % 2, c)])

            def dnp(ex, half, wb):
                nonlocal dn
                ab = aT[half % 2]
                for sub in range(4):
                    s8 = half * 4 + sub
                    t = tt * NS8 + s8
                    for ch in range(2):
                        bk = 4 + (dn % 4)
                        dn += 1
                        for c in range(2):
                            P.op(PE, lambda e, c=c, sub=sub, ch=ch, bk=bk: e.matmul(
                                psb(bk), ab[:, c, sub * 128:(sub + 1) * 128], wdn[wb][:, c, ch * 512:(ch + 1) * 512],
                                start=(c == 0), stop=(c == 1)),
                                reads=[("aT", half % 2, 0), ("aT", half % 2, 1), ("wdn", wb)], writes=[("ps", bk)])
                        P.op(DVE, lambda e, s8=s8, ch=ch, bk=bk, t=t: e.scalar_tensor_tensor(
                            yacc[:, s8, ch * 512:(ch + 1) * 512], psb(bk), comb[:, t, ex:ex + 1],
                            yacc[:, s8, ch * 512:(ch + 1) * 512], ALU.mult, ALU.add),
                            reads=[("ps", bk), ("yacc", s8)], writes=[("yacc", s8)])

            items = [(ex, half) for ex in range(NE) for half in range(TT // 512)]
            wbs = {}
            for ex in range(NE):
                wbs[ex] = it % 2
                it += 1

            def gu_item(i):
                ex, half = items[i]
                if half == 0:
                    wloads(ex, wbs[ex])
                gu(ex, half, wbs[ex])

            gu_item(0)
            for i in range(len(items)):
                if i + 1 < len(items):
                    gu_item(i + 1)
                ex, half = items[i]
                dnp(ex, half, wbs[ex])
            for s8 in range(NS8):
                t = tt * NS8 + s8
                P.op(ACT, lambda e, s8=s8, t=t: e.activation(sq6[:], yacc[:, s8, :], AF.Square, accum_out=st6[:, t, 0:1]),
                     reads=[("yacc", s8), "st6"], writes=["sq6", ("st6", t)])
                P.op(ACT, lambda e, t=t: e.activation(st6[:, t, 1:2], st6[:, t, 0:1], AF.Sqrt, bias=EPS, scale=1.0 / D),
                     reads=[("st6", t)], writes=[("st61", t)])
                P.op(DVE, lambda e, t=t: e.reciprocal(st6[:, t, 1:2], st6[:, t, 1:2]), reads=[("st61", t)], writes=[("st61", t)])
                P.op(DVE, lambda e, s8=s8, t=t: e.scalar_tensor_tensor(
                    yacc[:, s8, :], yacc[:, s8, :], st6[:, t, 1:2], gfin[:], ALU.mult, ALU.mult),
                    reads=[("yacc", s8), ("st61", t), "gfin"], writes=[("yacc", s8)])
                P.op(SP, lambda e, s8=s8, t=t: e.dma_start(out=out_d[t * 128:(t + 1) * 128, :], in_=yacc[:, s8, :]),
                     reads=[("yacc", s8)], dma=True)
    P.emit("p6")
    return nc, dbg_outs


def _consts():
    cfm = np.zeros((128, 512), np.float32)
    cfm[:, 0:128] = np.eye(128, dtype=np.float32)
    cfm[:, 128:256] = np.triu(np.ones((128, 128), np.float32))
    cfm[:, 256:384] = 1.0
    p = np.arange(128)
    j = p % 16
    freq = (10000.0 ** (-(j.astype(np.float32)) / np.float32(16.0))).astype(np.float32)
    sgn = np.where((p % 32) < 16, -1.0, 1.0).astype(np.float32)
    cfm[:, 384] = sgn * freq
    cfm[:, 385] = freq
    cfm[:, 386] = sgn
    cbm = np.zeros((128, 1024), np.float32)
    cbm[:, 0:128] = np.eye(128)
    k = np.arange(128)[:, None]
    q = np.arange(128)[None, :]
    cbm[:, 128:256] = np.where(k > q, NEG, 0.0)
    cbm[:, 256:384] = np.where((k // 64) > (q // 64), NEG, 0.0)
    for h in range(8):
        cbm[h, 384 + h * 65 + 64] = 1.0
    return cfm, cbm.astype(ml_dtypes.bfloat16)


def _prep_inputs(inp):
    f = lambda a: np.ascontiguousarray(np.asarray(a, dtype=np.float32))
    w_in = f(inp["w_in"])
    kr = w_in[:, 384:416]
    swap = np.concatenate([np.arange(16, 32), np.arange(0, 16)])
    w_kr2 = np.concatenate([kr, kr, kr, kr, kr, kr[:, swap]], axis=1)
    w_uq = f(inp["w_uq"])
    idx = np.arange(768).reshape(8, 96).copy()
    idx[:, 64:96] = idx[:, 64:96][:, swap]
    w_uq_sw = w_uq[:, idx.reshape(-1)]
    cfm, cbm = _consts()
    shared = {
        "g_mix": f(inp["g_mix"]).reshape(1, -1),
        "g_ffn": f(inp["g_ffn"]).reshape(1, -1),
        "g_final": f(inp["g_final"]).reshape(1, -1),
        "g_cq": f(inp["g_cq"]).reshape(1, -1),
        "g_ckv": f(inp["g_ckv"]).reshape(1, -1),
        "b_forget": f(inp["b_forget"]).reshape(1, -1),
        "b_r36": np.concatenate([f(inp["b_group"]), f(inp["b_router"])]).reshape(1, -1),
        "w_lat": np.ascontiguousarray(w_in[:, 0:384]),
        "w_kr2": np.ascontiguousarray(w_kr2),
        "w_fq": np.ascontiguousarray(w_in[:, 416:928]),
        "w_fk": np.ascontiguousarray(w_in[:, 928:1440]),
        "w_fv": np.ascontiguousarray(w_in[:, 1440:1952]),
        "w_f": np.ascontiguousarray(w_in[:, 1952:1960]),
        "w_ga": np.ascontiguousarray(w_in[:, 1960:2984]),
        "w_gb": np.ascontiguousarray(w_in[:, 2984:4008]),
        "w_uq": w_uq,
        "w_uq_sw": np.ascontiguousarray(w_uq_sw),
        "w_uk": f(inp["w_uk"]),
        "w_uv": f(inp["w_uv"]),
        "w_o_mla": f(inp["w_o_mla"]),
        "w_o_fox": f(inp["w_o_fox"]),
        "w_out": f(inp["w_out"]),
        "w_r36": np.ascontiguousarray(np.concatenate([f(inp["w_group"]), f(inp["w_router"])], axis=1)),
        "w_e_gate": f(inp["w_e_gate"]).reshape(-1, 2048),
        "w_e_up": f(inp["w_e_up"]).reshape(-1, 2048),
        "w_e_down": f(inp["w_e_down"]).reshape(-1, 2048),
        "consts_f": cfm,
        "consts_b": cbm,
    }
    x = f(inp["x"])
    pos = np.ascontiguousarray(np.asarray(inp["positions"], dtype=np.int32))
    in_maps = []
    for b in range(8):
        m = dict(shared)
        m["x"] = x[b]
        m["pos"] = pos[b].reshape(1, -1)
        in_maps.append(m)
    return in_maps


def kernel(**inputs):
    in_maps = _prep_inputs(inputs)
    nc, _ = build_program()
    res = run_bass_kernel_spmd(nc, in_maps, core_ids=list(range(8)))
    out = np.stack([np.asarray(r["out"], dtype=np.float32) for r in res.results], axis=0)
    return out
```

```python
import contextlib
import numpy as np
import ml_dtypes
import concourse.bass as bass
import concourse.mybir as mybir
from concourse.bass_utils import run_bass_kernel_spmd

F32 = mybir.dt.float32
BF16 = mybir.dt.bfloat16
I32 = mybir.dt.int32
ALU = mybir.AluOpType
AF = mybir.ActivationFunctionType
AX = mybir.AxisListType

PE, ACT, DVE, POOL, SP = "tensor", "scalar", "vector", "gpsimd", "sync"
ENGINES = [PE, ACT, DVE, POOL, SP]

S = 4096
D = 1024
NT = 32
NQ = 8
NH = 8
NE = 32
EFF = 256
EPS = 1e-6
MLA_SCALE = 96.0 ** -0.5
FOX_SCALE = 0.125
NEG = -30000.0
TWO_PI_HI = 6.28125
TWO_PI_LO = 6.283185307179586 - 6.28125


class Op:
    __slots__ = ("eng", "fn", "deps", "is_dma", "marked", "count", "sem", "semval", "prewait", "nosem")

    def __init__(self, eng, fn, is_dma):
        self.eng = eng
        self.fn = fn
        self.deps = []
        self.is_dma = is_dma
        self.marked = False
        self.count = 0
        self.sem = None
        self.semval = 0
        self.prewait = None
        self.nosem = False


class Prog:
    def __init__(self, nc, dma_pool=8):
        self.nc = nc
        self.ops = {e: [] for e in ENGINES}
        self.last_write = {}
        self.readers = {}
        self.dma_pool = dma_pool
        self.dma_count = {e: 0 for e in ENGINES}

    def op(self, eng, fn, reads=(), writes=(), dma=False, nosem=False):
        o = Op(eng, fn, dma)
        o.nosem = nosem
        deps = {}
        for r in reads:
            w = self.last_write.get(r)
            if w is not None:
                deps[id(w)] = (w, "raw")
        for w_ in writes:
            for rd in self.readers.get(w_, ()):
                if id(rd) not in deps:
                    deps[id(rd)] = (rd, "war")
            w = self.last_write.get(w_)
            if w is not None:
                deps[id(w)] = (w, "waw")
        for d, kind in deps.values():
            if d is o:
                continue
            if (not d.is_dma) and (not dma) and d.eng == eng:
                if eng == PE or kind == "war":
                    continue
            o.deps.append(d)
            d.marked = True
        for r in reads:
            self.readers.setdefault(r, []).append(o)
        for w_ in writes:
            self.last_write[w_] = o
            self.readers[w_] = []
        if dma and not nosem:
            k = self.dma_count[eng]
            self.dma_count[eng] = k + 1
            o.sem = (eng, k % self.dma_pool)
            o.semval = 16 * (k // self.dma_pool + 1)
            if k >= self.dma_pool:
                o.prewait = (o.sem, o.semval - 16)
        self.ops[eng].append(o)
        return o

    def emit(self, name):
        nc = self.nc
        for e in ENGINES:
            c = 0
            for o in self.ops[e]:
                if not o.is_dma and o.marked:
                    c += 1
                    o.count = c
        with contextlib.ExitStack() as st:
            esem = {e: st.enter_context(nc.semaphore("s_%s_%s" % (name, e))) for e in ENGINES}
            dsem = {}
            for e in ENGINES:
                for i in range(min(self.dma_pool, self.dma_count[e])):
                    dsem[(e, i)] = st.enter_context(nc.semaphore("d_%s_%s_%d" % (name, e, i)))
            allsems = list(esem.values()) + list(dsem.values())
            with nc.Block() as cblk:
                def _clr(engobj):
                    for s_ in allsems:
                        engobj.sem_clear(s_)
                cblk.sync(_clr)
            block = st.enter_context(nc.Block())

            def run_engine(e, engobj):
                seen = {}
                for o in self.ops[e]:
                    need = {}
                    for d in o.deps:
                        if d.is_dma:
                            key = ("d", d.sem)
                            val = d.semval
                        else:
                            key = ("e", d.eng)
                            val = d.count
                        if need.get(key, 0) < val:
                            need[key] = val
                    if o.prewait is not None:
                        key = ("d", o.prewait[0])
                        if need.get(key, 0) < o.prewait[1]:
                            need[key] = o.prewait[1]
                    for key, val in need.items():
                        if seen.get(key, 0) >= val:
                            continue
                        seen[key] = val
                        s = dsem[key[1]] if key[0] == "d" else esem[key[1]]
                        engobj.wait_ge(s, val)
                    ins = o.fn(engobj)
                    if o.nosem:
                        continue
                    if o.is_dma:
                        ins.then_inc(dsem[o.sem], 16)
                    elif o.marked:
                        ins.then_inc(esem[e], 1)
                k = self.dma_count[e]
                for i in range(min(self.dma_pool, k)):
                    uses = (k - 1 - i) // self.dma_pool + 1
                    if uses > 0:
                        engobj.wait_ge(dsem[(e, i)], 16 * uses)

            for e in ENGINES:
                if not self.ops[e]:
                    continue
                getattr(block, e)(lambda engobj, e=e: run_engine(e, engobj))


def build_program(stop_after=99, debug=None):
    nc = bass.Bass("TRN2", target_bir_lowering=False)
    debug = debug or []

    def din(name, shape, dt=F32):
        return nc.dram_tensor(name, list(shape), dt, kind="ExternalInput").ap()

    x_d = din("x", [S, D])
    pos_d = din("pos", [1, S], I32)
    g_mix_d = din("g_mix", [1, D])
    g_ffn_d = din("g_ffn", [1, D])
    g_fin_d = din("g_final", [1, D])
    g_cq_d = din("g_cq", [1, 256])
    g_ckv_d = din("g_ckv", [1, 128])
    bf_d = din("b_forget", [1, 8])
    br_d = din("b_r36", [1, 36])
    w_lat_d = din("w_lat", [D, 384])
    w_kr_d = din("w_kr2", [D, 192])
    w_fq_d = din("w_fq", [D, 512])
    w_fk_d = din("w_fk", [D, 512])
    w_fv_d = din("w_fv", [D, 512])
    w_f_d = din("w_f", [D, 8])
    w_ga_d = din("w_ga", [D, D])
    w_gb_d = din("w_gb", [D, D])
    w_uq_d = din("w_uq", [256, 768])
    w_uqs_d = din("w_uq_sw", [256, 768])
    w_uk_d = din("w_uk", [128, 512])
    w_uv_d = din("w_uv", [128, 512])
    w_oa_d = din("w_o_mla", [512, D])
    w_ob_d = din("w_o_fox", [512, D])
    w_out_d = din("w_out", [D, D])
    w_r_d = din("w_r36", [D, 36])
    w_eg_d = din("w_e_gate", [NE * D * EFF // 2048, 2048])
    w_eu_d = din("w_e_up", [NE * D * EFF // 2048, 2048])
    w_ed_d = din("w_e_down", [NE * EFF * D // 2048, 2048])
    cf_d = din("consts_f", [128, 512])
    cb_d = din("consts_b", [128, 1024], BF16)
    out_d = nc.dram_tensor("out", [S, D], F32, kind="ExternalOutput").ap()

    ga_s = nc.dram_tensor("ga_s", [D, S], BF16).ap()
    gb_s = nc.dram_tensor("gb_s", [D, S], BF16).ap()
    oa_s = nc.dram_tensor("oa_s", [512, S], BF16).ap()
    ob_s = nc.dram_tensor("ob_s", [512, S], BF16).ap()
    h_s = nc.dram_tensor("h_s", [S, D], F32).ap()

    dbg_outs = {}

    cf = nc.alloc_sbuf_tensor("cf", [128, 512], F32)
    cb = nc.alloc_sbuf_tensor("cb", [128, 1024], BF16)
    actT = nc.alloc_sbuf_tensor("actT", [128, 8, S], BF16)
    comb = nc.alloc_sbuf_tensor("comb", [128, NT, NE], F32)
    ps = nc.alloc_psum_tensor("ps", [128, 8, 512], F32)

    identf = cf[:, 0:128]
    utri = cf[:, 128:256]
    onesf = cf[:, 256:384]
    freq_col = cf[:, 384:385]
    freq_abs = cf[:, 385:386]
    identb = cb[:, 0:128]
    mask_fox = cb[:, 128:256]
    mask_mla = cb[:, 256:384]
    esel = cb[0:8, 384:384 + 8 * 65]


    def psb(b):
        return ps[:, b, :]

    def psb16(b):
        return ps[:, b, :].bitcast(BF16)

    def add_dbg(P, name, ap, shape, dt, reads):
        t = nc.dram_tensor("dbg_" + name, list(shape), dt, kind="ExternalOutput").ap()
        dbg_outs[name] = t
        P.op(SP, lambda e: e.dma_start(out=t, in_=ap), reads=reads, dma=True)

    def wload(P, dst, src_ap, res, eng=POOL):
        P.op(eng, lambda e: e.dma_start(out=dst, in_=src_ap), writes=[res], dma=True)

    def kc_view(w_ap):
        return w_ap.rearrange("(kc p) n -> p kc n", p=128)

    P = Prog(nc)
    wload(P, cf[:], cf_d, "cf", eng=SP)
    wload(P, cb[:], cb_d, "cb", eng=SP)
    with contextlib.ExitStack() as st:
        gmix = st.enter_context(nc.sbuf_tensor("gmix", [128, D], F32))
        xt = [st.enter_context(nc.sbuf_tensor("xt%d" % i, [128, D], F32)) for i in range(3)]
        xs = [st.enter_context(nc.sbuf_tensor("xs%d" % i, [128, D], BF16)) for i in range(2)]
        sq = st.enter_context(nc.sbuf_tensor("sq", [128, D], BF16))
        stat = st.enter_context(nc.sbuf_tensor("stat", [128, NT, 2], F32))
        P.op(SP, lambda e: e.dma_start(out=gmix[:], in_=g_mix_d[0].partition_broadcast(128)), writes=["gmix"], dma=True)
        P.op(DVE, lambda e: e.memset(stat[:], 0.0), writes=["stat"])

        def p1_copy(t):
            bk = t % 2
            P.op(ACT, lambda e: e.copy(
                actT[:, :, t * 128:(t + 1) * 128], psb16(bk).rearrange("p (a b) -> p a b", a=8)),
                reads=[("ps", bk)], writes=[("actT", t)])

        for t in range(NT):
            xb = xt[t % 3]
            xsb = xs[t % 2]
            bk = t % 2
            P.op(SP, lambda e, xb=xb, t=t: e.dma_start(out=xb[:], in_=x_d[t * 128:(t + 1) * 128, :]),
                 writes=[("xt", t % 3)], dma=True)
            P.op(ACT, lambda e, xb=xb, t=t: e.activation(sq[:], xb[:], AF.Square, accum_out=stat[:, t, 0:1]),
                 reads=[("xt", t % 3), "stat"], writes=["sq", ("stat", t)])
            P.op(ACT, lambda e, t=t: e.activation(stat[:, t, 1:2], stat[:, t, 0:1], AF.Sqrt, bias=EPS, scale=1.0 / D),
                 reads=[("stat", t)], writes=[("stat1", t)])
            if t > 0:
                p1_copy(t - 1)
            P.op(DVE, lambda e, t=t: e.reciprocal(stat[:, t, 1:2], stat[:, t, 1:2]),
                 reads=[("stat1", t)], writes=[("stat1", t)])
            P.op(DVE, lambda e, xb=xb, xsb=xsb, t=t: e.scalar_tensor_tensor(
                xsb[:], xb[:], stat[:, t, 1:2], gmix[:], ALU.mult, ALU.mult),
                reads=[("xt", t % 3), ("stat1", t), "gmix"], writes=[("xs", t % 2)])
            for kc in range(8):
                P.op(PE, lambda e, xsb=xsb, kc=kc, bk=bk: e.transpose(
                    psb16(bk)[:, kc * 128:(kc + 1) * 128], xsb[:, kc * 128:(kc + 1) * 128], identb),
                    reads=[("xs", t % 2), "cb"], writes=[("ps", bk)])
        p1_copy(NT - 1)
        if "xnT" in debug:
            add_dbg(P, "xnT", actT[:, :, 0:512], [128, 8, 512], BF16, [("actT", t) for t in range(4)])
    P.emit("p1")
    if stop_after <= 1:
        return nc, dbg_outs

    st_f = contextlib.ExitStack()
    Gc = st_f.enter_context(nc.sbuf_tensor("Gc", [128, NT, 8], F32))
    FsT = st_f.enter_context(nc.sbuf_tensor("FsT", [8, S], BF16))
    P = Prog(nc)
    with contextlib.ExitStack() as st:
        wf = st.enter_context(nc.sbuf_tensor("wf", [128, 8, 8], BF16))
        wga = st.enter_context(nc.sbuf_tensor("wga", [128, 8, D], BF16))
        wgb = st.enter_context(nc.sbuf_tensor("wgb", [128, 8, D], BF16))
        bfb = st.enter_context(nc.sbuf_tensor("bfb", [128, 8], F32))
        lf = st.enter_context(nc.sbuf_tensor("lf", [128, NT, 8], F32))
        tot = st.enter_context(nc.sbuf_tensor("tot", [128, NT, 8], F32))
        off = st.enter_context(nc.sbuf_tensor("off", [128, NT, 8], F32))
        gst = [st.enter_context(nc.sbuf_tensor("gst%d" % i, [128, 512], BF16)) for i in range(4)]
        wload(P, wf[:], kc_view(w_f_d), "wf")
        wload(P, wga[:], kc_view(w_ga_d), "wga")
        wload(P, wgb[:], kc_view(w_gb_d), "wgb")
        P.op(SP, lambda e: e.dma_start(out=bfb[:], in_=bf_d[0].partition_broadcast(128)), writes=["bfb"], dma=True)
        for t in range(NT):
            for kc in range(8):
                P.op(PE, lambda e, t=t, kc=kc: e.matmul(
                    psb(0)[:, t * 8:(t + 1) * 8], actT[:, kc, t * 128:(t + 1) * 128], wf[:, kc, :],
                    start=(kc == 0), stop=(kc == 7)),
                    reads=[("actT", t), "wf"], writes=[("ps", 0)])
        P.op(DVE, lambda e: e.tensor_tensor(
            lf[:], psb(0)[:, 0:256].rearrange("p (t h) -> p t h", h=8),
            bfb[:].unsqueeze(1).broadcast_to([128, NT, 8]), ALU.add),
            reads=[("ps", 0), "bfb"], writes=["lf"])
        P.op(ACT, lambda e: e.activation(lf[:], lf[:], AF.Exp, scale=-1.0), reads=["lf"], writes=["lf"])
        P.op(ACT, lambda e: e.activation(lf[:], lf[:], AF.Ln, bias=1.0), reads=["lf"], writes=["lf"])
        lf2 = lf[:].rearrange("p t h -> p (t h)")
        P.op(PE, lambda e: e.matmul(psb(1)[:, 0:256], utri, lf2, start=True, stop=True),
             reads=["lf", "cf"], writes=[("ps", 1)])
        P.op(PE, lambda e: e.matmul(psb(2)[:, 0:256], onesf, lf2, start=True, stop=True),
             reads=["lf", "cf"], writes=[("ps", 2)])
        P.op(DVE, lambda e: e.tensor_copy(tot[:], psb(2)[:, 0:256].rearrange("p (t h) -> p t h", h=8)),
             reads=[("ps", 2)], writes=["tot"])
        P.op(DVE, lambda e: e.memset(off[:, 0, :], 0.0), writes=["off"])
        for t in range(1, NT):
            P.op(DVE, lambda e, t=t: e.tensor_tensor(off[:, t, :], off[:, t - 1, :], tot[:, t - 1, :], ALU.add),
                 reads=["off", "tot"], writes=["off"])
        P.op(DVE, lambda e: e.tensor_tensor(
            Gc[:], psb(1)[:, 0:256].rearrange("p (t h) -> p t h", h=8), off[:], ALU.add),
            reads=[("ps", 1), "off"], writes=["Gc"])
        for g4 in range(8):
            bk = 3 + (g4 % 2)
            for i in range(4):
                t = g4 * 4 + i
                P.op(PE, lambda e, t=t, i=i, bk=bk: e.transpose(
                    psb(bk)[0:8, i * 128:(i + 1) * 128], Gc[:, t, :], identf),
                    reads=["Gc", "cf"], writes=[("ps", bk)])
            P.op(ACT, lambda e, g4=g4, bk=bk: e.mul(FsT[:, g4 * 512:(g4 + 1) * 512], psb(bk)[0:8, :], -0.5 / FOX_SCALE),
                 reads=[("ps", bk)], writes=["FsT"])
        n = 0
        for (wg_, dst) in ((wga, ga_s), (wgb, gb_s)):
            wname = "wga" if wg_ is wga else "wgb"
            for dc in range(8):
                for q in range(NQ):
                    bk = 5 + (n % 3)
                    sb = gst[n % 4]
                    for kc in range(8):
                        P.op(PE, lambda e, wg_=wg_, dc=dc, q=q, kc=kc, bk=bk: e.matmul(
                            psb(bk), wg_[:, kc, dc * 128:(dc + 1) * 128], actT[:, kc, q * 512:(q + 1) * 512],
                            start=(kc == 0), stop=(kc == 7)),
                            reads=[wname] + [("actT", q * 4 + i) for i in range(4)], writes=[("ps", bk)])
                    P.op(ACT, lambda e, sb=sb, bk=bk: e.activation(sb[:], psb(bk), AF.Sigmoid),
                         reads=[("ps", bk)], writes=[("gst", n % 4)])
                    P.op(SP, lambda e, sb=sb, dst=dst, dc=dc, q=q: e.dma_start(
                        out=dst[dc * 128:(dc + 1) * 128, q * 512:(q + 1) * 512], in_=sb[:]),
                        reads=[("gst", n % 4)], dma=True)
                    n += 1
        if "Gc" in debug:
            add_dbg(P, "Gc", Gc[:], [128, NT, 8], F32, ["Gc"])
            add_dbg(P, "FsT", FsT[:], [8, S], BF16, ["FsT"])
    P.emit("p2a")
    if stop_after <= 2:
        return nc, dbg_outs

    def attention_head(P, h, kdim, qT, kT, vx, scale, bias_fn, mask, o_dst, bufs, hname, interleave=()):
        pt, rrow, rbc, ost = bufs
        pairs = [(I, J) for I in range(NQ) for J in range(4 * I + 4)]
        npairs = len(pairs)

        def emit_qk(n):
            I, J = pairs[n]
            j = J - 4 * I
            c0 = max(0, j) * 128
            sbk = n % 3
            P.op(PE, lambda e: e.matmul(
                psb(sbk)[:, c0:512], kT[0:kdim, J * 128:(J + 1) * 128], qT[0:kdim, I * 512 + c0:(I + 1) * 512],
                start=True, stop=(j < 0)),
                reads=[hname + "q", hname + "k"], writes=[("ps", sbk)])
            if j >= 0:
                P.op(PE, lambda e: e.matmul(
                    psb(sbk)[:, c0:c0 + 128], identb, mask, start=False, stop=True),
                    reads=["cb"], writes=[("ps", sbk)])

        def emit_exp(n):
            I, J = pairs[n]
            j = J - 4 * I
            c0 = max(0, j) * 128
            sbk = n % 3
            ptb = pt[n % 3]
            bias = bias_fn(J)
            P.op(ACT, lambda e: e.activation(
                ptb[:, c0:512], psb(sbk)[:, c0:512], AF.Exp, bias=bias, scale=scale),
                reads=[("ps", sbk), "Gc"], writes=[("pt", n % 3)])

        def emit_pv(n):
            I, J = pairs[n]
            j = J - 4 * I
            c0 = max(0, j) * 128
            ob = 3 + (I % 2)
            nJ = 4 * I + 4
            ptb = pt[n % 3]
            P.op(PE, lambda e: e.matmul(
                psb(ob)[0:65, c0:512], vx[:, J, :], ptb[:, c0:512], start=(J == 0), stop=(J == nJ - 1)),
                reads=[("pt", n % 3), hname + "v"], writes=[("ps", ob)])

        def finalize(I):
            ob = 3 + (I % 2)
            P.op(DVE, lambda e: e.reciprocal(rrow[64:65, :], psb(ob)[64:65, :]),
                 reads=[("ps", ob)], writes=["rrow"])
            P.op(PE, lambda e: e.matmul(psb(5)[0:64, :], onesf[64:65, 0:64], rrow[64:65, :], start=True, stop=True),
                 reads=["rrow", "cf"], writes=[("ps", 5)])
            P.op(ACT, lambda e: e.copy(rbc[0:64, :], psb(5)[0:64, :]), reads=[("ps", 5)], writes=["rbc"])
            osb = ost[I % 2]
            P.op(DVE, lambda e: e.tensor_tensor(osb[0:64, :], psb(ob)[0:64, :], rbc[0:64, :], ALU.mult),
                 reads=[("ps", ob), "rbc"], writes=[("ost", I % 2)])
            P.op(SP, lambda e: e.dma_start(
                out=o_dst[h * 64:(h + 1) * 64, I * 512:(I + 1) * 512], in_=osb[0:64, :]),
                reads=[("ost", I % 2)], writes=["odram"], dma=True)

        pending = []
        chunks = list(interleave)
        evq = []
        step = max(1, (npairs - 8) // (len(chunks) + 1)) if chunks else 0
        emit_qk(0)
        emit_exp(0)
        emit_qk(1)
        emit_exp(1)
        for n in range(npairs):
            if n + 2 < npairs:
                emit_qk(n + 2)
                emit_exp(n + 2)
            emit_pv(n)
            I, J = pairs[n]
            if J == 4 * I + 3:
                pending.append((n + 3, I))
            while pending and pending[0][0] <= n:
                finalize(pending.pop(0)[1])
            while evq and evq[0][0] <= n:
                evq.pop(0)[1]()
            if chunks and n % step == step - 1:
                ev = chunks.pop(0)()
                if ev:
                    evq.append((n + 2, ev))
        for _, I in pending:
            finalize(I)
        for _, ev in evq:
            ev()
        for c in chunks:
            ev = c()
            if ev:
                ev()

    P = Prog(nc)
    with contextlib.ExitStack() as st:
        wfq = st.enter_context(nc.sbuf_tensor("wfq", [128, 8, 512], BF16))
        wfk = st.enter_context(nc.sbuf_tensor("wfk", [128, 8, 512], BF16))
        wfv = st.enter_context(nc.sbuf_tensor("wfv", [128, 8, 512], BF16))
        qTs = [st.enter_context(nc.sbuf_tensor("fqT%d" % i, [65, S], BF16)) for i in range(2)]
        kTs = [st.enter_context(nc.sbuf_tensor("fkT%d" % i, [65, S], BF16)) for i in range(2)]
        vall = st.enter_context(nc.sbuf_tensor("fvall", [128, NT, 8, 65], BF16))
        pt = [st.enter_context(nc.sbuf_tensor("pt%d" % i, [128, 512], BF16)) for i in range(3)]
        rrow = st.enter_context(nc.sbuf_tensor("rrow", [65, 512], F32))
        rbc = st.enter_context(nc.sbuf_tensor("rbc", [64, 512], F32))
        ost = [st.enter_context(nc.sbuf_tensor("ost%d" % i, [64, 512], BF16)) for i in range(2)]
        wload(P, wfq[:], kc_view(w_fq_d), "wfq")
        wload(P, wfk[:], kc_view(w_fk_d), "wfk")
        wload(P, wfv[:], kc_view(w_fv_d), "wfv")
        for i in range(2):
            P.op(POOL, lambda e, i=i: e.memset(kTs[i][64:65, :], 1.0), writes=[("fk", i)])
        P.op(POOL, lambda e: e.memset(vall[:, :, :, 64:65], 1.0), writes=["fvones"])
        allact = [("actT", t) for t in range(NT)]
        for t in range(NT):
            bk = 6 + (t % 2)
            for kc in range(8):
                P.op(PE, lambda e, t=t, kc=kc, bk=bk: e.matmul(
                    psb(bk), actT[:, kc, t * 128:(t + 1) * 128], wfv[:, kc, :], start=(kc == 0), stop=(kc == 7)),
                    reads=["wfv", ("actT", t)], writes=[("ps", bk)])
            if t % 2 == 0:
                P.op(DVE, lambda e, t=t, bk=bk: e.tensor_copy(vall[:, t, :, 0:64], psb(bk).rearrange("p (h d) -> p h d", d=64)),
                     reads=[("ps", bk), "fvones"], writes=["f0v", "f1v"])
            else:
                P.op(ACT, lambda e, t=t, bk=bk: e.copy(vall[:, t, :, 0:64], psb(bk).rearrange("p (h d) -> p h d", d=64)),
                     reads=[("ps", bk), "fvones"], writes=["f0v", "f1v"])
        def fox_chunks(h):
            b = h % 2
            qT, kT = qTs[b], kTs[b]
            hn = "f%d" % b
            out = []

            def cq(q):
                bk = 6 + (q % 2)
                P.op(PE, lambda e: e.matmul(
                    psb(bk)[0:65, :], esel[:, h * 65:(h + 1) * 65], FsT[:, q * 512:(q + 1) * 512], start=True, stop=False),
                    reads=["FsT", "cb"], writes=[("ps", bk)])
                for kc in range(8):
                    P.op(PE, lambda e, kc=kc: e.matmul(
                        psb(bk)[0:64, :], wfq[:, kc, h * 64:(h + 1) * 64], actT[:, kc, q * 512:(q + 1) * 512],
                        start=False, stop=False),
                        reads=["wfq"] + allact[q * 4:q * 4 + 4], writes=[("ps", bk)])
                P.op(PE, lambda e: e.matmul(
                    psb(bk)[0:65, :], esel[:, h * 65:(h + 1) * 65], FsT[:, q * 512:(q + 1) * 512], start=False, stop=True),
                    reads=["FsT", "cb"], writes=[("ps", bk)])
                return lambda: P.op(ACT, lambda e: e.copy(qT[0:65, q * 512:(q + 1) * 512], psb(bk)[0:65, :]),
                                    reads=[("ps", bk)], writes=[hn + "q"])

            def ck(q):
                bk = 6 + (q % 2)
                for kc in range(8):
                    P.op(PE, lambda e, kc=kc: e.matmul(
                        psb(bk)[0:64, :], wfk[:, kc, h * 64:(h + 1) * 64], actT[:, kc, q * 512:(q + 1) * 512],
                        start=(kc == 0), stop=(kc == 7)),
                        reads=["wfk"] + allact[q * 4:q * 4 + 4], writes=[("ps", bk)])
                return lambda: P.op(DVE, lambda e: e.tensor_copy(kT[0:64, q * 512:(q + 1) * 512], psb(bk)[0:64, :]),
                                    reads=[("ps", bk), ("fk", b)], writes=[hn + "k"])

            for q in range(NQ):
                out.append(lambda q=q: cq(q))
            for q in range(NQ):
                out.append(lambda q=q: ck(q))
            return out

        for c in fox_chunks(0):
            c()()
        for h in range(NH):
            b = h % 2
            nxt = fox_chunks(h + 1) if h + 1 < NH else []
            attention_head(P, h, 65, qTs[b], kTs[b], vall[:, :, h, :], FOX_SCALE, lambda J, h=h: Gc[:, J, h:h + 1], mask_fox,
                           ob_s, (pt, rrow, rbc, ost), "f%d" % b, interleave=nxt)
        if "ob" in debug:
            add_dbg(P, "ob", ob_s, [512, S], BF16, ["odram"])
    P.emit("p3")
    st_f.close()
    if stop_after <= 3:
        return nc, dbg_outs

    st_a = contextlib.ExitStack()
    cqnT = st_a.enter_context(nc.sbuf_tensor("cqnT", [128, 3, S], BF16))
    krT = st_a.enter_context(nc.sbuf_tensor("krT", [96, S], BF16))
    cosT = st_a.enter_context(nc.sbuf_tensor("cosT", [96, S], BF16))
    sinT = st_a.enter_context(nc.sbuf_tensor("sinT", [96, S], BF16))
    R = slice(64, 96)
    allc = [("cqnT", t) for t in range(NT)]
    P = Prog(nc)
    with contextlib.ExitStack() as st:
        wlat = st.enter_context(nc.sbuf_tensor("wlat", [128, 8, 384], BF16))
        wkr = st.enter_context(nc.sbuf_tensor("wkr", [128, 8, 192], BF16))
        gcq = st.enter_context(nc.sbuf_tensor("gcq", [128, 384], F32))
        lat = [st.enter_context(nc.sbuf_tensor("lat%d" % i, [128, 384], BF16)) for i in range(2)]
        lsq = st.enter_context(nc.sbuf_tensor("lsq", [128, 384], BF16))
        lst = st.enter_context(nc.sbuf_tensor("lst", [128, NT, 4], F32))
        posi = st.enter_context(nc.sbuf_tensor("posi", [96, 512], I32))
        ang = st.enter_context(nc.sbuf_tensor("ang", [96, 512], F32))
        uu = st.enter_context(nc.sbuf_tensor("uu", [96, 512], F32))
        ki = st.enter_context(nc.sbuf_tensor("ki", [96, 512], I32))
        kf = st.enter_context(nc.sbuf_tensor("kf", [96, 512], F32))
        rr = st.enter_context(nc.sbuf_tensor("rr", [96, 512], F32))
        hs_ = st.enter_context(nc.sbuf_tensor("hs_", [96, 512], F32))
        t1 = st.enter_context(nc.sbuf_tensor("t1", [96, 512], F32))
        t2 = st.enter_context(nc.sbuf_tensor("t2", [96, 512], F32))
        wload(P, wlat[:], kc_view(w_lat_d), "wlat")
        wload(P, wkr[:], kc_view(w_kr_d), "wkr")
        P.op(SP, lambda e: e.dma_start(out=gcq[:, 0:256], in_=g_cq_d[0].partition_broadcast(128)), writes=["gcq"], dma=True)
        P.op(SP, lambda e: e.dma_start(out=gcq[:, 256:384], in_=g_ckv_d[0].partition_broadcast(128)), writes=["gcq"], dma=True)
        for q in range(NQ):
            tk = slice(q * 512, (q + 1) * 512)
            P.op(SP, lambda e, tk=tk: e.dma_start(out=posi[R, :], in_=pos_d[0, tk].partition_broadcast(32)),
                 writes=["posi"], dma=True)
            P.op(DVE, lambda e: e.tensor_copy(ang[R, :], posi[R, :]), reads=["posi"], writes=["ang"])
            P.op(DVE, lambda e: e.tensor_scalar(ang[R, :], ang[R, :], freq_abs[R, :], None, ALU.mult),
                 reads=["ang", "cf"], writes=["ang"])
            P.op(DVE, lambda e: e.tensor_scalar(uu[R, :], ang[R, :], 1.0 / (2 * np.pi), None, ALU.mult),
                 reads=["ang"], writes=["uu"])
            P.op(DVE, lambda e: e.tensor_copy(ki[R, :], uu[R, :]), reads=["uu"], writes=["ki"])
            P.op(DVE, lambda e: e.tensor_copy(kf[R, :], ki[R, :]), reads=["ki"], writes=["kf"])
            P.op(DVE, lambda e: e.scalar_tensor_tensor(rr[R, :], kf[R, :], -TWO_PI_HI, ang[R, :], ALU.mult, ALU.add),
                 reads=["kf", "ang"], writes=["rr"])
            P.op(DVE, lambda e: e.scalar_tensor_tensor(rr[R, :], kf[R, :], -TWO_PI_LO, rr[R, :], ALU.mult, ALU.add),
                 reads=["kf", "rr"], writes=["rr"])
            P.op(DVE, lambda e: e.tensor_scalar(rr[R, :], rr[R, :], 3.1415925, -3.1415925, ALU.min, ALU.max),
                 reads=["rr"], writes=["rr"])
            P.op(ACT, lambda e, tk=tk: e.activation(sinT[R, tk], rr[R, :], AF.Sin, scale=cf[R, 386:387]),
                 reads=["rr", "cf"], writes=["sinT"])
            P.op(ACT, lambda e: e.activation(hs_[R, :], rr[R, :], AF.Sin, scale=0.5), reads=["rr"], writes=["hs_"])
            P.op(DVE, lambda e: e.tensor_tensor(hs_[R, :], hs_[R, :], hs_[R, :], ALU.mult), reads=["hs_"], writes=["hs_"])
            P.op(DVE, lambda e, tk=tk: e.tensor_scalar(cosT[R, tk], hs_[R, :], -2.0, 1.0, ALU.mult, ALU.add),
                 reads=["hs_"], writes=["cosT"])
        P.op(DVE, lambda e: e.memset(lst[:], 0.0), writes=["lst"])
        for t in range(NT):
            bk = 6 + (t % 2)
            lb = lat[t % 2]
            for kc in range(8):
                P.op(PE, lambda e, t=t, kc=kc, bk=bk: e.matmul(
                    psb(bk)[:, 0:384], actT[:, kc, t * 128:(t + 1) * 128], wlat[:, kc, :], start=(kc == 0), stop=(kc == 7)),
                    reads=["wlat", ("actT", t)], writes=[("ps", bk)])
            P.op(ACT, lambda e, t=t, bk=bk: e.activation(lsq[:, 0:256], psb(bk)[:, 0:256], AF.Square, accum_out=lst[:, t, 0:1]),
                 reads=[("ps", bk), "lst"], writes=["lsq", ("lst", t)])
            P.op(ACT, lambda e, t=t, bk=bk: e.activation(lsq[:, 256:384], psb(bk)[:, 256:384], AF.Square, accum_out=lst[:, t, 1:2]),
                 reads=[("ps", bk), ("lst", t)], writes=["lsq", ("lst", t)])
            P.op(ACT, lambda e, t=t: e.activation(lst[:, t, 2:3], lst[:, t, 0:1], AF.Sqrt, bias=EPS, scale=1.0 / 256),
                 reads=[("lst", t)], writes=[("lst2", t)])
            P.op(ACT, lambda e, t=t: e.activation(lst[:, t, 3:4], lst[:, t, 1:2], AF.Sqrt, bias=EPS, scale=1.0 / 128),
                 reads=[("lst", t)], writes=[("lst3", t)])
            P.op(DVE, lambda e, t=t: e.reciprocal(lst[:, t, 2:4], lst[:, t, 2:4]),
                 reads=[("lst2", t), ("lst3", t)], writes=[("lst2", t), ("lst3", t)])
            P.op(DVE, lambda e, t=t, bk=bk, lb=lb: e.scalar_tensor_tensor(
                lb[:, 0:256], psb(bk)[:, 0:256], lst[:, t, 2:3], gcq[:, 0:256], ALU.mult, ALU.mult),
                reads=[("ps", bk), ("lst2", t), "gcq"], writes=[("lat", t % 2)])
            P.op(DVE, lambda e, t=t, bk=bk, lb=lb: e.scalar_tensor_tensor(
                lb[:, 256:384], psb(bk)[:, 256:384], lst[:, t, 3:4], gcq[:, 256:384], ALU.mult, ALU.mult),
                reads=[("ps", bk), ("lst3", t), "gcq"], writes=[("lat", t % 2)])
            tb = 4 + (t % 2)
            for c in range(3):
                P.op(PE, lambda e, lb=lb, c=c, tb=tb: e.transpose(
                    psb16(tb)[:, c * 128:(c + 1) * 128], lb[:, c * 128:(c + 1) * 128], identb),
                    reads=[("lat", t % 2), "cb"], writes=[("ps", tb)])
            P.op(ACT, lambda e, t=t, tb=tb: e.copy(
                cqnT[:, :, t * 128:(t + 1) * 128], psb16(tb)[:, 0:384].rearrange("p (a b) -> p a b", a=3)),
                reads=[("ps", tb)], writes=[("cqnT", t)])
        for q in range(NQ):
            tk = slice(q * 512, (q + 1) * 512)
            for v in range(2):
                for kc in range(8):
                    P.op(PE, lambda e, q=q, v=v, kc=kc: e.matmul(
                        psb(6 + v)[0:96, :], wkr[:, kc, v * 96:(v + 1) * 96], actT[:, kc, q * 512:(q + 1) * 512],
                        start=(kc == 0), stop=(kc == 7)),
                        reads=["wkr"] + [("actT", q * 4 + i) for i in range(4)], writes=[("ps", 6 + v)])
            P.op(DVE, lambda e, tk=tk: e.tensor_tensor(t1[R, :], psb(6)[R, :], cosT[R, tk], ALU.mult),
                 reads=[("ps", 6), "cosT"], writes=["t1"])
            P.op(DVE, lambda e, tk=tk: e.tensor_tensor(t2[R, :], psb(7)[R, :], sinT[R, tk], ALU.mult),
                 reads=[("ps", 7), "sinT"], writes=["t2"])
            P.op(POOL, lambda e, tk=tk: e.tensor_tensor(krT[R, tk], t1[R, :], t2[R, :], ALU.add),
                 reads=["t1", "t2"], writes=["krT"])
        if "lat" in debug:
            add_dbg(P, "cqnT", cqnT[:, :, 0:512], [128, 3, 512], BF16, allc)
            add_dbg(P, "krT", krT[R, 0:512], [32, 512], BF16, ["krT"])
    P.emit("p4a")

    P = Prog(nc)
    with contextlib.ExitStack() as st:
        wuq = st.enter_context(nc.sbuf_tensor("wuq", [128, 2, 768], BF16))
        wuqs = st.enter_context(nc.sbuf_tensor("wuqs", [128, 2, 768], BF16))
        wuk = st.enter_context(nc.sbuf_tensor("wuk", [128, 512], BF16))
        wuv = st.enter_context(nc.sbuf_tensor("wuv", [128, 512], BF16))
        qTs = [st.enter_context(nc.sbuf_tensor("aqT%d" % i, [96, S], BF16)) for i in range(2)]
        kTs = [st.enter_context(nc.sbuf_tensor("akT%d" % i, [96, S], BF16)) for i in range(2)]
        vxs = [st.enter_context(nc.sbuf_tensor("avx%d" % i, [128, NT, 65], BF16)) for i in range(2)]
        pt = [st.enter_context(nc.sbuf_tensor("apt%d" % i, [128, 512], BF16)) for i in range(3)]
        rrow = st.enter_context(nc.sbuf_tensor("arrow", [65, 512], F32))
        rbc = st.enter_context(nc.sbuf_tensor("arbc", [64, 512], F32))
        ost = [st.enter_context(nc.sbuf_tensor("aost%d" % i, [64, 512], BF16)) for i in range(2)]
        t1 = st.enter_context(nc.sbuf_tensor("t1b", [96, 512], F32))
        t2 = st.enter_context(nc.sbuf_tensor("t2b", [96, 512], F32))
        wload(P, wuq[:], kc_view(w_uq_d), "wuq")
        wload(P, wuqs[:], kc_view(w_uqs_d), "wuqs")
        wload(P, wuk[:], w_uk_d, "wuk")
        wload(P, wuv[:], w_uv_d, "wuv")
        for i in range(2):
            P.op(POOL, lambda e, i=i: e.memset(vxs[i][:, :, 64:65], 1.0), writes=[("av", i)])
        def mla_chunks(h):
            b = h % 2
            qT, kT, vx = qTs[b], kTs[b], vxs[b]
            hn = "a%d" % b
            out = []

            def cq(q):
                tk = slice(q * 512, (q + 1) * 512)
                for v, w_ in enumerate((wuq, wuqs)):
                    for kc in range(2):
                        P.op(PE, lambda e, kc=kc, v=v, w_=w_: e.matmul(
                            psb(6 + v)[0:96, :], w_[:, kc, h * 96:(h + 1) * 96], cqnT[:, kc, q * 512:(q + 1) * 512],
                            start=(kc == 0), stop=(kc == 1)),
                            reads=["wuq", "wuqs"] + allc[q * 4:q * 4 + 4], writes=[("ps", 6 + v)])
                def ev():
                    P.op(ACT, lambda e: e.copy(qT[0:64, tk], psb(6)[0:64, :]),
                         reads=[("ps", 6)], writes=[hn + "q"])
                    P.op(DVE, lambda e: e.tensor_tensor(t1[R, :], psb(6)[R, :], cosT[R, tk], ALU.mult),
                         reads=[("ps", 6), "cosT"], writes=["t1"])
                    P.op(DVE, lambda e: e.tensor_tensor(t2[R, :], psb(7)[R, :], sinT[R, tk], ALU.mult),
                         reads=[("ps", 7), "sinT"], writes=["t2"])
                    P.op(POOL, lambda e: e.tensor_tensor(qT[R, tk], t1[R, :], t2[R, :], ALU.add),
                         reads=["t1", "t2"], writes=[hn + "q"])
                return ev

            def ck(q):
                tk = slice(q * 512, (q + 1) * 512)
                bk = 6 + (q % 2)
                P.op(PE, lambda e: e.matmul(
                    psb(bk)[0:64, :], wuk[:, h * 64:(h + 1) * 64], cqnT[:, 2, q * 512:(q + 1) * 512], start=True, stop=True),
                    reads=["wuk"] + allc[q * 4:q * 4 + 4], writes=[("ps", bk)])
                return lambda: P.op(ACT, lambda e: e.copy(kT[0:64, tk], psb(bk)[0:64, :]),
                                    reads=[("ps", bk)], writes=[hn + "k"])

            def ck2(q):
                e0 = ck(q)
                e1 = ck(q + 1)
                return lambda: (e0(), e1())

            def ckr():
                P.op(POOL, lambda e: e.tensor_copy(kT[R, :], krT[R, :]), reads=["krT"], writes=[hn + "k"])
                return None

            def cv(g8):
                bk = 6 + (g8 % 2)
                for i in range(8):
                    t = g8 * 8 + i
                    P.op(PE, lambda e, t=t, i=i: e.matmul(
                        psb(bk)[:, i * 64:(i + 1) * 64], cqnT[:, 2, t * 128:(t + 1) * 128], wuv[:, h * 64:(h + 1) * 64],
                        start=True, stop=True),
                        reads=["wuv", ("cqnT", t)], writes=[("ps", bk)])
                return lambda: P.op(DVE, lambda e: e.tensor_copy(
                    vx[:, g8 * 8:(g8 + 1) * 8, 0:64], psb(bk).rearrange("p (t d) -> p t d", d=64)),
                    reads=[("ps", bk), ("av", b)], writes=[hn + "v"])

            for q in range(NQ):
                out.append(lambda q=q: cq(q))
            for q in range(0, NQ, 2):
                out.append(lambda q=q: ck2(q))
            out.append(ckr)
            for g8 in range(4):
                out.append(lambda g8=g8: cv(g8))
            return out

        for c in mla_chunks(0):
            ev = c()
            if ev:
                ev()
        for h in range(NH):
            b = h % 2
            nxt = mla_chunks(h + 1) if h + 1 < NH else []
            attention_head(P, h, 96, qTs[b], kTs[b], vxs[b], MLA_SCALE, lambda J: 0.0, mask_mla,
                           oa_s, (pt, rrow, rbc, ost), "a%d" % b, interleave=nxt)
        if "oa" in debug:
            add_dbg(P, "oa", oa_s, [512, S], BF16, ["odram"])
    P.emit("p4")
    st_a.close()
    if stop_after <= 4:
        return nc, dbg_outs

    P = Prog(nc)
    with contextlib.ExitStack() as st:
        woa = st.enter_context(nc.sbuf_tensor("woa", [128, 4, D], BF16))
        wob = st.enter_context(nc.sbuf_tensor("wob", [128, 4, D], BF16))
        wout = st.enter_context(nc.sbuf_tensor("wout", [128, 8, D], BF16))
        wr = st.enter_context(nc.sbuf_tensor("wr", [128, 8, 36], BF16))
        gffn = st.enter_context(nc.sbuf_tensor("gffn", [128, D], F32))
        brb = st.enter_context(nc.sbuf_tensor("brb", [128, 36], F32))
        gat = [[st.enter_context(nc.sbuf_tensor("gat%d_%d" % (b_, i), [128, 8, 512], BF16)) for i in range(2)] for b_ in range(2)]
        oin = [[st.enter_context(nc.sbuf_tensor("oin%d_%d" % (b_, i), [128, 4, 512], BF16)) for i in range(2)] for b_ in range(2)]
        mixT = [st.enter_context(nc.sbuf_tensor("mixT%d" % i, [128, 8, 512], BF16)) for i in range(2)]
        m1 = [st.enter_context(nc.sbuf_tensor("m1_0", [128, 512], F32))] * 2
        m2 = [st.enter_context(nc.sbuf_tensor("m2_0", [128, 512], F32))] * 2
        xh = [st.enter_context(nc.sbuf_tensor("xh%d" % i, [128, D], F32)) for i in range(3)]
        hsb = [st.enter_context(nc.sbuf_tensor("hsb%d" % i, [128, D], BF16)) for i in range(2)]
        sq5 = st.enter_context(nc.sbuf_tensor("sq5", [128, D], BF16))
        st5 = st.enter_context(nc.sbuf_tensor("st5", [128, NT, 2], F32))
        Lr = st.enter_context(nc.sbuf_tensor("Lr", [128, NT, 36], F32))
        wload(P, woa[:], w_oa_d.rearrange("(c p) n -> p c n", p=128), "woa")
        wload(P, wob[:], w_ob_d.rearrange("(c p) n -> p c n", p=128), "wob")
        wload(P, wout[:], kc_view(w_out_d), "wout")
        wload(P, wr[:], kc_view(w_r_d), "wr")
        P.op(SP, lambda e: e.dma_start(out=gffn[:], in_=g_ffn_d[0].partition_broadcast(128)), writes=["gffn"], dma=True)
        P.op(SP, lambda e: e.dma_start(out=brb[:], in_=br_d[0].partition_broadcast(128)), writes=["brb"], dma=True)
        P.op(DVE, lambda e: e.memset(st5[:], 0.0), writes=["st5"])
        xi = [0]

        def merge_loads(q):
            tk = slice(q * 512, (q + 1) * 512)
            b_ = q % 2
            P.op(SP, lambda e: e.dma_start(out=gat[b_][0][:], in_=ga_s[:, tk].rearrange("(c p) t -> p c t", p=128)),
                 writes=[("gat", b_, 0)], dma=True)
            P.op(SP, lambda e: e.dma_start(out=oin[b_][0][:], in_=oa_s[:, tk].rearrange("(c p) t -> p c t", p=128)),
                 writes=[("oin", b_, 0)], dma=True)
            P.op(SP, lambda e: e.dma_start(out=gat[b_][1][:], in_=gb_s[:, tk].rearrange("(c p) t -> p c t", p=128)),
                 writes=[("gat", b_, 1)], dma=True)
            P.op(SP, lambda e: e.dma_start(out=oin[b_][1][:], in_=ob_s[:, tk].rearrange("(c p) t -> p c t", p=128)),
                 writes=[("oin", b_, 1)], dma=True)

        def merge_dc(q, dc):
            b_ = q % 2
            mb = 0
            for v, w_ in enumerate((woa, wob)):
                for pc in range(4):
                    P.op(PE, lambda e, v=v, w_=w_, pc=pc: e.matmul(
                        psb(v), w_[:, pc, dc * 128:(dc + 1) * 128], oin[b_][v][:, pc, :], start=(pc == 0), stop=(pc == 3)),
                        reads=["woa", "wob", ("oin", b_, v)], writes=[("ps", v)])
            P.op(DVE, lambda e: e.tensor_tensor(m1[mb][:], psb(0), gat[b_][0][:, dc, :], ALU.mult),
                 reads=[("ps", 0), ("gat", b_, 0)], writes=[("m1", mb)])
            P.op(DVE, lambda e: e.tensor_tensor(m2[mb][:], psb(1), gat[b_][1][:, dc, :], ALU.mult),
                 reads=[("ps", 1), ("gat", b_, 1)], writes=[("m2", mb)])
            P.op(POOL, lambda e: e.tensor_tensor(mixT[b_][:, dc, :], m1[mb][:], m2[mb][:], ALU.add),
                 reads=[("m1", mb), ("m2", mb)], writes=[("mixT", b_, dc)])

        merge_loads(0)
        for dc in range(8):
            merge_dc(0, dc)
        for q in range(NQ):
            if q + 1 < NQ:
                merge_loads(q + 1)
            for sub in range(4):
                t = q * 4 + sub
                xb = xh[xi[0] % 3]
                xr = ("xh", xi[0] % 3)
                xi[0] += 1
                P.op(SP, lambda e, xb=xb, t=t: e.dma_start(out=xb[:], in_=x_d[t * 128:(t + 1) * 128, :]),
                     writes=[xr], dma=True)
                for ch in range(2):
                    bk = 2 + ch
                    for kc in range(8):
                        P.op(PE, lambda e, sub=sub, ch=ch, kc=kc, bk=bk, q=q: e.matmul(
                            psb(bk), mixT[q % 2][:, kc, sub * 128:(sub + 1) * 128], wout[:, kc, ch * 512:(ch + 1) * 512],
                            start=(kc == 0), stop=(kc == 7)),
                            reads=["wout"] + [("mixT", q % 2, d_) for d_ in range(8)], writes=[("ps", bk)])
                    P.op(DVE, lambda e, xb=xb, ch=ch, bk=bk: e.tensor_tensor(
                        xb[:, ch * 512:(ch + 1) * 512], psb(bk), xb[:, ch * 512:(ch + 1) * 512], ALU.add),
                        reads=[("ps", bk), xr], writes=[xr])
                P.op(SP, lambda e, xb=xb, t=t: e.dma_start(out=h_s[t * 128:(t + 1) * 128, :], in_=xb[:]),
                     reads=[xr], dma=True)
                if q + 1 < NQ:
                    merge_dc(q + 1, 2 * sub)
                P.op(ACT, lambda e, xb=xb, t=t: e.activation(sq5[:], xb[:], AF.Square, accum_out=st5[:, t, 0:1]),
                     reads=[xr, "st5"], writes=["sq5", ("st5", t)])
                P.op(ACT, lambda e, t=t: e.activation(st5[:, t, 1:2], st5[:, t, 0:1], AF.Sqrt, bias=EPS, scale=1.0 / D),
                     reads=[("st5", t)], writes=[("st51", t)])
                P.op(DVE, lambda e, t=t: e.reciprocal(st5[:, t, 1:2], st5[:, t, 1:2]),
                     reads=[("st51", t)], writes=[("st51", t)])
                hb = hsb[t % 2]
                P.op(DVE, lambda e, xb=xb, hb=hb, t=t: e.scalar_tensor_tensor(
                    hb[:], xb[:], st5[:, t, 1:2], gffn[:], ALU.mult, ALU.mult),
                    reads=[xr, ("st51", t), "gffn"], writes=[("hsb", t % 2)])
                tb = 4 + (t % 2)
                for kc in range(8):
                    P.op(PE, lambda e, hb=hb, kc=kc, tb=tb: e.transpose(
                        psb16(tb)[:, kc * 128:(kc + 1) * 128], hb[:, kc * 128:(kc + 1) * 128], identb),
                        reads=[("hsb", t % 2), "cb"], writes=[("ps", tb)])
                P.op(ACT, lambda e, t=t, tb=tb: e.copy(
                    actT[:, :, t * 128:(t + 1) * 128], psb16(tb).rearrange("p (a b) -> p a b", a=8)),
                    reads=[("ps", tb)], writes=[("actT", t)])
                if q + 1 < NQ:
                    merge_dc(q + 1, 2 * sub + 1)
                for kc in range(8):
                    P.op(PE, lambda e, t=t, kc=kc: e.matmul(
                        psb(6)[:, 0:36], actT[:, kc, t * 128:(t + 1) * 128], wr[:, kc, :], start=(kc == 0), stop=(kc == 7)),
                        reads=[("actT", t), "wr"], writes=[("ps", 6)])
                P.op(DVE, lambda e, t=t: e.tensor_tensor(Lr[:, t, :], psb(6)[:, 0:36], brb[:], ALU.add),
                     reads=[("ps", 6), "brb"], writes=[("Lr", t)])

        allL = [("Lr", t) for t in range(NT)]
        scr = mixT[0][:].rearrange("p a b -> p (a b)").bitcast(F32)

        class V:
            def __init__(self, ap):
                self.ap = ap

            def __getitem__(self, idx):
                return self.ap if (isinstance(idx, slice) and idx == slice(None)) else self.ap[idx]

        def v3(o, k):
            return V(scr[:, o:o + NT * k].rearrange("p (t k) -> p t k", k=k))

        def v2(o):
            return V(scr[:, o:o + NT])

        sel, sel2, tmp8, is1, is2 = v3(0, 8), v3(256, 8), v3(512, 8), v3(768, 8), v3(1024, 8)
        gd, grp = v3(1280, 4), v3(1408, 4)
        gmax, gp, mx1, mx2, e2, w1, w2 = (v2(1536 + 32 * i) for i in range(7))
        mixall = [("mixT", 0, d_) for d_ in range(8)]

        def b3(ap2, k):
            return ap2.unsqueeze(2).broadcast_to([128, NT, k])

        def dv(fn, reads, writes):
            P.op(DVE, fn, reads=reads, writes=writes)

        Lg = Lr[:, :, 0:4]
        dv(lambda e: e.reduce_max(gmax[:], Lg, axis=AX.X), allL, ["gmax"] + mixall)
        dv(lambda e: e.tensor_tensor(gd[:], Lg, b3(gmax[:], 4), ALU.subtract), allL + ["gmax"], ["gd"])
        P.op(ACT, lambda e: e.activation(gd[:], gd[:], AF.Exp), reads=["gd"], writes=["gd"])
        dv(lambda e: e.reduce_sum(gp[:], gd[:], axis=AX.X), ["gd"], ["gp"])
        dv(lambda e: e.reciprocal(gp[:], gp[:]), ["gp"], ["gp"])
        dv(lambda e: e.tensor_tensor(grp[:], Lg, b3(gmax[:], 4), ALU.is_equal), allL + ["gmax"], ["grp"])
        dv(lambda e: e.tensor_tensor(sel[:], Lr[:, :, 4:12], grp[:, :, 0:1].broadcast_to([128, NT, 8]), ALU.mult),
           allL + ["grp"], ["sel"])
        for g in range(1, 4):
            dv(lambda e, g=g: e.tensor_tensor(tmp8[:], Lr[:, :, 4 + 8 * g:12 + 8 * g],
                                              grp[:, :, g:g + 1].broadcast_to([128, NT, 8]), ALU.mult),
               allL + ["grp", "sel"], ["tmp8"])
            dv(lambda e: e.tensor_tensor(sel[:], sel[:], tmp8[:], ALU.add), ["sel", "tmp8"], ["sel"])
        dv(lambda e: e.reduce_max(mx1[:], sel[:], axis=AX.X), ["sel"], ["mx1"])
        dv(lambda e: e.tensor_tensor(is1[:], sel[:], b3(mx1[:], 8), ALU.is_equal), ["sel", "mx1"], ["is1"])
        dv(lambda e: e.scalar_tensor_tensor(sel2[:], is1[:], -1e30, sel[:], ALU.mult, ALU.add), ["is1", "sel"], ["sel2"])
        dv(lambda e: e.reduce_max(mx2[:], sel2[:], axis=AX.X), ["sel2"], ["mx2"])
        dv(lambda e: e.tensor_tensor(is2[:], sel2[:], b3(mx2[:], 8), ALU.is_equal), ["sel2", "mx2"], ["is2"])
        dv(lambda e: e.tensor_tensor(e2[:], mx2[:], mx1[:], ALU.subtract), ["mx1", "mx2"], ["e2"])
        P.op(ACT, lambda e: e.activation(e2[:], e2[:], AF.Exp), reads=["e2"], writes=["e2"])
        dv(lambda e: e.tensor_scalar(w1[:], e2[:], 1.0, None, ALU.add), ["e2"], ["w1"])
        dv(lambda e: e.reciprocal(w1[:], w1[:]), ["w1"], ["w1"])
        dv(lambda e: e.tensor_tensor(w1[:], w1[:], gp[:], ALU.mult), ["w1", "gp"], ["w1"])
        dv(lambda e: e.tensor_tensor(w2[:], w1[:], e2[:], ALU.mult), ["w1", "e2"], ["w2"])
        dv(lambda e: e.tensor_tensor(is1[:], is1[:], b3(w1[:], 8), ALU.mult), ["is1", "w1", "sel2"], ["is1"])
        dv(lambda e: e.tensor_tensor(is2[:], is2[:], b3(w2[:], 8), ALU.mult), ["is2", "w2"], ["is2"])
        dv(lambda e: e.tensor_tensor(is1[:], is1[:], is2[:], ALU.add), ["is1", "is2"], ["is1"])
        for g in range(4):
            dv(lambda e, g=g: e.tensor_tensor(comb[:, :, 8 * g:8 * g + 8], is1[:],
                                              grp[:, :, g:g + 1].broadcast_to([128, NT, 8]), ALU.mult),
               ["is1", "grp"], [("comb", g)])
        if "p5" in debug:
            add_dbg(P, "comb", comb[:], [128, NT, NE], F32, [("comb", g) for g in range(4)])
            add_dbg(P, "hnT", actT[:, :, 2048:2560], [128, 8, 512], BF16, [("actT", t) for t in range(16, 20)])
    P.emit("p5")
    if stop_after <= 5:
        return nc, dbg_outs

    P = Prog(nc)
    TT = 2048
    NS8 = TT // 128
    with contextlib.ExitStack() as st:
        gfin = st.enter_context(nc.sbuf_tensor("gfin", [128, D], F32))
        yacc = st.enter_context(nc.sbuf_tensor("yacc", [128, TT // 128, D], F32))
        wgu = [st.enter_context(nc.sbuf_tensor("wgu%d" % i, [128, 2, 8, EFF], BF16)) for i in range(2)]
        wdn = [st.enter_context(nc.sbuf_tensor("wdn%d" % i, [128, 2, D], BF16)) for i in range(2)]
        sg = [st.enter_context(nc.sbuf_tensor("sg%d" % i, [128, 512], F32)) for i in range(2)]
        aT = [st.enter_context(nc.sbuf_tensor("aT%d" % i, [128, 2, 512], BF16)) for i in range(2)]
        sq6 = st.enter_context(nc.sbuf_tensor("sq6", [128, D], BF16))
        st6 = st.enter_context(nc.sbuf_tensor("st6", [128, NT, 2], F32))
        P.op(SP, lambda e: e.dma_start(out=gfin[:], in_=g_fin_d[0].partition_broadcast(128)), writes=["gfin"], dma=True)
        P.op(DVE, lambda e: e.memset(st6[:], 0.0), writes=["st6"])
        wg_v = w_eg_d.rearrange("(e r) c -> e (r c)", e=NE).rearrange("e (kc p f) -> e p kc f", p=128, f=EFF)
        wu_v = w_eu_d.rearrange("(e r) c -> e (r c)", e=NE).rearrange("e (kc p f) -> e p kc f", p=128, f=EFF)
        wd_v = w_ed_d.rearrange("(e r) c -> e (r c)", e=NE).rearrange("e (c p n) -> e p c n", p=128, n=D)
        it = 0
        dn = 0
        for tt in range(S // TT):
            for s8 in range(NS8):
                t = tt * NS8 + s8
                P.op(SP, lambda e, s8=s8, t=t: e.dma_start(out=yacc[:, s8, :], in_=h_s[t * 128:(t + 1) * 128, :]),
                     writes=[("yacc", s8)], dma=True)
            if "y0" in debug and tt == 1:
                add_dbg(P, "y0", yacc[:, 0, :], [128, D], F32, [("yacc", 0)])
            def wloads(ex, wb):
                P.op(POOL, lambda e: e.dma_start(out=wgu[wb][:, 0, :, :], in_=wg_v[ex]), writes=[("wgu", wb)], dma=True)
                P.op(POOL, lambda e: e.dma_start(out=wgu[wb][:, 1, :, :], in_=wu_v[ex]), writes=[("wgu", wb)], dma=True)
                P.op(POOL, lambda e: e.dma_start(out=wdn[wb][:], in_=wd_v[ex]), writes=[("wdn", wb)], dma=True)

            def gu(ex, half, wb):
                tok0 = tt * TT + half * 512
                ab = aT[half % 2]
                for c in range(2):
                    gb_, ub_ = 2 * c, 2 * c + 1
                    for v, bk in ((0, gb_), (1, ub_)):
                        for kc in range(8):
                            P.op(PE, lambda e, v=v, kc=kc, c=c, bk=bk: e.matmul(
                                psb(bk), wgu[wb][:, v, kc, c * 128:(c + 1) * 128], actT[:, kc, tok0:tok0 + 512],
                                start=(kc == 0), stop=(kc == 7)),
                                reads=[("wgu", wb)], writes=[("ps", bk)])
                    sgb = sg[c]
                    P.op(ACT, lambda e, sgb=sgb, gb_=gb_: e.activation(sgb[:], psb(gb_), AF.Silu),
                         reads=[("ps", gb_)], writes=[("sg", c)])
                    P.op(DVE, lambda e, c=c, sgb=sgb, ub_=ub_: e.tensor_tensor(ab[:, c, :], psb(ub_), sgb[:], ALU.mult),
                         reads=[("ps", ub_), ("sg", c)], writes=[("aT", half % 2, c)])

            def dnp(ex, half, wb):
                nonlocal dn
                ab = aT[half % 2]
                for sub in range(4):
                    s8 = half * 4 + sub
                    t = tt * NS8 + s8
                    for ch in range(2):
                        bk = 4 + (dn % 4)
                        dn += 1
                        for c in range(2):
                            P.op(PE, lambda e, c=c, sub=sub, ch=ch, bk=bk: e.matmul(
                                psb(bk), ab[:, c, sub * 128:(sub + 1) * 128], wdn[wb][:, c, ch * 512:(ch + 1) * 512],
                                start=(c == 0), stop=(c == 1)),
                                reads=[("aT", half % 2, 0), ("aT", half % 2, 1), ("wdn", wb)], writes=[("ps", bk)])
                        P.op(DVE, lambda e, s8=s8, ch=ch, bk=bk, t=t: e.scalar_tensor_tensor(
                            yacc[:, s8, ch * 512:(ch + 1) * 512], psb(bk), comb[:, t, ex:ex + 1],
                            yacc[:, s8, ch * 512:(ch + 1) * 512], ALU.mult, ALU.add),
                            reads=[("ps", bk), ("yacc", s8)], writes=[("yacc", s8)])

            items = [(ex, half) for ex in range(NE) for half in range(TT // 512)]
            wbs = {}
            for ex in range(NE):
                wbs[ex] = it % 2
                it += 1

            def gu_item(i):
                ex, half = items[i]
                if half == 0:
                    wloads(ex, wbs[ex])
                gu(ex, half, wbs[ex])

            gu_item(0)
            for i in range(len(items)):
                if i + 1 < len(items):
                    gu_item(i + 1)
                ex, half = items[i]
                dnp(ex, half, wbs[ex])
            for s8 in range(NS8):
                t = tt * NS8 + s8
                P.op(ACT, lambda e, s8=s8, t=t: e.activation(sq6[:], yacc[:, s8, :], AF.Square, accum_out=st6[:, t, 0:1]),
                     reads=[("yacc", s8), "st6"], writes=["sq6", ("st6", t)])
                P.op(ACT, lambda e, t=t: e.activation(st6[:, t, 1:2], st6[:, t, 0:1], AF.Sqrt, bias=EPS, scale=1.0 / D),
                     reads=[("st6", t)], writes=[("st61", t)])
                P.op(DVE, lambda e, t=t: e.reciprocal(st6[:, t, 1:2], st6[:, t, 1:2]), reads=[("st61", t)], writes=[("st61", t)])
                P.op(DVE, lambda e, s8=s8, t=t: e.scalar_tensor_tensor(
                    yacc[:, s8, :], yacc[:, s8, :], st6[:, t, 1:2], gfin[:], ALU.mult, ALU.mult),
                    reads=[("yacc", s8), ("st61", t), "gfin"], writes=[("yacc", s8)])
                P.op(SP, lambda e, s8=s8, t=t: e.dma_start(out=out_d[t * 128:(t + 1) * 128, :], in_=yacc[:, s8, :]),
                     reads=[("yacc", s8)], dma=True)
    P.emit("p6")
    return nc, dbg_outs


def _consts():
    cfm = np.zeros((128, 512), np.float32)
    cfm[:, 0:128] = np.eye(128, dtype=np.float32)
    cfm[:, 128:256] = np.triu(np.ones((128, 128), np.float32))
    cfm[:, 256:384] = 1.0
    p = np.arange(128)
    j = p % 16
    freq = (10000.0 ** (-(j.astype(np.float32)) / np.float32(16.0))).astype(np.float32)
    sgn = np.where((p % 32) < 16, -1.0, 1.0).astype(np.float32)
    cfm[:, 384] = sgn * freq
    cfm[:, 385] = freq
    cfm[:, 386] = sgn
    cbm = np.zeros((128, 1024), np.float32)
    cbm[:, 0:128] = np.eye(128)
    k = np.arange(128)[:, None]
    q = np.arange(128)[None, :]
    cbm[:, 128:256] = np.where(k > q, NEG, 0.0)
    cbm[:, 256:384] = np.where((k // 64) > (q // 64), NEG, 0.0)
    for h in range(8):
        cbm[h, 384 + h * 65 + 64] = 1.0
    return cfm, cbm.astype(ml_dtypes.bfloat16)


def _prep_inputs(inp):
    f = lambda a: np.ascontiguousarray(np.asarray(a, dtype=np.float32))
    w_in = f(inp["w_in"])
    kr = w_in[:, 384:416]
    swap = np.concatenate([np.arange(16, 32), np.arange(0, 16)])
    w_kr2 = np.concatenate([kr, kr, kr, kr, kr, kr[:, swap]], axis=1)
    w_uq = f(inp["w_uq"])
    idx = np.arange(768).reshape(8, 96).copy()
    idx[:, 64:96] = idx[:, 64:96][:, swap]
    w_uq_sw = w_uq[:, idx.reshape(-1)]
    cfm, cbm = _consts()
    shared = {
        "g_mix": f(inp["g_mix"]).reshape(1, -1),
        "g_ffn": f(inp["g_ffn"]).reshape(1, -1),
        "g_final": f(inp["g_final"]).reshape(1, -1),
        "g_cq": f(inp["g_cq"]).reshape(1, -1),
        "g_ckv": f(inp["g_ckv"]).reshape(1, -1),
        "b_forget": f(inp["b_forget"]).reshape(1, -1),
        "b_r36": np.concatenate([f(inp["b_group"]), f(inp["b_router"])]).reshape(1, -1),
        "w_lat": np.ascontiguousarray(w_in[:, 0:384]),
        "w_kr2": np.ascontiguousarray(w_kr2),
        "w_fq": np.ascontiguousarray(w_in[:, 416:928]),
        "w_fk": np.ascontiguousarray(w_in[:, 928:1440]),
        "w_fv": np.ascontiguousarray(w_in[:, 1440:1952]),
        "w_f": np.ascontiguousarray(w_in[:, 1952:1960]),
        "w_ga": np.ascontiguousarray(w_in[:, 1960:2984]),
        "w_gb": np.ascontiguousarray(w_in[:, 2984:4008]),
        "w_uq": w_uq,
        "w_uq_sw": np.ascontiguousarray(w_uq_sw),
        "w_uk": f(inp["w_uk"]),
        "w_uv": f(inp["w_uv"]),
        "w_o_mla": f(inp["w_o_mla"]),
        "w_o_fox": f(inp["w_o_fox"]),
        "w_out": f(inp["w_out"]),
        "w_r36": np.ascontiguousarray(np.concatenate([f(inp["w_group"]), f(inp["w_router"])], axis=1)),
        "w_e_gate": f(inp["w_e_gate"]).reshape(-1, 2048),
        "w_e_up": f(inp["w_e_up"]).reshape(-1, 2048),
        "w_e_down": f(inp["w_e_down"]).reshape(-1, 2048),
        "consts_f": cfm,
        "consts_b": cbm,
    }
    x = f(inp["x"])
    pos = np.ascontiguousarray(np.asarray(inp["positions"], dtype=np.int32))
    in_maps = []
    for b in range(8):
        m = dict(shared)
        m["x"] = x[b]
        m["pos"] = pos[b].reshape(1, -1)
        in_maps.append(m)
    return in_maps


def kernel(**inputs):
    in_maps = _prep_inputs(inputs)
    nc, _ = build_program()
    res = run_bass_kernel_spmd(nc, in_maps, core_ids=list(range(8)))
    out = np.stack([np.asarray(r["out"], dtype=np.float32) for r in res.results], axis=0)
    return out
```

```python
import contextlib
import numpy as np
import ml_dtypes
import concourse.bass as bass
import concourse.mybir as mybir
from concourse.bass_utils import run_bass_kernel_spmd

F32 = mybir.dt.float32
BF16 = mybir.dt.bfloat16
I32 = mybir.dt.int32
ALU = mybir.AluOpType
AF = mybir.ActivationFunctionType
AX = mybir.AxisListType

PE, ACT, DVE, POOL, SP = "tensor", "scalar", "vector", "gpsimd", "sync"
ENGINES = [PE, ACT, DVE, POOL, SP]

S = 4096
D = 1024
NT = 32
NQ = 8
NH = 8
NE = 32
EFF = 256
EPS = 1e-6
MLA_SCALE = 96.0 ** -0.5
FOX_SCALE = 0.125
NEG = -30000.0
TWO_PI_HI = 6.28125
TWO_PI_LO = 6.283185307179586 - 6.28125


class Op:
    __slots__ = ("eng", "fn", "deps", "is_dma", "marked", "count", "sem", "semval", "prewait", "nosem")

    def __init__(self, eng, fn, is_dma):
        self.eng = eng
        self.fn = fn
        self.deps = []
        self.is_dma = is_dma
        self.marked = False
        self.count = 0
        self.sem = None
        self.semval = 0
        self.prewait = None
        self.nosem = False


class Prog:
    def __init__(self, nc, dma_pool=8):
        self.nc = nc
        self.ops = {e: [] for e in ENGINES}
        self.last_write = {}
        self.readers = {}
        self.dma_pool = dma_pool
        self.dma_count = {e: 0 for e in ENGINES}

    def op(self, eng, fn, reads=(), writes=(), dma=False, nosem=False):
        o = Op(eng, fn, dma)
        o.nosem = nosem
        deps = {}
        for r in reads:
            w = self.last_write.get(r)
            if w is not None:
                deps[id(w)] = (w, "raw")
        for w_ in writes:
            for rd in self.readers.get(w_, ()):
                if id(rd) not in deps:
                    deps[id(rd)] = (rd, "war")
            w = self.last_write.get(w_)
            if w is not None:
                deps[id(w)] = (w, "waw")
        for d, kind in deps.values():
            if d is o:
                continue
            if (not d.is_dma) and (not dma) and d.eng == eng:
                if eng == PE or kind == "war":
                    continue
            o.deps.append(d)
            d.marked = True
        for r in reads:
            self.readers.setdefault(r, []).append(o)
        for w_ in writes:
            self.last_write[w_] = o
            self.readers[w_] = []
        if dma and not nosem:
            k = self.dma_count[eng]
            self.dma_count[eng] = k + 1
            o.sem = (eng, k % self.dma_pool)
            o.semval = 16 * (k // self.dma_pool + 1)
            if k >= self.dma_pool:
                o.prewait = (o.sem, o.semval - 16)
        self.ops[eng].append(o)
        return o

    def emit(self, name):
        nc = self.nc
        for e in ENGINES:
            c = 0
            for o in self.ops[e]:
                if not o.is_dma and o.marked:
                    c += 1
                    o.count = c
        with contextlib.ExitStack() as st:
            esem = {e: st.enter_context(nc.semaphore("s_%s_%s" % (name, e))) for e in ENGINES}
            dsem = {}
            for e in ENGINES:
                for i in range(min(self.dma_pool, self.dma_count[e])):
                    dsem[(e, i)] = st.enter_context(nc.semaphore("d_%s_%s_%d" % (name, e, i)))
            allsems = list(esem.values()) + list(dsem.values())
            with nc.Block() as cblk:
                def _clr(engobj):
                    for s_ in allsems:
                        engobj.sem_clear(s_)
                cblk.sync(_clr)
            block = st.enter_context(nc.Block())

            def run_engine(e, engobj):
                seen = {}
                for o in self.ops[e]:
                    need = {}
                    for d in o.deps:
                        if d.is_dma:
                            key = ("d", d.sem)
                            val = d.semval
                        else:
                            key = ("e", d.eng)
                            val = d.count
                        if need.get(key, 0) < val:
                            need[key] = val
                    if o.prewait is not None:
                        key = ("d", o.prewait[0])
                        if need.get(key, 0) < o.prewait[1]:
                            need[key] = o.prewait[1]
                    for key, val in need.items():
                        if seen.get(key, 0) >= val:
                            continue
                        seen[key] = val
                        s = dsem[key[1]] if key[0] == "d" else esem[key[1]]
                        engobj.wait_ge(s, val)
                    ins = o.fn(engobj)
                    if o.nosem:
                        continue
                    if o.is_dma:
                        ins.then_inc(dsem[o.sem], 16)
                    elif o.marked:
                        ins.then_inc(esem[e], 1)
                k = self.dma_count[e]
                for i in range(min(self.dma_pool, k)):
                    uses = (k - 1 - i) // self.dma_pool + 1
                    if uses > 0:
                        engobj.wait_ge(dsem[(e, i)], 16 * uses)

            for e in ENGINES:
                if not self.ops[e]:
                    continue
                getattr(block, e)(lambda engobj, e=e: run_engine(e, engobj))


def build_program(stop_after=99, debug=None):
    nc = bass.Bass("TRN2", target_bir_lowering=False)
    debug = debug or []

    def din(name, shape, dt=F32):
        return nc.dram_tensor(name, list(shape), dt, kind="ExternalInput").ap()

    x_d = din("x", [S, D])
    pos_d = din("pos", [1, S], I32)
    g_mix_d = din("g_mix", [1, D])
    g_ffn_d = din("g_ffn", [1, D])
    g_fin_d = din("g_final", [1, D])
    g_cq_d = din("g_cq", [1, 256])
    g_ckv_d = din("g_ckv", [1, 128])
    bf_d = din("b_forget", [1, 8])
    br_d = din("b_r36", [1, 36])
    w_lat_d = din("w_lat", [D, 384])
    w_kr_d = din("w_kr2", [D, 192])
    w_fq_d = din("w_fq", [D, 512])
    w_fk_d = din("w_fk", [D, 512])
    w_fv_d = din("w_fv", [D, 512])
    w_f_d = din("w_f", [D, 8])
    w_ga_d = din("w_ga", [D, D])
    w_gb_d = din("w_gb", [D, D])
    w_uq_d = din("w_uq", [256, 768])
    w_uqs_d = din("w_uq_sw", [256, 768])
    w_uk_d = din("w_uk", [128, 512])
    w_uv_d = din("w_uv", [128, 512])
    w_oa_d = din("w_o_mla", [512, D])
    w_ob_d = din("w_o_fox", [512, D])
    w_out_d = din("w_out", [D, D])
    w_r_d = din("w_r36", [D, 36])
    w_eg_d = din("w_e_gate", [NE * D * EFF // 2048, 2048])
    w_eu_d = din("w_e_up", [NE * D * EFF // 2048, 2048])
    w_ed_d = din("w_e_down", [NE * EFF * D // 2048, 2048])
    cf_d = din("consts_f", [128, 512])
    cb_d = din("consts_b", [128, 1024], BF16)
    out_d = nc.dram_tensor("out", [S, D], F32, kind="ExternalOutput").ap()

    ga_s = nc.dram_tensor("ga_s", [D, S], BF16).ap()
    gb_s = nc.dram_tensor("gb_s", [D, S], BF16).ap()
    oa_s = nc.dram_tensor("oa_s", [512, S], BF16).ap()
    ob_s = nc.dram_tensor("ob_s", [512, S], BF16).ap()
    h_s = nc.dram_tensor("h_s", [S, D], F32).ap()

    dbg_outs = {}

    cf = nc.alloc_sbuf_tensor("cf", [128, 512], F32)
    cb = nc.alloc_sbuf_tensor("cb", [128, 1024], BF16)
    actT = nc.alloc_sbuf_tensor("actT", [128, 8, S], BF16)
    comb = nc.alloc_sbuf_tensor("comb", [128, NT, NE], F32)
    ps = nc.alloc_psum_tensor("ps", [128, 8, 512], F32)

    identf = cf[:, 0:128]
    utri = cf[:, 128:256]
    onesf = cf[:, 256:384]
    freq_col = cf[:, 384:385]
    freq_abs = cf[:, 385:386]
    identb = cb[:, 0:128]
    mask_fox = cb[:, 128:256]
    mask_mla = cb[:, 256:384]
    esel = cb[0:8, 384:384 + 8 * 65]


    def psb(b):
        return ps[:, b, :]

    def psb16(b):
        return ps[:, b, :].bitcast(BF16)

    def add_dbg(P, name, ap, shape, dt, reads):
        t = nc.dram_tensor("dbg_" + name, list(shape), dt, kind="ExternalOutput").ap()
        dbg_outs[name] = t
        P.op(SP, lambda e: e.dma_start(out=t, in_=ap), reads=reads, dma=True)

    def wload(P, dst, src_ap, res, eng=POOL):
        P.op(eng, lambda e: e.dma_start(out=dst, in_=src_ap), writes=[res], dma=True)

    def kc_view(w_ap):
        return w_ap.rearrange("(kc p) n -> p kc n", p=128)

    P = Prog(nc)
    wload(P, cf[:], cf_d, "cf", eng=SP)
    wload(P, cb[:], cb_d, "cb", eng=SP)
    with contextlib.ExitStack() as st:
        gmix = st.enter_context(nc.sbuf_tensor("gmix", [128, D], F32))
        xt = [st.enter_context(nc.sbuf_tensor("xt%d" % i, [128, D], F32)) for i in range(3)]
        xs = [st.enter_context(nc.sbuf_tensor("xs%d" % i, [128, D], BF16)) for i in range(2)]
        sq = st.enter_context(nc.sbuf_tensor("sq", [128, D], BF16))
        stat = st.enter_context(nc.sbuf_tensor("stat", [128, NT, 2], F32))
        P.op(SP, lambda e: e.dma_start(out=gmix[:], in_=g_mix_d[0].partition_broadcast(128)), writes=["gmix"], dma=True)
        P.op(DVE, lambda e: e.memset(stat[:], 0.0), writes=["stat"])

        def p1_copy(t):
            bk = t % 2
            P.op(ACT, lambda e: e.copy(
                actT[:, :, t * 128:(t + 1) * 128], psb16(bk).rearrange("p (a b) -> p a b", a=8)),
                reads=[("ps", bk)], writes=[("actT", t)])

        for t in range(NT):
            xb = xt[t % 3]
            xsb = xs[t % 2]
            bk = t % 2
            P.op(SP, lambda e, xb=xb, t=t: e.dma_start(out=xb[:], in_=x_d[t * 128:(t + 1) * 128, :]),
                 writes=[("xt", t % 3)], dma=True)
            P.op(ACT, lambda e, xb=xb, t=t: e.activation(sq[:], xb[:], AF.Square, accum_out=stat[:, t, 0:1]),
                 reads=[("xt", t % 3), "stat"], writes=["sq", ("stat", t)])
            P.op(ACT, lambda e, t=t: e.activation(stat[:, t, 1:2], stat[:, t, 0:1], AF.Sqrt, bias=EPS, scale=1.0 / D),
                 reads=[("stat", t)], writes=[("stat1", t)])
            if t > 0:
                p1_copy(t - 1)
            P.op(DVE, lambda e, t=t: e.reciprocal(stat[:, t, 1:2], stat[:, t, 1:2]),
                 reads=[("stat1", t)], writes=[("stat1", t)])
            P.op(DVE, lambda e, xb=xb, xsb=xsb, t=t: e.scalar_tensor_tensor(
                xsb[:], xb[:], stat[:, t, 1:2], gmix[:], ALU.mult, ALU.mult),
                reads=[("xt", t % 3), ("stat1", t), "gmix"], writes=[("xs", t % 2)])
            for kc in range(8):
                P.op(PE, lambda e, xsb=xsb, kc=kc, bk=bk: e.transpose(
                    psb16(bk)[:, kc * 128:(kc + 1) * 128], xsb[:, kc * 128:(kc + 1) * 128], identb),
                    reads=[("xs", t % 2), "cb"], writes=[("ps", bk)])
        p1_copy(NT - 1)
        if "xnT" in debug:
            add_dbg(P, "xnT", actT[:, :, 0:512], [128, 8, 512], BF16, [("actT", t) for t in range(4)])
    P.emit("p1")
    if stop_after <= 1:
        return nc, dbg_outs

    st_f = contextlib.ExitStack()
    Gc = st_f.enter_context(nc.sbuf_tensor("Gc", [128, NT, 8], F32))
    FsT = st_f.enter_context(nc.sbuf_tensor("FsT", [8, S], BF16))
    P = Prog(nc)
    with contextlib.ExitStack() as st:
        wf = st.enter_context(nc.sbuf_tensor("wf", [128, 8, 8], BF16))
        wga = st.enter_context(nc.sbuf_tensor("wga", [128, 8, D], BF16))
        wgb = st.enter_context(nc.sbuf_tensor("wgb", [128, 8, D], BF16))
        bfb = st.enter_context(nc.sbuf_tensor("bfb", [128, 8], F32))
        lf = st.enter_context(nc.sbuf_tensor("lf", [128, NT, 8], F32))
        tot = st.enter_context(nc.sbuf_tensor("tot", [128, NT, 8], F32))
        off = st.enter_context(nc.sbuf_tensor("off", [128, NT, 8], F32))
        gst = [st.enter_context(nc.sbuf_tensor("gst%d" % i, [128, 512], BF16)) for i in range(4)]
        wload(P, wf[:], kc_view(w_f_d), "wf")
        wload(P, wga[:], kc_view(w_ga_d), "wga")
        wload(P, wgb[:], kc_view(w_gb_d), "wgb")
        P.op(SP, lambda e: e.dma_start(out=bfb[:], in_=bf_d[0].partition_broadcast(128)), writes=["bfb"], dma=True)
        for t in range(NT):
            for kc in range(8):
                P.op(PE, lambda e, t=t, kc=kc: e.matmul(
                    psb(0)[:, t * 8:(t + 1) * 8], actT[:, kc, t * 128:(t + 1) * 128], wf[:, kc, :],
                    start=(kc == 0), stop=(kc == 7)),
                    reads=[("actT", t), "wf"], writes=[("ps", 0)])
        P.op(DVE, lambda e: e.tensor_tensor(
            lf[:], psb(0)[:, 0:256].rearrange("p (t h) -> p t h", h=8),
            bfb[:].unsqueeze(1).broadcast_to([128, NT, 8]), ALU.add),
            reads=[("ps", 0), "bfb"], writes=["lf"])
        P.op(ACT, lambda e: e.activation(lf[:], lf[:], AF.Exp, scale=-1.0), reads=["lf"], writes=["lf"])
        P.op(ACT, lambda e: e.activation(lf[:], lf[:], AF.Ln, bias=1.0), reads=["lf"], writes=["lf"])
        lf2 = lf[:].rearrange("p t h -> p (t h)")
        P.op(PE, lambda e: e.matmul(psb(1)[:, 0:256], utri, lf2, start=True, stop=True),
             reads=["lf", "cf"], writes=[("ps", 1)])
        P.op(PE, lambda e: e.matmul(psb(2)[:, 0:256], onesf, lf2, start=True, stop=True),
             reads=["lf", "cf"], writes=[("ps", 2)])
        P.op(DVE, lambda e: e.tensor_copy(tot[:], psb(2)[:, 0:256].rearrange("p (t h) -> p t h", h=8)),
             reads=[("ps", 2)], writes=["tot"])
        P.op(DVE, lambda e: e.memset(off[:, 0, :], 0.0), writes=["off"])
        for t in range(1, NT):
            P.op(DVE, lambda e, t=t: e.tensor_tensor(off[:, t, :], off[:, t - 1, :], tot[:, t - 1, :], ALU.add),
                 reads=["off", "tot"], writes=["off"])
        P.op(DVE, lambda e: e.tensor_tensor(
            Gc[:], psb(1)[:, 0:256].rearrange("p (t h) -> p t h", h=8), off[:], ALU.add),
            reads=[("ps", 1), "off"], writes=["Gc"])
        for g4 in range(8):
            bk = 3 + (g4 % 2)
            for i in range(4):
                t = g4 * 4 + i
                P.op(PE, lambda e, t=t, i=i, bk=bk: e.transpose(
                    psb(bk)[0:8, i * 128:(i + 1) * 128], Gc[:, t, :], identf),
                    reads=["Gc", "cf"], writes=[("ps", bk)])
            P.op(ACT, lambda e, g4=g4, bk=bk: e.mul(FsT[:, g4 * 512:(g4 + 1) * 512], psb(bk)[0:8, :], -0.5 / FOX_SCALE),
                 reads=[("ps", bk)], writes=["FsT"])
        n = 0
        for (wg_, dst) in ((wga, ga_s), (wgb, gb_s)):
            wname = "wga" if wg_ is wga else "wgb"
            for dc in range(8):
                for q in range(NQ):
                    bk = 5 + (n % 3)
                    sb = gst[n % 4]
                    for kc in range(8):
                        P.op(PE, lambda e, wg_=wg_, dc=dc, q=q, kc=kc, bk=bk: e.matmul(
                            psb(bk), wg_[:, kc, dc * 128:(dc + 1) * 128], actT[:, kc, q * 512:(q + 1) * 512],
                            start=(kc == 0), stop=(kc == 7)),
                            reads=[wname] + [("actT", q * 4 + i) for i in range(4)], writes=[("ps", bk)])
                    P.op(ACT, lambda e, sb=sb, bk=bk: e.activation(sb[:], psb(bk), AF.Sigmoid),
                         reads=[("ps", bk)], writes=[("gst", n % 4)])
                    P.op(SP, lambda e, sb=sb, dst=dst, dc=dc, q=q: e.dma_start(
                        out=dst[dc * 128:(dc + 1) * 128, q * 512:(q + 1) * 512], in_=sb[:]),
                        reads=[("gst", n % 4)], dma=True)
                    n += 1
        if "Gc" in debug:
            add_dbg(P, "Gc", Gc[:], [128, NT, 8], F32, ["Gc"])
            add_dbg(P, "FsT", FsT[:], [8, S], BF16, ["FsT"])
    P.emit("p2a")
    if stop_after <= 2:
        return nc, dbg_outs

    def attention_head(P, h, kdim, qT, kT, vx, scale, bias_fn, mask, o_dst, bufs, hname, interleave=()):
        pt, rrow, rbc, ost = bufs
        pairs = [(I, J) for I in range(NQ) for J in range(4 * I + 4)]
        npairs = len(pairs)

        def emit_qk(n):
            I, J = pairs[n]
            j = J - 4 * I
            c0 = max(0, j) * 128
            sbk = n % 3
            P.op(PE, lambda e: e.matmul(
                psb(sbk)[:, c0:512], kT[0:kdim, J * 128:(J + 1) * 128], qT[0:kdim, I * 512 + c0:(I + 1) * 512],
                start=True, stop=(j < 0)),
                reads=[hname + "q", hname + "k"], writes=[("ps", sbk)])
            if j >= 0:
                P.op(PE, lambda e: e.matmul(
                    psb(sbk)[:, c0:c0 + 128], identb, mask, start=False, stop=True),
                    reads=["cb"], writes=[("ps", sbk)])

        def emit_exp(n):
            I, J = pairs[n]
            j = J - 4 * I
            c0 = max(0, j) * 128
            sbk = n % 3
            ptb = pt[n % 3]
            bias = bias_fn(J)
            P.op(ACT, lambda e: e.activation(
                ptb[:, c0:512], psb(sbk)[:, c0:512], AF.Exp, bias=bias, scale=scale),
                reads=[("ps", sbk), "Gc"], writes=[("pt", n % 3)])

        def emit_pv(n):
            I, J = pairs[n]
            j = J - 4 * I
            c0 = max(0, j) * 128
            ob = 3 + (I % 2)
            nJ = 4 * I + 4
            ptb = pt[n % 3]
            P.op(PE, lambda e: e.matmul(
                psb(ob)[0:65, c0:512], vx[:, J, :], ptb[:, c0:512], start=(J == 0), stop=(J == nJ - 1)),
                reads=[("pt", n % 3), hname + "v"], writes=[("ps", ob)])

        def finalize(I):
            ob = 3 + (I % 2)
            P.op(DVE, lambda e: e.reciprocal(rrow[64:65, :], psb(ob)[64:65, :]),
                 reads=[("ps", ob)], writes=["rrow"])
            P.op(PE, lambda e: e.matmul(psb(5)[0:64, :], onesf[64:65, 0:64], rrow[64:65, :], start=True, stop=True),
                 reads=["rrow", "cf"], writes=[("ps", 5)])
            P.op(ACT, lambda e: e.copy(rbc[0:64, :], psb(5)[0:64, :]), reads=[("ps", 5)], writes=["rbc"])
            osb = ost[I % 2]
            P.op(DVE, lambda e: e.tensor_tensor(osb[0:64, :], psb(ob)[0:64, :], rbc[0:64, :], ALU.mult),
                 reads=[("ps", ob), "rbc"], writes=[("ost", I % 2)])
            P.op(SP, lambda e: e.dma_start(
                out=o_dst[h * 64:(h + 1) * 64, I * 512:(I + 1) * 512], in_=osb[0:64, :]),
                reads=[("ost", I % 2)], writes=["odram"], dma=True)

        pending = []
        chunks = list(interleave)
        evq = []
        step = max(1, (npairs - 8) // (len(chunks) + 1)) if chunks else 0
        emit_qk(0)
        emit_exp(0)
        emit_qk(1)
        emit_exp(1)
        for n in range(npairs):
            if n + 2 < npairs:
                emit_qk(n + 2)
                emit_exp(n + 2)
            emit_pv(n)
            I, J = pairs[n]
            if J == 4 * I + 3:
                pending.append((n + 3, I))
            while pending and pending[0][0] <= n:
                finalize(pending.pop(0)[1])
            while evq and evq[0][0] <= n:
                evq.pop(0)[1]()
            if chunks and n % step == step - 1:
                ev = chunks.pop(0)()
                if ev:
                    evq.append((n + 2, ev))
        for _, I in pending:
            finalize(I)
        for _, ev in evq:
            ev()
        for c in chunks:
            ev = c()
            if ev:
                ev()

    P = Prog(nc)
    with contextlib.ExitStack() as st:
        wfq = st.enter_context(nc.sbuf_tensor("wfq", [128, 8, 512], BF16))
        wfk = st.enter_context(nc.sbuf_tensor("wfk", [128, 8, 512], BF16))
        wfv = st.enter_context(nc.sbuf_tensor("wfv", [128, 8, 512], BF16))
        qTs = [st.enter_context(nc.sbuf_tensor("fqT%d" % i, [65, S], BF16)) for i in range(2)]
        kTs = [st.enter_context(nc.sbuf_tensor("fkT%d" % i, [65, S], BF16)) for i in range(2)]
        vxs = [st.enter_context(nc.sbuf_tensor("fvx%d" % i, [128, NT, 65], BF16)) for i in range(2)]
        pt = [st.enter_context(nc.sbuf_tensor("pt%d" % i, [128, 512], BF16)) for i in range(3)]
        rrow = st.enter_context(nc.sbuf_tensor("rrow", [65, 512], F32))
        rbc = st.enter_context(nc.sbuf_tensor("rbc", [64, 512], F32))
        ost = [st.enter_context(nc.sbuf_tensor("ost%d" % i, [64, 512], BF16)) for i in range(2)]
        wload(P, wfq[:], kc_view(w_fq_d), "wfq")
        wload(P, wfk[:], kc_view(w_fk_d), "wfk")
        wload(P, wfv[:], kc_view(w_fv_d), "wfv")
        for i in range(2):
            P.op(POOL, lambda e, i=i: e.memset(kTs[i][64:65, :], 1.0), writes=[("fk", i)])
            P.op(POOL, lambda e, i=i: e.memset(vxs[i][:, :, 64:65], 1.0), writes=[("fv", i)])
        allact = [("actT", t) for t in range(NT)]
        def fox_chunks(h):
            b = h % 2
            qT, kT, vx = qTs[b], kTs[b], vxs[b]
            hn = "f%d" % b
            out = []

            def cq(q):
                bk = 6 + (q % 2)
                P.op(PE, lambda e: e.matmul(
                    psb(bk)[0:65, :], esel[:, h * 65:(h + 1) * 65], FsT[:, q * 512:(q + 1) * 512], start=True, stop=False),
                    reads=["FsT", "cb"], writes=[("ps", bk)])
                for kc in range(8):
                    P.op(PE, lambda e, kc=kc: e.matmul(
                        psb(bk)[0:64, :], wfq[:, kc, h * 64:(h + 1) * 64], actT[:, kc, q * 512:(q + 1) * 512],
                        start=False, stop=False),
                        reads=["wfq"] + allact[q * 4:q * 4 + 4], writes=[("ps", bk)])
                P.op(PE, lambda e: e.matmul(
                    psb(bk)[0:65, :], esel[:, h * 65:(h + 1) * 65], FsT[:, q * 512:(q + 1) * 512], start=False, stop=True),
                    reads=["FsT", "cb"], writes=[("ps", bk)])
                return lambda: P.op(ACT, lambda e: e.copy(qT[0:65, q * 512:(q + 1) * 512], psb(bk)[0:65, :]),
                                    reads=[("ps", bk)], writes=[hn + "q"])

            def ck(q):
                bk = 6 + (q % 2)
                for kc in range(8):
                    P.op(PE, lambda e, kc=kc: e.matmul(
                        psb(bk)[0:64, :], wfk[:, kc, h * 64:(h + 1) * 64], actT[:, kc, q * 512:(q + 1) * 512],
                        start=(kc == 0), stop=(kc == 7)),
                        reads=["wfk"] + allact[q * 4:q * 4 + 4], writes=[("ps", bk)])
                return lambda: P.op(DVE, lambda e: e.tensor_copy(kT[0:64, q * 512:(q + 1) * 512], psb(bk)[0:64, :]),
                                    reads=[("ps", bk), ("fk", b)], writes=[hn + "k"])

            def cv(g8):
                bk = 6 + (g8 % 2)
                for i in range(8):
                    t = g8 * 8 + i
                    for kc in range(8):
                        P.op(PE, lambda e, t=t, i=i, kc=kc: e.matmul(
                            psb(bk)[:, i * 64:(i + 1) * 64], actT[:, kc, t * 128:(t + 1) * 128], wfv[:, kc, h * 64:(h + 1) * 64],
                            start=(kc == 0), stop=(kc == 7)),
                            reads=["wfv", ("actT", t)], writes=[("ps", bk)])
                return lambda: P.op(DVE, lambda e: e.tensor_copy(
                    vx[:, g8 * 8:(g8 + 1) * 8, 0:64], psb(bk).rearrange("p (t d) -> p t d", d=64)),
                    reads=[("ps", bk), ("fv", b)], writes=[hn + "v"])

            for q in range(NQ):
                out.append(lambda q=q: cq(q))
            for q in range(NQ):
                out.append(lambda q=q: ck(q))
            for g8 in range(4):
                out.append(lambda g8=g8: cv(g8))
            return out

        for c in fox_chunks(0):
            c()()
        for h in range(NH):
            b = h % 2
            nxt = fox_chunks(h + 1) if h + 1 < NH else []
            attention_head(P, h, 65, qTs[b], kTs[b], vxs[b], FOX_SCALE, lambda J, h=h: Gc[:, J, h:h + 1], mask_fox,
                           ob_s, (pt, rrow, rbc, ost), "f%d" % b, interleave=nxt)
        if "ob" in debug:
            add_dbg(P, "ob", ob_s, [512, S], BF16, ["odram"])
    P.emit("p3")
    st_f.close()
    if stop_after <= 3:
        return nc, dbg_outs

    st_a = contextlib.ExitStack()
    cqnT = st_a.enter_context(nc.sbuf_tensor("cqnT", [128, 3, S], BF16))
    krT = st_a.enter_context(nc.sbuf_tensor("krT", [96, S], BF16))
    cosT = st_a.enter_context(nc.sbuf_tensor("cosT", [96, S], BF16))
    sinT = st_a.enter_context(nc.sbuf_tensor("sinT", [96, S], BF16))
    R = slice(64, 96)
    allc = [("cqnT", t) for t in range(NT)]
    P = Prog(nc)
    with contextlib.ExitStack() as st:
        wlat = st.enter_context(nc.sbuf_tensor("wlat", [128, 8, 384], BF16))
        wkr = st.enter_context(nc.sbuf_tensor("wkr", [128, 8, 192], BF16))
        gcq = st.enter_context(nc.sbuf_tensor("gcq", [128, 384], F32))
        lat = [st.enter_context(nc.sbuf_tensor("lat%d" % i, [128, 384], BF16)) for i in range(2)]
        lsq = st.enter_context(nc.sbuf_tensor("lsq", [128, 384], BF16))
        lst = st.enter_context(nc.sbuf_tensor("lst", [128, NT, 4], F32))
        posi = st.enter_context(nc.sbuf_tensor("posi", [96, 512], I32))
        ang = st.enter_context(nc.sbuf_tensor("ang", [96, 512], F32))
        uu = st.enter_context(nc.sbuf_tensor("uu", [96, 512], F32))
        ki = st.enter_context(nc.sbuf_tensor("ki", [96, 512], I32))
        kf = st.enter_context(nc.sbuf_tensor("kf", [96, 512], F32))
        rr = st.enter_context(nc.sbuf_tensor("rr", [96, 512], F32))
        hs_ = st.enter_context(nc.sbuf_tensor("hs_", [96, 512], F32))
        t1 = st.enter_context(nc.sbuf_tensor("t1", [96, 512], F32))
        t2 = st.enter_context(nc.sbuf_tensor("t2", [96, 512], F32))
        wload(P, wlat[:], kc_view(w_lat_d), "wlat")
        wload(P, wkr[:], kc_view(w_kr_d), "wkr")
        P.op(SP, lambda e: e.dma_start(out=gcq[:, 0:256], in_=g_cq_d[0].partition_broadcast(128)), writes=["gcq"], dma=True)
        P.op(SP, lambda e: e.dma_start(out=gcq[:, 256:384], in_=g_ckv_d[0].partition_broadcast(128)), writes=["gcq"], dma=True)
        for q in range(NQ):
            tk = slice(q * 512, (q + 1) * 512)
            P.op(SP, lambda e, tk=tk: e.dma_start(out=posi[R, :], in_=pos_d[0, tk].partition_broadcast(32)),
                 writes=["posi"], dma=True)
            P.op(DVE, lambda e: e.tensor_copy(ang[R, :], posi[R, :]), reads=["posi"], writes=["ang"])
            P.op(DVE, lambda e: e.tensor_scalar(ang[R, :], ang[R, :], freq_abs[R, :], None, ALU.mult),
                 reads=["ang", "cf"], writes=["ang"])
            P.op(DVE, lambda e: e.tensor_scalar(uu[R, :], ang[R, :], 1.0 / (2 * np.pi), None, ALU.mult),
                 reads=["ang"], writes=["uu"])
            P.op(DVE, lambda e: e.tensor_copy(ki[R, :], uu[R, :]), reads=["uu"], writes=["ki"])
            P.op(DVE, lambda e: e.tensor_copy(kf[R, :], ki[R, :]), reads=["ki"], writes=["kf"])
            P.op(DVE, lambda e: e.scalar_tensor_tensor(rr[R, :], kf[R, :], -TWO_PI_HI, ang[R, :], ALU.mult, ALU.add),
                 reads=["kf", "ang"], writes=["rr"])
            P.op(DVE, lambda e: e.scalar_tensor_tensor(rr[R, :], kf[R, :], -TWO_PI_LO, rr[R, :], ALU.mult, ALU.add),
                 reads=["kf", "rr"], writes=["rr"])
            P.op(DVE, lambda e: e.tensor_scalar(rr[R, :], rr[R, :], 3.1415925, -3.1415925, ALU.min, ALU.max),
                 reads=["rr"], writes=["rr"])
            P.op(ACT, lambda e, tk=tk: e.activation(sinT[R, tk], rr[R, :], AF.Sin, scale=cf[R, 386:387]),
                 reads=["rr", "cf"], writes=["sinT"])
            P.op(ACT, lambda e: e.activation(hs_[R, :], rr[R, :], AF.Sin, scale=0.5), reads=["rr"], writes=["hs_"])
            P.op(DVE, lambda e: e.tensor_tensor(hs_[R, :], hs_[R, :], hs_[R, :], ALU.mult), reads=["hs_"], writes=["hs_"])
            P.op(DVE, lambda e, tk=tk: e.tensor_scalar(cosT[R, tk], hs_[R, :], -2.0, 1.0, ALU.mult, ALU.add),
                 reads=["hs_"], writes=["cosT"])
        P.op(DVE, lambda e: e.memset(lst[:], 0.0), writes=["lst"])
        for t in range(NT):
            bk = 6 + (t % 2)
            lb = lat[t % 2]
            for kc in range(8):
                P.op(PE, lambda e, t=t, kc=kc, bk=bk: e.matmul(
                    psb(bk)[:, 0:384], actT[:, kc, t * 128:(t + 1) * 128], wlat[:, kc, :], start=(kc == 0), stop=(kc == 7)),
                    reads=["wlat", ("actT", t)], writes=[("ps", bk)])
            P.op(ACT, lambda e, t=t, bk=bk: e.activation(lsq[:, 0:256], psb(bk)[:, 0:256], AF.Square, accum_out=lst[:, t, 0:1]),
                 reads=[("ps", bk), "lst"], writes=["lsq", ("lst", t)])
            P.op(ACT, lambda e, t=t, bk=bk: e.activation(lsq[:, 256:384], psb(bk)[:, 256:384], AF.Square, accum_out=lst[:, t, 1:2]),
                 reads=[("ps", bk), ("lst", t)], writes=["lsq", ("lst", t)])
            P.op(ACT, lambda e, t=t: e.activation(lst[:, t, 2:3], lst[:, t, 0:1], AF.Sqrt, bias=EPS, scale=1.0 / 256),
                 reads=[("lst", t)], writes=[("lst2", t)])
            P.op(ACT, lambda e, t=t: e.activation(lst[:, t, 3:4], lst[:, t, 1:2], AF.Sqrt, bias=EPS, scale=1.0 / 128),
                 reads=[("lst", t)], writes=[("lst3", t)])
            P.op(DVE, lambda e, t=t: e.reciprocal(lst[:, t, 2:4], lst[:, t, 2:4]),
                 reads=[("lst2", t), ("lst3", t)], writes=[("lst2", t), ("lst3", t)])
            P.op(DVE, lambda e, t=t, bk=bk, lb=lb: e.scalar_tensor_tensor(
                lb[:, 0:256], psb(bk)[:, 0:256], lst[:, t, 2:3], gcq[:, 0:256], ALU.mult, ALU.mult),
                reads=[("ps", bk), ("lst2", t), "gcq"], writes=[("lat", t % 2)])
            P.op(DVE, lambda e, t=t, bk=bk, lb=lb: e.scalar_tensor_tensor(
                lb[:, 256:384], psb(bk)[:, 256:384], lst[:, t, 3:4], gcq[:, 256:384], ALU.mult, ALU.mult),
                reads=[("ps", bk), ("lst3", t), "gcq"], writes=[("lat", t % 2)])
            tb = 4 + (t % 2)
            for c in range(3):
                P.op(PE, lambda e, lb=lb, c=c, tb=tb: e.transpose(
                    psb16(tb)[:, c * 128:(c + 1) * 128], lb[:, c * 128:(c + 1) * 128], identb),
                    reads=[("lat", t % 2), "cb"], writes=[("ps", tb)])
            P.op(ACT, lambda e, t=t, tb=tb: e.copy(
                cqnT[:, :, t * 128:(t + 1) * 128], psb16(tb)[:, 0:384].rearrange("p (a b) -> p a b", a=3)),
                reads=[("ps", tb)], writes=[("cqnT", t)])
        for q in range(NQ):
            tk = slice(q * 512, (q + 1) * 512)
            for v in range(2):
                for kc in range(8):
                    P.op(PE, lambda e, q=q, v=v, kc=kc: e.matmul(
                        psb(6 + v)[0:96, :], wkr[:, kc, v * 96:(v + 1) * 96], actT[:, kc, q * 512:(q + 1) * 512],
                        start=(kc == 0), stop=(kc == 7)),
                        reads=["wkr"] + [("actT", q * 4 + i) for i in range(4)], writes=[("ps", 6 + v)])
            P.op(DVE, lambda e, tk=tk: e.tensor_tensor(t1[R, :], psb(6)[R, :], cosT[R, tk], ALU.mult),
                 reads=[("ps", 6), "cosT"], writes=["t1"])
            P.op(DVE, lambda e, tk=tk: e.tensor_tensor(t2[R, :], psb(7)[R, :], sinT[R, tk], ALU.mult),
                 reads=[("ps", 7), "sinT"], writes=["t2"])
            P.op(POOL, lambda e, tk=tk: e.tensor_tensor(krT[R, tk], t1[R, :], t2[R, :], ALU.add),
                 reads=["t1", "t2"], writes=["krT"])
        if "lat" in debug:
            add_dbg(P, "cqnT", cqnT[:, :, 0:512], [128, 3, 512], BF16, allc)
            add_dbg(P, "krT", krT[R, 0:512], [32, 512], BF16, ["krT"])
    P.emit("p4a")

    P = Prog(nc)
    with contextlib.ExitStack() as st:
        wuq = st.enter_context(nc.sbuf_tensor("wuq", [128, 2, 768], BF16))
        wuqs = st.enter_context(nc.sbuf_tensor("wuqs", [128, 2, 768], BF16))
        wuk = st.enter_context(nc.sbuf_tensor("wuk", [128, 512], BF16))
        wuv = st.enter_context(nc.sbuf_tensor("wuv", [128, 512], BF16))
        qTs = [st.enter_context(nc.sbuf_tensor("aqT%d" % i, [96, S], BF16)) for i in range(2)]
        kTs = [st.enter_context(nc.sbuf_tensor("akT%d" % i, [96, S], BF16)) for i in range(2)]
        vxs = [st.enter_context(nc.sbuf_tensor("avx%d" % i, [128, NT, 65], BF16)) for i in range(2)]
        pt = [st.enter_context(nc.sbuf_tensor("apt%d" % i, [128, 512], BF16)) for i in range(3)]
        rrow = st.enter_context(nc.sbuf_tensor("arrow", [65, 512], F32))
        rbc = st.enter_context(nc.sbuf_tensor("arbc", [64, 512], F32))
        ost = [st.enter_context(nc.sbuf_tensor("aost%d" % i, [64, 512], BF16)) for i in range(2)]
        t1 = st.enter_context(nc.sbuf_tensor("t1b", [96, 512], F32))
        t2 = st.enter_context(nc.sbuf_tensor("t2b", [96, 512], F32))
        wload(P, wuq[:], kc_view(w_uq_d), "wuq")
        wload(P, wuqs[:], kc_view(w_uqs_d), "wuqs")
        wload(P, wuk[:], w_uk_d, "wuk")
        wload(P, wuv[:], w_uv_d, "wuv")
        for i in range(2):
            P.op(POOL, lambda e, i=i: e.memset(vxs[i][:, :, 64:65], 1.0), writes=[("av", i)])
        def mla_chunks(h):
            b = h % 2
            qT, kT, vx = qTs[b], kTs[b], vxs[b]
            hn = "a%d" % b
            out = []

            def cq(q):
                tk = slice(q * 512, (q + 1) * 512)
                for v, w_ in enumerate((wuq, wuqs)):
                    for kc in range(2):
                        P.op(PE, lambda e, kc=kc, v=v, w_=w_: e.matmul(
                            psb(6 + v)[0:96, :], w_[:, kc, h * 96:(h + 1) * 96], cqnT[:, kc, q * 512:(q + 1) * 512],
                            start=(kc == 0), stop=(kc == 1)),
                            reads=["wuq", "wuqs"] + allc[q * 4:q * 4 + 4], writes=[("ps", 6 + v)])
                def ev():
                    P.op(ACT, lambda e: e.copy(qT[0:64, tk], psb(6)[0:64, :]),
                         reads=[("ps", 6)], writes=[hn + "q"])
                    P.op(DVE, lambda e: e.tensor_tensor(t1[R, :], psb(6)[R, :], cosT[R, tk], ALU.mult),
                         reads=[("ps", 6), "cosT"], writes=["t1"])
                    P.op(DVE, lambda e: e.tensor_tensor(t2[R, :], psb(7)[R, :], sinT[R, tk], ALU.mult),
                         reads=[("ps", 7), "sinT"], writes=["t2"])
                    P.op(POOL, lambda e: e.tensor_tensor(qT[R, tk], t1[R, :], t2[R, :], ALU.add),
                         reads=["t1", "t2"], writes=[hn + "q"])
                return ev

            def ck(q):
                tk = slice(q * 512, (q + 1) * 512)
                bk = 6 + (q % 2)
                P.op(PE, lambda e: e.matmul(
                    psb(bk)[0:64, :], wuk[:, h * 64:(h + 1) * 64], cqnT[:, 2, q * 512:(q + 1) * 512], start=True, stop=True),
                    reads=["wuk"] + allc[q * 4:q * 4 + 4], writes=[("ps", bk)])
                return lambda: P.op(ACT, lambda e: e.copy(kT[0:64, tk], psb(bk)[0:64, :]),
                                    reads=[("ps", bk)], writes=[hn + "k"])

            def ck2(q):
                e0 = ck(q)
                e1 = ck(q + 1)
                return lambda: (e0(), e1())

            def ckr():
                P.op(POOL, lambda e: e.tensor_copy(kT[R, :], krT[R, :]), reads=["krT"], writes=[hn + "k"])
                return None

            def cv(g8):
                bk = 6 + (g8 % 2)
                for i in range(8):
                    t = g8 * 8 + i
                    P.op(PE, lambda e, t=t, i=i: e.matmul(
                        psb(bk)[:, i * 64:(i + 1) * 64], cqnT[:, 2, t * 128:(t + 1) * 128], wuv[:, h * 64:(h + 1) * 64],
                        start=True, stop=True),
                        reads=["wuv", ("cqnT", t)], writes=[("ps", bk)])
                return lambda: P.op(DVE, lambda e: e.tensor_copy(
                    vx[:, g8 * 8:(g8 + 1) * 8, 0:64], psb(bk).rearrange("p (t d) -> p t d", d=64)),
                    reads=[("ps", bk), ("av", b)], writes=[hn + "v"])

            for q in range(NQ):
                out.append(lambda q=q: cq(q))
            for q in range(0, NQ, 2):
                out.append(lambda q=q: ck2(q))
            out.append(ckr)
            for g8 in range(4):
                out.append(lambda g8=g8: cv(g8))
            return out

        for c in mla_chunks(0):
            ev = c()
            if ev:
                ev()
        for h in range(NH):
            b = h % 2
            nxt = mla_chunks(h + 1) if h + 1 < NH else []
            attention_head(P, h, 96, qTs[b], kTs[b], vxs[b], MLA_SCALE, lambda J: 0.0, mask_mla,
                           oa_s, (pt, rrow, rbc, ost), "a%d" % b, interleave=nxt)
        if "oa" in debug:
            add_dbg(P, "oa", oa_s, [512, S], BF16, ["odram"])
    P.emit("p4")
    st_a.close()
    if stop_after <= 4:
        return nc, dbg_outs

    P = Prog(nc)
    with contextlib.ExitStack() as st:
        woa = st.enter_context(nc.sbuf_tensor("woa", [128, 4, D], BF16))
        wob = st.enter_context(nc.sbuf_tensor("wob", [128, 4, D], BF16))
        wout = st.enter_context(nc.sbuf_tensor("wout", [128, 8, D], BF16))
        wr = st.enter_context(nc.sbuf_tensor("wr", [128, 8, 36], BF16))
        gffn = st.enter_context(nc.sbuf_tensor("gffn", [128, D], F32))
        brb = st.enter_context(nc.sbuf_tensor("brb", [128, 36], F32))
        gat = [[st.enter_context(nc.sbuf_tensor("gat%d_%d" % (b_, i), [128, 8, 512], BF16)) for i in range(2)] for b_ in range(2)]
        oin = [[st.enter_context(nc.sbuf_tensor("oin%d_%d" % (b_, i), [128, 4, 512], BF16)) for i in range(2)] for b_ in range(2)]
        mixT = [st.enter_context(nc.sbuf_tensor("mixT%d" % i, [128, 8, 512], BF16)) for i in range(2)]
        m1 = [st.enter_context(nc.sbuf_tensor("m1_0", [128, 512], F32))] * 2
        m2 = [st.enter_context(nc.sbuf_tensor("m2_0", [128, 512], F32))] * 2
        xh = [st.enter_context(nc.sbuf_tensor("xh%d" % i, [128, D], F32)) for i in range(3)]
        hsb = [st.enter_context(nc.sbuf_tensor("hsb%d" % i, [128, D], BF16)) for i in range(2)]
        sq5 = st.enter_context(nc.sbuf_tensor("sq5", [128, D], BF16))
        st5 = st.enter_context(nc.sbuf_tensor("st5", [128, NT, 2], F32))
        Lr = st.enter_context(nc.sbuf_tensor("Lr", [128, NT, 36], F32))
        wload(P, woa[:], w_oa_d.rearrange("(c p) n -> p c n", p=128), "woa")
        wload(P, wob[:], w_ob_d.rearrange("(c p) n -> p c n", p=128), "wob")
        wload(P, wout[:], kc_view(w_out_d), "wout")
        wload(P, wr[:], kc_view(w_r_d), "wr")
        P.op(SP, lambda e: e.dma_start(out=gffn[:], in_=g_ffn_d[0].partition_broadcast(128)), writes=["gffn"], dma=True)
        P.op(SP, lambda e: e.dma_start(out=brb[:], in_=br_d[0].partition_broadcast(128)), writes=["brb"], dma=True)
        P.op(DVE, lambda e: e.memset(st5[:], 0.0), writes=["st5"])
        xi = [0]

        def merge_loads(q):
            tk = slice(q * 512, (q + 1) * 512)
            b_ = q % 2
            P.op(SP, lambda e: e.dma_start(out=gat[b_][0][:], in_=ga_s[:, tk].rearrange("(c p) t -> p c t", p=128)),
                 writes=[("gat", b_, 0)], dma=True)
            P.op(SP, lambda e: e.dma_start(out=oin[b_][0][:], in_=oa_s[:, tk].rearrange("(c p) t -> p c t", p=128)),
                 writes=[("oin", b_, 0)], dma=True)
            P.op(SP, lambda e: e.dma_start(out=gat[b_][1][:], in_=gb_s[:, tk].rearrange("(c p) t -> p c t", p=128)),
                 writes=[("gat", b_, 1)], dma=True)
            P.op(SP, lambda e: e.dma_start(out=oin[b_][1][:], in_=ob_s[:, tk].rearrange("(c p) t -> p c t", p=128)),
                 writes=[("oin", b_, 1)], dma=True)

        def merge_dc(q, dc):
            b_ = q % 2
            mb = 0
            for v, w_ in enumerate((woa, wob)):
                for pc in range(4):
                    P.op(PE, lambda e, v=v, w_=w_, pc=pc: e.matmul(
                        psb(v), w_[:, pc, dc * 128:(dc + 1) * 128], oin[b_][v][:, pc, :], start=(pc == 0), stop=(pc == 3)),
                        reads=["woa", "wob", ("oin", b_, v)], writes=[("ps", v)])
            P.op(DVE, lambda e: e.tensor_tensor(m1[mb][:], psb(0), gat[b_][0][:, dc, :], ALU.mult),
                 reads=[("ps", 0), ("gat", b_, 0)], writes=[("m1", mb)])
            P.op(DVE, lambda e: e.tensor_tensor(m2[mb][:], psb(1), gat[b_][1][:, dc, :], ALU.mult),
                 reads=[("ps", 1), ("gat", b_, 1)], writes=[("m2", mb)])
            P.op(POOL, lambda e: e.tensor_tensor(mixT[b_][:, dc, :], m1[mb][:], m2[mb][:], ALU.add),
                 reads=[("m1", mb), ("m2", mb)], writes=[("mixT", b_, dc)])

        merge_loads(0)
        for dc in range(8):
            merge_dc(0, dc)
        for q in range(NQ):
            if q + 1 < NQ:
                merge_loads(q + 1)
            for sub in range(4):
                t = q * 4 + sub
                xb = xh[xi[0] % 3]
                xr = ("xh", xi[0] % 3)
                xi[0] += 1
                P.op(SP, lambda e, xb=xb, t=t: e.dma_start(out=xb[:], in_=x_d[t * 128:(t + 1) * 128, :]),
                     writes=[xr], dma=True)
                for ch in range(2):
                    bk = 2 + ch
                    for kc in range(8):
                        P.op(PE, lambda e, sub=sub, ch=ch, kc=kc, bk=bk, q=q: e.matmul(
                            psb(bk), mixT[q % 2][:, kc, sub * 128:(sub + 1) * 128], wout[:, kc, ch * 512:(ch + 1) * 512],
                            start=(kc == 0), stop=(kc == 7)),
                            reads=["wout"] + [("mixT", q % 2, d_) for d_ in range(8)], writes=[("ps", bk)])
                    P.op(DVE, lambda e, xb=xb, ch=ch, bk=bk: e.tensor_tensor(
                        xb[:, ch * 512:(ch + 1) * 512], psb(bk), xb[:, ch * 512:(ch + 1) * 512], ALU.add),
                        reads=[("ps", bk), xr], writes=[xr])
                P.op(SP, lambda e, xb=xb, t=t: e.dma_start(out=h_s[t * 128:(t + 1) * 128, :], in_=xb[:]),
                     reads=[xr], dma=True)
                if q + 1 < NQ:
                    merge_dc(q + 1, 2 * sub)
                P.op(ACT, lambda e, xb=xb, t=t: e.activation(sq5[:], xb[:], AF.Square, accum_out=st5[:, t, 0:1]),
                     reads=[xr, "st5"], writes=["sq5", ("st5", t)])
                P.op(ACT, lambda e, t=t: e.activation(st5[:, t, 1:2], st5[:, t, 0:1], AF.Sqrt, bias=EPS, scale=1.0 / D),
                     reads=[("st5", t)], writes=[("st51", t)])
                P.op(DVE, lambda e, t=t: e.reciprocal(st5[:, t, 1:2], st5[:, t, 1:2]),
                     reads=[("st51", t)], writes=[("st51", t)])
                hb = hsb[t % 2]
                P.op(DVE, lambda e, xb=xb, hb=hb, t=t: e.scalar_tensor_tensor(
                    hb[:], xb[:], st5[:, t, 1:2], gffn[:], ALU.mult, ALU.mult),
                    reads=[xr, ("st51", t), "gffn"], writes=[("hsb", t % 2)])
                tb = 4 + (t % 2)
                for kc in range(8):
                    P.op(PE, lambda e, hb=hb, kc=kc, tb=tb: e.transpose(
                        psb16(tb)[:, kc * 128:(kc + 1) * 128], hb[:, kc * 128:(kc + 1) * 128], identb),
                        reads=[("hsb", t % 2), "cb"], writes=[("ps", tb)])
                P.op(ACT, lambda e, t=t, tb=tb: e.copy(
                    actT[:, :, t * 128:(t + 1) * 128], psb16(tb).rearrange("p (a b) -> p a b", a=8)),
                    reads=[("ps", tb)], writes=[("actT", t)])
                if q + 1 < NQ:
                    merge_dc(q + 1, 2 * sub + 1)
                for kc in range(8):
                    P.op(PE, lambda e, t=t, kc=kc: e.matmul(
                        psb(6)[:, 0:36], actT[:, kc, t * 128:(t + 1) * 128], wr[:, kc, :], start=(kc == 0), stop=(kc == 7)),
                        reads=[("actT", t), "wr"], writes=[("ps", 6)])
                P.op(DVE, lambda e, t=t: e.tensor_tensor(Lr[:, t, :], psb(6)[:, 0:36], brb[:], ALU.add),
                     reads=[("ps", 6), "brb"], writes=[("Lr", t)])

        allL = [("Lr", t) for t in range(NT)]
        scr = mixT[0][:].rearrange("p a b -> p (a b)").bitcast(F32)

        class V:
            def __init__(self, ap):
                self.ap = ap

            def __getitem__(self, idx):
                return self.ap if (isinstance(idx, slice) and idx == slice(None)) else self.ap[idx]

        def v3(o, k):
            return V(scr[:, o:o + NT * k].rearrange("p (t k) -> p t k", k=k))

        def v2(o):
            return V(scr[:, o:o + NT])

        sel, sel2, tmp8, is1, is2 = v3(0, 8), v3(256, 8), v3(512, 8), v3(768, 8), v3(1024, 8)
        gd, grp = v3(1280, 4), v3(1408, 4)
        gmax, gp, mx1, mx2, e2, w1, w2 = (v2(1536 + 32 * i) for i in range(7))
        mixall = [("mixT", 0, d_) for d_ in range(8)]

        def b3(ap2, k):
            return ap2.unsqueeze(2).broadcast_to([128, NT, k])

        def dv(fn, reads, writes):
            P.op(DVE, fn, reads=reads, writes=writes)

        Lg = Lr[:, :, 0:4]
        dv(lambda e: e.reduce_max(gmax[:], Lg, axis=AX.X), allL, ["gmax"] + mixall)
        dv(lambda e: e.tensor_tensor(gd[:], Lg, b3(gmax[:], 4), ALU.subtract), allL + ["gmax"], ["gd"])
        P.op(ACT, lambda e: e.activation(gd[:], gd[:], AF.Exp), reads=["gd"], writes=["gd"])
        dv(lambda e: e.reduce_sum(gp[:], gd[:], axis=AX.X), ["gd"], ["gp"])
        dv(lambda e: e.reciprocal(gp[:], gp[:]), ["gp"], ["gp"])
        dv(lambda e: e.tensor_tensor(grp[:], Lg, b3(gmax[:], 4), ALU.is_equal), allL + ["gmax"], ["grp"])
        dv(lambda e: e.tensor_tensor(sel[:], Lr[:, :, 4:12], grp[:, :, 0:1].broadcast_to([128, NT, 8]), ALU.mult),
           allL + ["grp"], ["sel"])
        for g in range(1, 4):
            dv(lambda e, g=g: e.tensor_tensor(tmp8[:], Lr[:, :, 4 + 8 * g:12 + 8 * g],
                                              grp[:, :, g:g + 1].broadcast_to([128, NT, 8]), ALU.mult),
               allL + ["grp", "sel"], ["tmp8"])
            dv(lambda e: e.tensor_tensor(sel[:], sel[:], tmp8[:], ALU.add), ["sel", "tmp8"], ["sel"])
        dv(lambda e: e.reduce_max(mx1[:], sel[:], axis=AX.X), ["sel"], ["mx1"])
        dv(lambda e: e.tensor_tensor(is1[:], sel[:], b3(mx1[:], 8), ALU.is_equal), ["sel", "mx1"], ["is1"])
        dv(lambda e: e.scalar_tensor_tensor(sel2[:], is1[:], -1e30, sel[:], ALU.mult, ALU.add), ["is1", "sel"], ["sel2"])
        dv(lambda e: e.reduce_max(mx2[:], sel2[:], axis=AX.X), ["sel2"], ["mx2"])
        dv(lambda e: e.tensor_tensor(is2[:], sel2[:], b3(mx2[:], 8), ALU.is_equal), ["sel2", "mx2"], ["is2"])
        dv(lambda e: e.tensor_tensor(e2[:], mx2[:], mx1[:], ALU.subtract), ["mx1", "mx2"], ["e2"])
        P.op(ACT, lambda e: e.activation(e2[:], e2[:], AF.Exp), reads=["e2"], writes=["e2"])
        dv(lambda e: e.tensor_scalar(w1[:], e2[:], 1.0, None, ALU.add), ["e2"], ["w1"])
        dv(lambda e: e.reciprocal(w1[:], w1[:]), ["w1"], ["w1"])
        dv(lambda e: e.tensor_tensor(w1[:], w1[:], gp[:], ALU.mult), ["w1", "gp"], ["w1"])
        dv(lambda e: e.tensor_tensor(w2[:], w1[:], e2[:], ALU.mult), ["w1", "e2"], ["w2"])
        dv(lambda e: e.tensor_tensor(is1[:], is1[:], b3(w1[:], 8), ALU.mult), ["is1", "w1", "sel2"], ["is1"])
        dv(lambda e: e.tensor_tensor(is2[:], is2[:], b3(w2[:], 8), ALU.mult), ["is2", "w2"], ["is2"])
        dv(lambda e: e.tensor_tensor(is1[:], is1[:], is2[:], ALU.add), ["is1", "is2"], ["is1"])
        for g in range(4):
            dv(lambda e, g=g: e.tensor_tensor(comb[:, :, 8 * g:8 * g + 8], is1[:],
                                              grp[:, :, g:g + 1].broadcast_to([128, NT, 8]), ALU.mult),
               ["is1", "grp"], [("comb", g)])
        if "p5" in debug:
            add_dbg(P, "comb", comb[:], [128, NT, NE], F32, [("comb", g) for g in range(4)])
            add_dbg(P, "hnT", actT[:, :, 2048:2560], [128, 8, 512], BF16, [("actT", t) for t in range(16, 20)])
    P.emit("p5")
    if stop_after <= 5:
        return nc, dbg_outs

    P = Prog(nc)
    TT = 2048
    NS8 = TT // 128
    with contextlib.ExitStack() as st:
        gfin = st.enter_context(nc.sbuf_tensor("gfin", [128, D], F32))
        yacc = st.enter_context(nc.sbuf_tensor("yacc", [128, TT // 128, D], F32))
        wgu = [st.enter_context(nc.sbuf_tensor("wgu%d" % i, [128, 2, 8, EFF], BF16)) for i in range(2)]
        wdn = [st.enter_context(nc.sbuf_tensor("wdn%d" % i, [128, 2, D], BF16)) for i in range(2)]
        sg = [st.enter_context(nc.sbuf_tensor("sg%d" % i, [128, 512], F32)) for i in range(2)]
        aT = [st.enter_context(nc.sbuf_tensor("aT%d" % i, [128, 2, 512], BF16)) for i in range(2)]
        sq6 = st.enter_context(nc.sbuf_tensor("sq6", [128, D], BF16))
        st6 = st.enter_context(nc.sbuf_tensor("st6", [128, NT, 2], F32))
        P.op(SP, lambda e: e.dma_start(out=gfin[:], in_=g_fin_d[0].partition_broadcast(128)), writes=["gfin"], dma=True)
        P.op(DVE, lambda e: e.memset(st6[:], 0.0), writes=["st6"])
        wg_v = w_eg_d.rearrange("(e r) c -> e (r c)", e=NE).rearrange("e (kc p f) -> e p kc f", p=128, f=EFF)
        wu_v = w_eu_d.rearrange("(e r) c -> e (r c)", e=NE).rearrange("e (kc p f) -> e p kc f", p=128, f=EFF)
        wd_v = w_ed_d.rearrange("(e r) c -> e (r c)", e=NE).rearrange("e (c p n) -> e p c n", p=128, n=D)
        it = 0
        dn = 0
        for tt in range(S // TT):
            for s8 in range(NS8):
                t = tt * NS8 + s8
                P.op(SP, lambda e, s8=s8, t=t: e.dma_start(out=yacc[:, s8, :], in_=h_s[t * 128:(t + 1) * 128, :]),
                     writes=[("yacc", s8)], dma=True)
            if "y0" in debug and tt == 1:
                add_dbg(P, "y0", yacc[:, 0, :], [128, D], F32, [("yacc", 0)])
            def wloads(ex, wb):
                P.op(POOL, lambda e: e.dma_start(out=wgu[wb][:, 0, :, :], in_=wg_v[ex]), writes=[("wgu", wb)], dma=True)
                P.op(POOL, lambda e: e.dma_start(out=wgu[wb][:, 1, :, :], in_=wu_v[ex]), writes=[("wgu", wb)], dma=True)
                P.op(POOL, lambda e: e.dma_start(out=wdn[wb][:], in_=wd_v[ex]), writes=[("wdn", wb)], dma=True)

            def gu(ex, half, wb):
                tok0 = tt * TT + half * 512
                ab = aT[half % 2]
                for c in range(2):
                    gb_, ub_ = 2 * c, 2 * c + 1
                    for v, bk in ((0, gb_), (1, ub_)):
                        for kc in range(8):
                            P.op(PE, lambda e, v=v, kc=kc, c=c, bk=bk: e.matmul(
                                psb(bk), wgu[wb][:, v, kc, c * 128:(c + 1) * 128], actT[:, kc, tok0:tok0 + 512],
                                start=(kc == 0), stop=(kc == 7)),
                                reads=[("wgu", wb)], writes=[("ps", bk)])
                    sgb = sg[c]
                    P.op(ACT, lambda e, sgb=sgb, gb_=gb_: e.activation(sgb[:], psb(gb_), AF.Silu),
                         reads=[("ps", gb_)], writes=[("sg", c)])
                    P.op(DVE, lambda e, c=c, sgb=sgb, ub_=ub_: e.tensor_tensor(ab[:, c, :], psb(ub_), sgb[:], ALU.mult),
                         reads=[("ps", ub_), ("sg", c)], writes=[("aT", half % 2, c)])

            def dnp(ex, half, wb):
                nonlocal dn
                ab = aT[half % 2]
                for sub in range(4):
                    s8 = half * 4 + sub
                    t = tt * NS8 + s8
                    for ch in range(2):
                        bk = 4 + (dn % 4)
                        dn += 1
                        for c in range(2):
                            P.op(PE, lambda e, c=c, sub=sub, ch=ch, bk=bk: e.matmul(
                                psb(bk), ab[:, c, sub * 128:(sub + 1) * 128], wdn[wb][:, c, ch * 512:(ch + 1) * 512],
                                start=(c == 0), stop=(c == 1)),
                                reads=[("aT", half % 2, 0), ("aT", half % 2, 1), ("wdn", wb)], writes=[("ps", bk)])
                        P.op(DVE, lambda e, s8=s8, ch=ch, bk=bk, t=t: e.scalar_tensor_tensor(
                            yacc[:, s8, ch * 512:(ch + 1) * 512], psb(bk), comb[:, t, ex:ex + 1],
                            yacc[:, s8, ch * 512:(ch + 1) * 512], ALU.mult, ALU.add),
                            reads=[("ps", bk), ("yacc", s8)], writes=[("yacc", s8)])

            items = [(ex, half) for ex in range(NE) for half in range(TT // 512)]
            wbs = {}
            for ex in range(NE):
                wbs[ex] = it % 2
                it += 1

            def gu_item(i):
                ex, half = items[i]
                if half == 0:
                    wloads(ex, wbs[ex])
                gu(ex, half, wbs[ex])

            gu_item(0)
            for i in range(len(items)):
                if i + 1 < len(items):
                    gu_item(i + 1)
                ex, half = items[i]
                dnp(ex, half, wbs[ex])
            for s8 in range(NS8):
                t = tt * NS8 + s8
                P.op(ACT, lambda e, s8=s8, t=t: e.activation(sq6[:], yacc[:, s8, :], AF.Square, accum_out=st6[:, t, 0:1]),
                     reads=[("yacc", s8), "st6"], writes=["sq6", ("st6", t)])
                P.op(ACT, lambda e, t=t: e.activation(st6[:, t, 1:2], st6[:, t, 0:1], AF.Sqrt, bias=EPS, scale=1.0 / D),
                     reads=[("st6", t)], writes=[("st61", t)])
                P.op(DVE, lambda e, t=t: e.reciprocal(st6[:, t, 1:2], st6[:, t, 1:2]), reads=[("st61", t)], writes=[("st61", t)])
                P.op(DVE, lambda e, s8=s8, t=t: e.scalar_tensor_tensor(
                    yacc[:, s8, :], yacc[:, s8, :], st6[:, t, 1:2], gfin[:], ALU.mult, ALU.mult),
                    reads=[("yacc", s8), ("st61", t), "gfin"], writes=[("yacc", s8)])
                P.op(SP, lambda e, s8=s8, t=t: e.dma_start(out=out_d[t * 128:(t + 1) * 128, :], in_=yacc[:, s8, :]),
                     reads=[("yacc", s8)], dma=True)
    P.emit("p6")
    return nc, dbg_outs


def _consts():
    cfm = np.zeros((128, 512), np.float32)
    cfm[:, 0:128] = np.eye(128, dtype=np.float32)
    cfm[:, 128:256] = np.triu(np.ones((128, 128), np.float32))
    cfm[:, 256:384] = 1.0
    p = np.arange(128)
    j = p % 16
    freq = (10000.0 ** (-(j.astype(np.float32)) / np.float32(16.0))).astype(np.float32)
    sgn = np.where((p % 32) < 16, -1.0, 1.0).astype(np.float32)
    cfm[:, 384] = sgn * freq
    cfm[:, 385] = freq
    cfm[:, 386] = sgn
    cbm = np.zeros((128, 1024), np.float32)
    cbm[:, 0:128] = np.eye(128)
    k = np.arange(128)[:, None]
    q = np.arange(128)[None, :]
    cbm[:, 128:256] = np.where(k > q, NEG, 0.0)
    cbm[:, 256:384] = np.where((k // 64) > (q // 64), NEG, 0.0)
    for h in range(8):
        cbm[h, 384 + h * 65 + 64] = 1.0
    return cfm, cbm.astype(ml_dtypes.bfloat16)


def _prep_inputs(inp):
    f = lambda a: np.ascontiguousarray(np.asarray(a, dtype=np.float32))
    w_in = f(inp["w_in"])
    kr = w_in[:, 384:416]
    swap = np.concatenate([np.arange(16, 32), np.arange(0, 16)])
    w_kr2 = np.concatenate([kr, kr, kr, kr, kr, kr[:, swap]], axis=1)
    w_uq = f(inp["w_uq"])
    idx = np.arange(768).reshape(8, 96).copy()
    idx[:, 64:96] = idx[:, 64:96][:, swap]
    w_uq_sw = w_uq[:, idx.reshape(-1)]
    cfm, cbm = _consts()
    shared = {
        "g_mix": f(inp["g_mix"]).reshape(1, -1),
        "g_ffn": f(inp["g_ffn"]).reshape(1, -1),
        "g_final": f(inp["g_final"]).reshape(1, -1),
        "g_cq": f(inp["g_cq"]).reshape(1, -1),
        "g_ckv": f(inp["g_ckv"]).reshape(1, -1),
        "b_forget": f(inp["b_forget"]).reshape(1, -1),
        "b_r36": np.concatenate([f(inp["b_group"]), f(inp["b_router"])]).reshape(1, -1),
        "w_lat": np.ascontiguousarray(w_in[:, 0:384]),
        "w_kr2": np.ascontiguousarray(w_kr2),
        "w_fq": np.ascontiguousarray(w_in[:, 416:928]),
        "w_fk": np.ascontiguousarray(w_in[:, 928:1440]),
        "w_fv": np.ascontiguousarray(w_in[:, 1440:1952]),
        "w_f": np.ascontiguousarray(w_in[:, 1952:1960]),
        "w_ga": np.ascontiguousarray(w_in[:, 1960:2984]),
        "w_gb": np.ascontiguousarray(w_in[:, 2984:4008]),
        "w_uq": w_uq,
        "w_uq_sw": np.ascontiguousarray(w_uq_sw),
        "w_uk": f(inp["w_uk"]),
        "w_uv": f(inp["w_uv"]),
        "w_o_mla": f(inp["w_o_mla"]),
        "w_o_fox": f(inp["w_o_fox"]),
        "w_out": f(inp["w_out"]),
        "w_r36": np.ascontiguousarray(np.concatenate([f(inp["w_group"]), f(inp["w_router"])], axis=1)),
        "w_e_gate": f(inp["w_e_gate"]).reshape(-1, 2048),
        "w_e_up": f(inp["w_e_up"]).reshape(-1, 2048),
        "w_e_down": f(inp["w_e_down"]).reshape(-1, 2048),
        "consts_f": cfm,
        "consts_b": cbm,
    }
    x = f(inp["x"])
    pos = np.ascontiguousarray(np.asarray(inp["positions"], dtype=np.int32))
    in_maps = []
    for b in range(8):
        m = dict(shared)
        m["x"] = x[b]
        m["pos"] = pos[b].reshape(1, -1)
        in_maps.append(m)
    return in_maps


def kernel(**inputs):
    in_maps = _prep_inputs(inputs)
    nc, _ = build_program()
    res = run_bass_kernel_spmd(nc, in_maps, core_ids=list(range(8)))
    out = np.stack([np.asarray(r["out"], dtype=np.float32) for r in res.results], axis=0)
    return out
```

```python
import contextlib
import numpy as np
import ml_dtypes
import concourse.bass as bass
import concourse.mybir as mybir
from concourse.bass_utils import run_bass_kernel_spmd

F32 = mybir.dt.float32
BF16 = mybir.dt.bfloat16
I32 = mybir.dt.int32
ALU = mybir.AluOpType
AF = mybir.ActivationFunctionType
AX = mybir.AxisListType

PE, ACT, DVE, POOL, SP = "tensor", "scalar", "vector", "gpsimd", "sync"
ENGINES = [PE, ACT, DVE, POOL, SP]

S = 4096
D = 1024
NT = 32
NQ = 8
NH = 8
NE = 32
EFF = 256
EPS = 1e-6
MLA_SCALE = 96.0 ** -0.5
FOX_SCALE = 0.125
NEG = -30000.0
TWO_PI_HI = 6.28125
TWO_PI_LO = 6.283185307179586 - 6.28125


class Op:
    __slots__ = ("eng", "fn", "deps", "is_dma", "marked", "count", "sem", "semval", "prewait", "nosem")

    def __init__(self, eng, fn, is_dma):
        self.eng = eng
        self.fn = fn
        self.deps = []
        self.is_dma = is_dma
        self.marked = False
        self.count = 0
        self.sem = None
        self.semval = 0
        self.prewait = None
        self.nosem = False


class Prog:
    def __init__(self, nc, dma_pool=8):
        self.nc = nc
        self.ops = {e: [] for e in ENGINES}
        self.last_write = {}
        self.readers = {}
        self.dma_pool = dma_pool
        self.dma_count = {e: 0 for e in ENGINES}

    def op(self, eng, fn, reads=(), writes=(), dma=False, nosem=False):
        o = Op(eng, fn, dma)
        o.nosem = nosem
        deps = {}
        for r in reads:
            w = self.last_write.get(r)
            if w is not None:
                deps[id(w)] = (w, "raw")
        for w_ in writes:
            for rd in self.readers.get(w_, ()):
                if id(rd) not in deps:
                    deps[id(rd)] = (rd, "war")
            w = self.last_write.get(w_)
            if w is not None:
                deps[id(w)] = (w, "waw")
        for d, kind in deps.values():
            if d is o:
                continue
            if (not d.is_dma) and (not dma) and d.eng == eng:
                if eng == PE or kind == "war":
                    continue
            o.deps.append(d)
            d.marked = True
        for r in reads:
            self.readers.setdefault(r, []).append(o)
        for w_ in writes:
            self.last_write[w_] = o
            self.readers[w_] = []
        if dma and not nosem:
            k = self.dma_count[eng]
            self.dma_count[eng] = k + 1
            o.sem = (eng, k % self.dma_pool)
            o.semval = 16 * (k // self.dma_pool + 1)
            if k >= self.dma_pool:
                o.prewait = (o.sem, o.semval - 16)
        self.ops[eng].append(o)
        return o

    def emit(self, name):
        nc = self.nc
        for e in ENGINES:
            c = 0
            for o in self.ops[e]:
                if not o.is_dma and o.marked:
                    c += 1
                    o.count = c
        with contextlib.ExitStack() as st:
            esem = {e: st.enter_context(nc.semaphore("s_%s_%s" % (name, e))) for e in ENGINES}
            dsem = {}
            for e in ENGINES:
                for i in range(min(self.dma_pool, self.dma_count[e])):
                    dsem[(e, i)] = st.enter_context(nc.semaphore("d_%s_%s_%d" % (name, e, i)))
            allsems = list(esem.values()) + list(dsem.values())
            with nc.Block() as cblk:
                def _clr(engobj):
                    for s_ in allsems:
                        engobj.sem_clear(s_)
                cblk.sync(_clr)
            block = st.enter_context(nc.Block())

            def run_engine(e, engobj):
                seen = {}
                for o in self.ops[e]:
                    need = {}
                    for d in o.deps:
                        if d.is_dma:
                            key = ("d", d.sem)
                            val = d.semval
                        else:
                            key = ("e", d.eng)
                            val = d.count
                        if need.get(key, 0) < val:
                            need[key] = val
                    if o.prewait is not None:
                        key = ("d", o.prewait[0])
                        if need.get(key, 0) < o.prewait[1]:
                            need[key] = o.prewait[1]
                    for key, val in need.items():
                        if seen.get(key, 0) >= val:
                            continue
                        seen[key] = val
                        s = dsem[key[1]] if key[0] == "d" else esem[key[1]]
                        engobj.wait_ge(s, val)
                    ins = o.fn(engobj)
                    if o.nosem:
                        continue
                    if o.is_dma:
                        ins.then_inc(dsem[o.sem], 16)
                    elif o.marked:
                        ins.then_inc(esem[e], 1)
                k = self.dma_count[e]
                for i in range(min(self.dma_pool, k)):
                    uses = (k - 1 - i) // self.dma_pool + 1
                    if uses > 0:
                        engobj.wait_ge(dsem[(e, i)], 16 * uses)

            for e in ENGINES:
                if not self.ops[e]:
                    continue
                getattr(block, e)(lambda engobj, e=e: run_engine(e, engobj))


def build_program(stop_after=99, debug=None):
    nc = bass.Bass("TRN2", target_bir_lowering=False)
    debug = debug or []

    def din(name, shape, dt=F32):
        return nc.dram_tensor(name, list(shape), dt, kind="ExternalInput").ap()

    x_d = din("x", [S, D])
    pos_d = din("pos", [1, S], I32)
    g_mix_d = din("g_mix", [1, D])
    g_ffn_d = din("g_ffn", [1, D])
    g_fin_d = din("g_final", [1, D])
    g_cq_d = din("g_cq", [1, 256])
    g_ckv_d = din("g_ckv", [1, 128])
    bf_d = din("b_forget", [1, 8])
    br_d = din("b_r36", [1, 36])
    w_lat_d = din("w_lat", [D, 384])
    w_kr_d = din("w_kr2", [D, 192])
    w_fq_d = din("w_fq", [D, 512])
    w_fk_d = din("w_fk", [D, 512])
    w_fv_d = din("w_fv", [D, 512])
    w_f_d = din("w_f", [D, 8])
    w_ga_d = din("w_ga", [D, D])
    w_gb_d = din("w_gb", [D, D])
    w_uq_d = din("w_uq", [256, 768])
    w_uqs_d = din("w_uq_sw", [256, 768])
    w_uk_d = din("w_uk", [128, 512])
    w_uv_d = din("w_uv", [128, 512])
    w_oa_d = din("w_o_mla", [512, D])
    w_ob_d = din("w_o_fox", [512, D])
    w_out_d = din("w_out", [D, D])
    w_r_d = din("w_r36", [D, 36])
    w_eg_d = din("w_e_gate", [NE * D * EFF // 2048, 2048])
    w_eu_d = din("w_e_up", [NE * D * EFF // 2048, 2048])
    w_ed_d = din("w_e_down", [NE * EFF * D // 2048, 2048])
    cf_d = din("consts_f", [128, 512])
    cb_d = din("consts_b", [128, 1024], BF16)
    out_d = nc.dram_tensor("out", [S, D], F32, kind="ExternalOutput").ap()

    ga_s = nc.dram_tensor("ga_s", [D, S], BF16).ap()
    gb_s = nc.dram_tensor("gb_s", [D, S], BF16).ap()
    oa_s = nc.dram_tensor("oa_s", [512, S], BF16).ap()
    ob_s = nc.dram_tensor("ob_s", [512, S], BF16).ap()
    h_s = nc.dram_tensor("h_s", [S, D], F32).ap()

    dbg_outs = {}

    cf = nc.alloc_sbuf_tensor("cf", [128, 512], F32)
    cb = nc.alloc_sbuf_tensor("cb", [128, 1024], BF16)
    actT = nc.alloc_sbuf_tensor("actT", [128, 8, S], BF16)
    comb = nc.alloc_sbuf_tensor("comb", [128, NT, NE], F32)
    ps = nc.alloc_psum_tensor("ps", [128, 8, 512], F32)

    identf = cf[:, 0:128]
    utri = cf[:, 128:256]
    onesf = cf[:, 256:384]
    freq_col = cf[:, 384:385]
    freq_abs = cf[:, 385:386]
    identb = cb[:, 0:128]
    mask_fox = cb[:, 128:256]
    mask_mla = cb[:, 256:384]
    esel = cb[0:8, 384:384 + 8 * 65]


    def psb(b):
        return ps[:, b, :]

    def psb16(b):
        return ps[:, b, :].bitcast(BF16)

    def add_dbg(P, name, ap, shape, dt, reads):
        t = nc.dram_tensor("dbg_" + name, list(shape), dt, kind="ExternalOutput").ap()
        dbg_outs[name] = t
        P.op(SP, lambda e: e.dma_start(out=t, in_=ap), reads=reads, dma=True)

    def wload(P, dst, src_ap, res, eng=POOL):
        P.op(eng, lambda e: e.dma_start(out=dst, in_=src_ap), writes=[res], dma=True)

    def kc_view(w_ap):
        return w_ap.rearrange("(kc p) n -> p kc n", p=128)

    P = Prog(nc)
    wload(P, cf[:], cf_d, "cf", eng=SP)
    wload(P, cb[:], cb_d, "cb", eng=SP)
    with contextlib.ExitStack() as st:
        gmix = st.enter_context(nc.sbuf_tensor("gmix", [128, D], F32))
        xt = [st.enter_context(nc.sbuf_tensor("xt%d" % i, [128, D], F32)) for i in range(3)]
        xs = [st.enter_context(nc.sbuf_tensor("xs%d" % i, [128, D], BF16)) for i in range(2)]
        sq = st.enter_context(nc.sbuf_tensor("sq", [128, D], BF16))
        stat = st.enter_context(nc.sbuf_tensor("stat", [128, NT, 2], F32))
        P.op(SP, lambda e: e.dma_start(out=gmix[:], in_=g_mix_d[0].partition_broadcast(128)), writes=["gmix"], dma=True)
        P.op(DVE, lambda e: e.memset(stat[:], 0.0), writes=["stat"])

        def p1_copy(t):
            bk = t % 2
            P.op(ACT, lambda e: e.copy(
                actT[:, :, t * 128:(t + 1) * 128], psb16(bk).rearrange("p (a b) -> p a b", a=8)),
                reads=[("ps", bk)], writes=[("actT", t)])

        for t in range(NT):
            xb = xt[t % 3]
            xsb = xs[t % 2]
            bk = t % 2
            P.op(SP, lambda e, xb=xb, t=t: e.dma_start(out=xb[:], in_=x_d[t * 128:(t + 1) * 128, :]),
                 writes=[("xt", t % 3)], dma=True)
            P.op(ACT, lambda e, xb=xb, t=t: e.activation(sq[:], xb[:], AF.Square, accum_out=stat[:, t, 0:1]),
                 reads=[("xt", t % 3), "stat"], writes=["sq", ("stat", t)])
            P.op(ACT, lambda e, t=t: e.activation(stat[:, t, 1:2], stat[:, t, 0:1], AF.Sqrt, bias=EPS, scale=1.0 / D),
                 reads=[("stat", t)], writes=[("stat1", t)])
            if t > 0:
                p1_copy(t - 1)
            P.op(DVE, lambda e, t=t: e.reciprocal(stat[:, t, 1:2], stat[:, t, 1:2]),
                 reads=[("stat1", t)], writes=[("stat1", t)])
            P.op(DVE, lambda e, xb=xb, xsb=xsb, t=t: e.scalar_tensor_tensor(
                xsb[:], xb[:], stat[:, t, 1:2], gmix[:], ALU.mult, ALU.mult),
                reads=[("xt", t % 3), ("stat1", t), "gmix"], writes=[("xs", t % 2)])
            for kc in range(8):
                P.op(PE, lambda e, xsb=xsb, kc=kc, bk=bk: e.transpose(
                    psb16(bk)[:, kc * 128:(kc + 1) * 128], xsb[:, kc * 128:(kc + 1) * 128], identb),
                    reads=[("xs", t % 2), "cb"], writes=[("ps", bk)])
        p1_copy(NT - 1)
        if "xnT" in debug:
            add_dbg(P, "xnT", actT[:, :, 0:512], [128, 8, 512], BF16, [("actT", t) for t in range(4)])
    P.emit("p1")
    if stop_after <= 1:
        return nc, dbg_outs

    st_f = contextlib.ExitStack()
    Gc = st_f.enter_context(nc.sbuf_tensor("Gc", [128, NT, 8], F32))
    FsT = st_f.enter_context(nc.sbuf_tensor("FsT", [8, S], BF16))
    P = Prog(nc)
    with contextlib.ExitStack() as st:
        wf = st.enter_context(nc.sbuf_tensor("wf", [128, 8, 8], BF16))
        wga = st.enter_context(nc.sbuf_tensor("wga", [128, 8, D], BF16))
        wgb = st.enter_context(nc.sbuf_tensor("wgb", [128, 8, D], BF16))
        bfb = st.enter_context(nc.sbuf_tensor("bfb", [128, 8], F32))
        lf = st.enter_context(nc.sbuf_tensor("lf", [128, NT, 8], F32))
        tot = st.enter_context(nc.sbuf_tensor("tot", [128, NT, 8], F32))
        off = st.enter_context(nc.sbuf_tensor("off", [128, NT, 8], F32))
        gst = [st.enter_context(nc.sbuf_tensor("gst%d" % i, [128, 512], BF16)) for i in range(4)]
        wload(P, wf[:], kc_view(w_f_d), "wf")
        wload(P, wga[:], kc_view(w_ga_d), "wga")
        wload(P, wgb[:], kc_view(w_gb_d), "wgb")
        P.op(SP, lambda e: e.dma_start(out=bfb[:], in_=bf_d[0].partition_broadcast(128)), writes=["bfb"], dma=True)
        for t in range(NT):
            for kc in range(8):
                P.op(PE, lambda e, t=t, kc=kc: e.matmul(
                    psb(0)[:, t * 8:(t + 1) * 8], actT[:, kc, t * 128:(t + 1) * 128], wf[:, kc, :],
                    start=(kc == 0), stop=(kc == 7)),
                    reads=[("actT", t), "wf"], writes=[("ps", 0)])
        P.op(DVE, lambda e: e.tensor_tensor(
            lf[:], psb(0)[:, 0:256].rearrange("p (t h) -> p t h", h=8),
            bfb[:].unsqueeze(1).broadcast_to([128, NT, 8]), ALU.add),
            reads=[("ps", 0), "bfb"], writes=["lf"])
        P.op(ACT, lambda e: e.activation(lf[:], lf[:], AF.Exp, scale=-1.0), reads=["lf"], writes=["lf"])
        P.op(ACT, lambda e: e.activation(lf[:], lf[:], AF.Ln, bias=1.0), reads=["lf"], writes=["lf"])
        lf2 = lf[:].rearrange("p t h -> p (t h)")
        P.op(PE, lambda e: e.matmul(psb(1)[:, 0:256], utri, lf2, start=True, stop=True),
             reads=["lf", "cf"], writes=[("ps", 1)])
        P.op(PE, lambda e: e.matmul(psb(2)[:, 0:256], onesf, lf2, start=True, stop=True),
             reads=["lf", "cf"], writes=[("ps", 2)])
        P.op(DVE, lambda e: e.tensor_copy(tot[:], psb(2)[:, 0:256].rearrange("p (t h) -> p t h", h=8)),
             reads=[("ps", 2)], writes=["tot"])
        P.op(DVE, lambda e: e.memset(off[:, 0, :], 0.0), writes=["off"])
        for t in range(1, NT):
            P.op(DVE, lambda e, t=t: e.tensor_tensor(off[:, t, :], off[:, t - 1, :], tot[:, t - 1, :], ALU.add),
                 reads=["off", "tot"], writes=["off"])
        P.op(DVE, lambda e: e.tensor_tensor(
            Gc[:], psb(1)[:, 0:256].rearrange("p (t h) -> p t h", h=8), off[:], ALU.add),
            reads=[("ps", 1), "off"], writes=["Gc"])
        for g4 in range(8):
            bk = 3 + (g4 % 2)
            for i in range(4):
                t = g4 * 4 + i
                P.op(PE, lambda e, t=t, i=i, bk=bk: e.transpose(
                    psb(bk)[0:8, i * 128:(i + 1) * 128], Gc[:, t, :], identf),
                    reads=["Gc", "cf"], writes=[("ps", bk)])
            P.op(ACT, lambda e, g4=g4, bk=bk: e.mul(FsT[:, g4 * 512:(g4 + 1) * 512], psb(bk)[0:8, :], -0.5 / FOX_SCALE),
                 reads=[("ps", bk)], writes=["FsT"])
        n = 0
        for (wg_, dst) in ((wga, ga_s), (wgb, gb_s)):
            wname = "wga" if wg_ is wga else "wgb"
            for dc in range(8):
                for q in range(NQ):
                    bk = 5 + (n % 3)
                    sb = gst[n % 4]
                    for kc in range(8):
                        P.op(PE, lambda e, wg_=wg_, dc=dc, q=q, kc=kc, bk=bk: e.matmul(
                            psb(bk), wg_[:, kc, dc * 128:(dc + 1) * 128], actT[:, kc, q * 512:(q + 1) * 512],
                            start=(kc == 0), stop=(kc == 7)),
                            reads=[wname] + [("actT", q * 4 + i) for i in range(4)], writes=[("ps", bk)])
                    P.op(ACT, lambda e, sb=sb, bk=bk: e.activation(sb[:], psb(bk), AF.Sigmoid),
                         reads=[("ps", bk)], writes=[("gst", n % 4)])
                    P.op(SP, lambda e, sb=sb, dst=dst, dc=dc, q=q: e.dma_start(
                        out=dst[dc * 128:(dc + 1) * 128, q * 512:(q + 1) * 512], in_=sb[:]),
                        reads=[("gst", n % 4)], dma=True)
                    n += 1
        if "Gc" in debug:
            add_dbg(P, "Gc", Gc[:], [128, NT, 8], F32, ["Gc"])
            add_dbg(P, "FsT", FsT[:], [8, S], BF16, ["FsT"])
    P.emit("p2a")
    if stop_after <= 2:
        return nc, dbg_outs

    def attention_head(P, h, kdim, qT, kT, vx, scale, bias_fn, mask, o_dst, bufs, hname, interleave=()):
        pt, rrow, rbc, ost = bufs
        pairs = [(I, J) for I in range(NQ) for J in range(4 * I + 4)]
        npairs = len(pairs)

        def emit_qk(n):
            I, J = pairs[n]
            j = J - 4 * I
            c0 = max(0, j) * 128
            sbk = n % 3
            P.op(PE, lambda e: e.matmul(
                psb(sbk)[:, c0:512], kT[0:kdim, J * 128:(J + 1) * 128], qT[0:kdim, I * 512 + c0:(I + 1) * 512],
                start=True, stop=(j < 0)),
                reads=[hname + "q", hname + "k"], writes=[("ps", sbk)])
            if j >= 0:
                P.op(PE, lambda e: e.matmul(
                    psb(sbk)[:, c0:c0 + 128], identb, mask, start=False, stop=True),
                    reads=["cb"], writes=[("ps", sbk)])

        def emit_exp(n):
            I, J = pairs[n]
            j = J - 4 * I
            c0 = max(0, j) * 128
            sbk = n % 3
            ptb = pt[n % 3]
            bias = bias_fn(J)
            P.op(ACT, lambda e: e.activation(
                ptb[:, c0:512], psb(sbk)[:, c0:512], AF.Exp, bias=bias, scale=scale),
                reads=[("ps", sbk), "Gc"], writes=[("pt", n % 3)])

        def emit_pv(n):
            I, J = pairs[n]
            j = J - 4 * I
            c0 = max(0, j) * 128
            ob = 3 + (I % 2)
            nJ = 4 * I + 4
            ptb = pt[n % 3]
            P.op(PE, lambda e: e.matmul(
                psb(ob)[0:65, c0:512], vx[:, J, :], ptb[:, c0:512], start=(J == 0), stop=(J == nJ - 1)),
                reads=[("pt", n % 3), hname + "v"], writes=[("ps", ob)])

        def finalize(I):
            ob = 3 + (I % 2)
            P.op(DVE, lambda e: e.reciprocal(rrow[64:65, :], psb(ob)[64:65, :]),
                 reads=[("ps", ob)], writes=["rrow"])
            P.op(PE, lambda e: e.matmul(psb(5)[0:64, :], onesf[64:65, 0:64], rrow[64:65, :], start=True, stop=True),
                 reads=["rrow", "cf"], writes=[("ps", 5)])
            P.op(DVE, lambda e: e.tensor_copy(rbc[0:64, :], psb(5)[0:64, :]), reads=[("ps", 5)], writes=["rbc"])
            osb = ost[I % 2]
            P.op(DVE, lambda e: e.tensor_tensor(osb[0:64, :], psb(ob)[0:64, :], rbc[0:64, :], ALU.mult),
                 reads=[("ps", ob), "rbc"], writes=[("ost", I % 2)])
            P.op(SP, lambda e: e.dma_start(
                out=o_dst[h * 64:(h + 1) * 64, I * 512:(I + 1) * 512], in_=osb[0:64, :]),
                reads=[("ost", I % 2)], writes=["odram"], dma=True)

        pending = []
        chunks = list(interleave)
        evq = []
        step = max(1, (npairs - 8) // (len(chunks) + 1)) if chunks else 0
        emit_qk(0)
        emit_exp(0)
        emit_qk(1)
        emit_exp(1)
        for n in range(npairs):
            if n + 2 < npairs:
                emit_qk(n + 2)
                emit_exp(n + 2)
            emit_pv(n)
            I, J = pairs[n]
            if J == 4 * I + 3:
                pending.append((n + 3, I))
            while pending and pending[0][0] <= n:
                finalize(pending.pop(0)[1])
            while evq and evq[0][0] <= n:
                evq.pop(0)[1]()
            if chunks and n % step == step - 1:
                ev = chunks.pop(0)()
                if ev:
                    evq.append((n + 2, ev))
        for _, I in pending:
            finalize(I)
        for _, ev in evq:
            ev()
        for c in chunks:
            ev = c()
            if ev:
                ev()

    P = Prog(nc)
    with contextlib.ExitStack() as st:
        wfq = st.enter_context(nc.sbuf_tensor("wfq", [128, 8, 512], BF16))
        wfk = st.enter_context(nc.sbuf_tensor("wfk", [128, 8, 512], BF16))
        wfv = st.enter_context(nc.sbuf_tensor("wfv", [128, 8, 512], BF16))
        qTs = [st.enter_context(nc.sbuf_tensor("fqT%d" % i, [65, S], BF16)) for i in range(2)]
        kTs = [st.enter_context(nc.sbuf_tensor("fkT%d" % i, [65, S], BF16)) for i in range(2)]
        vxs = [st.enter_context(nc.sbuf_tensor("fvx%d" % i, [128, NT, 65], BF16)) for i in range(2)]
        pt = [st.enter_context(nc.sbuf_tensor("pt%d" % i, [128, 512], BF16)) for i in range(3)]
        rrow = st.enter_context(nc.sbuf_tensor("rrow", [65, 512], F32))
        rbc = st.enter_context(nc.sbuf_tensor("rbc", [64, 512], F32))
        ost = [st.enter_context(nc.sbuf_tensor("ost%d" % i, [64, 512], BF16)) for i in range(2)]
        wload(P, wfq[:], kc_view(w_fq_d), "wfq")
        wload(P, wfk[:], kc_view(w_fk_d), "wfk")
        wload(P, wfv[:], kc_view(w_fv_d), "wfv")
        for i in range(2):
            P.op(POOL, lambda e, i=i: e.memset(kTs[i][64:65, :], 1.0), writes=[("fk", i)])
            P.op(POOL, lambda e, i=i: e.memset(vxs[i][:, :, 64:65], 1.0), writes=[("fv", i)])
        allact = [("actT", t) for t in range(NT)]
        def fox_chunks(h):
            b = h % 2
            qT, kT, vx = qTs[b], kTs[b], vxs[b]
            hn = "f%d" % b
            out = []

            def cq(q):
                bk = 6 + (q % 2)
                P.op(PE, lambda e: e.matmul(
                    psb(bk)[0:65, :], esel[:, h * 65:(h + 1) * 65], FsT[:, q * 512:(q + 1) * 512], start=True, stop=False),
                    reads=["FsT", "cb"], writes=[("ps", bk)])
                for kc in range(8):
                    P.op(PE, lambda e, kc=kc: e.matmul(
                        psb(bk)[0:64, :], wfq[:, kc, h * 64:(h + 1) * 64], actT[:, kc, q * 512:(q + 1) * 512],
                        start=False, stop=False),
                        reads=["wfq"] + allact[q * 4:q * 4 + 4], writes=[("ps", bk)])
                P.op(PE, lambda e: e.matmul(
                    psb(bk)[0:65, :], esel[:, h * 65:(h + 1) * 65], FsT[:, q * 512:(q + 1) * 512], start=False, stop=True),
                    reads=["FsT", "cb"], writes=[("ps", bk)])
                return lambda: P.op(ACT, lambda e: e.copy(qT[0:65, q * 512:(q + 1) * 512], psb(bk)[0:65, :]),
                                    reads=[("ps", bk)], writes=[hn + "q"])

            def ck(q):
                bk = 6 + (q % 2)
                for kc in range(8):
                    P.op(PE, lambda e, kc=kc: e.matmul(
                        psb(bk)[0:64, :], wfk[:, kc, h * 64:(h + 1) * 64], actT[:, kc, q * 512:(q + 1) * 512],
                        start=(kc == 0), stop=(kc == 7)),
                        reads=["wfk"] + allact[q * 4:q * 4 + 4], writes=[("ps", bk)])
                return lambda: P.op(DVE, lambda e: e.tensor_copy(kT[0:64, q * 512:(q + 1) * 512], psb(bk)[0:64, :]),
                                    reads=[("ps", bk), ("fk", b)], writes=[hn + "k"])

            def cv(g8):
                bk = 6 + (g8 % 2)
                for i in range(8):
                    t = g8 * 8 + i
                    for kc in range(8):
                        P.op(PE, lambda e, t=t, i=i, kc=kc: e.matmul(
                            psb(bk)[:, i * 64:(i + 1) * 64], actT[:, kc, t * 128:(t + 1) * 128], wfv[:, kc, h * 64:(h + 1) * 64],
                            start=(kc == 0), stop=(kc == 7)),
                            reads=["wfv", ("actT", t)], writes=[("ps", bk)])
                return lambda: P.op(DVE, lambda e: e.tensor_copy(
                    vx[:, g8 * 8:(g8 + 1) * 8, 0:64], psb(bk).rearrange("p (t d) -> p t d", d=64)),
                    reads=[("ps", bk), ("fv", b)], writes=[hn + "v"])

            for q in range(NQ):
                out.append(lambda q=q: cq(q))
            for q in range(NQ):
                out.append(lambda q=q: ck(q))
            for g8 in range(4):
                out.append(lambda g8=g8: cv(g8))
            return out

        for c in fox_chunks(0):
            c()()
        for h in range(NH):
            b = h % 2
            nxt = fox_chunks(h + 1) if h + 1 < NH else []
            attention_head(P, h, 65, qTs[b], kTs[b], vxs[b], FOX_SCALE, lambda J, h=h: Gc[:, J, h:h + 1], mask_fox,
                           ob_s, (pt, rrow, rbc, ost), "f%d" % b, interleave=nxt)
        if "ob" in debug:
            add_dbg(P, "ob", ob_s, [512, S], BF16, ["odram"])
    P.emit("p3")
    st_f.close()
    if stop_after <= 3:
        return nc, dbg_outs

    st_a = contextlib.ExitStack()
    cqnT = st_a.enter_context(nc.sbuf_tensor("cqnT", [128, 3, S], BF16))
    krT = st_a.enter_context(nc.sbuf_tensor("krT", [96, S], BF16))
    cosT = st_a.enter_context(nc.sbuf_tensor("cosT", [96, S], BF16))
    sinT = st_a.enter_context(nc.sbuf_tensor("sinT", [96, S], BF16))
    R = slice(64, 96)
    allc = [("cqnT", t) for t in range(NT)]
    P = Prog(nc)
    with contextlib.ExitStack() as st:
        wlat = st.enter_context(nc.sbuf_tensor("wlat", [128, 8, 384], BF16))
        wkr = st.enter_context(nc.sbuf_tensor("wkr", [128, 8, 192], BF16))
        gcq = st.enter_context(nc.sbuf_tensor("gcq", [128, 384], F32))
        lat = [st.enter_context(nc.sbuf_tensor("lat%d" % i, [128, 384], BF16)) for i in range(2)]
        lsq = st.enter_context(nc.sbuf_tensor("lsq", [128, 384], BF16))
        lst = st.enter_context(nc.sbuf_tensor("lst", [128, NT, 4], F32))
        posi = st.enter_context(nc.sbuf_tensor("posi", [96, 512], I32))
        ang = st.enter_context(nc.sbuf_tensor("ang", [96, 512], F32))
        uu = st.enter_context(nc.sbuf_tensor("uu", [96, 512], F32))
        ki = st.enter_context(nc.sbuf_tensor("ki", [96, 512], I32))
        kf = st.enter_context(nc.sbuf_tensor("kf", [96, 512], F32))
        rr = st.enter_context(nc.sbuf_tensor("rr", [96, 512], F32))
        hs_ = st.enter_context(nc.sbuf_tensor("hs_", [96, 512], F32))
        t1 = st.enter_context(nc.sbuf_tensor("t1", [96, 512], F32))
        t2 = st.enter_context(nc.sbuf_tensor("t2", [96, 512], F32))
        wload(P, wlat[:], kc_view(w_lat_d), "wlat")
        wload(P, wkr[:], kc_view(w_kr_d), "wkr")
        P.op(SP, lambda e: e.dma_start(out=gcq[:, 0:256], in_=g_cq_d[0].partition_broadcast(128)), writes=["gcq"], dma=True)
        P.op(SP, lambda e: e.dma_start(out=gcq[:, 256:384], in_=g_ckv_d[0].partition_broadcast(128)), writes=["gcq"], dma=True)
        for q in range(NQ):
            tk = slice(q * 512, (q + 1) * 512)
            P.op(SP, lambda e, tk=tk: e.dma_start(out=posi[R, :], in_=pos_d[0, tk].partition_broadcast(32)),
                 writes=["posi"], dma=True)
            P.op(DVE, lambda e: e.tensor_copy(ang[R, :], posi[R, :]), reads=["posi"], writes=["ang"])
            P.op(DVE, lambda e: e.tensor_scalar(ang[R, :], ang[R, :], freq_abs[R, :], None, ALU.mult),
                 reads=["ang", "cf"], writes=["ang"])
            P.op(DVE, lambda e: e.tensor_scalar(uu[R, :], ang[R, :], 1.0 / (2 * np.pi), None, ALU.mult),
                 reads=["ang"], writes=["uu"])
            P.op(DVE, lambda e: e.tensor_copy(ki[R, :], uu[R, :]), reads=["uu"], writes=["ki"])
            P.op(DVE, lambda e: e.tensor_copy(kf[R, :], ki[R, :]), reads=["ki"], writes=["kf"])
            P.op(DVE, lambda e: e.scalar_tensor_tensor(rr[R, :], kf[R, :], -TWO_PI_HI, ang[R, :], ALU.mult, ALU.add),
                 reads=["kf", "ang"], writes=["rr"])
            P.op(DVE, lambda e: e.scalar_tensor_tensor(rr[R, :], kf[R, :], -TWO_PI_LO, rr[R, :], ALU.mult, ALU.add),
                 reads=["kf", "rr"], writes=["rr"])
            P.op(DVE, lambda e: e.tensor_scalar(rr[R, :], rr[R, :], 3.1415925, -3.1415925, ALU.min, ALU.max),
                 reads=["rr"], writes=["rr"])
            P.op(ACT, lambda e, tk=tk: e.activation(sinT[R, tk], rr[R, :], AF.Sin, scale=cf[R, 386:387]),
                 reads=["rr", "cf"], writes=["sinT"])
            P.op(ACT, lambda e: e.activation(hs_[R, :], rr[R, :], AF.Sin, scale=0.5), reads=["rr"], writes=["hs_"])
            P.op(DVE, lambda e: e.tensor_tensor(hs_[R, :], hs_[R, :], hs_[R, :], ALU.mult), reads=["hs_"], writes=["hs_"])
            P.op(DVE, lambda e, tk=tk: e.tensor_scalar(cosT[R, tk], hs_[R, :], -2.0, 1.0, ALU.mult, ALU.add),
                 reads=["hs_"], writes=["cosT"])
        P.op(DVE, lambda e: e.memset(lst[:], 0.0), writes=["lst"])
        for t in range(NT):
            bk = 6 + (t % 2)
            lb = lat[t % 2]
            for kc in range(8):
                P.op(PE, lambda e, t=t, kc=kc, bk=bk: e.matmul(
                    psb(bk)[:, 0:384], actT[:, kc, t * 128:(t + 1) * 128], wlat[:, kc, :], start=(kc == 0), stop=(kc == 7)),
                    reads=["wlat", ("actT", t)], writes=[("ps", bk)])
            P.op(ACT, lambda e, t=t, bk=bk: e.activation(lsq[:, 0:256], psb(bk)[:, 0:256], AF.Square, accum_out=lst[:, t, 0:1]),
                 reads=[("ps", bk), "lst"], writes=["lsq", ("lst", t)])
            P.op(ACT, lambda e, t=t, bk=bk: e.activation(lsq[:, 256:384], psb(bk)[:, 256:384], AF.Square, accum_out=lst[:, t, 1:2]),
                 reads=[("ps", bk), ("lst", t)], writes=["lsq", ("lst", t)])
            P.op(ACT, lambda e, t=t: e.activation(lst[:, t, 2:3], lst[:, t, 0:1], AF.Sqrt, bias=EPS, scale=1.0 / 256),
                 reads=[("lst", t)], writes=[("lst2", t)])
            P.op(ACT, lambda e, t=t: e.activation(lst[:, t, 3:4], lst[:, t, 1:2], AF.Sqrt, bias=EPS, scale=1.0 / 128),
                 reads=[("lst", t)], writes=[("lst3", t)])
            P.op(DVE, lambda e, t=t: e.reciprocal(lst[:, t, 2:4], lst[:, t, 2:4]),
                 reads=[("lst2", t), ("lst3", t)], writes=[("lst2", t), ("lst3", t)])
            P.op(DVE, lambda e, t=t, bk=bk, lb=lb: e.scalar_tensor_tensor(
                lb[:, 0:256], psb(bk)[:, 0:256], lst[:, t, 2:3], gcq[:, 0:256], ALU.mult, ALU.mult),
                reads=[("ps", bk), ("lst2", t), "gcq"], writes=[("lat", t % 2)])
            P.op(DVE, lambda e, t=t, bk=bk, lb=lb: e.scalar_tensor_tensor(
                lb[:, 256:384], psb(bk)[:, 256:384], lst[:, t, 3:4], gcq[:, 256:384], ALU.mult, ALU.mult),
                reads=[("ps", bk), ("lst3", t), "gcq"], writes=[("lat", t % 2)])
            tb = 4 + (t % 2)
            for c in range(3):
                P.op(PE, lambda e, lb=lb, c=c, tb=tb: e.transpose(
                    psb16(tb)[:, c * 128:(c + 1) * 128], lb[:, c * 128:(c + 1) * 128], identb),
                    reads=[("lat", t % 2), "cb"], writes=[("ps", tb)])
            P.op(ACT, lambda e, t=t, tb=tb: e.copy(
                cqnT[:, :, t * 128:(t + 1) * 128], psb16(tb)[:, 0:384].rearrange("p (a b) -> p a b", a=3)),
                reads=[("ps", tb)], writes=[("cqnT", t)])
        for q in range(NQ):
            tk = slice(q * 512, (q + 1) * 512)
            for v in range(2):
                for kc in range(8):
                    P.op(PE, lambda e, q=q, v=v, kc=kc: e.matmul(
                        psb(6 + v)[0:96, :], wkr[:, kc, v * 96:(v + 1) * 96], actT[:, kc, q * 512:(q + 1) * 512],
                        start=(kc == 0), stop=(kc == 7)),
                        reads=["wkr"] + [("actT", q * 4 + i) for i in range(4)], writes=[("ps", 6 + v)])
            P.op(DVE, lambda e, tk=tk: e.tensor_tensor(t1[R, :], psb(6)[R, :], cosT[R, tk], ALU.mult),
                 reads=[("ps", 6), "cosT"], writes=["t1"])
            P.op(DVE, lambda e, tk=tk: e.tensor_tensor(t2[R, :], psb(7)[R, :], sinT[R, tk], ALU.mult),
                 reads=[("ps", 7), "sinT"], writes=["t2"])
            P.op(POOL, lambda e, tk=tk: e.tensor_tensor(krT[R, tk], t1[R, :], t2[R, :], ALU.add),
                 reads=["t1", "t2"], writes=["krT"])
        if "lat" in debug:
            add_dbg(P, "cqnT", cqnT[:, :, 0:512], [128, 3, 512], BF16, allc)
            add_dbg(P, "krT", krT[R, 0:512], [32, 512], BF16, ["krT"])
    P.emit("p4a")

    P = Prog(nc)
    with contextlib.ExitStack() as st:
        wuq = st.enter_context(nc.sbuf_tensor("wuq", [128, 2, 768], BF16))
        wuqs = st.enter_context(nc.sbuf_tensor("wuqs", [128, 2, 768], BF16))
        wuk = st.enter_context(nc.sbuf_tensor("wuk", [128, 512], BF16))
        wuv = st.enter_context(nc.sbuf_tensor("wuv", [128, 512], BF16))
        qTs = [st.enter_context(nc.sbuf_tensor("aqT%d" % i, [96, S], BF16)) for i in range(2)]
        kTs = [st.enter_context(nc.sbuf_tensor("akT%d" % i, [96, S], BF16)) for i in range(2)]
        vxs = [st.enter_context(nc.sbuf_tensor("avx%d" % i, [128, NT, 65], BF16)) for i in range(2)]
        pt = [st.enter_context(nc.sbuf_tensor("apt%d" % i, [128, 512], BF16)) for i in range(3)]
        rrow = st.enter_context(nc.sbuf_tensor("arrow", [65, 512], F32))
        rbc = st.enter_context(nc.sbuf_tensor("arbc", [64, 512], F32))
        ost = [st.enter_context(nc.sbuf_tensor("aost%d" % i, [64, 512], BF16)) for i in range(2)]
        t1 = st.enter_context(nc.sbuf_tensor("t1b", [96, 512], F32))
        t2 = st.enter_context(nc.sbuf_tensor("t2b", [96, 512], F32))
        wload(P, wuq[:], kc_view(w_uq_d), "wuq")
        wload(P, wuqs[:], kc_view(w_uqs_d), "wuqs")
        wload(P, wuk[:], w_uk_d, "wuk")
        wload(P, wuv[:], w_uv_d, "wuv")
        for i in range(2):
            P.op(POOL, lambda e, i=i: e.memset(vxs[i][:, :, 64:65], 1.0), writes=[("av", i)])
        def mla_chunks(h):
            b = h % 2
            qT, kT, vx = qTs[b], kTs[b], vxs[b]
            hn = "a%d" % b
            out = []

            def cq(q):
                tk = slice(q * 512, (q + 1) * 512)
                for v, w_ in enumerate((wuq, wuqs)):
                    for kc in range(2):
                        P.op(PE, lambda e, kc=kc, v=v, w_=w_: e.matmul(
                            psb(6 + v)[0:96, :], w_[:, kc, h * 96:(h + 1) * 96], cqnT[:, kc, q * 512:(q + 1) * 512],
                            start=(kc == 0), stop=(kc == 1)),
                            reads=["wuq", "wuqs"] + allc[q * 4:q * 4 + 4], writes=[("ps", 6 + v)])
                def ev():
                    P.op(ACT, lambda e: e.copy(qT[0:64, tk], psb(6)[0:64, :]),
                         reads=[("ps", 6)], writes=[hn + "q"])
                    P.op(DVE, lambda e: e.tensor_tensor(t1[R, :], psb(6)[R, :], cosT[R, tk], ALU.mult),
                         reads=[("ps", 6), "cosT"], writes=["t1"])
                    P.op(DVE, lambda e: e.tensor_tensor(t2[R, :], psb(7)[R, :], sinT[R, tk], ALU.mult),
                         reads=[("ps", 7), "sinT"], writes=["t2"])
                    P.op(POOL, lambda e: e.tensor_tensor(qT[R, tk], t1[R, :], t2[R, :], ALU.add),
                         reads=["t1", "t2"], writes=[hn + "q"])
                return ev

            def ck(q):
                tk = slice(q * 512, (q + 1) * 512)
                bk = 6 + (q % 2)
                P.op(PE, lambda e: e.matmul(
                    psb(bk)[0:64, :], wuk[:, h * 64:(h + 1) * 64], cqnT[:, 2, q * 512:(q + 1) * 512], start=True, stop=True),
                    reads=["wuk"] + allc[q * 4:q * 4 + 4], writes=[("ps", bk)])
                return lambda: P.op(ACT, lambda e: e.copy(kT[0:64, tk], psb(bk)[0:64, :]),
                                    reads=[("ps", bk)], writes=[hn + "k"])

            def ck2(q):
                e0 = ck(q)
                e1 = ck(q + 1)
                return lambda: (e0(), e1())

            def ckr():
                P.op(POOL, lambda e: e.tensor_copy(kT[R, :], krT[R, :]), reads=["krT"], writes=[hn + "k"])
                return None

            def cv(g8):
                bk = 6 + (g8 % 2)
                for i in range(8):
                    t = g8 * 8 + i
                    P.op(PE, lambda e, t=t, i=i: e.matmul(
                        psb(bk)[:, i * 64:(i + 1) * 64], cqnT[:, 2, t * 128:(t + 1) * 128], wuv[:, h * 64:(h + 1) * 64],
                        start=True, stop=True),
                        reads=["wuv", ("cqnT", t)], writes=[("ps", bk)])
                return lambda: P.op(DVE, lambda e: e.tensor_copy(
                    vx[:, g8 * 8:(g8 + 1) * 8, 0:64], psb(bk).rearrange("p (t d) -> p t d", d=64)),
                    reads=[("ps", bk), ("av", b)], writes=[hn + "v"])

            for q in range(NQ):
                out.append(lambda q=q: cq(q))
            for q in range(0, NQ, 2):
                out.append(lambda q=q: ck2(q))
            out.append(ckr)
            for g8 in range(4):
                out.append(lambda g8=g8: cv(g8))
            return out

        for c in mla_chunks(0):
            ev = c()
            if ev:
                ev()
        for h in range(NH):
            b = h % 2
            nxt = mla_chunks(h + 1) if h + 1 < NH else []
            attention_head(P, h, 96, qTs[b], kTs[b], vxs[b], MLA_SCALE, lambda J: 0.0, mask_mla,
                           oa_s, (pt, rrow, rbc, ost), "a%d" % b, interleave=nxt)
        if "oa" in debug:
            add_dbg(P, "oa", oa_s, [512, S], BF16, ["odram"])
    P.emit("p4")
    st_a.close()
    if stop_after <= 4:
        return nc, dbg_outs

    P = Prog(nc)
    with contextlib.ExitStack() as st:
        woa = st.enter_context(nc.sbuf_tensor("woa", [128, 4, D], BF16))
        wob = st.enter_context(nc.sbuf_tensor("wob", [128, 4, D], BF16))
        wout = st.enter_context(nc.sbuf_tensor("wout", [128, 8, D], BF16))
        wr = st.enter_context(nc.sbuf_tensor("wr", [128, 8, 36], BF16))
        gffn = st.enter_context(nc.sbuf_tensor("gffn", [128, D], F32))
        brb = st.enter_context(nc.sbuf_tensor("brb", [128, 36], F32))
        gat = [[st.enter_context(nc.sbuf_tensor("gat%d_%d" % (b_, i), [128, 8, 512], BF16)) for i in range(2)] for b_ in range(2)]
        oin = [[st.enter_context(nc.sbuf_tensor("oin%d_%d" % (b_, i), [128, 4, 512], BF16)) for i in range(2)] for b_ in range(2)]
        mixT = [st.enter_context(nc.sbuf_tensor("mixT%d" % i, [128, 8, 512], BF16)) for i in range(2)]
        m1 = [st.enter_context(nc.sbuf_tensor("m1_0", [128, 512], F32))] * 2
        m2 = [st.enter_context(nc.sbuf_tensor("m2_0", [128, 512], F32))] * 2
        xh = [st.enter_context(nc.sbuf_tensor("xh%d" % i, [128, D], F32)) for i in range(3)]
        hsb = [st.enter_context(nc.sbuf_tensor("hsb%d" % i, [128, D], BF16)) for i in range(2)]
        sq5 = st.enter_context(nc.sbuf_tensor("sq5", [128, D], BF16))
        st5 = st.enter_context(nc.sbuf_tensor("st5", [128, NT, 2], F32))
        Lr = st.enter_context(nc.sbuf_tensor("Lr", [128, NT, 36], F32))
        wload(P, woa[:], w_oa_d.rearrange("(c p) n -> p c n", p=128), "woa")
        wload(P, wob[:], w_ob_d.rearrange("(c p) n -> p c n", p=128), "wob")
        wload(P, wout[:], kc_view(w_out_d), "wout")
        wload(P, wr[:], kc_view(w_r_d), "wr")
        P.op(SP, lambda e: e.dma_start(out=gffn[:], in_=g_ffn_d[0].partition_broadcast(128)), writes=["gffn"], dma=True)
        P.op(SP, lambda e: e.dma_start(out=brb[:], in_=br_d[0].partition_broadcast(128)), writes=["brb"], dma=True)
        P.op(DVE, lambda e: e.memset(st5[:], 0.0), writes=["st5"])
        xi = [0]

        def merge_loads(q):
            tk = slice(q * 512, (q + 1) * 512)
            b_ = q % 2
            P.op(SP, lambda e: e.dma_start(out=gat[b_][0][:], in_=ga_s[:, tk].rearrange("(c p) t -> p c t", p=128)),
                 writes=[("gat", b_, 0)], dma=True)
            P.op(SP, lambda e: e.dma_start(out=oin[b_][0][:], in_=oa_s[:, tk].rearrange("(c p) t -> p c t", p=128)),
                 writes=[("oin", b_, 0)], dma=True)
            P.op(SP, lambda e: e.dma_start(out=gat[b_][1][:], in_=gb_s[:, tk].rearrange("(c p) t -> p c t", p=128)),
                 writes=[("gat", b_, 1)], dma=True)
            P.op(SP, lambda e: e.dma_start(out=oin[b_][1][:], in_=ob_s[:, tk].rearrange("(c p) t -> p c t", p=128)),
                 writes=[("oin", b_, 1)], dma=True)

        def merge_dc(q, dc):
            b_ = q % 2
            mb = 0
            for v, w_ in enumerate((woa, wob)):
                for pc in range(4):
                    P.op(PE, lambda e, v=v, w_=w_, pc=pc: e.matmul(
                        psb(v), w_[:, pc, dc * 128:(dc + 1) * 128], oin[b_][v][:, pc, :], start=(pc == 0), stop=(pc == 3)),
                        reads=["woa", "wob", ("oin", b_, v)], writes=[("ps", v)])
            P.op(DVE, lambda e: e.tensor_tensor(m1[mb][:], psb(0), gat[b_][0][:, dc, :], ALU.mult),
                 reads=[("ps", 0), ("gat", b_, 0)], writes=[("m1", mb)])
            P.op(DVE, lambda e: e.tensor_tensor(m2[mb][:], psb(1), gat[b_][1][:, dc, :], ALU.mult),
                 reads=[("ps", 1), ("gat", b_, 1)], writes=[("m2", mb)])
            P.op(POOL, lambda e: e.tensor_tensor(mixT[b_][:, dc, :], m1[mb][:], m2[mb][:], ALU.add),
                 reads=[("m1", mb), ("m2", mb)], writes=[("mixT", b_, dc)])

        merge_loads(0)
        for dc in range(8):
            merge_dc(0, dc)
        for q in range(NQ):
            if q + 1 < NQ:
                merge_loads(q + 1)
            for sub in range(4):
                t = q * 4 + sub
                xb = xh[xi[0] % 3]
                xr = ("xh", xi[0] % 3)
                xi[0] += 1
                P.op(SP, lambda e, xb=xb, t=t: e.dma_start(out=xb[:], in_=x_d[t * 128:(t + 1) * 128, :]),
                     writes=[xr], dma=True)
                for ch in range(2):
                    bk = 2 + ch
                    for kc in range(8):
                        P.op(PE, lambda e, sub=sub, ch=ch, kc=kc, bk=bk, q=q: e.matmul(
                            psb(bk), mixT[q % 2][:, kc, sub * 128:(sub + 1) * 128], wout[:, kc, ch * 512:(ch + 1) * 512],
                            start=(kc == 0), stop=(kc == 7)),
                            reads=["wout"] + [("mixT", q % 2, d_) for d_ in range(8)], writes=[("ps", bk)])
                    P.op(DVE, lambda e, xb=xb, ch=ch, bk=bk: e.tensor_tensor(
                        xb[:, ch * 512:(ch + 1) * 512], psb(bk), xb[:, ch * 512:(ch + 1) * 512], ALU.add),
                        reads=[("ps", bk), xr], writes=[xr])
                P.op(SP, lambda e, xb=xb, t=t: e.dma_start(out=h_s[t * 128:(t + 1) * 128, :], in_=xb[:]),
                     reads=[xr], dma=True)
                if q + 1 < NQ:
                    merge_dc(q + 1, 2 * sub)
                P.op(ACT, lambda e, xb=xb, t=t: e.activation(sq5[:], xb[:], AF.Square, accum_out=st5[:, t, 0:1]),
                     reads=[xr, "st5"], writes=["sq5", ("st5", t)])
                P.op(ACT, lambda e, t=t: e.activation(st5[:, t, 1:2], st5[:, t, 0:1], AF.Sqrt, bias=EPS, scale=1.0 / D),
                     reads=[("st5", t)], writes=[("st51", t)])
                P.op(DVE, lambda e, t=t: e.reciprocal(st5[:, t, 1:2], st5[:, t, 1:2]),
                     reads=[("st51", t)], writes=[("st51", t)])
                hb = hsb[t % 2]
                P.op(DVE, lambda e, xb=xb, hb=hb, t=t: e.scalar_tensor_tensor(
                    hb[:], xb[:], st5[:, t, 1:2], gffn[:], ALU.mult, ALU.mult),
                    reads=[xr, ("st51", t), "gffn"], writes=[("hsb", t % 2)])
                tb = 4 + (t % 2)
                for kc in range(8):
                    P.op(PE, lambda e, hb=hb, kc=kc, tb=tb: e.transpose(
                        psb16(tb)[:, kc * 128:(kc + 1) * 128], hb[:, kc * 128:(kc + 1) * 128], identb),
                        reads=[("hsb", t % 2), "cb"], writes=[("ps", tb)])
                P.op(ACT, lambda e, t=t, tb=tb: e.copy(
                    actT[:, :, t * 128:(t + 1) * 128], psb16(tb).rearrange("p (a b) -> p a b", a=8)),
                    reads=[("ps", tb)], writes=[("actT", t)])
                if q + 1 < NQ:
                    merge_dc(q + 1, 2 * sub + 1)
                for kc in range(8):
                    P.op(PE, lambda e, t=t, kc=kc: e.matmul(
                        psb(6)[:, 0:36], actT[:, kc, t * 128:(t + 1) * 128], wr[:, kc, :], start=(kc == 0), stop=(kc == 7)),
                        reads=[("actT", t), "wr"], writes=[("ps", 6)])
                P.op(DVE, lambda e, t=t: e.tensor_tensor(Lr[:, t, :], psb(6)[:, 0:36], brb[:], ALU.add),
                     reads=[("ps", 6), "brb"], writes=[("Lr", t)])

        allL = [("Lr", t) for t in range(NT)]
        scr = mixT[0][:].rearrange("p a b -> p (a b)").bitcast(F32)

        class V:
            def __init__(self, ap):
                self.ap = ap

            def __getitem__(self, idx):
                return self.ap if (isinstance(idx, slice) and idx == slice(None)) else self.ap[idx]

        def v3(o, k):
            return V(scr[:, o:o + NT * k].rearrange("p (t k) -> p t k", k=k))

        def v2(o):
            return V(scr[:, o:o + NT])

        sel, sel2, tmp8, is1, is2 = v3(0, 8), v3(256, 8), v3(512, 8), v3(768, 8), v3(1024, 8)
        gd, grp = v3(1280, 4), v3(1408, 4)
        gmax, gp, mx1, mx2, e2, w1, w2 = (v2(1536 + 32 * i) for i in range(7))
        mixall = [("mixT", 0, d_) for d_ in range(8)]

        def b3(ap2, k):
            return ap2.unsqueeze(2).broadcast_to([128, NT, k])

        def dv(fn, reads, writes):
            P.op(DVE, fn, reads=reads, writes=writes)

        Lg = Lr[:, :, 0:4]
        dv(lambda e: e.reduce_max(gmax[:], Lg, axis=AX.X), allL, ["gmax"] + mixall)
        dv(lambda e: e.tensor_tensor(gd[:], Lg, b3(gmax[:], 4), ALU.subtract), allL + ["gmax"], ["gd"])
        P.op(ACT, lambda e: e.activation(gd[:], gd[:], AF.Exp), reads=["gd"], writes=["gd"])
        dv(lambda e: e.reduce_sum(gp[:], gd[:], axis=AX.X), ["gd"], ["gp"])
        dv(lambda e: e.reciprocal(gp[:], gp[:]), ["gp"], ["gp"])
        dv(lambda e: e.tensor_tensor(grp[:], Lg, b3(gmax[:], 4), ALU.is_equal), allL + ["gmax"], ["grp"])
        dv(lambda e: e.tensor_tensor(sel[:], Lr[:, :, 4:12], grp[:, :, 0:1].broadcast_to([128, NT, 8]), ALU.mult),
           allL + ["grp"], ["sel"])
        for g in range(1, 4):
            dv(lambda e, g=g: e.tensor_tensor(tmp8[:], Lr[:, :, 4 + 8 * g:12 + 8 * g],
                                              grp[:, :, g:g + 1].broadcast_to([128, NT, 8]), ALU.mult),
               allL + ["grp", "sel"], ["tmp8"])
            dv(lambda e: e.tensor_tensor(sel[:], sel[:], tmp8[:], ALU.add), ["sel", "tmp8"], ["sel"])
        dv(lambda e: e.reduce_max(mx1[:], sel[:], axis=AX.X), ["sel"], ["mx1"])
        dv(lambda e: e.tensor_tensor(is1[:], sel[:], b3(mx1[:], 8), ALU.is_equal), ["sel", "mx1"], ["is1"])
        dv(lambda e: e.scalar_tensor_tensor(sel2[:], is1[:], -1e30, sel[:], ALU.mult, ALU.add), ["is1", "sel"], ["sel2"])
        dv(lambda e: e.reduce_max(mx2[:], sel2[:], axis=AX.X), ["sel2"], ["mx2"])
        dv(lambda e: e.tensor_tensor(is2[:], sel2[:], b3(mx2[:], 8), ALU.is_equal), ["sel2", "mx2"], ["is2"])
        dv(lambda e: e.tensor_tensor(e2[:], mx2[:], mx1[:], ALU.subtract), ["mx1", "mx2"], ["e2"])
        P.op(ACT, lambda e: e.activation(e2[:], e2[:], AF.Exp), reads=["e2"], writes=["e2"])
        dv(lambda e: e.tensor_scalar(w1[:], e2[:], 1.0, None, ALU.add), ["e2"], ["w1"])
        dv(lambda e: e.reciprocal(w1[:], w1[:]), ["w1"], ["w1"])
        dv(lambda e: e.tensor_tensor(w1[:], w1[:], gp[:], ALU.mult), ["w1", "gp"], ["w1"])
        dv(lambda e: e.tensor_tensor(w2[:], w1[:], e2[:], ALU.mult), ["w1", "e2"], ["w2"])
        dv(lambda e: e.tensor_tensor(is1[:], is1[:], b3(w1[:], 8), ALU.mult), ["is1", "w1", "sel2"], ["is1"])
        dv(lambda e: e.tensor_tensor(is2[:], is2[:], b3(w2[:], 8), ALU.mult), ["is2", "w2"], ["is2"])
        dv(lambda e: e.tensor_tensor(is1[:], is1[:], is2[:], ALU.add), ["is1", "is2"], ["is1"])
        for g in range(4):
            dv(lambda e, g=g: e.tensor_tensor(comb[:, :, 8 * g:8 * g + 8], is1[:],
                                              grp[:, :, g:g + 1].broadcast_to([128, NT, 8]), ALU.mult),
               ["is1", "grp"], [("comb", g)])
        if "p5" in debug:
            add_dbg(P, "comb", comb[:], [128, NT, NE], F32, [("comb", g) for g in range(4)])
            add_dbg(P, "hnT", actT[:, :, 2048:2560], [128, 8, 512], BF16, [("actT", t) for t in range(16, 20)])
    P.emit("p5")
    if stop_after <= 5:
        return nc, dbg_outs

    P = Prog(nc)
    TT = 2048
    NS8 = TT // 128
    with contextlib.ExitStack() as st:
        gfin = st.enter_context(nc.sbuf_tensor("gfin", [128, D], F32))
        yacc = st.enter_context(nc.sbuf_tensor("yacc", [128, TT // 128, D], F32))
        wgu = [st.enter_context(nc.sbuf_tensor("wgu%d" % i, [128, 2, 8, EFF], BF16)) for i in range(2)]
        wdn = [st.enter_context(nc.sbuf_tensor("wdn%d" % i, [128, 2, D], BF16)) for i in range(2)]
        sg = [st.enter_context(nc.sbuf_tensor("sg%d" % i, [128, 512], F32)) for i in range(2)]
        aT = [st.enter_context(nc.sbuf_tensor("aT%d" % i, [128, 2, 512], BF16)) for i in range(2)]
        sq6 = st.enter_context(nc.sbuf_tensor("sq6", [128, D], BF16))
        st6 = st.enter_context(nc.sbuf_tensor("st6", [128, NT, 2], F32))
        P.op(SP, lambda e: e.dma_start(out=gfin[:], in_=g_fin_d[0].partition_broadcast(128)), writes=["gfin"], dma=True)
        P.op(DVE, lambda e: e.memset(st6[:], 0.0), writes=["st6"])
        wg_v = w_eg_d.rearrange("(e r) c -> e (r c)", e=NE).rearrange("e (kc p f) -> e p kc f", p=128, f=EFF)
        wu_v = w_eu_d.rearrange("(e r) c -> e (r c)", e=NE).rearrange("e (kc p f) -> e p kc f", p=128, f=EFF)
        wd_v = w_ed_d.rearrange("(e r) c -> e (r c)", e=NE).rearrange("e (c p n) -> e p c n", p=128, n=D)
        it = 0
        dn = 0
        for tt in range(S // TT):
            for s8 in range(NS8):
                t = tt * NS8 + s8
                P.op(SP, lambda e, s8=s8, t=t: e.dma_start(out=yacc[:, s8, :], in_=h_s[t * 128:(t + 1) * 128, :]),
                     writes=[("yacc", s8)], dma=True)
            if "y0" in debug and tt == 1:
                add_dbg(P, "y0", yacc[:, 0, :], [128, D], F32, [("yacc", 0)])
            def wloads(ex, wb):
                P.op(POOL, lambda e: e.dma_start(out=wgu[wb][:, 0, :, :], in_=wg_v[ex]), writes=[("wgu", wb)], dma=True)
                P.op(POOL, lambda e: e.dma_start(out=wgu[wb][:, 1, :, :], in_=wu_v[ex]), writes=[("wgu", wb)], dma=True)
                P.op(POOL, lambda e: e.dma_start(out=wdn[wb][:], in_=wd_v[ex]), writes=[("wdn", wb)], dma=True)

            def gu(ex, half, wb):
                tok0 = tt * TT + half * 512
                ab = aT[half % 2]
                for c in range(2):
                    gb_, ub_ = 2 * c, 2 * c + 1
                    for v, bk in ((0, gb_), (1, ub_)):
                        for kc in range(8):
                            P.op(PE, lambda e, v=v, kc=kc, c=c, bk=bk: e.matmul(
                                psb(bk), wgu[wb][:, v, kc, c * 128:(c + 1) * 128], actT[:, kc, tok0:tok0 + 512],
                                start=(kc == 0), stop=(kc == 7)),
                                reads=[("wgu", wb)], writes=[("ps", bk)])
                    sgb = sg[c]
                    P.op(ACT, lambda e, sgb=sgb, gb_=gb_: e.activation(sgb[:], psb(gb_), AF.Silu),
                         reads=[("ps", gb_)], writes=[("sg", c)])
                    P.op(DVE, lambda e, c=c, sgb=sgb, ub_=ub_: e.tensor_tensor(ab[:, c, :], psb(ub_), sgb[:], ALU.mult),
                         reads=[("ps", ub_), ("sg", c)], writes=[("aT", half % 2, c)])

            def dnp(ex, half, wb):
                nonlocal dn
                ab = aT[half % 2]
                for sub in range(4):
                    s8 = half * 4 + sub
                    t = tt * NS8 + s8
                    for ch in range(2):
                        bk = 4 + (dn % 4)
                        dn += 1
                        for c in range(2):
                            P.op(PE, lambda e, c=c, sub=sub, ch=ch, bk=bk: e.matmul(
                                psb(bk), ab[:, c, sub * 128:(sub + 1) * 128], wdn[wb][:, c, ch * 512:(ch + 1) * 512],
                                start=(c == 0), stop=(c == 1)),
                                reads=[("aT", half % 2, 0), ("aT", half % 2, 1), ("wdn", wb)], writes=[("ps", bk)])
                        P.op(DVE, lambda e, s8=s8, ch=ch, bk=bk, t=t: e.scalar_tensor_tensor(
                            yacc[:, s8, ch * 512:(ch + 1) * 512], psb(bk), comb[:, t, ex:ex + 1],
                            yacc[:, s8, ch * 512:(ch + 1) * 512], ALU.mult, ALU.add),
                            reads=[("ps", bk), ("yacc", s8)], writes=[("yacc", s8)])

            items = [(ex, half) for ex in range(NE) for half in range(TT // 512)]
            wbs = {}
            for ex in range(NE):
                wbs[ex] = it % 2
                it += 1

            def gu_item(i):
                ex, half = items[i]
                if half == 0:
                    wloads(ex, wbs[ex])
                gu(ex, half, wbs[ex])

            gu_item(0)
            for i in range(len(items)):
                if i + 1 < len(items):
                    gu_item(i + 1)
                ex, half = items[i]
                dnp(ex, half, wbs[ex])
            for s8 in range(NS8):
                t = tt * NS8 + s8
                P.op(ACT, lambda e, s8=s8, t=t: e.activation(sq6[:], yacc[:, s8, :], AF.Square, accum_out=st6[:, t, 0:1]),
                     reads=[("yacc", s8), "st6"], writes=["sq6", ("st6", t)])
                P.op(ACT, lambda e, t=t: e.activation(st6[:, t, 1:2], st6[:, t, 0:1], AF.Sqrt, bias=EPS, scale=1.0 / D),
                     reads=[("st6", t)], writes=[("st61", t)])
                P.op(DVE, lambda e, t=t: e.reciprocal(st6[:, t, 1:2], st6[:, t, 1:2]), reads=[("st61", t)], writes=[("st61", t)])
                P.op(DVE, lambda e, s8=s8, t=t: e.scalar_tensor_tensor(
                    yacc[:, s8, :], yacc[:, s8, :], st6[:, t, 1:2], gfin[:], ALU.mult, ALU.mult),
                    reads=[("yacc", s8), ("st61", t), "gfin"], writes=[("yacc", s8)])
                P.op(SP, lambda e, s8=s8, t=t: e.dma_start(out=out_d[t * 128:(t + 1) * 128, :], in_=yacc[:, s8, :]),
                     reads=[("yacc", s8)], dma=True)
    P.emit("p6")
    return nc, dbg_outs


def _consts():
    cfm = np.zeros((128, 512), np.float32)
    cfm[:, 0:128] = np.eye(128, dtype=np.float32)
    cfm[:, 128:256] = np.triu(np.ones((128, 128), np.float32))
    cfm[:, 256:384] = 1.0
    p = np.arange(128)
    j = p % 16
    freq = (10000.0 ** (-(j.astype(np.float32)) / np.float32(16.0))).astype(np.float32)
    sgn = np.where((p % 32) < 16, -1.0, 1.0).astype(np.float32)
    cfm[:, 384] = sgn * freq
    cfm[:, 385] = freq
    cfm[:, 386] = sgn
    cbm = np.zeros((128, 1024), np.float32)
    cbm[:, 0:128] = np.eye(128)
    k = np.arange(128)[:, None]
    q = np.arange(128)[None, :]
    cbm[:, 128:256] = np.where(k > q, NEG, 0.0)
    cbm[:, 256:384] = np.where((k // 64) > (q // 64), NEG, 0.0)
    for h in range(8):
        cbm[h, 384 + h * 65 + 64] = 1.0
    return cfm, cbm.astype(ml_dtypes.bfloat16)


def _prep_inputs(inp):
    f = lambda a: np.ascontiguousarray(np.asarray(a, dtype=np.float32))
    w_in = f(inp["w_in"])
    kr = w_in[:, 384:416]
    swap = np.concatenate([np.arange(16, 32), np.arange(0, 16)])
    w_kr2 = np.concatenate([kr, kr, kr, kr, kr, kr[:, swap]], axis=1)
    w_uq = f(inp["w_uq"])
    idx = np.arange(768).reshape(8, 96).copy()
    idx[:, 64:96] = idx[:, 64:96][:, swap]
    w_uq_sw = w_uq[:, idx.reshape(-1)]
    cfm, cbm = _consts()
    shared = {
        "g_mix": f(inp["g_mix"]).reshape(1, -1),
        "g_ffn": f(inp["g_ffn"]).reshape(1, -1),
        "g_final": f(inp["g_final"]).reshape(1, -1),
        "g_cq": f(inp["g_cq"]).reshape(1, -1),
        "g_ckv": f(inp["g_ckv"]).reshape(1, -1),
        "b_forget": f(inp["b_forget"]).reshape(1, -1),
        "b_r36": np.concatenate([f(inp["b_group"]), f(inp["b_router"])]).reshape(1, -1),
        "w_lat": np.ascontiguousarray(w_in[:, 0:384]),
        "w_kr2": np.ascontiguousarray(w_kr2),
        "w_fq": np.ascontiguousarray(w_in[:, 416:928]),
        "w_fk": np.ascontiguousarray(w_in[:, 928:1440]),
        "w_fv": np.ascontiguousarray(w_in[:, 1440:1952]),
        "w_f": np.ascontiguousarray(w_in[:, 1952:1960]),
        "w_ga": np.ascontiguousarray(w_in[:, 1960:2984]),
        "w_gb": np.ascontiguousarray(w_in[:, 2984:4008]),
        "w_uq": w_uq,
        "w_uq_sw": np.ascontiguousarray(w_uq_sw),
        "w_uk": f(inp["w_uk"]),
        "w_uv": f(inp["w_uv"]),
        "w_o_mla": f(inp["w_o_mla"]),
        "w_o_fox": f(inp["w_o_fox"]),
        "w_out": f(inp["w_out"]),
        "w_r36": np.ascontiguousarray(np.concatenate([f(inp["w_group"]), f(inp["w_router"])], axis=1)),
        "w_e_gate": f(inp["w_e_gate"]).reshape(-1, 2048),
        "w_e_up": f(inp["w_e_up"]).reshape(-1, 2048),
        "w_e_down": f(inp["w_e_down"]).reshape(-1, 2048),
        "consts_f": cfm,
        "consts_b": cbm,
    }
    x = f(inp["x"])
    pos = np.ascontiguousarray(np.asarray(inp["positions"], dtype=np.int32))
    in_maps = []
    for b in range(8):
        m = dict(shared)
        m["x"] = x[b]
        m["pos"] = pos[b].reshape(1, -1)
        in_maps.append(m)
    return in_maps


def kernel(**inputs):
    in_maps = _prep_inputs(inputs)
    nc, _ = build_program()
    res = run_bass_kernel_spmd(nc, in_maps, core_ids=list(range(8)))
    out = np.stack([np.asarray(r["out"], dtype=np.float32) for r in res.results], axis=0)
    return out
```
